# Optimizing a Trainium2 kernel written in Bass

```python
import math
import jax, jax.numpy as jnp
from jax import lax
import numpy as np

D_MODEL = 4096
BATCH = 4
SEQ = 4096
DEPTH = 2

N_MEM = 256
EPS = 1e-6
NEG_INF = -1e30
HEAD_DIM_A = 128
WIDTH_A = 3 * D_MODEL // 8
N_HEADS_A = WIDTH_A // HEAD_DIM_A
ROT_DIM = HEAD_DIM_A // 4
ROPE_THETA = 500000.0
DILATION_PAIRS = ((128, 1), (512, 4), (2048, 16))
WIDTH_B = 3 * D_MODEL // 8
N_HEADS_B = 6
V_HEAD_B = WIDTH_B // N_HEADS_B
QK_HEAD_B = V_HEAD_B // 2
RET_CHUNK = 128
RET_THETA = 10000.0
WIDTH_C = D_MODEL - WIDTH_A - WIDTH_B
S5_GROUP = 16
S5_GROUPS = WIDTH_C // S5_GROUP
S5_STATE = 64
S5_DT_MIN = 1e-3
S5_DT_MAX = 1e-1
IN_WIDTHS = (WIDTH_A, WIDTH_A, WIDTH_A, N_HEADS_B * QK_HEAD_B, N_HEADS_B * QK_HEAD_B, WIDTH_B, WIDTH_B, WIDTH_C)
IN_WIDTH = sum(IN_WIDTHS)
IN_SPLITS = tuple(int(v) for v in np.cumsum(IN_WIDTHS)[:-1])
N_HEADS_X = 4
HEAD_DIM_X = D_MODEL // N_HEADS_X
N_EXPERTS = 16
EXPERT_FF = D_MODEL // 4
EC_CAPACITY_FACTOR = 2

kernel_name = 'hybrid_dilated_retention_s5_ec_encoder'


def rmsnorm(x, w):
    xf = x.astype(jnp.float32)
    y = xf * lax.rsqrt(jnp.mean(xf * xf, axis=-1, keepdims=True) + EPS)
    return (y * w.astype(jnp.float32)).astype(x.dtype)


def rotate(x, pos, freqs):
    half = freqs.shape[0]
    ang = pos.astype(jnp.float32)[:, :, None, None] * freqs
    cos, sin = jnp.cos(ang), jnp.sin(ang)
    xf = x.astype(jnp.float32)
    x1, x2, rest = xf[..., :half], xf[..., half:2 * half], xf[..., 2 * half:]
    return jnp.concatenate([x1 * cos - x2 * sin, x2 * cos + x1 * sin, rest], axis=-1)


def dilated_branch(q, k, v, dilation, radius):
    bn, s, nh, hd = q.shape
    blk = radius
    sub = s // dilation
    nb = -(-sub // blk)
    lp = nb * blk

    def regroup(t):
        return t.reshape(bn, sub, dilation, nh, hd).transpose(0, 2, 3, 1, 4)

    qg = jnp.pad(regroup(q), ((0, 0), (0, 0), (0, 0), (0, lp - sub), (0, 0)))
    qg = qg.reshape(bn, dilation, nh, nb, blk, hd)

    def band(t):
        tp = jnp.pad(regroup(t), ((0, 0), (0, 0), (0, 0), (blk, lp - sub + blk), (0, 0)))
        tp = tp.reshape(bn, dilation, nh, nb + 2, blk, hd)
        return jnp.concatenate([tp[:, :, :, 0:nb], tp[:, :, :, 1:nb + 1], tp[:, :, :, 2:nb + 2]], axis=4)

    kb, vb = band(k), band(v)
    jb = jnp.arange(nb)[:, None, None]
    qa = jnp.arange(blk)[None, :, None]
    ka = jnp.arange(3 * blk)[None, None, :]
    qi = jb * blk + qa
    ki = (jb - 1) * blk + ka
    mask = (jnp.abs(ki - qi) <= radius) & (ki >= 0) & (ki < sub)
    sc = jnp.einsum('bdhnqe,bdhnke->bdhnqk', qg, kb)
    sc = jnp.where(mask, sc, NEG_INF)
    m = jnp.max(sc, axis=-1, keepdims=True)
    p = jnp.exp(sc - m)
    den = jnp.sum(p, axis=-1, keepdims=True)
    o = jnp.einsum('bdhnqk,bdhnke->bdhnqe', p, vb) / den
    lse = (m + jnp.log(den))[..., 0]
    o = o.reshape(bn, dilation, nh, lp, hd)[:, :, :, :sub].transpose(0, 3, 1, 2, 4).reshape(bn, s, nh, hd)
    lse = lse.reshape(bn, dilation, nh, lp)[..., :sub].transpose(0, 3, 1, 2).reshape(bn, s, nh)
    return o, lse


def retention_direction(q, k, v, log_g, strict):
    bn, nh, s, dk = q.shape
    dv = v.shape[-1]
    cb = RET_CHUNK
    nc = s // cb
    qc = q.reshape(bn, nh, nc, cb, dk)
    kc = k.reshape(bn, nh, nc, cb, dk)
    vc = v.reshape(bn, nh, nc, cb, dv)
    i = jnp.arange(cb)[:, None]
    j = jnp.arange(cb)[None, :]
    keep = (i > j) if strict else (i >= j)
    diff = jnp.where(keep, i - j, 0).astype(jnp.float32)
    dmask = jnp.where(keep[None], jnp.exp(diff[None] * log_g[:, None, None]), 0.0)
    pos = jnp.arange(cb, dtype=jnp.float32)
    k_dec = jnp.exp((cb - 1 - pos)[None, :] * log_g[:, None])
    q_dec = jnp.exp((pos + 1)[None, :] * log_g[:, None])
    c_dec = jnp.exp(cb * log_g)
    scores = jnp.einsum('bhncd,bhnmd->bhncm', qc, kc) * dmask[None, :, None]
    intra = jnp.einsum('bhncm,bhnmv->bhncv', scores, vc)
    chunk_kv = jnp.einsum('bhnmd,hm,bhnmv->bhndv', kc, k_dec, vc)

    def step(state, kv_n):
        return state * c_dec[None, :, None, None] + kv_n, state

    _, prev = lax.scan(step, jnp.zeros((bn, nh, dk, dv), jnp.float32), jnp.moveaxis(chunk_kv, 2, 0))
    prev = jnp.moveaxis(prev, 0, 2)
    cross = jnp.einsum('bhncd,bhndv->bhncv', qc * q_dec[None, :, None, :, None], prev)
    return (intra + cross).reshape(bn, nh, s, dv)


def _cplx_combine(e1, e2):
    a1r, a1i, b1r, b1i = e1
    a2r, a2i, b2r, b2i = e2
    return (a2r * a1r - a2i * a1i,
            a2r * a1i + a2i * a1r,
            a2r * b1r - a2i * b1i + b2r,
            a2r * b1i + a2i * b1r + b2i)


def s5_direction(u, lam_re, lam_im, log_dt, b_re, b_im, c_re, c_im, reverse):
    s = u.shape[1]
    lam_re = lam_re.astype(jnp.float32)
    lam_im = lam_im.astype(jnp.float32)
    dt = jnp.exp(log_dt.astype(jnp.float32))[:, None]
    mag = jnp.exp(lam_re * dt)
    ar = mag * jnp.cos(lam_im * dt)
    ai = mag * jnp.sin(lam_im * dt)
    den = lam_re * lam_re + lam_im * lam_im
    nr = ar - 1.0
    cr = (nr * lam_re + ai * lam_im) / den
    ci = (ai * lam_re - nr * lam_im) / den
    b_re = b_re.astype(jnp.float32)
    b_im = b_im.astype(jnp.float32)
    bbr = cr[..., None] * b_re - ci[..., None] * b_im
    bbi = cr[..., None] * b_im + ci[..., None] * b_re
    xr = jnp.einsum('gpc,bsgc->bsgp', bbr, u)
    xi = jnp.einsum('gpc,bsgc->bsgp', bbi, u)
    ar_s = jnp.broadcast_to(ar, (s,) + ar.shape)
    ai_s = jnp.broadcast_to(ai, (s,) + ai.shape)

    def scan_one(br, bi):
        out = lax.associative_scan(_cplx_combine, (ar_s, ai_s, br, bi), reverse=reverse, axis=0)
        return out[2], out[3]

    hr, hi = jax.vmap(scan_one)(xr, xi)
    return (jnp.einsum('gcp,bsgp->bsgc', c_re.astype(jnp.float32), hr)
            - jnp.einsum('gcp,bsgp->bsgc', c_im.astype(jnp.float32), hi))


def setup_inputs(seed: int = 0) -> dict:
    key = jax.random.key(seed)
    ks = jax.random.split(key, 28)
    f32 = jnp.float32

    def normal(k, shape, scale):
        return jax.random.normal(k, shape, f32) * scale

    def gain(k, shape):
        return 1.0 + 0.02 * jax.random.normal(k, shape, f32)

    x = normal(ks[0], (BATCH, SEQ, D_MODEL), 1.0)
    mem = normal(ks[1], (BATCH, N_MEM, D_MODEL), 1.0)
    positions = (jax.random.randint(ks[2], (BATCH, 1), 0, 1024, dtype=jnp.int32)
                 + jnp.arange(SEQ, dtype=jnp.int32)[None, :])
    w_in = normal(ks[3], (DEPTH, D_MODEL, IN_WIDTH), D_MODEL ** -0.5)
    w_out = normal(ks[4], (DEPTH, D_MODEL, D_MODEL), D_MODEL ** -0.5)
    norm_mix_w = gain(ks[5], (DEPTH, D_MODEL))
    norm_cross_w = gain(ks[6], (DEPTH, D_MODEL))
    norm_mem_w = gain(ks[7], (DEPTH, D_MODEL))
    norm_ffn_w = gain(ks[8], (DEPTH, D_MODEL))
    final_norm_w = gain(ks[9], (D_MODEL,))
    ret_logit = np.log(2.0 ** (5.0 + np.arange(N_HEADS_B)) - 1.0).astype(np.float32)
    ret_decay = jnp.asarray(ret_logit)[None, None, :] + normal(ks[10], (DEPTH, 2, N_HEADS_B), 0.05)
    ret_gn_w = gain(ks[11], (DEPTH, WIDTH_B))
    s5_lam_re = -0.5 + normal(ks[12], (DEPTH, 2, S5_GROUPS, S5_STATE), 0.01)
    s5_lam_im = math.pi * jnp.arange(S5_STATE, dtype=f32) + normal(ks[13], (DEPTH, 2, S5_GROUPS, S5_STATE), 0.01)
    s5_log_dt = jax.random.uniform(ks[14], (DEPTH, 2, S5_GROUPS), f32, math.log(S5_DT_MIN), math.log(S5_DT_MAX))
    s5_b_re = normal(ks[15], (DEPTH, 2, S5_GROUPS, S5_STATE, S5_GROUP), (2 * S5_GROUP) ** -0.5)
    s5_b_im = normal(ks[16], (DEPTH, 2, S5_GROUPS, S5_STATE, S5_GROUP), (2 * S5_GROUP) ** -0.5)
    s5_c_re = normal(ks[17], (DEPTH, 2, S5_GROUPS, S5_GROUP, S5_STATE), S5_STATE ** -0.5)
    s5_c_im = normal(ks[18], (DEPTH, 2, S5_GROUPS, S5_GROUP, S5_STATE), S5_STATE ** -0.5)
    s5_d = normal(ks[19], (DEPTH, WIDTH_C), 1.0)
    s5_glu_w = normal(ks[20], (DEPTH, WIDTH_C, WIDTH_C), WIDTH_C ** -0.5)
    cross_wq = normal(ks[21], (DEPTH, D_MODEL, D_MODEL), D_MODEL ** -0.5)
    cross_wkv = normal(ks[22], (DEPTH, D_MODEL, 2 * D_MODEL), D_MODEL ** -0.5)
    cross_wo = normal(ks[23], (DEPTH, D_MODEL, D_MODEL), D_MODEL ** -0.5)
    router_w = normal(ks[24], (DEPTH, D_MODEL, N_EXPERTS), D_MODEL ** -0.5)
    expert_w_gate = normal(ks[25], (DEPTH, N_EXPERTS, D_MODEL, EXPERT_FF), D_MODEL ** -0.5)
    expert_w_up = normal(ks[26], (DEPTH, N_EXPERTS, D_MODEL, EXPERT_FF), D_MODEL ** -0.5)
    expert_w_down = normal(ks[27], (DEPTH, N_EXPERTS, EXPERT_FF, D_MODEL), EXPERT_FF ** -0.5)
    return {'x': x, 'mem': mem, 'positions': positions, 'w_in': w_in, 'w_out': w_out,
            'norm_mix_w': norm_mix_w, 'norm_cross_w': norm_cross_w, 'norm_mem_w': norm_mem_w,
            'norm_ffn_w': norm_ffn_w, 'final_norm_w': final_norm_w, 'ret_decay': ret_decay,
            'ret_gn_w': ret_gn_w, 's5_lam_re': s5_lam_re, 's5_lam_im': s5_lam_im, 's5_log_dt': s5_log_dt,
            's5_b_re': s5_b_re, 's5_b_im': s5_b_im, 's5_c_re': s5_c_re, 's5_c_im': s5_c_im,
            's5_d': s5_d, 's5_glu_w': s5_glu_w, 'cross_wq': cross_wq, 'cross_wkv': cross_wkv,
            'cross_wo': cross_wo, 'router_w': router_w, 'expert_w_gate': expert_w_gate,
            'expert_w_up': expert_w_up, 'expert_w_down': expert_w_down}


def reference(x, mem, positions, w_in, w_out, norm_mix_w, norm_cross_w, norm_mem_w, norm_ffn_w,
              final_norm_w, ret_decay, ret_gn_w, s5_lam_re, s5_lam_im, s5_log_dt, s5_b_re, s5_b_im,
              s5_c_re, s5_c_im, s5_d, s5_glu_w, cross_wq, cross_wkv, cross_wo, router_w,
              expert_w_gate, expert_w_up, expert_w_down):
    bn, s, _ = x.shape
    dt = x.dtype
    rope_freqs = ROPE_THETA ** (-(jnp.arange(ROT_DIM // 2, dtype=jnp.float32) * 2.0 / ROT_DIM))
    ret_freqs = RET_THETA ** (-jnp.linspace(0.0, 1.0, QK_HEAD_B // 2, dtype=jnp.float32))
    capacity = EC_CAPACITY_FACTOR * s // N_EXPERTS
    bidx = jnp.arange(bn)[:, None, None]
    for l in range(DEPTH):
        h = rmsnorm(x, norm_mix_w[l])
        proj = h @ w_in[l]
        qa, ka, va, qb, kb, vb, gb, uc = jnp.split(proj, IN_SPLITS, axis=-1)

        qa = rotate(qa.reshape(bn, s, N_HEADS_A, HEAD_DIM_A), positions, rope_freqs) * (HEAD_DIM_A ** -0.5)
        ka = rotate(ka.reshape(bn, s, N_HEADS_A, HEAD_DIM_A), positions, rope_freqs)
        va = va.reshape(bn, s, N_HEADS_A, HEAD_DIM_A).astype(jnp.float32)
        outs, lses = [], []
        for window, dil in DILATION_PAIRS:
            o_b, lse_b = dilated_branch(qa, ka, va, dil, window // (2 * dil))
            outs.append(o_b)
            lses.append(lse_b)
        wts = jax.nn.softmax(jnp.stack(lses, axis=-1), axis=-1)
        a_out = jnp.einsum('bshr,bshre->bshe', wts, jnp.stack(outs, axis=3)).reshape(bn, s, WIDTH_A).astype(dt)

        qr = rotate(qb.reshape(bn, s, N_HEADS_B, QK_HEAD_B), positions, ret_freqs).transpose(0, 2, 1, 3)
        kr = (rotate(kb.reshape(bn, s, N_HEADS_B, QK_HEAD_B), positions, ret_freqs)
              * (QK_HEAD_B ** -0.5)).transpose(0, 2, 1, 3)
        vr = vb.reshape(bn, s, N_HEADS_B, V_HEAD_B).astype(jnp.float32).transpose(0, 2, 1, 3)
        log_g = jax.nn.log_sigmoid(ret_decay[l].astype(jnp.float32))
        r_fwd = retention_direction(qr, kr, vr, log_g[0], strict=False)
        r_bwd = jnp.flip(retention_direction(jnp.flip(qr, 2), jnp.flip(kr, 2), jnp.flip(vr, 2), log_g[1], strict=True), 2)
        r = (r_fwd + r_bwd).transpose(0, 2, 1, 3)
        mu = jnp.mean(r, axis=-1, keepdims=True)
        var = jnp.mean(jnp.square(r - mu), axis=-1, keepdims=True)
        r = (r - mu) * lax.rsqrt(var + EPS) * ret_gn_w[l].astype(jnp.float32).reshape(N_HEADS_B, V_HEAD_B)
        b_out = (r.reshape(bn, s, WIDTH_B) * jax.nn.silu(gb.astype(jnp.float32))).astype(dt)

        u = uc.astype(jnp.float32).reshape(bn, s, S5_GROUPS, S5_GROUP)
        y = (s5_direction(u, s5_lam_re[l, 0], s5_lam_im[l, 0], s5_log_dt[l, 0], s5_b_re[l, 0], s5_b_im[l, 0],
                          s5_c_re[l, 0], s5_c_im[l, 0], reverse=False)
             + s5_direction(u, s5_lam_re[l, 1], s5_lam_im[l, 1], s5_log_dt[l, 1], s5_b_re[l, 1], s5_b_im[l, 1],
                            s5_c_re[l, 1], s5_c_im[l, 1], reverse=True)
             + u * s5_d[l].astype(jnp.float32).reshape(S5_GROUPS, S5_GROUP))
        y = jax.nn.gelu(y.reshape(bn, s, WIDTH_C))
        c_out = (y * jax.nn.sigmoid(y @ s5_glu_w[l].astype(jnp.float32))).astype(dt)

        x = x + jnp.concatenate([a_out, b_out, c_out], axis=-1) @ w_out[l]

        h = rmsnorm(x, norm_cross_w[l])
        mn = rmsnorm(mem, norm_mem_w[l])
        q = (h @ cross_wq[l]).reshape(bn, s, N_HEADS_X, HEAD_DIM_X).astype(jnp.float32)
        kv = mn @ cross_wkv[l]
        km = kv[..., :D_MODEL].reshape(bn, N_MEM, N_HEADS_X, HEAD_DIM_X).astype(jnp.float32)
        vm = kv[..., D_MODEL:].reshape(bn, N_MEM, N_HEADS_X, HEAD_DIM_X).astype(jnp.float32)
        p = jax.nn.softmax(jnp.einsum('bshd,bmhd->bhsm', q, km) * (HEAD_DIM_X ** -0.5), axis=-1)
        o = jnp.einsum('bhsm,bmhd->bshd', p, vm).reshape(bn, s, D_MODEL).astype(dt)
        x = x + o @ cross_wo[l]

        h = rmsnorm(x, norm_ffn_w[l])
        aff = jax.nn.softmax((h @ router_w[l]).astype(jnp.float32), axis=-1)
        gate, idx = lax.top_k(aff.transpose(0, 2, 1), capacity)
        xe = jax.vmap(lambda hb, ib: hb[ib])(h, idx)
        g = jnp.einsum('becd,edf->becf', xe, expert_w_gate[l])
        up = jnp.einsum('becd,edf->becf', xe, expert_w_up[l])
        ye = jnp.einsum('becf,efd->becd', jax.nn.silu(g) * up, expert_w_down[l]) * gate[..., None].astype(dt)
        x = x + jnp.zeros_like(x).at[bidx, idx].add(ye)
    return rmsnorm(x, final_norm_w)
```

```python
import numpy as np
import ml_dtypes
import concourse.bass as bass
import concourse.mybir as mybir
from concourse.bass_utils import run_bass_kernel_spmd

F32 = mybir.dt.float32
BF16 = mybir.dt.bfloat16
I32 = mybir.dt.int32
U32 = mybir.dt.uint32
AF = mybir.ActivationFunctionType
ALU = mybir.AluOpType
AX = mybir.AxisListType

D = 4096
NCORES = 8
EPS = 1e-6


class Buf:
    __slots__ = ("name", "w", "r", "dsem", "dcnt", "ws")

    def __init__(self, name):
        self.name = name
        self.ws = {}
        self.w = None
        self.r = {}
        self.dsem = None
        self.dcnt = 0


class Sched:
    def __init__(self, nc):
        self.nc = nc
        self.eng = {"pe": nc.tensor, "dve": nc.vector, "act": nc.scalar,
                    "pool": nc.gpsimd, "sp": nc.sync}
        self.sem = {k: nc.alloc_semaphore("s_" + k) for k in self.eng}
        self.cnt = {k: 0 for k in self.eng}
        self.seen = {k: {} for k in self.eng}
        self.nbuf = 0
        self.ndsem = 0
        self.final = []

    def buf(self, name=None):
        self.nbuf += 1
        return Buf(name or ("b%d" % self.nbuf))

    def bufs(self, n, name="b"):
        return [self.buf("%s%d" % (name, i)) for i in range(n)]

    def _waits(self, e, reads, writes):
        need = {}

        def add(ev):
            if ev is None:
                return
            k = ev[0]
            if k not in need or need[k][2] < ev[2]:
                need[k] = ev

        for b in reads:
            add(b.w)
            for ev in b.ws.values():
                add(ev)
        for b in writes:
            add(b.w)
            for ev in b.ws.values():
                add(ev)
            for ev in b.r.values():
                add(ev)
        eng = self.eng[e]
        seen = self.seen[e]
        for k, ev in need.items():
            if e == "pe" and k == "pe":
                continue
            if seen.get(k, 0) >= ev[2]:
                continue
            eng.wait_ge(ev[1], ev[2])
            seen[k] = ev[2]

    def _record(self, ev, reads, writes):
        for b in reads:
            b.r[ev[0]] = ev
        for b in writes:
            b.w = ev
            b.ws[ev[0]] = ev
            b.r = {}

    def op(self, e, fn, reads=(), writes=()):
        self._waits(e, reads, writes)
        ins = fn(self.eng[e])
        self.cnt[e] += 1
        ev = (e, self.sem[e], self.cnt[e])
        ins.then_inc(self.sem[e], 1)
        if e != "pe":
            pass
        self._record(ev, reads, writes)
        return ev

    def dma(self, e, fn, sb, reads=(), writes=()):
        self._waits(e, reads, writes)
        if sb.dsem is None:
            sb.dsem = self.nc.alloc_semaphore("d_%s" % sb.name)
            self.ndsem += 1
        ins = fn(self.eng[e])
        sb.dcnt += 16
        ev = ("d_" + sb.name, sb.dsem, sb.dcnt)
        ins.then_inc(sb.dsem, 16)
        self._record(ev, reads, writes)
        return ev

    def finish(self, evs, e="sp"):
        eng = self.eng[e]
        best = {}
        for ev in evs:
            if ev[0] not in best or best[ev[0]][2] < ev[2]:
                best[ev[0]] = ev
        for ev in best.values():
            eng.wait_ge(ev[1], ev[2])


def _host_consts():
    c = {}
    c["ident_bf"] = np.eye(128, dtype=np.float32).astype(ml_dtypes.bfloat16)
    c["ident_f"] = np.eye(128, dtype=np.float32)
    c["ones_bf"] = np.ones((128, 128), np.float32).astype(ml_dtypes.bfloat16)
    return c


class Ctx:
    def __init__(self):
        self.nc = bass.Bass("TRN2", target_bir_lowering=False)
        self.S = Sched(self.nc)
        self.ins = {}
        self.outs = []
        self.out_evs = []
        nc, S = self.nc, self.S
        self.ident_bf = nc.alloc_sbuf_tensor("sb_ident_bf", [128, 128], BF16)
        self.ident_f = nc.alloc_sbuf_tensor("sb_ident_f", [128, 128], F32)
        self.ones_bf = nc.alloc_sbuf_tensor("sb_ones_bf", [128, 128], BF16)
        self.eps_t = nc.alloc_sbuf_tensor("eps_t", [128, 1], F32)
        self.b_const = S.buf("consts")
        for nm, t, dt in (("ident_bf", self.ident_bf, BF16), ("ident_f", self.ident_f, F32),
                          ("ones_bf", self.ones_bf, BF16)):
            d = self.inp(nm, [128, 128], dt)
            S.dma("sp", lambda q, t=t, d=d: q.dma_start(out=t[:], in_=d), self.b_const,
                  writes=[self.b_const])
        S.op("dve", lambda v: v.memset(self.eps_t[:], EPS), writes=[self.b_const])
        self.halfpi_t = nc.alloc_sbuf_tensor("halfpi_t", [128, 1], F32)
        S.op("dve", lambda v: v.memset(self.halfpi_t[:], 1.5707963267948966), writes=[self.b_const])

    def inp(self, name, shape, dt):
        t = self.nc.dram_tensor(name, list(shape), dt, kind="ExternalInput")
        self.ins[name] = t
        return t.ap()

    def out(self, name, shape, dt):
        t = self.nc.dram_tensor(name, list(shape), dt, kind="ExternalOutput")
        self.outs.append(name)
        return t.ap()

    def scratch(self, name, shape, dt):
        return self.nc.dram_tensor(name, list(shape), dt, kind="Internal").ap()

    def run(self, in_maps):
        self.S.finish(self.out_evs)
        cst = _host_consts()
        maps = []
        for m in in_maps:
            mm = dict(cst)
            mm.update(m)
            maps.append(mm)
        res = run_bass_kernel_spmd(self.nc, maps, core_ids=list(range(len(maps))))
        return res.results


class Pool:
    def __init__(self, C, name, n, shape, dt, psum=False):
        self.tiles = []
        for i in range(n):
            nm = "%s%d" % (name, i)
            if psum:
                t = C.nc.alloc_psum_tensor(nm, list(shape), dt)
            else:
                t = C.nc.alloc_sbuf_tensor(nm, list(shape), dt)
            self.tiles.append((t, C.S.buf(nm)))
        self.i = 0

    def next(self):
        t = self.tiles[self.i % len(self.tiles)]
        self.i += 1
        return t


def rmsnorm_tile(C, xt, bx, wb, bw, hb, bh, P, out_f32=None):
    S = C.S
    ss, rs, junk = P["ss"], P["rs"], P["junk"]
    bs = P["b"]
    S.op("act", lambda a: a.activation(out=junk[:], in_=xt[:], func=AF.Square, accum_out=ss[:]),
         reads=[bx], writes=[bs])
    S.op("act", lambda a: a.activation(out=rs[:], in_=ss[:], func=AF.Sqrt, bias=C.eps_t[:], scale=1.0 / D),
         reads=[bs, C.b_const], writes=[bs])
    S.op("dve", lambda v: v.reciprocal(out=rs[:], in_=rs[:]), reads=[bs], writes=[bs])
    S.op("dve", lambda v: v.scalar_tensor_tensor(out=hb[:], in0=xt[:], scalar=rs[:, 0:1], in1=wb[:],
                                                 op0=ALU.mult, op1=ALU.mult),
         reads=[bx, bs, bw], writes=[bh])


def transpose_to(C, src, bsrc, dst_fn, bdst, nk, pst_pool, dt=BF16, evac=("act", "dve")):
    S = C.S
    ident = C.ident_bf if dt == BF16 else C.ident_f
    per = 8 if dt == BF16 else 4
    gi = 0
    for k0 in range(0, nk, per):
        n = min(per, nk - k0)
        pt, bpt = pst_pool.next()
        for j in range(n):
            S.op("pe", lambda p, j=j, pt=pt: p.transpose(out=pt[:, j, :], in_=src[:, (k0 + j) * 128:(k0 + j + 1) * 128],
                                                          identity=ident[:]),
                 reads=[bsrc, C.b_const], writes=[bpt])
        e = evac[gi % len(evac)]
        gi += 1
        dst = dst_fn(k0, n)
        if e == "act":
            S.op("act", lambda a, pt=pt, dst=dst, n=n: a.copy(out=dst, in_=pt[:, 0:n, :]), reads=[bpt], writes=[bdst])
        else:
            S.op("dve", lambda v, pt=pt, dst=dst, n=n: v.tensor_copy(out=dst, in_=pt[:, 0:n, :]), reads=[bpt], writes=[bdst])


def load_w_cast(C, wt, bw, src_ap):
    C.S.dma("pool", lambda q: q.dma_start(out=wt, in_=src_ap), bw, writes=[bw])


TB = 1024


def norm_block_to_hT(C, x_ap, t0, ntile, wb, bwb, hT, bhT, pools, also_tok=None):
    S = C.S
    for i in range(ntile):
        xt, bx = pools["x"].next()
        r0 = t0 + i * 128
        S.dma("sp", lambda q, xt=xt, r0=r0: q.dma_start(out=xt[:], in_=x_ap[r0:r0 + 128, :]), bx, writes=[bx])
        hb, bh = pools["h"].next()
        rmsnorm_tile(C, xt, bx, wb, bwb, hb, bh, pools["small"])
        if also_tok is not None:
            also_tok(i, hb, bh)
        transpose_to(C, hb, bh, lambda k0, n, i=i: hT[:, k0:k0 + n, i * 128:(i + 1) * 128], bhT, 32, pools["pst"])


def make_norm_pools(C, tag=""):
    nc = C.nc
    pools = {
        "x": Pool(C, "xt" + tag, 2, [128, D], F32),
        "h": Pool(C, "hb" + tag, 2, [128, D], BF16),
        "pst": Pool(C, "pst" + tag, 2, [128, 8, 128], BF16, psum=True),
    }
    pools["small"] = {
        "ss": nc.alloc_sbuf_tensor("ss" + tag, [128, 1], F32),
        "rs": nc.alloc_sbuf_tensor("rs" + tag, [128, 1], F32),
        "junk": nc.alloc_sbuf_tensor("junk" + tag, [128, D], BF16),
        "b": C.S.buf("small" + tag),
    }
    return pools


def gemm_fm(C, hT, bhT, nk, ntok, w_ap, m0, nm, out_cb, wpool, pspool):
    S = C.S
    wv = w_ap.rearrange("(kc p) m -> p kc m", p=128)
    for mi in range(nm):
        m = m0 + mi
        wt, bw = wpool.next()
        load_w_cast(C, wt[:, 0:nk, :], bw, wv[:, :, m * 128:(m + 1) * 128])
        for ts in range(ntok // 512):
            ps, bps = pspool.next()
            for k in range(nk):
                S.op("pe", lambda p, k=k, ps=ps, wt=wt, ts=ts: p.matmul(
                    ps[:], lhsT=wt[:, k, :], rhs=hT[:, k, ts * 512:(ts + 1) * 512],
                    start=(k == 0), stop=(k == nk - 1)),
                    reads=[bw, bhT], writes=[bps])
            out_cb(mi, ts, ps, bps)


def build_k1(l):
    C = Ctx()
    nc, S = C.nc, C.S
    Tc = 2048
    x = C.inp("x", [Tc, D], F32)
    nw = C.inp("nw", [128, D], F32)
    w_in = C.inp("w_in", [D, 10240], F32)
    projT = C.out("projT", [10240, Tc], BF16)
    wb = nc.alloc_sbuf_tensor("wb", [128, D], F32)
    bwb = S.buf("wb")
    S.dma("sp", lambda q: q.dma_start(out=wb[:], in_=nw), bwb, writes=[bwb])
    pools = make_norm_pools(C)
    hT = nc.alloc_sbuf_tensor("hT", [128, 32, TB], BF16)
    bhT = S.buf("hT")
    wpool = Pool(C, "w", 3, [128, 32, 128], BF16)
    pspool = Pool(C, "ps", 4, [128, 512], F32, psum=True)
    opool = Pool(C, "ot", 4, [128, 512], BF16)
    bproj = S.buf("projT")
    evs = []
    cnt = [0]
    for tb in range(Tc // TB):
        norm_block_to_hT(C, x, tb * TB, TB // 128, wb, bwb, hT, bhT, pools)

        def out_cb(mi, ts, ps, bps, tb=tb):
            ot, bo = opool.next()
            e = ("act", "dve")[cnt[0] % 2]
            cnt[0] += 1
            if e == "act":
                S.op("act", lambda a: a.copy(out=ot[:], in_=ps[:]), reads=[bps], writes=[bo])
            else:
                S.op("dve", lambda v: v.tensor_copy(out=ot[:], in_=ps[:]), reads=[bps], writes=[bo])
            c0 = tb * TB + ts * 512
            evs.append(S.dma("sp", lambda q: q.dma_start(out=projT[mi * 128:(mi + 1) * 128, c0:c0 + 512], in_=ot[:]),
                             bo, reads=[bo]))

        gemm_fm(C, hT, bhT, 32, TB, w_in, 0, 80, out_cb, wpool, pspool)
    C.out_evs += evs[-8:] + evs
    return C


_UID = [0]


class Phase:
    def __init__(self, C, name):
        self.C = C
        self.name = name
        self.cms = []
        self.n = 0

    def sb(self, shape, dt, nm=None):
        self.n += 1
        _UID[0] += 1
        cm = self.C.nc.sbuf_tensor("%s_%s%d_%d" % (self.name, nm or "t", self.n, _UID[0]), list(shape), dt)
        t = cm.__enter__()
        self.cms.append(cm)
        return t

    def ps(self, shape, dt, nm=None):
        self.n += 1
        _UID[0] += 1
        cm = self.C.nc.psum_tensor("%s_%s%d_%d" % (self.name, nm or "p", self.n, _UID[0]), list(shape), dt)
        t = cm.__enter__()
        self.cms.append(cm)
        return t

    def pool(self, nm, n, shape, dt, psum=False):
        p = Pool.__new__(Pool)
        p.tiles = []
        p.i = 0
        for i in range(n):
            t = self.ps(shape, dt, nm) if psum else self.sb(shape, dt, nm)
            p.tiles.append((t, self.C.S.buf("%s_%s%d" % (self.name, nm, i))))
        return p

    def close(self):
        self.C.S.barrier()
        for cm in reversed(self.cms):
            cm.__exit__(None, None, None)
        self.cms = []


def _sched_barrier(self):
    evs = [(k, self.sem[k], self.cnt[k]) for k in self.eng if self.cnt[k] > 0]
    evs += list(self.all_dma.values())
    for e, eng in self.eng.items():
        seen = self.seen[e]
        for ev in evs:
            if ev[0] == e:
                continue
            if seen.get(ev[0], 0) >= ev[2]:
                continue
            eng.wait_ge(ev[1], ev[2])
            seen[ev[0]] = ev[2]


Sched.barrier = _sched_barrier
_old_dma = Sched.dma


def _dma_track(self, e, fn, sb, reads=(), writes=()):
    ev = _old_dma(self, e, fn, sb, reads, writes)
    if not hasattr(self, "all_dma"):
        self.all_dma = {}
    self.all_dma[ev[0]] = ev
    return ev


Sched.dma = _dma_track
_old_init = Sched.__init__


def _init2(self, nc):
    _old_init(self, nc)
    self.all_dma = {}


Sched.__init__ = _init2


class _DSem:
    __slots__ = ("sem", "cnt", "key")


def _dma_v2(self, e, fn, sb, reads=(), writes=()):
    self._waits(e, reads, writes)
    if sb.dsem is None:
        if self.free_dsems:
            sb.dsem = self.free_dsems.pop()
        else:
            d = _DSem()
            d.key = "d%d" % self.ndsem
            d.sem = self.nc.alloc_semaphore("dsem%d" % self.ndsem)
            d.cnt = 0
            self.ndsem += 1
            sb.dsem = d
        self.bound.append(sb)
    d = sb.dsem
    ins = fn(self.eng[e])
    d.cnt += 16
    ev = (d.key, d.sem, d.cnt)
    ins.then_inc(d.sem, 16)
    self._record(ev, reads, writes)
    self.all_dma[d.key] = ev
    return ev


def _barrier_v2(self):
    _sched_barrier(self)
    for b in self.bound:
        self.free_dsems.append(b.dsem)
        b.dsem = None
    self.bound = []


def _init3(self, nc):
    _old_init(self, nc)
    self.all_dma = {}
    self.free_dsems = []
    self.bound = []


Sched.dma = _dma_v2
Sched.barrier = _barrier_v2
Sched.__init__ = _init3


import math

TWO_PI = 2.0 * math.pi
CW1 = 6.28125
CW2 = TWO_PI - CW1


def _mixer_consts():
    c = {}
    fa = 500000.0 ** (-(np.arange(16, dtype=np.float32) * 2.0 / 32.0))
    fcol = np.zeros((128, 1), np.float32)
    fcol[0:16, 0] = fa
    fcol[16:32, 0] = fa
    c["freq_a"] = fcol
    pa = np.zeros((128, 128), np.float32)
    for d in range(16):
        pa[d + 16, d] = -1.0
        pa[d, d + 16] = 1.0
    c["pmat_a"] = pa.astype(ml_dtypes.bfloat16)
    fb = 10000.0 ** (-np.linspace(0.0, 1.0, 64, dtype=np.float32))
    fcolb = np.concatenate([fb, fb]).reshape(128, 1).astype(np.float32)
    c["freq_b"] = fcolb
    pb = np.zeros((128, 128), np.float32)
    for d in range(64):
        pb[d + 64, d] = -1.0
        pb[d, d + 64] = 1.0
    c["pmat_b"] = pb.astype(ml_dtypes.bfloat16)
    a = np.arange(128)[:, None]
    b = np.arange(128)[None, :]
    m = np.stack([(a >= b), (a <= b), (a >= b) & (a >= 64), (a <= b) & (a < 64)]).astype(np.float32)
    c["amask"] = np.ascontiguousarray(m.transpose(1, 0, 2)).astype(ml_dtypes.bfloat16)
    return c


def load_const(C, ph, name, shape, dt, b):
    d = C.inp(name, shape, dt) if name not in C.ins else C.ins[name].ap()
    t = ph.sb(shape, dt, name)
    C.S.dma("sp", lambda q: q.dma_start(out=t[:], in_=d), b, writes=[b])
    return t


def make_trig_tables(C, ph, posb_ap, freq_t, bfreq, nrows, T, cos_t, sin_t, btab, offset=0.0):
    S = C.S
    CH = 2048
    pi_t = ph.sb([nrows, CH], I32, "posi")
    ang = ph.sb([nrows, CH], F32, "ang")
    qi = ph.sb([nrows, CH], I32, "qi")
    qf = ph.sb([nrows, CH], F32, "qf")
    rr = ph.sb([nrows, CH], F32, "rr")
    b = S.buf("trig_tmp")
    bp = S.buf("trig_pos")
    for c0 in range(0, T, CH):
        S.dma("sp", lambda q: q.dma_start(out=pi_t[:], in_=posb_ap[0:nrows, c0:c0 + CH]), bp, writes=[bp])
        S.op("dve", lambda v: v.tensor_copy(out=ang[:], in_=pi_t[:]), reads=[bp], writes=[b])
        S.op("dve", lambda v: v.tensor_scalar(out=ang[:], in0=ang[:], scalar1=freq_t[0:nrows, 0:1], scalar2=offset,
                                              op0=ALU.mult, op1=ALU.add), reads=[b, bfreq], writes=[b])
        S.op("dve", lambda v: v.tensor_scalar(out=qi[:], in0=ang[:], scalar1=1.0 / TWO_PI, scalar2=None,
                                              op0=ALU.mult), reads=[b], writes=[b])
        S.op("dve", lambda v: v.tensor_copy(out=qf[:], in_=qi[:]), reads=[b], writes=[b])
        S.op("dve", lambda v: v.scalar_tensor_tensor(out=rr[:], in0=qf[:], scalar=-CW1, in1=ang[:],
                                                     op0=ALU.mult, op1=ALU.add), reads=[b], writes=[b])
        S.op("dve", lambda v: v.scalar_tensor_tensor(out=rr[:], in0=qf[:], scalar=-CW2, in1=rr[:],
                                                     op0=ALU.mult, op1=ALU.add), reads=[b], writes=[b])
        S.op("dve", lambda v: v.tensor_scalar(out=qf[:], in0=rr[:], scalar1=math.pi, scalar2=None, op0=ALU.is_gt),
             reads=[b], writes=[b])
        S.op("dve", lambda v: v.scalar_tensor_tensor(out=ang[:], in0=qf[:], scalar=-TWO_PI, in1=rr[:],
                                                     op0=ALU.mult, op1=ALU.add), reads=[b], writes=[b])
        S.op("act", lambda a: a.activation(out=sin_t[:, c0:c0 + CH], in_=ang[:], func=AF.Sin), reads=[b], writes=[btab])
        S.op("dve", lambda v: v.tensor_scalar(out=qf[:], in0=rr[:], scalar1=math.pi / 2, scalar2=None, op0=ALU.is_gt),
             reads=[b], writes=[b])
        S.op("dve", lambda v: v.scalar_tensor_tensor(out=ang[:], in0=qf[:], scalar=-TWO_PI, in1=rr[:],
                                                     op0=ALU.mult, op1=ALU.add), reads=[b, btab], writes=[b])
        S.op("act", lambda a: a.activation(out=cos_t[:, c0:c0 + CH], in_=ang[:], func=AF.Sin, bias=C.halfpi_t[0:nrows, :]),
             reads=[b, C.b_const], writes=[btab])


def rope_inplace(C, ph, xt, bx, nrows, T, pmat, bconst, cos_t, sin_t, btab, pools):
    S = C.S
    for c0 in range(0, T, 512):
        pp, bpp = pools["rp"].next()
        S.op("pe", lambda p: p.matmul(pp[0:nrows, :], lhsT=pmat[0:nrows, 0:nrows], rhs=xt[0:nrows, c0:c0 + 512],
                                      start=True, stop=True), reads=[bx, bconst], writes=[bpp])
        t1, bt1 = pools["rt"].next()
        t2, bt2 = pools["rt"].next()
        S.op("dve", lambda v: v.tensor_tensor(out=t1[0:nrows, :], in0=pp[0:nrows, :], in1=sin_t[0:nrows, c0:c0 + 512],
                                              op=ALU.mult), reads=[bpp, btab], writes=[bt1])
        S.op("pool", lambda g: g.tensor_tensor(out=t2[0:nrows, :], in0=xt[0:nrows, c0:c0 + 512],
                                               in1=cos_t[0:nrows, c0:c0 + 512], op=ALU.mult),
             reads=[bx, btab], writes=[bt2])
        S.op("dve", lambda v: v.tensor_tensor(out=xt[0:nrows, c0:c0 + 512], in0=t1[0:nrows, :], in1=t2[0:nrows, :],
                                              op=ALU.add), reads=[bt1, bt2], writes=[bx])


SEQ = 4096
PADA = 1024


def mixer_a_head(C, ph, ld, out_ap, T, W):
    S = C.S
    qt, bq = W["q"]
    kp, bk = W["kp"]
    vp, bv = W["vp"]
    ld("q", qt, bq, 0)
    ld("k", kp, bk, PADA)
    ld("v", vp, bv, PADA)
    rope_inplace(C, ph, qt, bq, 32, T, W["pmat"], W["bconst"], W["cos"], W["sin"], W["btab"], W)
    rope_inplace(C, ph, kp[:, PADA:PADA + T], bk, 32, T, W["pmat"], W["bconst"], W["cos"], W["sin"], W["btab"], W)
    num, bnum = W["num"]
    den, bden = W["den"]
    amask = W["amask"]
    scale = 128.0 ** -0.5
    first = True
    for d in (1, 4, 16):
        sub = T // d
        nb = sub // 128
        for r in range(d):
            vprev = None
            for j in range(nb):
                po, bpo = W["po"].next()
                pd, bpd = W["pd"].next()
                for side in (0, 1):
                    K0 = 128 * j - 64 + 128 * side
                    c_lo = PADA + r + d * K0
                    ksl = kp[:, c_lo:c_lo + d * 127 + 1:d]
                    vsl = vp[:, c_lo:c_lo + d * 127 + 1:d]
                    q_lo = r + d * 128 * j
                    qsl = qt[:, q_lo:q_lo + d * 127 + 1:d]
                    if side == 0 and vprev is not None:
                        vb, bvb = vprev
                    else:
                        pv, bpv = W["pv"].next()
                        S.op("pe", lambda p: p.transpose(out=pv[:], in_=vsl, identity=C.ident_bf[:]),
                             reads=[bv, C.b_const], writes=[bpv])
                        vb, bvb = W["vb"].next()
                        S.op("dve", lambda v: v.tensor_copy(out=vb[:], in_=pv[:]), reads=[bpv], writes=[bvb])
                    if side == 1:
                        vprev = (vb, bvb)
                    pss, bps = W["pss"].next()
                    S.op("pe", lambda p: p.matmul(pss[:], lhsT=ksl, rhs=qsl, start=True, stop=True),
                         reads=[bk, bq], writes=[bps])
                    pt, bpt = W["pt"].next()
                    S.op("act", lambda a: a.activation(out=pt[:], in_=pss[:], func=AF.Exp, scale=scale),
                         reads=[bps], writes=[bpt])
                    mi = side
                    if j == 0 and side == 0:
                        mi = 2
                    if j == nb - 1 and side == 1:
                        mi = 3
                    S.op("pool", lambda g: g.tensor_tensor(out=pt[:], in0=pt[:], in1=amask[:, mi, :], op=ALU.mult),
                         reads=[bpt, W["bconst"]], writes=[bpt])
                    S.op("pe", lambda p: p.matmul(po[:], lhsT=vb[:], rhs=pt[:], start=(side == 0), stop=(side == 1)),
                         reads=[bvb, bpt], writes=[bpo])
                    S.op("pe", lambda p: p.matmul(pd[:], lhsT=C.ones_bf[:], rhs=pt[:], start=(side == 0), stop=(side == 1)),
                         reads=[C.b_const, bpt], writes=[bpd])
                q_lo = r + d * 128 * j
                nsl = num[:, q_lo:q_lo + d * 127 + 1:d]
                dsl = den[:, q_lo:q_lo + d * 127 + 1:d]
                if first:
                    S.op("act", lambda a: a.copy(out=nsl, in_=po[:]), reads=[bpo], writes=[bnum])
                    S.op("dve", lambda v: v.tensor_copy(out=dsl, in_=pd[:]), reads=[bpd], writes=[bden])
                else:
                    S.op("dve", lambda v: v.tensor_tensor(out=nsl, in0=po[:], in1=nsl, op=ALU.add),
                         reads=[bpo, bnum], writes=[bnum])
                    S.op("dve", lambda v: v.tensor_tensor(out=dsl, in0=pd[:], in1=dsl, op=ALU.add),
                         reads=[bpd, bden], writes=[bden])
        first = False
    ot, bo = W["ao"]
    for c0 in range(0, T, 1024):
        S.op("dve", lambda v: v.reciprocal(out=den[:, c0:c0 + 1024], in_=den[:, c0:c0 + 1024]), reads=[bden], writes=[bden])
        S.op("dve", lambda v: v.tensor_tensor(out=ot[:, c0:c0 + 1024], in0=num[:, c0:c0 + 1024],
                                              in1=den[:, c0:c0 + 1024], op=ALU.mult),
             reads=[bnum, bden], writes=[bo])
    return S.dma("sp", lambda q: q.dma_start(out=out_ap, in_=ot[:]), bo, reads=[bo])


def mixer_a_setup(C, ph, posb_ap, T):
    S = C.S
    W = {}
    bconst = S.buf("a_const")
    W["bconst"] = bconst
    W["pmat"] = load_const(C, ph, "pmat_a", [128, 128], BF16, bconst)
    W["amask"] = load_const(C, ph, "amask", [128, 4, 128], BF16, bconst)
    freq = load_const(C, ph, "freq_a", [128, 1], F32, bconst)
    W["cos"] = ph.sb([32, T], F32, "cos")
    W["sin"] = ph.sb([32, T], F32, "sin")
    W["btab"] = S.buf("a_tab")
    tp = Phase(C, ph.name + "_trig")
    make_trig_tables(C, tp, posb_ap, freq, bconst, 32, T, W["cos"], W["sin"], W["btab"])
    tp.close()
    W["q"] = (ph.sb([128, T], BF16, "q"), S.buf("a_q"))
    W["kp"] = (ph.sb([128, T + 2 * PADA], BF16, "kp"), S.buf("a_kp"))
    W["vp"] = (ph.sb([128, T + 2 * PADA], BF16, "vp"), S.buf("a_vp"))
    for nm in ("kp", "vp"):
        t, b = W[nm]
        S.op("pool", lambda g: g.memset(t[:, 0:PADA], 0.0), writes=[b])
        S.op("pool", lambda g: g.memset(t[:, PADA + T:], 0.0), writes=[b])
    W["num"] = (ph.sb([128, T], F32, "num"), S.buf("a_num"))
    W["den"] = (ph.sb([128, T], F32, "den"), S.buf("a_den"))
    W["ao"] = (ph.sb([128, T], BF16, "ao"), S.buf("a_ao"))
    W["rp"] = ph.pool("rp", 1, [128, 512], F32, psum=True)
    W["rt"] = ph.pool("rt", 4, [128, 512], F32)
    W["po"] = ph.pool("po", 2, [128, 128], F32, psum=True)
    W["pd"] = ph.pool("pd", 2, [128, 128], F32, psum=True)
    W["pv"] = ph.pool("pv", 1, [128, 128], BF16, psum=True)
    W["pss"] = ph.pool("pss", 2, [128, 128], F32, psum=True)
    W["vb"] = ph.pool("vb", 4, [128, 128], BF16)
    W["pt"] = ph.pool("pt", 3, [128, 128], BF16)
    return W


def _retention_consts():
    c = {}
    m = np.arange(128, dtype=np.float32)[:, None]
    cc = np.arange(128, dtype=np.float32)[None, :]
    diff = cc - m
    c["ret_dpos"] = np.maximum(diff, 0.0)
    c["ret_dneg"] = np.maximum(-diff, 0.0)
    c["ret_mge"] = (diff >= 0).astype(np.float32)
    c["ret_mlt"] = (diff < 0).astype(np.float32)
    c["ret_cp1"] = np.broadcast_to(cc + 1.0, (128, 128)).copy()
    c["ret_128mc"] = np.broadcast_to(128.0 - cc, (128, 128)).copy()
    col = np.zeros((128, 4), np.float32)
    col[:, 0] = 127.0 - m[:, 0]
    col[:, 1] = m[:, 0]
    col[:, 2] = 128.0
    c["ret_cols"] = col
    c["ones_f"] = np.ones((128, 128), np.float32)
    return c


def mixer_b_setup(C, ph, posb_ap, T):
    S = C.S
    W = {}
    bconst = S.buf("b_const")
    W["bconst"] = bconst
    W["pmat"] = load_const(C, ph, "pmat_b", [128, 128], BF16, bconst)
    for nm in ("ret_dpos", "ret_dneg", "ret_mge", "ret_mlt", "ret_cp1", "ret_128mc", "ones_f"):
        W[nm] = load_const(C, ph, nm, [128, 128], F32, bconst)
    W["ret_cols"] = load_const(C, ph, "ret_cols", [128, 4], F32, bconst)
    freq = load_const(C, ph, "freq_b", [128, 1], F32, bconst)
    W["cos"] = ph.sb([128, T], F32, "cos")
    W["sin"] = ph.sb([128, T], F32, "sin")
    W["btab"] = S.buf("b_tab")
    tp = Phase(C, ph.name + "_trig")
    make_trig_tables(C, tp, posb_ap, freq, bconst, 128, T, W["cos"], W["sin"], W["btab"])
    tp.close()
    return W


def mixer_b_head(C, ph0, ld, decay_t, bdec, hidx, gnw_t, bgn, out_ap, T, W):
    S = C.S
    ph = Phase(C, ph0.name + "_h")
    pp = Phase(C, ph0.name + "_pa")
    nch = T // 128
    sc = 128.0 ** -0.5
    bc = W["bconst"]
    sm = ph.sb([128, 16], F32, "sm")
    dm = ph.sb([128, 128], F32, "dm")
    tmp = ph.sb([128, 128], F32, "tmp")
    qdf = ph.sb([128, 128], F32, "qdf")
    qdb = ph.sb([128, 128], F32, "qdb")
    rT, brT = ph.sb([128, 2, T], F32, "rT"), S.buf("b_rT")
    qt, bq = pp.sb([128, T], BF16, "q"), S.buf("b_q")
    kt, bk = pp.sb([128, T], BF16, "k"), S.buf("b_k")
    vt, bv = pp.sb([128, 2, T], BF16, "v"), S.buf("b_v")
    ld("q", qt[:, :], bq)
    ld("k", kt[:, :], bk)
    ld("v0", vt[:, 0, :], bv)
    ld("v1", vt[:, 1, :], bv)
    rpools = {"rp": pp.pool("rp", 1, [128, 512], F32, psum=True), "rt": pp.pool("rt", 4, [128, 512], F32)}
    rope_inplace(C, ph, qt, bq, 128, T, W["pmat"], bc, W["cos"], W["sin"], W["btab"], rpools)
    rope_inplace(C, ph, kt, bk, 128, T, W["pmat"], bc, W["cos"], W["sin"], W["btab"], rpools)
    bsm = S.buf("b_sm")
    nh2 = decay_t.shape[1] // 2
    for dr in (0, 1):
        col = dr * nh2 + hidx
        S.op("act", lambda a: a.activation(out=sm[:, dr:dr + 1], in_=decay_t[:, col:col + 1], func=AF.Exp, scale=-1.0),
             reads=[bdec], writes=[bsm])
        S.op("dve", lambda v: v.tensor_scalar(out=sm[:, dr:dr + 1], in0=sm[:, dr:dr + 1], scalar1=1.0, scalar2=None,
                                              op0=ALU.add), reads=[bsm], writes=[bsm])
        S.op("act", lambda a: a.activation(out=sm[:, dr:dr + 1], in_=sm[:, dr:dr + 1], func=AF.Ln), reads=[bsm], writes=[bsm])
        S.op("dve", lambda v: v.tensor_scalar(out=sm[:, dr:dr + 1], in0=sm[:, dr:dr + 1], scalar1=-1.0, scalar2=None,
                                              op0=ALU.mult), reads=[bsm], writes=[bsm])
    lgf, lgb = sm[:, 0:1], sm[:, 1:2]
    cols = W["ret_cols"]
    S.op("act", lambda a: a.activation(out=sm[:, 2:3], in_=cols[:, 0:1], func=AF.Exp, scale=lgf), reads=[bsm, bc], writes=[bsm])
    S.op("act", lambda a: a.activation(out=sm[:, 3:4], in_=cols[:, 1:2], func=AF.Exp, scale=lgb), reads=[bsm, bc], writes=[bsm])
    S.op("act", lambda a: a.activation(out=sm[:, 4:5], in_=cols[:, 2:3], func=AF.Exp, scale=lgf), reads=[bsm, bc], writes=[bsm])
    S.op("act", lambda a: a.activation(out=sm[:, 5:6], in_=cols[:, 2:3], func=AF.Exp, scale=lgb), reads=[bsm, bc], writes=[bsm])
    bdm = S.buf("b_dm")
    S.op("act", lambda a: a.activation(out=dm[:], in_=W["ret_dpos"][:], func=AF.Exp, scale=lgf), reads=[bsm, bc], writes=[bdm])
    S.op("dve", lambda v: v.scalar_tensor_tensor(out=dm[:], in0=dm[:], scalar=sc, in1=W["ret_mge"][:], op0=ALU.mult, op1=ALU.mult),
         reads=[bdm, bc], writes=[bdm])
    S.op("act", lambda a: a.activation(out=tmp[:], in_=W["ret_dneg"][:], func=AF.Exp, scale=lgb), reads=[bsm, bc], writes=[bdm])
    S.op("dve", lambda v: v.scalar_tensor_tensor(out=tmp[:], in0=tmp[:], scalar=sc, in1=W["ret_mlt"][:], op0=ALU.mult, op1=ALU.mult),
         reads=[bdm, bc], writes=[bdm])
    S.op("dve", lambda v: v.tensor_tensor(out=dm[:], in0=dm[:], in1=tmp[:], op=ALU.add), reads=[bdm], writes=[bdm])
    S.op("act", lambda a: a.activation(out=qdf[:], in_=W["ret_cp1"][:], func=AF.Exp, scale=lgf), reads=[bsm, bc], writes=[bdm])
    S.op("act", lambda a: a.activation(out=qdb[:], in_=W["ret_128mc"][:], func=AF.Exp, scale=lgb), reads=[bsm, bc], writes=[bdm])
    S.op("dve", lambda v: v.tensor_scalar(out=qdf[:], in0=qdf[:], scalar1=sc, scalar2=None, op0=ALU.mult), reads=[bdm], writes=[bdm])
    S.op("dve", lambda v: v.tensor_scalar(out=qdb[:], in0=qdb[:], scalar1=sc, scalar2=None, op0=ALU.mult), reads=[bdm], writes=[bdm])
    qf, bqf = pp.sb([128, T], BF16, "qf"), S.buf("b_qf")
    qb, bqb = pp.sb([128, T], BF16, "qb"), S.buf("b_qb")
    for n in range(nch):
        sl = slice(n * 128, (n + 1) * 128)
        S.op("dve", lambda v: v.tensor_tensor(out=qf[:, sl], in0=qt[:, sl], in1=qdf[:], op=ALU.mult), reads=[bq, bdm], writes=[bqf])
        S.op("pool", lambda g: g.tensor_tensor(out=qb[:, sl], in0=qt[:, sl], in1=qdb[:], op=ALU.mult), reads=[bq, bdm], writes=[bqb])
    kf, bkf = pp.sb([128, nch, 128], BF16, "kf"), S.buf("b_kf")
    kb, bkb = pp.sb([128, nch, 128], BF16, "kb"), S.buf("b_kb")
    vtm, bvtm = pp.sb([128, nch, 256], BF16, "vtm"), S.buf("b_vtm")
    ptr = pp.pool("ptr", 1, [128, 3, 128], BF16, psum=True)
    for n in range(nch):
        sl = slice(n * 128, (n + 1) * 128)
        pt, bpt = ptr.next()
        S.op("pe", lambda p: p.transpose(out=pt[:, 0, :], in_=kt[:, sl], identity=C.ident_bf[:]), reads=[bk, C.b_const], writes=[bpt])
        S.op("pe", lambda p: p.transpose(out=pt[:, 1, :], in_=vt[:, 0, sl], identity=C.ident_bf[:]), reads=[bv, C.b_const], writes=[bpt])
        S.op("pe", lambda p: p.transpose(out=pt[:, 2, :], in_=vt[:, 1, sl], identity=C.ident_bf[:]), reads=[bv, C.b_const], writes=[bpt])
        S.op("act", lambda a: a.activation(out=kf[:, n, :], in_=pt[:, 0, :], func=AF.Copy, scale=sm[:, 2:3]), reads=[bpt, bsm], writes=[bkf])
        S.op("act", lambda a: a.activation(out=kb[:, n, :], in_=pt[:, 0, :], func=AF.Copy, scale=sm[:, 3:4]), reads=[bpt, bsm], writes=[bkb])
        S.op("dve", lambda v: v.tensor_copy(out=vtm[:, n, :], in_=pt[:, 1:3, :]), reads=[bpt], writes=[bvtm])
    sf, bsf = pp.sb([128, nch, 256], BF16, "sf"), S.buf("b_sf")
    sbk, bsb = pp.sb([128, nch, 256], BF16, "sbk"), S.buf("b_sb")
    st, bst = pp.sb([128, 256], F32, "st"), S.buf("b_st")
    pkv = pp.pool("pkv", 1, [128, 256], F32, psum=True)
    S.op("dve", lambda v: v.memset(st[:], 0.0), writes=[bst])
    S.op("pool", lambda g: g.memset(sf[:, 0, :], 0.0), writes=[bsf])
    for n in range(1, nch):
        pk, bpk = pkv.next()
        S.op("pe", lambda p: p.matmul(pk[:], lhsT=kf[:, n - 1, :], rhs=vtm[:, n - 1, :], start=True, stop=True),
             reads=[bkf, bvtm], writes=[bpk])
        S.op("dve", lambda v: v.scalar_tensor_tensor(out=st[:], in0=st[:], scalar=sm[:, 4:5], in1=pk[:], op0=ALU.mult, op1=ALU.add),
             reads=[bst, bsm, bpk], writes=[bst])
        S.op("act", lambda a: a.copy(out=sf[:, n, :], in_=st[:]), reads=[bst], writes=[bsf])
    S.op("dve", lambda v: v.memset(st[:], 0.0), reads=[bst], writes=[bst])
    S.op("pool", lambda g: g.memset(sbk[:, nch - 1, :], 0.0), writes=[bsb])
    for n in range(nch - 2, -1, -1):
        pk, bpk = pkv.next()
        S.op("pe", lambda p: p.matmul(pk[:], lhsT=kb[:, n + 1, :], rhs=vtm[:, n + 1, :], start=True, stop=True),
             reads=[bkb, bvtm], writes=[bpk])
        S.op("dve", lambda v: v.scalar_tensor_tensor(out=st[:], in0=st[:], scalar=sm[:, 5:6], in1=pk[:], op0=ALU.mult, op1=ALU.add),
             reads=[bst, bsm, bpk], writes=[bst])
        S.op("act", lambda a: a.copy(out=sbk[:, n, :], in_=st[:]), reads=[bst], writes=[bsb])
    pss = pp.pool("pss", 2, [128, 128], F32, psum=True)
    pout = pp.pool("pout", 2, [128, 128], F32, psum=True)
    pmt = pp.pool("pmt", 3, [128, 128], BF16)
    for n in range(nch):
        sl = slice(n * 128, (n + 1) * 128)
        ps_, bps = pss.next()
        S.op("pe", lambda p: p.matmul(ps_[:], lhsT=kt[:, sl], rhs=qt[:, sl], start=True, stop=True), reads=[bk, bq], writes=[bps])
        pm, bpm = pmt.next()
        S.op("dve", lambda v: v.tensor_tensor(out=pm[:], in0=ps_[:], in1=dm[:], op=ALU.mult), reads=[bps, bdm], writes=[bpm])
        for hf in (0, 1):
            po, bpo = pout.next()
            cs = slice(hf * 128, (hf + 1) * 128)
            S.op("pe", lambda p: p.matmul(po[:], lhsT=vtm[:, n, cs], rhs=pm[:], start=True, stop=False), reads=[bvtm, bpm], writes=[bpo])
            S.op("pe", lambda p: p.matmul(po[:], lhsT=sf[:, n, cs], rhs=qf[:, sl], start=False, stop=False), reads=[bsf, bqf], writes=[bpo])
            S.op("pe", lambda p: p.matmul(po[:], lhsT=sbk[:, n, cs], rhs=qb[:, sl], start=False, stop=True), reads=[bsb, bqb], writes=[bpo])
            if hf == 0:
                S.op("act", lambda a: a.copy(out=rT[:, hf, sl], in_=po[:]), reads=[bpo], writes=[brT])
            else:
                S.op("dve", lambda v: v.tensor_copy(out=rT[:, hf, sl], in_=po[:]), reads=[bpo], writes=[brT])
    pp.close()
    gt, bg = ph.sb([128, 2, T], BF16, "g"), S.buf("b_g")
    ld("g0", gt[:, 0, :], bg)
    ld("g1", gt[:, 1, :], bg)
    sq, bsq = ph.sb([128, 2, 512], F32, "sq"), S.buf("b_sq")
    pst = ph.pool("pst", 2, [128, 2, 512], F32, psum=True)
    mv, bmv = ph.sb([128, 4, 512], F32, "mv"), S.buf("b_mv")
    ot, bo = ph.sb([128, 2, T], BF16, "ot"), S.buf("b_ot")
    sg, bsg = ph.sb([128, 2, 512], F32, "sg"), S.buf("b_sg")
    onesf = W["ones_f"]
    for c0 in range(0, T, 512):
        cs = slice(c0, c0 + 512)
        S.op("act", lambda a: a.activation(out=sq[:], in_=rT[:, :, cs], func=AF.Square), reads=[brT], writes=[bsq])
        p2, bp2 = pst.next()
        for hf in (0, 1):
            S.op("pe", lambda p: p.matmul(p2[:, 0, :], lhsT=onesf[:], rhs=rT[:, hf, cs], start=(hf == 0), stop=(hf == 1)),
                 reads=[bc, brT], writes=[bp2])
        for hf in (0, 1):
            S.op("pe", lambda p: p.matmul(p2[:, 1, :], lhsT=onesf[:], rhs=sq[:, hf, :], start=(hf == 0), stop=(hf == 1)),
                 reads=[bc, bsq], writes=[bp2])
        S.op("dve", lambda v: v.tensor_scalar(out=mv[:, 0, :], in0=p2[:, 0, :], scalar1=1.0 / 256, scalar2=None, op0=ALU.mult),
             reads=[bp2], writes=[bmv])
        S.op("dve", lambda v: v.tensor_tensor(out=mv[:, 1, :], in0=mv[:, 0, :], in1=mv[:, 0, :], op=ALU.mult), reads=[bmv], writes=[bmv])
        S.op("dve", lambda v: v.scalar_tensor_tensor(out=mv[:, 1, :], in0=p2[:, 1, :], scalar=1.0 / 256, in1=mv[:, 1, :],
                                                     op0=ALU.mult, op1=ALU.subtract), reads=[bp2, bmv], writes=[bmv])
        S.op("act", lambda a: a.activation(out=mv[:, 1, :], in_=mv[:, 1, :], func=AF.Sqrt, bias=C.eps_t[:], scale=1.0),
             reads=[bmv, C.b_const], writes=[bmv])
        S.op("dve", lambda v: v.reciprocal(out=mv[:, 1, :], in_=mv[:, 1, :]), reads=[bmv], writes=[bmv])
        S.op("act", lambda a: a.activation(out=sg[:], in_=gt[:, :, cs], func=AF.Silu), reads=[bg], writes=[bsg])
        for hf in (0, 1):
            S.op("dve", lambda v: v.tensor_tensor(out=mv[:, 2, :], in0=rT[:, hf, cs], in1=mv[:, 0, :], op=ALU.subtract),
                 reads=[brT, bmv], writes=[bmv])
            S.op("dve", lambda v: v.scalar_tensor_tensor(out=mv[:, 2, :], in0=mv[:, 2, :], scalar=gnw_t[:, hf:hf + 1], in1=mv[:, 1, :],
                                                         op0=ALU.mult, op1=ALU.mult), reads=[bmv, bgn], writes=[bmv])
            S.op("dve", lambda v: v.tensor_tensor(out=ot[:, hf, cs], in0=mv[:, 2, :], in1=sg[:, hf, :], op=ALU.mult),
                 reads=[bmv, bsg], writes=[bo])
    ev = S.dma("sp", lambda q: q.dma_start(out=out_ap.rearrange("(h p) t -> p h t", p=128), in_=ot[:]), bo, reads=[bo])
    ph.close()
    return ev


def _s5_consts(T):
    return {"tidx": np.broadcast_to(np.arange(T, dtype=np.int32), (128, T)).copy()}


def sincos_col(C, ph, ang, bang, s_out, c_out, bout, tmp, btmp):
    S = C.S
    a2, qi, qf, rr = tmp["f"][:, 0:1], tmp["i"][:, 0:1], tmp["f"][:, 1:2], tmp["f"][:, 2:3]
    m = tmp["f"][:, 3:4]
    S.op("dve", lambda v: v.tensor_scalar(out=a2, in0=ang, scalar1=8 * math.pi, scalar2=None, op0=ALU.add), reads=[bang], writes=[btmp])
    S.op("dve", lambda v: v.tensor_scalar(out=qi, in0=a2, scalar1=1.0 / TWO_PI, scalar2=None, op0=ALU.mult), reads=[btmp], writes=[btmp])
    S.op("dve", lambda v: v.tensor_copy(out=qf, in_=qi), reads=[btmp], writes=[btmp])
    S.op("dve", lambda v: v.scalar_tensor_tensor(out=rr, in0=qf, scalar=-CW1, in1=a2, op0=ALU.mult, op1=ALU.add), reads=[btmp], writes=[btmp])
    S.op("dve", lambda v: v.scalar_tensor_tensor(out=rr, in0=qf, scalar=-CW2, in1=rr, op0=ALU.mult, op1=ALU.add), reads=[btmp], writes=[btmp])
    S.op("dve", lambda v: v.tensor_scalar(out=m, in0=rr, scalar1=math.pi, scalar2=None, op0=ALU.is_gt), reads=[btmp], writes=[btmp])
    S.op("dve", lambda v: v.scalar_tensor_tensor(out=a2, in0=m, scalar=-TWO_PI, in1=rr, op0=ALU.mult, op1=ALU.add), reads=[btmp], writes=[btmp])
    S.op("act", lambda a: a.activation(out=s_out, in_=a2, func=AF.Sin), reads=[btmp], writes=[bout])
    S.op("dve", lambda v: v.tensor_scalar(out=m, in0=rr, scalar1=math.pi / 2, scalar2=None, op0=ALU.is_gt), reads=[btmp], writes=[btmp])
    S.op("dve", lambda v: v.scalar_tensor_tensor(out=a2, in0=m, scalar=-TWO_PI, in1=rr, op0=ALU.mult, op1=ALU.add), reads=[btmp, bout], writes=[btmp])
    S.op("act", lambda a: a.activation(out=c_out, in_=a2, func=AF.Sin, bias=C.halfpi_t[:, :]), reads=[btmp, C.b_const], writes=[bout])


def mixer_c_pair(C, ph0, ld_u, prm, dcol_ap, tidx_ap, out_ap, T):
    S = C.S
    ph = Phase(C, ph0.name + "_c")
    ut, bu = ph.sb([32, T], BF16, "u"), S.buf("c_u")
    ld_u(ut, bu)
    Y, bY = ph.sb([32, T], F32, "Y"), S.buf("c_Y")
    cos_t = ph.sb([128, T], F32, "cos")
    sin_t = ph.sb([128, T], F32, "sin")
    btab = S.buf("c_tab")
    pr = ph.sb([128, 24], F32, "pr")
    bpr = S.buf("c_pr")
    tmpd = {"f": ph.sb([128, 4], F32, "tf"), "i": ph.sb([128, 1], I32, "ti")}
    btmp = S.buf("c_tmp")
    bmat = ph.sb([128, 4, 16], F32, "bmat")
    bd = ph.sb([128, 4, 32], F32, "bd")
    bbd = S.buf("c_bd")
    lhs_b = ph.sb([32, 2, 128], BF16, "lhsb")
    lhs_c = ph.sb([128, 2, 32], BF16, "lhsc")
    blhs = S.buf("c_lhs")
    RB = ph.sb([128, 512], F32, "RB")
    bRB = S.buf("c_RB")
    carry = ph.sb([128, 2], F32, "carry")
    bcar = S.buf("c_car")
    dcol = ph.sb([32, 1], F32, "dcol")
    bdc = S.buf("c_dcol")
    S.dma("sp", lambda q: q.dma_start(out=dcol[:], in_=dcol_ap), bdc, writes=[bdc])
    wk = ph.pool("wk", 12, [128, 512], F32)
    hb = ph.pool("hb", 4, [128, 512], BF16)
    px = ph.pool("px", 4, [128, 512], F32, psum=True)
    py = ph.pool("py", 2, [32, 512], F32, psum=True)
    ptp = ph.pool("ptp", 1, [32, 128], F32, psum=True)
    for dr in (0, 1):
        P = prm[dr]
        sg = 1.0 if dr == 0 else -1.0
        for i, nm in enumerate(("lam_re", "lam_im", "logdt")):
            S.dma("sp", lambda q, i=i, nm=nm: q.dma_start(out=pr[:, i:i + 1], in_=P[nm]), bpr, writes=[bpr])
        for i, nm in enumerate(("b_re", "b_im", "c_reT", "c_imT")):
            S.dma("sp", lambda q, i=i, nm=nm: q.dma_start(out=bmat[:, i, :], in_=P[nm]), bbd, writes=[bbd])
        c_ = lambda i: pr[:, i:i + 1]
        S.op("act", lambda a: a.activation(out=c_(2), in_=c_(2), func=AF.Exp), reads=[bpr], writes=[bpr])
        S.op("dve", lambda v: v.tensor_tensor(out=c_(3), in0=c_(0), in1=c_(2), op=ALU.mult), reads=[bpr], writes=[bpr])
        S.op("act", lambda a: a.activation(out=c_(3), in_=c_(3), func=AF.Exp), reads=[bpr], writes=[bpr])
        S.op("dve", lambda v: v.tensor_tensor(out=c_(4), in0=c_(1), in1=c_(2), op=ALU.mult), reads=[bpr], writes=[bpr])
        sincos_col(C, ph, c_(4), bpr, c_(5), c_(6), bpr, tmpd, btmp)
        S.op("dve", lambda v: v.tensor_tensor(out=c_(7), in0=c_(3), in1=c_(6), op=ALU.mult), reads=[bpr], writes=[bpr])
        S.op("dve", lambda v: v.tensor_tensor(out=c_(8), in0=c_(3), in1=c_(5), op=ALU.mult), reads=[bpr], writes=[bpr])
        S.op("dve", lambda v: v.tensor_tensor(out=c_(9), in0=c_(0), in1=c_(0), op=ALU.mult), reads=[bpr], writes=[bpr])
        S.op("dve", lambda v: v.scalar_tensor_tensor(out=c_(9), in0=c_(1), scalar=c_(1), in1=c_(9), op0=ALU.mult, op1=ALU.add), reads=[bpr], writes=[bpr])
        S.op("dve", lambda v: v.reciprocal(out=c_(9), in_=c_(9)), reads=[bpr], writes=[bpr])
        S.op("dve", lambda v: v.tensor_scalar(out=c_(10), in0=c_(7), scalar1=-1.0, scalar2=None, op0=ALU.add), reads=[bpr], writes=[bpr])
        S.op("dve", lambda v: v.tensor_tensor(out=c_(13), in0=c_(10), in1=c_(0), op=ALU.mult), reads=[bpr], writes=[bpr])
        S.op("dve", lambda v: v.scalar_tensor_tensor(out=c_(13), in0=c_(8), scalar=c_(1), in1=c_(13), op0=ALU.mult, op1=ALU.add), reads=[bpr], writes=[bpr])
        S.op("dve", lambda v: v.tensor_tensor(out=c_(11), in0=c_(13), in1=c_(9), op=ALU.mult), reads=[bpr], writes=[bpr])
        S.op("dve", lambda v: v.tensor_tensor(out=c_(13), in0=c_(8), in1=c_(0), op=ALU.mult), reads=[bpr], writes=[bpr])
        S.op("dve", lambda v: v.tensor_tensor(out=c_(14), in0=c_(10), in1=c_(1), op=ALU.mult), reads=[bpr], writes=[bpr])
        S.op("dve", lambda v: v.tensor_tensor(out=c_(13), in0=c_(13), in1=c_(14), op=ALU.subtract), reads=[bpr], writes=[bpr])
        S.op("dve", lambda v: v.tensor_tensor(out=c_(12), in0=c_(13), in1=c_(9), op=ALU.mult), reads=[bpr], writes=[bpr])
        S.op("dve", lambda v: v.tensor_scalar(out=c_(15), in0=c_(12), scalar1=-1.0, scalar2=None, op0=ALU.mult), reads=[bpr], writes=[bpr])
        S.op("pool", lambda g: g.memset(bd[:], 0.0), reads=[bbd], writes=[bbd])
        for gi in (0, 1):
            rs = slice(gi * 64, (gi + 1) * 64)
            cs = slice(gi * 16, (gi + 1) * 16)
            S.op("dve", lambda v: v.tensor_scalar(out=bd[rs, 0, cs], in0=bmat[rs, 0, :], scalar1=pr[rs, 11:12], scalar2=None, op0=ALU.mult),
                 reads=[bbd, bpr], writes=[bbd])
            S.op("dve", lambda v: v.scalar_tensor_tensor(out=bd[rs, 0, cs], in0=bmat[rs, 1, :], scalar=pr[rs, 15:16], in1=bd[rs, 0, cs],
                                                         op0=ALU.mult, op1=ALU.add), reads=[bbd, bpr], writes=[bbd])
            S.op("dve", lambda v: v.tensor_scalar(out=bd[rs, 1, cs], in0=bmat[rs, 1, :], scalar1=pr[rs, 11:12], scalar2=None, op0=ALU.mult),
                 reads=[bbd, bpr], writes=[bbd])
            S.op("dve", lambda v: v.scalar_tensor_tensor(out=bd[rs, 1, cs], in0=bmat[rs, 0, :], scalar=pr[rs, 12:13], in1=bd[rs, 1, cs],
                                                         op0=ALU.mult, op1=ALU.add), reads=[bbd, bpr], writes=[bbd])
            S.op("dve", lambda v: v.tensor_copy(out=bd[rs, 2, cs], in_=bmat[rs, 2, :]), reads=[bbd], writes=[bbd])
            S.op("dve", lambda v: v.tensor_scalar(out=bd[rs, 3, cs], in0=bmat[rs, 3, :], scalar1=-1.0, scalar2=None, op0=ALU.mult),
                 reads=[bbd], writes=[bbd])
        for i in (0, 1):
            pt, bpt = ptp.next()
            S.op("pe", lambda p: p.transpose(out=pt[:], in_=bd[:, i, :], identity=C.ident_f[:]), reads=[bbd, C.b_const], writes=[bpt])
            S.op("dve", lambda v: v.tensor_copy(out=lhs_b[:, i, :], in_=pt[:]), reads=[bpt], writes=[blhs])
        S.op("dve", lambda v: v.tensor_copy(out=lhs_c[:, :, :], in_=bd[:, 2:4, :]), reads=[bbd], writes=[blhs])
        S.op("dve", lambda v: v.memset(RB[:], 1.0), reads=[bRB], writes=[bRB])
        S.op("dve", lambda v: v.tensor_scalar(out=RB[:], in0=RB[:], scalar1=pr[:, 3:4], scalar2=None, op0=ALU.mult), reads=[bRB, bpr], writes=[bRB])
        tp = Phase(C, ph.name + "_trig%d" % dr)
        make_trig_tables(C, tp, tidx_ap, pr[:, 4:5], bpr, 128, T, cos_t, sin_t, btab, offset=8 * math.pi)
        tp.close()
        S.op("dve", lambda v: v.memset(carry[:], 0.0), reads=[bcar], writes=[bcar])
        ntile = T // 512
        order = range(ntile) if dr == 0 else range(ntile - 1, -1, -1)
        for ti in order:
            cs = slice(ti * 512, (ti + 1) * 512)
            pxr, bpxr = px.next()
            pxi, bpxi = px.next()
            S.op("pe", lambda p: p.matmul(pxr[:], lhsT=lhs_b[:, 0, :], rhs=ut[:, cs], start=True, stop=True), reads=[blhs, bu], writes=[bpxr])
            S.op("pe", lambda p: p.matmul(pxi[:], lhsT=lhs_b[:, 1, :], rhs=ut[:, cs], start=True, stop=True), reads=[blhs, bu], writes=[bpxi])
            (a1, ba1), (a2, ba2), (a3, ba3), (a4, ba4) = wk.next(), wk.next(), wk.next(), wk.next()
            S.op("dve", lambda v: v.tensor_tensor(out=a1[:], in0=pxr[:], in1=cos_t[:, cs], op=ALU.mult), reads=[bpxr, btab], writes=[ba1])
            S.op("dve", lambda v: v.tensor_tensor(out=a2[:], in0=pxi[:], in1=sin_t[:, cs], op=ALU.mult), reads=[bpxi, btab], writes=[ba2])
            S.op("dve", lambda v: v.tensor_tensor(out=a3[:], in0=pxi[:], in1=cos_t[:, cs], op=ALU.mult), reads=[bpxi, btab], writes=[ba3])
            S.op("dve", lambda v: v.tensor_tensor(out=a4[:], in0=pxr[:], in1=sin_t[:, cs], op=ALU.mult), reads=[bpxr, btab], writes=[ba4])
            opr = ALU.add if dr == 0 else ALU.subtract
            opi = ALU.subtract if dr == 0 else ALU.add
            S.op("pool", lambda g: g.tensor_tensor(out=a1[:], in0=a1[:], in1=a2[:], op=opr), reads=[ba1, ba2], writes=[ba1])
            S.op("pool", lambda g: g.tensor_tensor(out=a3[:], in0=a3[:], in1=a4[:], op=opi), reads=[ba3, ba4], writes=[ba3])
            (hr_, bhr), (hi_, bhi) = wk.next(), wk.next()
            rev = (lambda t: t[:, ::-1]) if dr == 1 else (lambda t: t[:, :])
            last = 0 if dr == 1 else 511
            S.op("dve", lambda v: v.tensor_tensor_scan(out=rev(hr_), data0=RB[:], data1=rev(a1), initial=carry[:, 0:1], op0=ALU.mult, op1=ALU.add),
                 reads=[bRB, ba1, bcar], writes=[bhr])
            S.op("dve", lambda v: v.tensor_tensor_scan(out=rev(hi_), data0=RB[:], data1=rev(a3), initial=carry[:, 1:2], op0=ALU.mult, op1=ALU.add),
                 reads=[bRB, ba3, bcar], writes=[bhi])
            S.op("dve", lambda v: v.tensor_copy(out=carry[:, 0:1], in_=hr_[:, last:last + 1]), reads=[bhr], writes=[bcar])
            S.op("dve", lambda v: v.tensor_copy(out=carry[:, 1:2], in_=hi_[:, last:last + 1]), reads=[bhi], writes=[bcar])
            (b1, bb1), (b2, bb2), (b3, bb3), (b4, bb4) = wk.next(), wk.next(), wk.next(), wk.next()
            S.op("pool", lambda g: g.tensor_tensor(out=b1[:], in0=hr_[:], in1=cos_t[:, cs], op=ALU.mult), reads=[bhr, btab], writes=[bb1])
            S.op("pool", lambda g: g.tensor_tensor(out=b2[:], in0=hi_[:], in1=sin_t[:, cs], op=ALU.mult), reads=[bhi, btab], writes=[bb2])
            S.op("pool", lambda g: g.tensor_tensor(out=b3[:], in0=hi_[:], in1=cos_t[:, cs], op=ALU.mult), reads=[bhi, btab], writes=[bb3])
            S.op("pool", lambda g: g.tensor_tensor(out=b4[:], in0=hr_[:], in1=sin_t[:, cs], op=ALU.mult), reads=[bhr, btab], writes=[bb4])
            (h1, bh1), (h2, bh2) = hb.next(), hb.next()
            S.op("dve", lambda v: v.scalar_tensor_tensor(out=h1[:], in0=b2[:], scalar=-sg, in1=b1[:], op0=ALU.mult, op1=ALU.add),
                 reads=[bb1, bb2], writes=[bh1])
            S.op("dve", lambda v: v.scalar_tensor_tensor(out=h2[:], in0=b4[:], scalar=sg, in1=b3[:], op0=ALU.mult, op1=ALU.add),
                 reads=[bb3, bb4], writes=[bh2])
            pyt, bpy = py.next()
            S.op("pe", lambda p: p.matmul(pyt[:], lhsT=lhs_c[:, 0, :], rhs=h1[:], start=True, stop=False), reads=[blhs, bh1], writes=[bpy])
            S.op("pe", lambda p: p.matmul(pyt[:], lhsT=lhs_c[:, 1, :], rhs=h2[:], start=False, stop=True), reads=[blhs, bh2], writes=[bpy])
            if dr == 0:
                S.op("act", lambda a: a.copy(out=Y[:, cs], in_=pyt[:]), reads=[bpy], writes=[bY])
            else:
                S.op("dve", lambda v: v.tensor_tensor(out=Y[:, cs], in0=pyt[:], in1=Y[:, cs], op=ALU.add), reads=[bpy, bY], writes=[bY])
    yo, byo = ph.sb([32, T], BF16, "yo"), S.buf("c_yo")
    for c0 in range(0, T, 2048):
        cs = slice(c0, c0 + 2048)
        S.op("dve", lambda v: v.scalar_tensor_tensor(out=Y[:, cs], in0=ut[:, cs], scalar=dcol[:, 0:1], in1=Y[:, cs], op0=ALU.mult, op1=ALU.add),
             reads=[bu, bdc, bY], writes=[bY])
        S.op("act", lambda a: a.activation(out=yo[:, cs], in_=Y[:, cs], func=AF.Gelu), reads=[bY], writes=[byo])
    ev = S.dma("sp", lambda q: q.dma_start(out=out_ap, in_=yo[:]), byo, reads=[byo])
    ph.close()
    return ev


def load_w(C, wt_ap, bw, src_ap):
    if src_ap.dtype == BF16:
        C.S.dma("sp", lambda q: q.dma_start(out=wt_ap, in_=src_ap), bw, writes=[bw])
    else:
        C.S.dma("pool", lambda q: q.dma_start(out=wt_ap, in_=src_ap), bw, writes=[bw])


class WMat:
    def __init__(self, ap3, col0=0):
        self.ap3 = ap3
        self.cw = ap3.shape[2]
        self.col0 = col0

    def blk(self, c0, wn):
        c0 += self.col0
        bi, off = c0 // self.cw, c0 % self.cw
        return self.ap3[bi].rearrange("(kc p) c -> p kc c", p=128)[:, :, off:off + wn]


def wblk(w_ap, c0, wn):
    if hasattr(w_ap, "blk"):
        return w_ap.blk(c0, wn)
    return w_ap.rearrange("(kc p) n -> p kc n", p=128)[:, :, c0:c0 + wn]


def gemm_tok(C, act_fn, bact, nk, ntt, w_ap, nn, cb, wpool, pspool, wn=512):
    S = C.S
    for n in range(nn):
        wt, bw = wpool.next()
        load_w(C, wt[:, 0:nk, 0:wn], bw, wblk(w_ap, n * wn, wn))
        for tt in range(ntt):
            ps, bps = pspool.next()
            for k in range(nk):
                S.op("pe", lambda p, k=k: p.matmul(ps[:, 0:wn], lhsT=act_fn(k, tt), rhs=wt[:, k, 0:wn],
                                                   start=(k == 0), stop=(k == nk - 1)),
                     reads=[bw, bact], writes=[bps])
            cb(tt, n, ps, bps)


def gemm_fm2(C, act_fn, bact, nk, ntok, w_ap, nmb, cb, wpool, pspool, wn=512):
    S = C.S
    for nb in range(nmb):
        wt, bw = wpool.next()
        load_w(C, wt[:, 0:nk, 0:wn], bw, wblk(w_ap, nb * wn, wn))
        for mi in range(wn // 128):
            ps, bps = pspool.next()
            for k in range(nk):
                S.op("pe", lambda p, k=k: p.matmul(ps[:, 0:ntok], lhsT=wt[:, k, mi * 128:(mi + 1) * 128], rhs=act_fn(k),
                                                   start=(k == 0), stop=(k == nk - 1)),
                     reads=[bw, bact], writes=[bps])
            cb(nb * (wn // 128) + mi, ps, bps)


def phase_mem_kv(C, mem_ap, nwb_ap, wkv_ap, kmT_d, vm_d, bkm, bvm):
    S = C.S
    ph = Phase(C, "mkv")
    wb = ph.sb([128, D], F32, "wb")
    bwb = S.buf("mkv_wb")
    S.dma("sp", lambda q: q.dma_start(out=wb[:], in_=nwb_ap), bwb, writes=[bwb])
    pools = {"x": ph.pool("x", 2, [128, D], F32), "h": ph.pool("h", 2, [128, D], BF16),
             "pst": ph.pool("pst", 2, [128, 8, 128], BF16, psum=True),
             "small": {"ss": ph.sb([128, 1], F32), "rs": ph.sb([128, 1], F32), "junk": ph.sb([128, D], BF16), "b": S.buf("mkv_small")}}
    mT = ph.sb([128, 32, 256], BF16, "mT")
    bmT = S.buf("mkv_mT")
    norm_block_to_hT(C, mem_ap, 0, 2, wb, bwb, mT, bmT, pools)
    wpool = ph.pool("w", 2, [128, 32, 512], BF16)
    pspool = ph.pool("ps", 3, [128, 512], F32, psum=True)
    opool = ph.pool("o", 3, [128, 512], BF16)

    def cb_k(m, ps, bps):
        ot, bo = opool.next()
        S.op("act", lambda a: a.copy(out=ot[:, 0:256], in_=ps[:, 0:256]), reads=[bps], writes=[bo])
        S.dma("sp", lambda q: q.dma_start(out=kmT_d[m * 128:(m + 1) * 128, :], in_=ot[:, 0:256]), bo, reads=[bo], writes=[bkm])

    gemm_fm2(C, lambda k: mT[:, k, :], bmT, 32, 256, wkv_ap[0] if isinstance(wkv_ap, tuple) else wkv_ap[:, 0:D], 8, cb_k, wpool, pspool)

    def cb_v(tt, n, ps, bps):
        ot, bo = opool.next()
        S.op("dve", lambda v: v.tensor_copy(out=ot[:], in_=ps[:]), reads=[bps], writes=[bo])
        S.dma("sp", lambda q: q.dma_start(out=vm_d[tt * 128:(tt + 1) * 128, n * 512:(n + 1) * 512], in_=ot[:]), bo, reads=[bo], writes=[bvm])

    gemm_tok(C, lambda k, tt: mT[:, k, tt * 128:(tt + 1) * 128], bmT, 32, 2, wkv_ap[1] if isinstance(wkv_ap, tuple) else wkv_ap[:, D:2 * D], 8, cb_v, wpool, pspool)
    ph.close()


TB3 = 512


def phase_k3(C, x_ap, cat_src, glu_ap, wout_ap, nwc_ap, wq_ap, kmT_d, vm_d, bkm, bvm, wo_ap, nwf_ap, rw_ap,
             x1_d, x2_d, hffn_d, aff_d, affT_d, ntok):
    S = C.S
    ph = Phase(C, "k3")
    bx1 = S.buf("k3_x1d")
    bx2 = S.buf("k3_x2d")
    bhf = S.buf("k3_hffn")
    baf = S.buf("k3_aff")
    glu = ph.sb([128, 8, 1024], BF16, "glu")
    bglu = S.buf("k3_glu")
    for nb_ in range(2):
        load_w(C, glu[:, :, nb_ * 512:(nb_ + 1) * 512], bglu, wblk(glu_ap, nb_ * 512, 512))
    wbc = ph.sb([128, D], F32, "wbc")
    wbf = wbc
    bwb = S.buf("k3_wb")
    brw = S.buf("k3_rw")
    rw = ph.sb([128, 32, 16], BF16, "rw")
    load_w(C, rw[:], brw, rw_ap.rearrange("(kc p) n -> p kc n", p=128))
    actT = ph.sb([128, 32, TB3], BF16, "actT")
    bact = S.buf("k3_act")
    qT = ph.sb([128, 32, TB3], BF16, "qT")
    bqT = S.buf("k3_qT")
    wpool = ph.pool("w", 2, [128, 32, 256], BF16)
    pspool = ph.pool("ps", 3, [128, 512], F32, psum=True)
    xpool = ph.pool("x", 1, [128, D], F32)
    hpool = ph.pool("h", 1, [128, D], BF16)
    small = {"ss": ph.sb([128, 1], F32), "rs": ph.sb([128, 1], F32), "junk": ph.sb([128, D], BF16), "b": S.buf("k3_small")}
    pst = ph.pool("pst", 2, [128, 8, 128], BF16, psum=True)
    npools = {"x": xpool, "h": hpool, "pst": pst, "small": small}
    sgp = ph.pool("sg", 2, [128, 512], F32)
    xo = ph.pool("xo", 3, [128, 512], F32)
    kmh = ph.pool("kmh", 2, [128, 8, 256], BF16)
    vmh = ph.pool("vmh", 2, [128, 2, 1024], BF16)
    ptp = ph.pool("ptp", 2, [128, 2, 512], BF16)
    rdn = ph.pool("rdn", 2, [128, 512], F32)
    rsm = ph.sb([128, 8], F32, "rsm")
    lg = ph.sb([128, 16], F32, "lg")
    aft = ph.sb([16, 128], F32, "aft")
    brs = S.buf("k3_rsm")
    ntt = TB3 // 128
    for t0 in range(0, ntok, TB3):
        for kc in range(32):
            cat_src(kc, t0, (qT if kc >= 24 else actT)[:, kc, :], bqT if kc >= 24 else bact)
        for m in range(8):
            ps, bps = pspool.next()
            for k in range(8):
                S.op("pe", lambda p, k=k: p.matmul(ps[:], lhsT=glu[:, k, m * 128:(m + 1) * 128], rhs=qT[:, 24 + k, :],
                                                   start=(k == 0), stop=(k == 7)), reads=[bglu, bqT], writes=[bps])
            sg, bsg = sgp.next()
            S.op("act", lambda a: a.activation(out=sg[:], in_=ps[:], func=AF.Sigmoid), reads=[bps], writes=[bsg])
            S.op("dve", lambda v: v.tensor_tensor(out=actT[:, 24 + m, :], in0=sg[:], in1=qT[:, 24 + m, :], op=ALU.mult),
                 reads=[bsg, bqT], writes=[bact])

        def cb_res(src_ap, dst_ap, bdst, bsrc=None):
            def cb(tt, n, ps, bps):
                xt, bxt = xo.next()
                r0 = t0 + tt * 128
                S.dma("sp", lambda q: q.dma_start(out=xt[:, 0:256], in_=src_ap[r0:r0 + 128, n * 256:(n + 1) * 256]), bxt,
                      reads=([bsrc] if bsrc else []), writes=[bxt])
                S.op("dve", lambda v: v.tensor_tensor(out=xt[:, 0:256], in0=ps[:, 0:256], in1=xt[:, 0:256], op=ALU.add), reads=[bps, bxt], writes=[bxt])
                S.dma("sp", lambda q: q.dma_start(out=dst_ap[r0:r0 + 128, n * 256:(n + 1) * 256], in_=xt[:, 0:256]), bxt,
                      reads=[bxt], writes=[bdst])
            return cb

        gemm_tok(C, lambda k, tt: actT[:, k, tt * 128:(tt + 1) * 128], bact, 32, ntt, wout_ap, 16, cb_res(x_ap, x1_d, bx1), wpool, pspool, wn=256)
        S.dma("sp", lambda q: q.dma_start(out=wbc[:], in_=nwc_ap), bwb, writes=[bwb])
        for i in range(ntt):
            xt, bx = xpool.next()
            r0 = t0 + i * 128
            S.dma("sp", lambda q: q.dma_start(out=xt[:], in_=x1_d[r0:r0 + 128, :]), bx, reads=[bx1], writes=[bx])
            hb, bh = hpool.next()
            rmsnorm_tile(C, xt, bx, wbc, bwb, hb, bh, small)
            transpose_to(C, hb, bh, lambda k0, n, i=i: actT[:, k0:k0 + n, i * 128:(i + 1) * 128], bact, 32, pst)

        def cb_q(m, ps, bps):
            S.op("act" if m % 2 else "dve",
                 (lambda a: a.copy(out=qT[:, m, :], in_=ps[:])) if m % 2 else (lambda v: v.tensor_copy(out=qT[:, m, :], in_=ps[:])),
                 reads=[bps], writes=[bqT])

        gemm_fm2(C, lambda k: actT[:, k, :], bact, 32, TB3, wq_ap, 16, cb_q, wpool, pspool, wn=256)
        for h in range(4):
            km, bkmh = kmh.next()
            vm, bvmh = vmh.next()
            S.dma("sp", lambda q: q.dma_start(out=km[:], in_=kmT_d[h * 1024:(h + 1) * 1024, :].rearrange("(c p) m -> p c m", p=128)),
                  bkmh, reads=[bkm], writes=[bkmh])
            S.dma("sp", lambda q: q.dma_start(out=vm[:], in_=vm_d[:, h * 1024:(h + 1) * 1024].rearrange("(b p) f -> p b f", p=128)),
                  bvmh, reads=[bvm], writes=[bvmh])
            pt, bpt = ptp.next()
            for mb in range(2):
                ps, bps = pspool.next()
                for c in range(8):
                    S.op("pe", lambda p, c=c: p.matmul(ps[:], lhsT=km[:, c, mb * 128:(mb + 1) * 128], rhs=qT[:, h * 8 + c, :],
                                                       start=(c == 0), stop=(c == 7)), reads=[bkmh, bqT], writes=[bps])
                S.op("act", lambda a: a.activation(out=pt[:, mb, :], in_=ps[:], func=AF.Exp, scale=1.0 / 32.0), reads=[bps], writes=[bpt])
            ps, bps = pspool.next()
            for mb in range(2):
                S.op("pe", lambda p: p.matmul(ps[:], lhsT=C.ones_bf[:], rhs=pt[:, mb, :], start=(mb == 0), stop=(mb == 1)),
                     reads=[C.b_const, bpt], writes=[bps])
            rd, brd = rdn.next()
            S.op("dve", lambda v: v.reciprocal(out=rd[:], in_=ps[:]), reads=[bps], writes=[brd])
            for c in range(8):
                ps, bps = pspool.next()
                for mb in range(2):
                    S.op("pe", lambda p: p.matmul(ps[:], lhsT=vm[:, mb, c * 128:(c + 1) * 128], rhs=pt[:, mb, :],
                                                  start=(mb == 0), stop=(mb == 1)), reads=[bvmh, bpt], writes=[bps])
                S.op("dve", lambda v: v.tensor_tensor(out=actT[:, h * 8 + c, :], in0=ps[:], in1=rd[:], op=ALU.mult),
                     reads=[bps, brd], writes=[bact])
        gemm_tok(C, lambda k, tt: actT[:, k, tt * 128:(tt + 1) * 128], bact, 32, ntt, wo_ap, 16, cb_res(x1_d, x2_d, bx2, bx1), wpool, pspool, wn=256)
        S.dma("sp", lambda q: q.dma_start(out=wbc[:], in_=nwf_ap), bwb, writes=[bwb])
        for i in range(ntt):
            xt, bx = xpool.next()
            r0 = t0 + i * 128
            S.dma("sp", lambda q: q.dma_start(out=xt[:], in_=x2_d[r0:r0 + 128, :]), bx, reads=[bx2], writes=[bx])
            hb, bh = hpool.next()
            rmsnorm_tile(C, xt, bx, wbf, bwb, hb, bh, small)
            S.dma("sp", lambda q: q.dma_start(out=hffn_d[r0:r0 + 128, :], in_=hb[:]), bh, reads=[bh], writes=[bhf])
            transpose_to(C, hb, bh, lambda k0, n, i=i: actT[:, k0:k0 + n, i * 128:(i + 1) * 128], bact, 32, pst)
            ps, bps = pspool.next()
            for k in range(32):
                S.op("pe", lambda p, k=k: p.matmul(ps[:, 0:16], lhsT=actT[:, k, i * 128:(i + 1) * 128], rhs=rw[:, k, :],
                                                   start=(k == 0), stop=(k == 31)), reads=[bact, brw], writes=[bps])
            S.op("dve", lambda v: v.tensor_reduce(out=rsm[:, 0:1], in_=ps[:, 0:16], axis=AX.X, op=ALU.max), reads=[bps], writes=[brs])
            S.op("dve", lambda v: v.tensor_scalar(out=rsm[:, 1:2], in0=rsm[:, 0:1], scalar1=-1.0, scalar2=None, op0=ALU.mult), reads=[brs], writes=[brs])
            S.op("act", lambda a: a.activation(out=lg[:], in_=ps[:, 0:16], func=AF.Exp, bias=rsm[:, 1:2], accum_out=rsm[:, 2:3]),
                 reads=[bps, brs], writes=[brs])
            S.op("dve", lambda v: v.reciprocal(out=rsm[:, 3:4], in_=rsm[:, 2:3]), reads=[brs], writes=[brs])
            S.op("dve", lambda v: v.tensor_scalar(out=lg[:], in0=lg[:], scalar1=rsm[:, 3:4], scalar2=None, op0=ALU.mult), reads=[brs], writes=[brs])
            S.dma("sp", lambda q: q.dma_start(out=aff_d[r0:r0 + 128, :], in_=lg[:]), brs, reads=[brs], writes=[baf])
            pa, bpa = pspool.next()
            S.op("pe", lambda p: p.transpose(out=pa[0:16, 0:128], in_=lg[:], identity=C.ident_f[:]), reads=[brs, C.b_const], writes=[bpa])
            S.op("dve", lambda v: v.tensor_copy(out=aft[:], in_=pa[0:16, 0:128]), reads=[bpa, brs], writes=[brs])
            S.dma("sp", lambda q: q.dma_start(out=affT_d[:, r0:r0 + 128], in_=aft[:]), brs, reads=[brs], writes=[baf])
    ph.close()


CAP = 512
TOWN = 4096
HALF = 2048
SW = 256


def _moe_consts():
    c = {}
    c["moe_iota"] = np.broadcast_to(np.arange(CAP, dtype=np.float32), (128, CAP)).copy()
    rh = np.zeros((128, TOWN // 128, 3), np.float32)
    rh[:, :, 0] = np.arange(128)[:, None]
    rh[:, :, 1] = np.arange(TOWN // 128)[None, :]
    rh[:, :, 2] = 1.0
    c["moe_rh"] = rh.astype(ml_dtypes.bfloat16)
    dm = np.zeros((128, 4), np.float32)
    for sc in range(4):
        dm[:, sc] = TOWN + sc * 128 + np.arange(128)
    c["moe_dmy"] = dm
    c["moe_ncol"] = np.broadcast_to(np.arange(D // SW, dtype=np.float32), (128, D // SW)).copy()
    return c


def phase_k4(C, affT_pair, affT_own, aff_own, hffn_d, xacc_d, wexp, experts, bxacc_in, dep_bufs=()):
    S = C.S
    ph = Phase(C, "k4")
    bc = S.buf("k4_const")
    iota = load_const(C, ph, "moe_iota", [128, CAP], F32, bc)
    rhc = load_const(C, ph, "moe_rh", [128, TOWN // 128, 3], BF16, bc)
    dmy = load_const(C, ph, "moe_dmy", [128, 4], F32, bc)
    ncol = load_const(C, ph, "moe_ncol", [128, D // SW], F32, bc)
    ntt = TOWN // 128
    pos_tok = ph.sb([128, ntt, 16], F32, "pos_tok")
    sel_tok = ph.sb([128, ntt, 16], F32, "sel_tok")
    aff_tok = ph.sb([128, ntt, 16], F32, "aff_tok")
    ahi = ph.sb([128, ntt, 16], BF16, "ahi")
    alo = ph.sb([128, ntt, 16], BF16, "alo")
    btok = S.buf("k4_tok")
    S.dma("sp", lambda q: q.dma_start(out=aff_tok[:], in_=aff_own.rearrange("(t p) e -> p t e", p=128)), btok, reads=list(dep_bufs), writes=[btok])
    S.op("dve", lambda v: v.tensor_copy(out=ahi[:], in_=aff_tok[:]), reads=[btok], writes=[btok])
    S.op("dve", lambda v: v.tensor_tensor(out=alo[:], in0=aff_tok[:], in1=ahi[:], op=ALU.subtract), reads=[btok], writes=[btok])
    p1 = Phase(C, "k4a")
    AT = p1.sb([16, TOWN], F32, "AT")
    junk = p1.sb([16, TOWN], F32, "junk")
    bAT = S.buf("k4_AT")
    for r in (0, 1):
        S.dma("sp", lambda q: q.dma_start(out=AT[:, r * HALF:(r + 1) * HALF], in_=affT_pair[r]), bAT, reads=list(dep_bufs), writes=[bAT])
    bs = p1.sb([16, 8], F32, "bs")
    bbs = S.buf("k4_bs")
    c_ = lambda i: bs[:, i:i + 1]
    S.op("dve", lambda v: v.memset(bs[:], 0.0), writes=[bbs])
    S.op("dve", lambda v: v.memset(c_(1), 1.0), reads=[bbs], writes=[bbs])
    S.op("dve", lambda v: v.memset(c_(6), 0.5), reads=[bbs], writes=[bbs])
    for it in range(30):
        S.op("dve", lambda v: v.scalar_tensor_tensor(out=c_(2), in0=c_(0), scalar=c_(1), in1=c_(6), op0=ALU.add, op1=ALU.mult), reads=[bbs], writes=[bbs])
        S.op("dve", lambda v: v.tensor_scalar(out=junk[:], in0=AT[:], scalar1=c_(2), scalar2=0.0, op0=ALU.is_gt, op1=ALU.add, accum_out=c_(3)),
             reads=[bAT, bbs], writes=[bbs])
        S.op("dve", lambda v: v.tensor_scalar(out=c_(4), in0=c_(3), scalar1=CAP - 0.5, scalar2=None, op0=ALU.is_gt), reads=[bbs], writes=[bbs])
        S.op("dve", lambda v: v.tensor_tensor(out=c_(5), in0=c_(2), in1=c_(0), op=ALU.subtract), reads=[bbs], writes=[bbs])
        S.op("dve", lambda v: v.scalar_tensor_tensor(out=c_(0), in0=c_(5), scalar=c_(4), in1=c_(0), op0=ALU.mult, op1=ALU.add), reads=[bbs], writes=[bbs])
        S.op("dve", lambda v: v.tensor_tensor(out=c_(5), in0=c_(1), in1=c_(2), op=ALU.subtract), reads=[bbs], writes=[bbs])
        S.op("dve", lambda v: v.scalar_tensor_tensor(out=c_(1), in0=c_(5), scalar=c_(4), in1=c_(2), op0=ALU.mult, op1=ALU.add), reads=[bbs], writes=[bbs])
    selT = p1.sb([16, TOWN], F32, "selT")
    posT = p1.sb([16, TOWN], F32, "posT")
    ones = p1.sb([16, TOWN], F32, "ones")
    bsel = S.buf("k4_sel")
    S.op("dve", lambda v: v.tensor_scalar(out=selT[:], in0=AT[:], scalar1=c_(0), scalar2=None, op0=ALU.is_gt), reads=[bAT, bbs], writes=[bsel])
    S.op("dve", lambda v: v.memset(ones[:], 1.0), writes=[bsel])
    S.op("dve", lambda v: v.tensor_tensor_scan(out=posT[:], data0=ones[:], data1=selT[:], initial=0.0, op0=ALU.mult, op1=ALU.add),
         reads=[bsel], writes=[bsel])
    S.op("dve", lambda v: v.tensor_tensor(out=posT[:], in0=posT[:], in1=selT[:], op=ALU.subtract), reads=[bsel], writes=[bsel])
    ptr = p1.pool("ptr", 2, [128, 2, 16], F32, psum=True)
    for tt in range(ntt):
        pt, bpt = ptr.next()
        cs = slice(tt * 128, (tt + 1) * 128)
        S.op("pe", lambda p: p.transpose(out=pt[:, 0, :], in_=posT[:, cs], identity=C.ident_f[0:16, 0:16]), reads=[bsel, C.b_const], writes=[bpt])
        S.op("pe", lambda p: p.transpose(out=pt[:, 1, :], in_=selT[:, cs], identity=C.ident_f[0:16, 0:16]), reads=[bsel, C.b_const], writes=[bpt])
        S.op("dve", lambda v: v.tensor_copy(out=pos_tok[:, tt, :], in_=pt[:, 0, :]), reads=[bpt], writes=[btok])
        S.op("dve", lambda v: v.tensor_copy(out=sel_tok[:, tt, :], in_=pt[:, 1, :]), reads=[bpt], writes=[btok])
    p1.close()
    rh = ph.sb([128, ntt, 5], BF16, "rh")
    brh = S.buf("k4_rh")
    S.op("dve", lambda v: v.tensor_copy(out=rh[:, :, 0:3], in_=rhc[:]), reads=[bc], writes=[brh])
    ohp = ph.pool("oh", ntt + 2, [128, CAP], BF16)
    pidx = ph.pool("pidx", 1, [128, 8], F32, psum=True)
    ixf = ph.sb([128, 4, 8], F32, "ixf")
    idx_g = ph.sb([128, 4], I32, "idx_g")
    idx_s = ph.sb([128, 4, D // SW], I32, "idx_s")
    idx_sf = ph.sb([128, D // SW], F32, "idx_sf")
    gate = ph.sb([128, 4], F32, "gate")
    bix = S.buf("k4_ix")
    xgp = ph.pool("xg", 2, [128, D], BF16)
    xeT = ph.sb([128, 32, CAP], BF16, "xeT")
    bxe = S.buf("k4_xeT")
    pst = ph.pool("pst", 2, [128, 8, 128], BF16, psum=True)
    wpool = ph.pool("w", 2, [128, 32, 256], BF16)
    pspool = ph.pool("ps", 3, [128, 512], F32, psum=True)
    sgl = ph.sb([128, 8, CAP], F32, "sgl")
    bsg = S.buf("k4_sgl")
    actT = ph.sb([128, 8, CAP], BF16, "actT")
    bact = S.buf("k4_act")
    yep = ph.pool("ye", 4, [128, SW], F32)
    bacc = [S.buf("k4_acc%d" % n) for n in range(D // SW)]
    for b in bacc:
        b.w = bxacc_in.w
    xacc_v = xacc_d.rearrange("r (a w) -> (r a) w", w=SW)
    bhf = S.buf("k4_hf")
    for e in experts:
        S.op("dve", lambda v: v.tensor_copy(out=rh[:, :, 3], in_=ahi[:, :, e]), reads=[btok, brh], writes=[brh])
        S.op("dve", lambda v: v.tensor_copy(out=rh[:, :, 4], in_=alo[:, :, e]), reads=[btok, brh], writes=[brh])
        ohs = []
        for tt in range(ntt):
            oh, boh = ohp.next()
            S.op("dve", lambda v: v.tensor_scalar(out=oh[:], in0=iota[:], scalar1=pos_tok[:, tt, e:e + 1], scalar2=sel_tok[:, tt, e:e + 1],
                                                  op0=ALU.is_equal, op1=ALU.mult), reads=[bc, btok], writes=[boh])
            ohs.append((oh, boh))
        for sc in range(4):
            pi, bpi = pidx.next()
            for tt in range(ntt):
                oh, boh = ohs[tt]
                S.op("pe", lambda p: p.matmul(pi[:, 0:5], lhsT=oh[:, sc * 128:(sc + 1) * 128], rhs=rh[:, tt, :], start=(tt == 0), stop=(tt == ntt - 1)),
                     reads=[boh, brh], writes=[bpi])
            S.op("dve", lambda v: v.tensor_copy(out=ixf[:, sc, 0:5], in_=pi[:, 0:5]), reads=[bpi, bix], writes=[bix])
            f = lambda i: ixf[:, sc, i:i + 1]
            S.op("dve", lambda v: v.scalar_tensor_tensor(out=f(5), in0=f(1), scalar=128.0, in1=f(0), op0=ALU.mult, op1=ALU.add), reads=[bix], writes=[bix])
            S.op("dve", lambda v: v.tensor_copy(out=idx_g[:, sc:sc + 1], in_=f(5)), reads=[bix], writes=[bix])
            S.op("dve", lambda v: v.tensor_tensor(out=gate[:, sc:sc + 1], in0=f(3), in1=f(4), op=ALU.add), reads=[bix], writes=[bix])
            S.op("dve", lambda v: v.tensor_tensor(out=f(6), in0=f(2), in1=dmy[:, sc:sc + 1], op=ALU.mult), reads=[bix, bc], writes=[bix])
            S.op("dve", lambda v: v.tensor_tensor(out=f(7), in0=dmy[:, sc:sc + 1], in1=f(6), op=ALU.subtract), reads=[bix, bc], writes=[bix])
            S.op("dve", lambda v: v.tensor_tensor(out=f(7), in0=f(7), in1=f(5), op=ALU.add), reads=[bix], writes=[bix])
            S.op("dve", lambda v: v.tensor_copy(out=idx_sf[:], in_=ncol[:]), reads=[bc, bix], writes=[bix])
            S.op("dve", lambda v: v.tensor_scalar(out=f(6), in0=f(7), scalar1=float(D // SW), scalar2=None, op0=ALU.mult), reads=[bix], writes=[bix])
            S.op("dve", lambda v: v.tensor_scalar(out=idx_sf[:], in0=idx_sf[:], scalar1=f(6), scalar2=None, op0=ALU.add), reads=[bix], writes=[bix])
            S.op("dve", lambda v: v.tensor_copy(out=idx_s[:, sc, :], in_=idx_sf[:]), reads=[bix], writes=[bix])
            xg, bxg = xgp.next()
            S.dma("pool", lambda q: q.indirect_dma_start(out=xg[:], out_offset=None, in_=hffn_d,
                                                         in_offset=bass.IndirectOffsetOnAxis(ap=idx_g[:, sc:sc + 1], axis=0)),
                  bxg, reads=[bix, bhf] + list(dep_bufs), writes=[bxg])
            transpose_to(C, xg, bxg, lambda k0, n: xeT[:, k0:k0 + n, sc * 128:(sc + 1) * 128], bxe, 32, pst)
        wg_e, wu_e, wd_e = wexp(e)

        def cb_g(m, ps, bps):
            S.op("act", lambda a: a.activation(out=sgl[:, m, :], in_=ps[:], func=AF.Silu), reads=[bps], writes=[bsg])

        gemm_fm2(C, lambda k: xeT[:, k, :], bxe, 32, CAP, wg_e, 4, cb_g, wpool, pspool, wn=256)

        def cb_u(m, ps, bps):
            S.op("dve", lambda v: v.tensor_tensor(out=actT[:, m, :], in0=ps[:], in1=sgl[:, m, :], op=ALU.mult), reads=[bps, bsg], writes=[bact])

        gemm_fm2(C, lambda k: xeT[:, k, :], bxe, 32, CAP, wu_e, 4, cb_u, wpool, pspool, wn=256)

        def cb_d(sc, n, ps, bps):
            ye, bye = yep.next()
            S.op("act", lambda a: a.activation(out=ye[:], in_=ps[:, 0:SW], func=AF.Copy, scale=gate[:, sc:sc + 1]), reads=[bps, bix], writes=[bye])
            S.dma("pool", lambda q: q.indirect_dma_start(out=xacc_v, out_offset=bass.IndirectOffsetOnAxis(ap=idx_s[:, sc, n:n + 1], axis=0),
                                                         in_=ye[:], in_offset=None, compute_op=ALU.add),
                  bye, reads=[bye, bix], writes=[bacc[n]])

        gemm_tok(C, lambda k, tt: actT[:, k, tt * 128:(tt + 1) * 128], bact, 8, 4, wd_e, D // SW, cb_d, wpool, pspool, wn=SW)
    ph.close()
    return bacc


def _coll(self, kind, groups, in_ap, out_ap, reads, writes):
    e = "pool"
    self._waits(e, reads, writes)
    if not hasattr(self, "cc_sem"):
        self.cc_sem = self.nc.alloc_semaphore("cc_sem")
        self.cc_cnt = 0
    ins = self.eng[e].collective_compute(kind, ALU.bypass, replica_groups=groups, ins=[in_ap.opt()], outs=[out_ap.opt()])
    ins.then_inc(self.cc_sem)
    self.cc_cnt += 1
    ev = ("cc", self.cc_sem, self.cc_cnt)
    self._record(ev, reads, writes)
    self.all_dma["cc"] = ev
    return ev


Sched.coll = _coll
G4 = [[0, 1, 2, 3], [4, 5, 6, 7]]
GP4 = [[0, 4], [1, 5], [2, 6], [3, 7]]
GP1 = [[0, 1], [2, 3], [4, 5], [6, 7]]

OFF_QA, OFF_KA, OFF_VA, OFF_QB, OFF_KB, OFF_VB, OFF_GB, OFF_UC = 0, 1536, 3072, 4608, 5376, 6144, 7680, 9216


def phase_k1(C, x_ap, bx_in, nw_ap, w_ap, bw_in, projT_d, bproj):
    S = C.S
    ph = Phase(C, "k1")
    Tc = 2048
    TBK = 1024
    wb = ph.sb([128, D], F32, "wb")
    bwb = S.buf("k1_wb")
    S.dma("sp", lambda q: q.dma_start(out=wb[:], in_=nw_ap), bwb, writes=[bwb])
    pools = {"x": ph.pool("x", 2, [128, D], F32), "h": ph.pool("h", 2, [128, D], BF16),
             "pst": ph.pool("pst", 2, [128, 8, 128], BF16, psum=True),
             "small": {"ss": ph.sb([128, 1], F32), "rs": ph.sb([128, 1], F32), "junk": ph.sb([128, D], BF16), "b": S.buf("k1_small")}}
    hT = ph.sb([128, 32, TBK], BF16, "hT")
    bhT = S.buf("k1_hT")
    wpool = ph.pool("w", 2, [128, 32, 256], BF16)
    pspool = ph.pool("ps", 4, [128, 512], F32, psum=True)
    opool = ph.pool("ot", 4, [128, 512], BF16)
    cnt = 0
    for tb in range(Tc // TBK):
        norm_block_to_hT(C, x_ap, tb * TBK, TBK // 128, wb, bwb, hT, bhT, pools)
        for nb in range(40):
            wt, bw = wpool.next()
            load_w(C, wt[:, :, :], bw, wblk(w_ap, nb * 256, 256))
            for mi in range(2):
                for th in range(TBK // 512):
                    ps, bps = pspool.next()
                    for k in range(32):
                        S.op("pe", lambda p: p.matmul(ps[:], lhsT=wt[:, k, mi * 128:(mi + 1) * 128], rhs=hT[:, k, th * 512:(th + 1) * 512],
                                                      start=(k == 0), stop=(k == 31)), reads=[bw, bhT], writes=[bps])
                    ot, bo = opool.next()
                    if cnt % 2:
                        S.op("act", lambda a: a.copy(out=ot[:], in_=ps[:]), reads=[bps], writes=[bo])
                    else:
                        S.op("dve", lambda v: v.tensor_copy(out=ot[:], in_=ps[:]), reads=[bps], writes=[bo])
                    cnt += 1
                    m = nb * 2 + mi
                    c0 = tb * TBK + th * 512
                    S.dma("sp", lambda q: q.dma_start(out=projT_d[m * 128:(m + 1) * 128, c0:c0 + 512], in_=ot[:]), bo, reads=[bo], writes=[bproj])
    ph.close()


def phase_k2(C, G1, bG1, posb, idx2_ap, P, mixT_d, bmix):
    S = C.S
    T = SEQ
    G1v = G1
    top = Phase(C, "k2")
    idx2 = top.sb([128, 80], I32, "idx2")
    bidx = S.buf("k2_idx")
    S.dma("sp", lambda q: q.dma_start(out=idx2[:], in_=idx2_ap), bidx, writes=[bidx])

    def gath(dst_fn, b, col):
        for h in (0, 1):
            S.dma("pool", lambda q: q.indirect_dma_start(out=dst_fn(h), out_offset=None, in_=G1v,
                                                         in_offset=bass.IndirectOffsetOnAxis(ap=idx2[:, col * 2 + h:col * 2 + h + 1], axis=0)),
                  b, reads=[bidx, bG1], writes=[b])

    ph = Phase(C, "k2a")
    W = mixer_a_setup(C, ph, posb, T)
    for j in range(6):
        def ld(which, t, b, off, j=j):
            ti = {"q": 0, "k": 1, "v": 2}[which]
            gath(lambda h: t[:, off + h * 2048: off + (h + 1) * 2048], b, j * 3 + ti)
        mixer_a_head(C, ph, ld, mixT_d[j * 128:(j + 1) * 128, :], T, W)
    S.barrier()
    bmix.w = None
    ph.close()
    ph = Phase(C, "k2b")
    Wb = mixer_b_setup(C, ph, posb, T)
    dect = ph.sb([128, 6], F32, "dect")
    gnt = ph.sb([128, 6], F32, "gnt")
    bd = S.buf("k2_dec")
    S.dma("sp", lambda q: q.dma_start(out=dect[:], in_=P["ret_dec"]), bd, writes=[bd])
    S.dma("sp", lambda q: q.dma_start(out=gnt[:], in_=P["gnw"]), bd, writes=[bd])
    for j in range(3):
        def ldb(which, ap, b, j=j):
            wi = {"q": 0, "k": 1, "v0": 2, "v1": 3, "g0": 4, "g1": 5}[which]
            gath(lambda h: ap[:, h * 2048:(h + 1) * 2048], b, 18 + j * 6 + wi)
        mixer_b_head(C, ph, ldb, dect, bd, j, gnt[:, 2 * j:2 * j + 2], bd, mixT_d[768 + 256 * j:768 + 256 * (j + 1), :], T, Wb)
    ph.close()
    ph = Phase(C, "k2c")
    stg = ph.sb([128, T], BF16, "stg")
    bstg = S.buf("k2_stg")
    for gp in range(16):
        if gp % 4 == 0:
            gath(lambda h: stg[:, h * 2048:(h + 1) * 2048], bstg, 36 + gp // 4)

        def ld_u(ut, bu, gp=gp):
            r0 = (gp % 4) * 32
            S.dma("sp", lambda q: q.dma_start(out=ut[:], in_=stg[r0:r0 + 32, :]), bu, reads=[bstg], writes=[bu])

        prm = []
        for dr in (0, 1):
            d = {}
            for i, nm in enumerate(("lam_re", "lam_im", "logdt")):
                d[nm] = P["s5_cols"][gp, dr, i]
            for i, nm in enumerate(("b_re", "b_im", "c_reT", "c_imT")):
                d[nm] = P["s5_mats"][gp, dr, :, i, :]
            prm.append(d)
        mixer_c_pair(C, ph, ld_u, prm, P["s5_d"][gp], P["tidx"], mixT_d[1536 + 32 * gp:1536 + 32 * (gp + 1), :], T)
    ph.close()
    top.close()


NC4 = 4
GRP = [[0, 1, 2, 3]]
WSPEC = (("w_in", 4096, 10240, 512, 1), ("w_out", 4096, D, 512, 1), ("wq", 4096, D, 512, 1), ("wkv", 4096, 2 * D, 512, 1),
         ("wo", 4096, D, 512, 1), ("glu", 1024, 1024, 512, 1), ("wg", 4096, 1024, 512, 16), ("wu", 4096, 1024, 512, 16),
         ("wd", 1024, D, 2048, 16))


def prologue_weight(C, name, src_ap, K, N, cw, ne, pool):
    S = C.S
    Ks = K // 4
    NB = N // cw
    sh = C.scratch(name + "_sh", [ne * NB, Ks, cw], BF16)
    full = C.scratch(name + "_full", [ne * NB, K, cw], BF16)
    for e in range(ne):
        bsh = S.buf("%s_sh%d" % (name, e))
        for r0 in range(0, Ks, 128):
            t, bt = pool.next()
            S.dma("pool", lambda q: q.dma_start(out=t[:, 0:N], in_=src_ap[e * Ks + r0:e * Ks + r0 + 128, :]), bt, writes=[bt])
            S.dma("sp", lambda q: q.dma_start(out=sh[e * NB:(e + 1) * NB, r0:r0 + 128, :].rearrange("nb p c -> p nb c"),
                                              in_=t[:, 0:N].rearrange("p (nb c) -> p nb c", c=cw)), bt, reads=[bt], writes=[bsh])
        for nb in range(NB):
            S.coll("AllGather", GRP, sh[e * NB + nb], full[e * NB + nb], [bsh], [])
    return full


def build_full(depth=2):
    C = Ctx()
    S = C.S
    x_in = C.inp("x", [SEQ, D], F32)
    mem_in = C.inp("mem", [256, D], F32)
    posb = C.inp("posb", [128, SEQ], I32)
    tidx = C.inp("tidx", [128, SEQ], I32)
    idx2_in = C.inp("idx2", [2, 128, 80], I32)
    idx3_in = C.inp("idx3", [2, 128, 32], F32)
    final_nw = C.inp("final_nw", [128, D], F32)
    out = C.out("out", [SEQ, D], F32)
    for nm, arr in list(_mixer_consts().items()) + list(_retention_consts().items()) + list(_moe_consts().items()):
        C.inp(nm, list(arr.shape), BF16 if arr.dtype == ml_dtypes.bfloat16 else (I32 if arr.dtype == np.int32 else F32))
    G1 = C.scratch("G1", [2 * 10240, 2048], BF16)
    G2 = C.scratch("G2", [2 * 2048, SEQ], BF16)
    x1_d = C.scratch("x1_d", [2048, D], F32)
    xacc_all = C.scratch("xacc", [SEQ + CAP, D], F32)
    hffn_all = C.scratch("hffn", [SEQ, D], BF16)
    aff_all = C.scratch("aff", [SEQ, 16], F32)
    xacc = [xacc_all[r * 2048:(r + 1) * 2048, :] for r in (0, 1)]
    hffn = [hffn_all[r * 2048:(r + 1) * 2048, :] for r in (0, 1)]
    aff = [aff_all[r * 2048:(r + 1) * 2048, :] for r in (0, 1)]
    affT_pair = C.scratch("affT_pair", [32, 2048], F32)
    kmT_d = C.scratch("kmT_d", [D, 256], BF16)
    vm_d = C.scratch("vm_d", [256, D], BF16)
    LW = []
    ph = Phase(C, "pro")
    cpool = ph.pool("cast", 3, [128, 10240], BF16)
    for l in range(depth):
        Wl = {}
        for nm, K, N, cw, ne in WSPEC:
            src = C.inp("%s_%d" % (nm, l), [ne * K // 4, N], F32)
            Wl[nm] = prologue_weight(C, "%s_%d" % (nm, l), src, K, N, cw, ne, cpool)
        LW.append(Wl)
    ph.close()
    for l in range(depth):
        Wl = LW[l]
        Pr = []
        rd_in = C.inp("ret_dec_%d" % l, [2, 128, 6], F32)
        gn_in = C.inp("gnw_%d" % l, [2, 128, 6], F32)
        sc_in = C.inp("s5_cols_%d" % l, [2, 16, 2, 3, 128, 1], F32)
        sm_in = C.inp("s5_mats_%d" % l, [2, 16, 2, 128, 4, 16], F32)
        sd_in = C.inp("s5_d_%d" % l, [2, 16, 32, 1], F32)
        for r in (0, 1):
            Pr.append({"ret_dec": rd_in[r], "gnw": gn_in[r], "s5_cols": sc_in[r], "s5_mats": sm_in[r], "s5_d": sd_in[r], "tidx": tidx})
        nw_mix = C.inp("nw_mix_%d" % l, [128, D], F32)
        nw_cross = C.inp("nw_cross_%d" % l, [128, D], F32)
        nw_mem = C.inp("nw_mem_%d" % l, [128, D], F32)
        nw_ffn = C.inp("nw_ffn_%d" % l, [128, D], F32)
        rw = C.inp("rw_%d" % l, [D, 16], F32)
        xs = [x_in[0:2048, :], x_in[2048:4096, :]] if l == 0 else [xacc[0], xacc[1]]
        dummy = S.buf("dummy")
        for r in (0, 1):
            phase_k1(C, xs[r], None, nw_mix, WMat(Wl["w_in"]), dummy, G1[r * 10240:(r + 1) * 10240, :], S.buf("proj"))
        for r in (0, 1):
            phase_k2(C, G1, S.buf("G1"), posb, idx2_in[r], Pr[r], G2[r * 2048:(r + 1) * 2048, :], S.buf("mix"))
        bkm, bvm = S.buf("kmd"), S.buf("vmd")
        phase_mem_kv(C, mem_in, nw_mem, (WMat(Wl["wkv"]), WMat(Wl["wkv"], col0=D)), kmT_d, vm_d, bkm, bvm)
        G2v = G2.rearrange("r (a w) -> (r a) w", w=512)
        for r in (0, 1):
            php = Phase(C, "k3idx")
            idx3f = php.sb([128, 32], F32, "idx3f")
            idx3b = php.sb([128, 4, 32], I32, "idx3b")
            bi3 = S.buf("k3_idx")
            S.dma("sp", lambda q: q.dma_start(out=idx3f[:], in_=idx3_in[r]), bi3, writes=[bi3])
            for blk in range(4):
                S.op("dve", lambda v: v.tensor_scalar(out=idx3b[:, blk, :], in0=idx3f[:], scalar1=float(blk), scalar2=None, op0=ALU.add),
                     reads=[bi3], writes=[bi3])

            def cat_load(kc, t0, dst, bd):
                blk = t0 // 512
                S.dma("pool", lambda q: q.indirect_dma_start(out=dst, out_offset=None, in_=G2v,
                                                             in_offset=bass.IndirectOffsetOnAxis(ap=idx3b[:, blk, kc:kc + 1], axis=0)),
                      bd, reads=[bi3], writes=[bd])

            phase_k3(C, xs[r], cat_load, WMat(Wl["glu"]), WMat(Wl["w_out"]), nw_cross, WMat(Wl["wq"]), kmT_d, vm_d, bkm, bvm,
                     WMat(Wl["wo"]), nw_ffn, rw, x1_d, xacc[r], hffn[r], aff[r], affT_pair[r * 16:(r + 1) * 16, :], 2048)
            php.close()
        affp_v = affT_pair.rearrange("(r e) t -> r e t", r=2)

        def wexp(e):
            return (WMat(Wl["wg"][2 * e:2 * e + 2]), WMat(Wl["wu"][2 * e:2 * e + 2]), WMat(Wl["wd"][2 * e:2 * e + 2]))

        phase_k4(C, affp_v, None, aff_all, hffn_all, xacc_all, wexp, list(range(16)), S.buf("xacc"))
    ph = Phase(C, "fin")
    wb = ph.sb([128, D], F32, "wb")
    bwb = S.buf("fin_wb")
    S.dma("sp", lambda q: q.dma_start(out=wb[:], in_=final_nw), bwb, writes=[bwb])
    xp = ph.pool("x", 2, [128, D], F32)
    hp = ph.pool("h", 2, [128, D], F32)
    small = {"ss": ph.sb([128, 1], F32), "rs": ph.sb([128, 1], F32), "junk": ph.sb([128, D], BF16), "b": S.buf("fin_small")}
    for r in (0, 1):
        for i in range(16):
            xt, bxt = xp.next()
            S.dma("sp", lambda q: q.dma_start(out=xt[:], in_=xacc[r][i * 128:(i + 1) * 128, :]), bxt, writes=[bxt])
            hb, bh = hp.next()
            rmsnorm_tile(C, xt, bxt, wb, bwb, hb, bh, small)
            r0 = r * 2048 + i * 128
            C.out_evs.append(S.dma("sp", lambda q: q.dma_start(out=out[r0:r0 + 128, :], in_=hb[:]), bh, reads=[bh]))
    ph.close()
    return C


def _idx_tables(r):
    idx2 = np.zeros((128, 80), np.int32)
    p = np.arange(128)
    cols = []
    for j in range(6):
        H = 6 * r + j
        cols += [OFF_QA + 128 * H, OFF_KA + 128 * H, OFF_VA + 128 * H]
    for j in range(3):
        H = 3 * r + j
        cols += [OFF_QB + 128 * H, OFF_KB + 128 * H, OFF_VB + 256 * H, OFF_VB + 256 * H + 128, OFF_GB + 256 * H, OFF_GB + 256 * H + 128]
    for cb in range(4):
        cols += [OFF_UC + 512 * r + 128 * cb]
    for ci, row0 in enumerate(cols):
        for h in (0, 1):
            idx2[:, ci * 2 + h] = h * 10240 + row0 + p
    idx3 = np.zeros((128, 32), np.float32)
    for kc in range(32):
        if kc < 12:
            rr, row0 = kc // 6, 128 * (kc % 6)
        elif kc < 24:
            rr, row0 = (kc - 12) // 6, 768 + 128 * ((kc - 12) % 6)
        else:
            rr, row0 = (kc - 24) // 4, 1536 + 128 * ((kc - 24) % 4)
        idx3[:, kc] = (rr * 2048 + row0 + p) * 8 + r * 4
    return idx2, idx3


_PROG = {}


def make_maps(inp, depth=2):
    f32 = lambda a: np.ascontiguousarray(np.asarray(a), dtype=np.float32)
    bc = lambda v: np.ascontiguousarray(np.broadcast_to(np.asarray(v, dtype=np.float32), (128, D)))
    consts = {}
    consts.update(_mixer_consts())
    consts.update(_retention_consts())
    consts.update(_moe_consts())
    consts.update(_s5_consts(SEQ))
    consts.update(_host_consts())
    t2 = [_idx_tables(r) for r in (0, 1)]
    consts["idx2"] = np.stack([t2[0][0], t2[1][0]])
    consts["idx3"] = np.stack([t2[0][1], t2[1][1]])
    consts["final_nw"] = bc(inp["final_norm_w"])
    per_layer = []
    for l in range(depth):
        d = {}
        d["rw_%d" % l] = f32(inp["router_w"][l])
        d["nw_mix_%d" % l] = bc(inp["norm_mix_w"][l])
        d["nw_cross_%d" % l] = bc(inp["norm_cross_w"][l])
        d["nw_mem_%d" % l] = bc(inp["norm_mem_w"][l])
        d["nw_ffn_%d" % l] = bc(inp["norm_ffn_w"][l])
        rd = np.asarray(inp["ret_decay"][l], dtype=np.float32)
        gw = np.asarray(inp["ret_gn_w"][l], dtype=np.float32)
        dec = np.zeros((2, 128, 6), np.float32)
        gn = np.zeros((2, 128, 6), np.float32)
        cols = np.zeros((2, 16, 2, 3, 128, 1), np.float32)
        mats = np.zeros((2, 16, 2, 128, 4, 16), np.float32)
        sd = np.zeros((2, 16, 32, 1), np.float32)
        lre, lim, ldt = (np.asarray(inp[k][l]) for k in ("s5_lam_re", "s5_lam_im", "s5_log_dt"))
        bre, bim, cre, cim = (np.asarray(inp[k][l]) for k in ("s5_b_re", "s5_b_im", "s5_c_re", "s5_c_im"))
        s5d = np.asarray(inp["s5_d"][l])
        for r in (0, 1):
            for j in range(3):
                H = 3 * r + j
                for dr in (0, 1):
                    dec[r, :, dr * 3 + j] = rd[dr, H]
                for hf in (0, 1):
                    gn[r, :, 2 * j + hf] = gw[H * 256 + hf * 128:H * 256 + (hf + 1) * 128]
            for gp in range(16):
                g0 = 32 * r + 2 * gp
                sd[r, gp, :, 0] = s5d[16 * g0:16 * g0 + 32]
                for dr in (0, 1):
                    for gi in (0, 1):
                        g = g0 + gi
                        ps = slice(gi * 64, (gi + 1) * 64)
                        cols[r, gp, dr, 0, ps, 0] = lre[dr][g]
                        cols[r, gp, dr, 1, ps, 0] = lim[dr][g]
                        cols[r, gp, dr, 2, ps, 0] = ldt[dr][g]
                        mats[r, gp, dr, ps, 0] = bre[dr][g]
                        mats[r, gp, dr, ps, 1] = bim[dr][g]
                        mats[r, gp, dr, ps, 2] = cre[dr][g].T
                        mats[r, gp, dr, ps, 3] = cim[dr][g].T
        d["ret_dec_%d" % l], d["gnw_%d" % l] = dec, gn
        d["s5_cols_%d" % l], d["s5_mats_%d" % l], d["s5_d_%d" % l] = cols, mats, sd
        per_layer.append(d)
    maps = []
    for c in range(NC4):
        m = dict(consts)
        m["x"] = f32(inp["x"][c])
        m["mem"] = f32(inp["mem"][c])
        m["posb"] = np.ascontiguousarray(np.broadcast_to(np.asarray(inp["positions"][c], dtype=np.int32), (128, SEQ)))
        for l in range(depth):
            m.update(per_layer[l])
            for nm, key, K in (("w_in", "w_in", 4096), ("w_out", "w_out", 4096), ("wq", "cross_wq", 4096), ("wkv", "cross_wkv", 4096),
                               ("wo", "cross_wo", 4096), ("glu", "s5_glu_w", 1024)):
                Ks = K // 4
                m["%s_%d" % (nm, l)] = f32(inp[key][l][c * Ks:(c + 1) * Ks])
            for nm, key, K in (("wg", "expert_w_gate", 4096), ("wu", "expert_w_up", 4096), ("wd", "expert_w_down", 1024)):
                Ks = K // 4
                a = np.asarray(inp[key][l])[:, c * Ks:(c + 1) * Ks, :]
                m["%s_%d" % (nm, l)] = f32(a.reshape(16 * Ks, a.shape[2]))
        maps.append(m)
    return maps


def kernel(**inp):
    depth = 2
    if "full" not in _PROG:
        C = build_full(depth)
        C.S.finish(C.out_evs)
        _PROG["full"] = C
    C = _PROG["full"]
    maps = make_maps(inp, depth)
    res = run_bass_kernel_spmd(C.nc, maps, core_ids=list(range(NC4)))
    full = np.zeros((4, SEQ, D), np.float32)
    for c in range(NC4):
        full[c] = np.asarray(res.results[c]["out"])
    return full
```

```python
import numpy as np
import ml_dtypes
import concourse.bass as bass
import concourse.mybir as mybir
from concourse.bass_utils import run_bass_kernel_spmd

F32 = mybir.dt.float32
BF16 = mybir.dt.bfloat16
I32 = mybir.dt.int32
U32 = mybir.dt.uint32
AF = mybir.ActivationFunctionType
ALU = mybir.AluOpType
AX = mybir.AxisListType

D = 4096
NCORES = 8
EPS = 1e-6


class Buf:
    __slots__ = ("name", "w", "r", "dsem", "dcnt", "ws")

    def __init__(self, name):
        self.name = name
        self.ws = {}
        self.w = None
        self.r = {}
        self.dsem = None
        self.dcnt = 0


class Sched:
    def __init__(self, nc):
        self.nc = nc
        self.eng = {"pe": nc.tensor, "dve": nc.vector, "act": nc.scalar,
                    "pool": nc.gpsimd, "sp": nc.sync}
        self.sem = {k: nc.alloc_semaphore("s_" + k) for k in self.eng}
        self.cnt = {k: 0 for k in self.eng}
        self.seen = {k: {} for k in self.eng}
        self.nbuf = 0
        self.ndsem = 0
        self.final = []

    def buf(self, name=None):
        self.nbuf += 1
        return Buf(name or ("b%d" % self.nbuf))

    def bufs(self, n, name="b"):
        return [self.buf("%s%d" % (name, i)) for i in range(n)]

    def _waits(self, e, reads, writes):
        need = {}

        def add(ev):
            if ev is None:
                return
            k = ev[0]
            if k not in need or need[k][2] < ev[2]:
                need[k] = ev

        for b in reads:
            add(b.w)
            for ev in b.ws.values():
                add(ev)
        for b in writes:
            add(b.w)
            for ev in b.ws.values():
                add(ev)
            for ev in b.r.values():
                add(ev)
        eng = self.eng[e]
        seen = self.seen[e]
        for k, ev in need.items():
            if e == "pe" and k == "pe":
                continue
            if seen.get(k, 0) >= ev[2]:
                continue
            eng.wait_ge(ev[1], ev[2])
            seen[k] = ev[2]

    def _record(self, ev, reads, writes):
        for b in reads:
            b.r[ev[0]] = ev
        for b in writes:
            b.w = ev
            b.ws[ev[0]] = ev
            b.r = {}

    def op(self, e, fn, reads=(), writes=()):
        self._waits(e, reads, writes)
        ins = fn(self.eng[e])
        self.cnt[e] += 1
        ev = (e, self.sem[e], self.cnt[e])
        ins.then_inc(self.sem[e], 1)
        if e != "pe":
            pass
        self._record(ev, reads, writes)
        return ev

    def dma(self, e, fn, sb, reads=(), writes=()):
        self._waits(e, reads, writes)
        if sb.dsem is None:
            sb.dsem = self.nc.alloc_semaphore("d_%s" % sb.name)
            self.ndsem += 1
        ins = fn(self.eng[e])
        sb.dcnt += 16
        ev = ("d_" + sb.name, sb.dsem, sb.dcnt)
        ins.then_inc(sb.dsem, 16)
        self._record(ev, reads, writes)
        return ev

    def finish(self, evs, e="sp"):
        eng = self.eng[e]
        best = {}
        for ev in evs:
            if ev[0] not in best or best[ev[0]][2] < ev[2]:
                best[ev[0]] = ev
        for ev in best.values():
            eng.wait_ge(ev[1], ev[2])


def _host_consts():
    c = {}
    c["ident_bf"] = np.eye(128, dtype=np.float32).astype(ml_dtypes.bfloat16)
    c["ident_f"] = np.eye(128, dtype=np.float32)
    c["ones_bf"] = np.ones((128, 128), np.float32).astype(ml_dtypes.bfloat16)
    return c


class Ctx:
    def __init__(self):
        self.nc = bass.Bass("TRN2", target_bir_lowering=False)
        self.S = Sched(self.nc)
        self.ins = {}
        self.outs = []
        self.out_evs = []
        nc, S = self.nc, self.S
        self.ident_bf = nc.alloc_sbuf_tensor("sb_ident_bf", [128, 128], BF16)
        self.ident_f = nc.alloc_sbuf_tensor("sb_ident_f", [128, 128], F32)
        self.ones_bf = nc.alloc_sbuf_tensor("sb_ones_bf", [128, 128], BF16)
        self.eps_t = nc.alloc_sbuf_tensor("eps_t", [128, 1], F32)
        self.b_const = S.buf("consts")
        for nm, t, dt in (("ident_bf", self.ident_bf, BF16), ("ident_f", self.ident_f, F32),
                          ("ones_bf", self.ones_bf, BF16)):
            d = self.inp(nm, [128, 128], dt)
            S.dma("sp", lambda q, t=t, d=d: q.dma_start(out=t[:], in_=d), self.b_const,
                  writes=[self.b_const])
        S.op("dve", lambda v: v.memset(self.eps_t[:], EPS), writes=[self.b_const])
        self.halfpi_t = nc.alloc_sbuf_tensor("halfpi_t", [128, 1], F32)
        S.op("dve", lambda v: v.memset(self.halfpi_t[:], 1.5707963267948966), writes=[self.b_const])

    def inp(self, name, shape, dt):
        t = self.nc.dram_tensor(name, list(shape), dt, kind="ExternalInput")
        self.ins[name] = t
        return t.ap()

    def out(self, name, shape, dt):
        t = self.nc.dram_tensor(name, list(shape), dt, kind="ExternalOutput")
        self.outs.append(name)
        return t.ap()

    def scratch(self, name, shape, dt):
        return self.nc.dram_tensor(name, list(shape), dt, kind="Internal").ap()

    def run(self, in_maps):
        self.S.finish(self.out_evs)
        cst = _host_consts()
        maps = []
        for m in in_maps:
            mm = dict(cst)
            mm.update(m)
            maps.append(mm)
        res = run_bass_kernel_spmd(self.nc, maps, core_ids=list(range(len(maps))))
        return res.results


class Pool:
    def __init__(self, C, name, n, shape, dt, psum=False):
        self.tiles = []
        for i in range(n):
            nm = "%s%d" % (name, i)
            if psum:
                t = C.nc.alloc_psum_tensor(nm, list(shape), dt)
            else:
                t = C.nc.alloc_sbuf_tensor(nm, list(shape), dt)
            self.tiles.append((t, C.S.buf(nm)))
        self.i = 0

    def next(self):
        t = self.tiles[self.i % len(self.tiles)]
        self.i += 1
        return t


def rmsnorm_tile(C, xt, bx, wb, bw, hb, bh, P, out_f32=None):
    S = C.S
    ss, rs, junk = P["ss"], P["rs"], P["junk"]
    bs = P["b"]
    S.op("act", lambda a: a.activation(out=junk[:], in_=xt[:], func=AF.Square, accum_out=ss[:]),
         reads=[bx], writes=[bs])
    S.op("act", lambda a: a.activation(out=rs[:], in_=ss[:], func=AF.Sqrt, bias=C.eps_t[:], scale=1.0 / D),
         reads=[bs, C.b_const], writes=[bs])
    S.op("dve", lambda v: v.reciprocal(out=rs[:], in_=rs[:]), reads=[bs], writes=[bs])
    S.op("dve", lambda v: v.scalar_tensor_tensor(out=hb[:], in0=xt[:], scalar=rs[:, 0:1], in1=wb[:],
                                                 op0=ALU.mult, op1=ALU.mult),
         reads=[bx, bs, bw], writes=[bh])


def transpose_to(C, src, bsrc, dst_fn, bdst, nk, pst_pool, dt=BF16, evac=("act", "dve")):
    S = C.S
    ident = C.ident_bf if dt == BF16 else C.ident_f
    per = 8 if dt == BF16 else 4
    gi = 0
    for k0 in range(0, nk, per):
        n = min(per, nk - k0)
        pt, bpt = pst_pool.next()
        for j in range(n):
            S.op("pe", lambda p, j=j, pt=pt: p.transpose(out=pt[:, j, :], in_=src[:, (k0 + j) * 128:(k0 + j + 1) * 128],
                                                          identity=ident[:]),
                 reads=[bsrc, C.b_const], writes=[bpt])
        e = evac[gi % len(evac)]
        gi += 1
        dst = dst_fn(k0, n)
        if e == "act":
            S.op("act", lambda a, pt=pt, dst=dst, n=n: a.copy(out=dst, in_=pt[:, 0:n, :]), reads=[bpt], writes=[bdst])
        else:
            S.op("dve", lambda v, pt=pt, dst=dst, n=n: v.tensor_copy(out=dst, in_=pt[:, 0:n, :]), reads=[bpt], writes=[bdst])


def load_w_cast(C, wt, bw, src_ap):
    C.S.dma("pool", lambda q: q.dma_start(out=wt, in_=src_ap), bw, writes=[bw])


TB = 1024


def norm_block_to_hT(C, x_ap, t0, ntile, wb, bwb, hT, bhT, pools, also_tok=None):
    S = C.S
    for i in range(ntile):
        xt, bx = pools["x"].next()
        r0 = t0 + i * 128
        S.dma("sp", lambda q, xt=xt, r0=r0: q.dma_start(out=xt[:], in_=x_ap[r0:r0 + 128, :]), bx, writes=[bx])
        hb, bh = pools["h"].next()
        rmsnorm_tile(C, xt, bx, wb, bwb, hb, bh, pools["small"])
        if also_tok is not None:
            also_tok(i, hb, bh)
        transpose_to(C, hb, bh, lambda k0, n, i=i: hT[:, k0:k0 + n, i * 128:(i + 1) * 128], bhT, 32, pools["pst"])


def make_norm_pools(C, tag=""):
    nc = C.nc
    pools = {
        "x": Pool(C, "xt" + tag, 2, [128, D], F32),
        "h": Pool(C, "hb" + tag, 2, [128, D], BF16),
        "pst": Pool(C, "pst" + tag, 2, [128, 8, 128], BF16, psum=True),
    }
    pools["small"] = {
        "ss": nc.alloc_sbuf_tensor("ss" + tag, [128, 1], F32),
        "rs": nc.alloc_sbuf_tensor("rs" + tag, [128, 1], F32),
        "junk": nc.alloc_sbuf_tensor("junk" + tag, [128, D], BF16),
        "b": C.S.buf("small" + tag),
    }
    return pools


def gemm_fm(C, hT, bhT, nk, ntok, w_ap, m0, nm, out_cb, wpool, pspool):
    S = C.S
    wv = w_ap.rearrange("(kc p) m -> p kc m", p=128)
    for mi in range(nm):
        m = m0 + mi
        wt, bw = wpool.next()
        load_w_cast(C, wt[:, 0:nk, :], bw, wv[:, :, m * 128:(m + 1) * 128])
        for ts in range(ntok // 512):
            ps, bps = pspool.next()
            for k in range(nk):
                S.op("pe", lambda p, k=k, ps=ps, wt=wt, ts=ts: p.matmul(
                    ps[:], lhsT=wt[:, k, :], rhs=hT[:, k, ts * 512:(ts + 1) * 512],
                    start=(k == 0), stop=(k == nk - 1)),
                    reads=[bw, bhT], writes=[bps])
            out_cb(mi, ts, ps, bps)


def build_k1(l):
    C = Ctx()
    nc, S = C.nc, C.S
    Tc = 2048
    x = C.inp("x", [Tc, D], F32)
    nw = C.inp("nw", [128, D], F32)
    w_in = C.inp("w_in", [D, 10240], F32)
    projT = C.out("projT", [10240, Tc], BF16)
    wb = nc.alloc_sbuf_tensor("wb", [128, D], F32)
    bwb = S.buf("wb")
    S.dma("sp", lambda q: q.dma_start(out=wb[:], in_=nw), bwb, writes=[bwb])
    pools = make_norm_pools(C)
    hT = nc.alloc_sbuf_tensor("hT", [128, 32, TB], BF16)
    bhT = S.buf("hT")
    wpool = Pool(C, "w", 3, [128, 32, 128], BF16)
    pspool = Pool(C, "ps", 4, [128, 512], F32, psum=True)
    opool = Pool(C, "ot", 4, [128, 512], BF16)
    bproj = S.buf("projT")
    evs = []
    cnt = [0]
    for tb in range(Tc // TB):
        norm_block_to_hT(C, x, tb * TB, TB // 128, wb, bwb, hT, bhT, pools)

        def out_cb(mi, ts, ps, bps, tb=tb):
            ot, bo = opool.next()
            e = ("act", "dve")[cnt[0] % 2]
            cnt[0] += 1
            if e == "act":
                S.op("act", lambda a: a.copy(out=ot[:], in_=ps[:]), reads=[bps], writes=[bo])
            else:
                S.op("dve", lambda v: v.tensor_copy(out=ot[:], in_=ps[:]), reads=[bps], writes=[bo])
            c0 = tb * TB + ts * 512
            evs.append(S.dma("sp", lambda q: q.dma_start(out=projT[mi * 128:(mi + 1) * 128, c0:c0 + 512], in_=ot[:]),
                             bo, reads=[bo]))

        gemm_fm(C, hT, bhT, 32, TB, w_in, 0, 80, out_cb, wpool, pspool)
    C.out_evs += evs[-8:] + evs
    return C


_UID = [0]


class Phase:
    def __init__(self, C, name):
        self.C = C
        self.name = name
        self.cms = []
        self.n = 0

    def sb(self, shape, dt, nm=None):
        self.n += 1
        _UID[0] += 1
        cm = self.C.nc.sbuf_tensor("%s_%s%d_%d" % (self.name, nm or "t", self.n, _UID[0]), list(shape), dt)
        t = cm.__enter__()
        self.cms.append(cm)
        return t

    def ps(self, shape, dt, nm=None):
        self.n += 1
        _UID[0] += 1
        cm = self.C.nc.psum_tensor("%s_%s%d_%d" % (self.name, nm or "p", self.n, _UID[0]), list(shape), dt)
        t = cm.__enter__()
        self.cms.append(cm)
        return t

    def pool(self, nm, n, shape, dt, psum=False):
        p = Pool.__new__(Pool)
        p.tiles = []
        p.i = 0
        for i in range(n):
            t = self.ps(shape, dt, nm) if psum else self.sb(shape, dt, nm)
            p.tiles.append((t, self.C.S.buf("%s_%s%d" % (self.name, nm, i))))
        return p

    def close(self):
        self.C.S.barrier()
        for cm in reversed(self.cms):
            cm.__exit__(None, None, None)
        self.cms = []


def _sched_barrier(self):
    evs = [(k, self.sem[k], self.cnt[k]) for k in self.eng if self.cnt[k] > 0]
    evs += list(self.all_dma.values())
    for e, eng in self.eng.items():
        seen = self.seen[e]
        for ev in evs:
            if ev[0] == e:
                continue
            if seen.get(ev[0], 0) >= ev[2]:
                continue
            eng.wait_ge(ev[1], ev[2])
            seen[ev[0]] = ev[2]


Sched.barrier = _sched_barrier
_old_dma = Sched.dma


def _dma_track(self, e, fn, sb, reads=(), writes=()):
    ev = _old_dma(self, e, fn, sb, reads, writes)
    if not hasattr(self, "all_dma"):
        self.all_dma = {}
    self.all_dma[ev[0]] = ev
    return ev


Sched.dma = _dma_track
_old_init = Sched.__init__


def _init2(self, nc):
    _old_init(self, nc)
    self.all_dma = {}


Sched.__init__ = _init2


class _DSem:
    __slots__ = ("sem", "cnt", "key")


def _dma_v2(self, e, fn, sb, reads=(), writes=()):
    self._waits(e, reads, writes)
    if sb.dsem is None:
        if self.free_dsems:
            sb.dsem = self.free_dsems.pop()
        else:
            d = _DSem()
            d.key = "d%d" % self.ndsem
            d.sem = self.nc.alloc_semaphore("dsem%d" % self.ndsem)
            d.cnt = 0
            self.ndsem += 1
            sb.dsem = d
        self.bound.append(sb)
    d = sb.dsem
    ins = fn(self.eng[e])
    d.cnt += 16
    ev = (d.key, d.sem, d.cnt)
    ins.then_inc(d.sem, 16)
    self._record(ev, reads, writes)
    self.all_dma[d.key] = ev
    return ev


def _barrier_v2(self):
    _sched_barrier(self)
    for b in self.bound:
        self.free_dsems.append(b.dsem)
        b.dsem = None
    self.bound = []


def _init3(self, nc):
    _old_init(self, nc)
    self.all_dma = {}
    self.free_dsems = []
    self.bound = []


Sched.dma = _dma_v2
Sched.barrier = _barrier_v2
Sched.__init__ = _init3


import math

TWO_PI = 2.0 * math.pi
CW1 = 6.28125
CW2 = TWO_PI - CW1


def _mixer_consts():
    c = {}
    fa = 500000.0 ** (-(np.arange(16, dtype=np.float32) * 2.0 / 32.0))
    fcol = np.zeros((128, 1), np.float32)
    fcol[0:16, 0] = fa
    fcol[16:32, 0] = fa
    c["freq_a"] = fcol
    pa = np.zeros((128, 128), np.float32)
    for d in range(16):
        pa[d + 16, d] = -1.0
        pa[d, d + 16] = 1.0
    c["pmat_a"] = pa.astype(ml_dtypes.bfloat16)
    fb = 10000.0 ** (-np.linspace(0.0, 1.0, 64, dtype=np.float32))
    fcolb = np.concatenate([fb, fb]).reshape(128, 1).astype(np.float32)
    c["freq_b"] = fcolb
    pb = np.zeros((128, 128), np.float32)
    for d in range(64):
        pb[d + 64, d] = -1.0
        pb[d, d + 64] = 1.0
    c["pmat_b"] = pb.astype(ml_dtypes.bfloat16)
    a = np.arange(128)[:, None]
    b = np.arange(128)[None, :]
    m = np.stack([(a >= b), (a <= b), (a >= b) & (a >= 64), (a <= b) & (a < 64)]).astype(np.float32)
    c["amask"] = np.ascontiguousarray(m.transpose(1, 0, 2)).astype(ml_dtypes.bfloat16)
    return c


def load_const(C, ph, name, shape, dt, b):
    d = C.inp(name, shape, dt) if name not in C.ins else C.ins[name].ap()
    t = ph.sb(shape, dt, name)
    C.S.dma("sp", lambda q: q.dma_start(out=t[:], in_=d), b, writes=[b])
    return t


def make_trig_tables(C, ph, posb_ap, freq_t, bfreq, nrows, T, cos_t, sin_t, btab, offset=0.0):
    S = C.S
    CH = 2048
    pi_t = ph.sb([nrows, CH], I32, "posi")
    ang = ph.sb([nrows, CH], F32, "ang")
    qi = ph.sb([nrows, CH], I32, "qi")
    qf = ph.sb([nrows, CH], F32, "qf")
    rr = ph.sb([nrows, CH], F32, "rr")
    b = S.buf("trig_tmp")
    bp = S.buf("trig_pos")
    for c0 in range(0, T, CH):
        S.dma("sp", lambda q: q.dma_start(out=pi_t[:], in_=posb_ap[0:nrows, c0:c0 + CH]), bp, writes=[bp])
        S.op("dve", lambda v: v.tensor_copy(out=ang[:], in_=pi_t[:]), reads=[bp], writes=[b])
        S.op("dve", lambda v: v.tensor_scalar(out=ang[:], in0=ang[:], scalar1=freq_t[0:nrows, 0:1], scalar2=offset,
                                              op0=ALU.mult, op1=ALU.add), reads=[b, bfreq], writes=[b])
        S.op("dve", lambda v: v.tensor_scalar(out=qi[:], in0=ang[:], scalar1=1.0 / TWO_PI, scalar2=None,
                                              op0=ALU.mult), reads=[b], writes=[b])
        S.op("dve", lambda v: v.tensor_copy(out=qf[:], in_=qi[:]), reads=[b], writes=[b])
        S.op("dve", lambda v: v.scalar_tensor_tensor(out=rr[:], in0=qf[:], scalar=-CW1, in1=ang[:],
                                                     op0=ALU.mult, op1=ALU.add), reads=[b], writes=[b])
        S.op("dve", lambda v: v.scalar_tensor_tensor(out=rr[:], in0=qf[:], scalar=-CW2, in1=rr[:],
                                                     op0=ALU.mult, op1=ALU.add), reads=[b], writes=[b])
        S.op("dve", lambda v: v.tensor_scalar(out=qf[:], in0=rr[:], scalar1=math.pi, scalar2=None, op0=ALU.is_gt),
             reads=[b], writes=[b])
        S.op("dve", lambda v: v.scalar_tensor_tensor(out=ang[:], in0=qf[:], scalar=-TWO_PI, in1=rr[:],
                                                     op0=ALU.mult, op1=ALU.add), reads=[b], writes=[b])
        S.op("act", lambda a: a.activation(out=sin_t[:, c0:c0 + CH], in_=ang[:], func=AF.Sin), reads=[b], writes=[btab])
        S.op("dve", lambda v: v.tensor_scalar(out=qf[:], in0=rr[:], scalar1=math.pi / 2, scalar2=None, op0=ALU.is_gt),
             reads=[b], writes=[b])
        S.op("dve", lambda v: v.scalar_tensor_tensor(out=ang[:], in0=qf[:], scalar=-TWO_PI, in1=rr[:],
                                                     op0=ALU.mult, op1=ALU.add), reads=[b, btab], writes=[b])
        S.op("act", lambda a: a.activation(out=cos_t[:, c0:c0 + CH], in_=ang[:], func=AF.Sin, bias=C.halfpi_t[0:nrows, :]),
             reads=[b, C.b_const], writes=[btab])


def rope_inplace(C, ph, xt, bx, nrows, T, pmat, bconst, cos_t, sin_t, btab, pools):
    S = C.S
    for c0 in range(0, T, 512):
        pp, bpp = pools["rp"].next()
        S.op("pe", lambda p: p.matmul(pp[0:nrows, :], lhsT=pmat[0:nrows, 0:nrows], rhs=xt[0:nrows, c0:c0 + 512],
                                      start=True, stop=True), reads=[bx, bconst], writes=[bpp])
        t1, bt1 = pools["rt"].next()
        t2, bt2 = pools["rt"].next()
        S.op("dve", lambda v: v.tensor_tensor(out=t1[0:nrows, :], in0=pp[0:nrows, :], in1=sin_t[0:nrows, c0:c0 + 512],
                                              op=ALU.mult), reads=[bpp, btab], writes=[bt1])
        S.op("pool", lambda g: g.tensor_tensor(out=t2[0:nrows, :], in0=xt[0:nrows, c0:c0 + 512],
                                               in1=cos_t[0:nrows, c0:c0 + 512], op=ALU.mult),
             reads=[bx, btab], writes=[bt2])
        S.op("dve", lambda v: v.tensor_tensor(out=xt[0:nrows, c0:c0 + 512], in0=t1[0:nrows, :], in1=t2[0:nrows, :],
                                              op=ALU.add), reads=[bt1, bt2], writes=[bx])


SEQ = 4096
PADA = 1024


def mixer_a_head(C, ph, ld, out_ap, T, W):
    S = C.S
    qt, bq = W["q"]
    kp, bk = W["kp"]
    vp, bv = W["vp"]
    ld("q", qt, bq, 0)
    ld("k", kp, bk, PADA)
    ld("v", vp, bv, PADA)
    rope_inplace(C, ph, qt, bq, 32, T, W["pmat"], W["bconst"], W["cos"], W["sin"], W["btab"], W)
    rope_inplace(C, ph, kp[:, PADA:PADA + T], bk, 32, T, W["pmat"], W["bconst"], W["cos"], W["sin"], W["btab"], W)
    num, bnum = W["num"]
    den, bden = W["den"]
    amask = W["amask"]
    scale = 128.0 ** -0.5
    first = True
    for d in (1, 4, 16):
        sub = T // d
        nb = sub // 128
        for r in range(d):
            vprev = None
            for j in range(nb):
                po, bpo = W["po"].next()
                pd, bpd = W["pd"].next()
                for side in (0, 1):
                    K0 = 128 * j - 64 + 128 * side
                    c_lo = PADA + r + d * K0
                    ksl = kp[:, c_lo:c_lo + d * 127 + 1:d]
                    vsl = vp[:, c_lo:c_lo + d * 127 + 1:d]
                    q_lo = r + d * 128 * j
                    qsl = qt[:, q_lo:q_lo + d * 127 + 1:d]
                    if side == 0 and vprev is not None:
                        vb, bvb = vprev
                    else:
                        pv, bpv = W["pv"].next()
                        S.op("pe", lambda p: p.transpose(out=pv[:], in_=vsl, identity=C.ident_bf[:]),
                             reads=[bv, C.b_const], writes=[bpv])
                        vb, bvb = W["vb"].next()
                        S.op("dve", lambda v: v.tensor_copy(out=vb[:], in_=pv[:]), reads=[bpv], writes=[bvb])
                    if side == 1:
                        vprev = (vb, bvb)
                    pss, bps = W["pss"].next()
                    S.op("pe", lambda p: p.matmul(pss[:], lhsT=ksl, rhs=qsl, start=True, stop=True),
                         reads=[bk, bq], writes=[bps])
                    pt, bpt = W["pt"].next()
                    S.op("act", lambda a: a.activation(out=pt[:], in_=pss[:], func=AF.Exp, scale=scale),
                         reads=[bps], writes=[bpt])
                    mi = side
                    if j == 0 and side == 0:
                        mi = 2
                    if j == nb - 1 and side == 1:
                        mi = 3
                    S.op("pool", lambda g: g.tensor_tensor(out=pt[:], in0=pt[:], in1=amask[:, mi, :], op=ALU.mult),
                         reads=[bpt, W["bconst"]], writes=[bpt])
                    S.op("pe", lambda p: p.matmul(po[:], lhsT=vb[:], rhs=pt[:], start=(side == 0), stop=(side == 1)),
                         reads=[bvb, bpt], writes=[bpo])
                    S.op("pe", lambda p: p.matmul(pd[:], lhsT=C.ones_bf[:], rhs=pt[:], start=(side == 0), stop=(side == 1)),
                         reads=[C.b_const, bpt], writes=[bpd])
                q_lo = r + d * 128 * j
                nsl = num[:, q_lo:q_lo + d * 127 + 1:d]
                dsl = den[:, q_lo:q_lo + d * 127 + 1:d]
                if first:
                    S.op("act", lambda a: a.copy(out=nsl, in_=po[:]), reads=[bpo], writes=[bnum])
                    S.op("dve", lambda v: v.tensor_copy(out=dsl, in_=pd[:]), reads=[bpd], writes=[bden])
                else:
                    S.op("dve", lambda v: v.tensor_tensor(out=nsl, in0=po[:], in1=nsl, op=ALU.add),
                         reads=[bpo, bnum], writes=[bnum])
                    S.op("dve", lambda v: v.tensor_tensor(out=dsl, in0=pd[:], in1=dsl, op=ALU.add),
                         reads=[bpd, bden], writes=[bden])
        first = False
    ot, bo = W["ao"]
    for c0 in range(0, T, 1024):
        S.op("dve", lambda v: v.reciprocal(out=den[:, c0:c0 + 1024], in_=den[:, c0:c0 + 1024]), reads=[bden], writes=[bden])
        S.op("dve", lambda v: v.tensor_tensor(out=ot[:, c0:c0 + 1024], in0=num[:, c0:c0 + 1024],
                                              in1=den[:, c0:c0 + 1024], op=ALU.mult),
             reads=[bnum, bden], writes=[bo])
    return S.dma("sp", lambda q: q.dma_start(out=out_ap, in_=ot[:]), bo, reads=[bo])


def mixer_a_setup(C, ph, posb_ap, T):
    S = C.S
    W = {}
    bconst = S.buf("a_const")
    W["bconst"] = bconst
    W["pmat"] = load_const(C, ph, "pmat_a", [128, 128], BF16, bconst)
    W["amask"] = load_const(C, ph, "amask", [128, 4, 128], BF16, bconst)
    freq = load_const(C, ph, "freq_a", [128, 1], F32, bconst)
    W["cos"] = ph.sb([32, T], F32, "cos")
    W["sin"] = ph.sb([32, T], F32, "sin")
    W["btab"] = S.buf("a_tab")
    tp = Phase(C, ph.name + "_trig")
    make_trig_tables(C, tp, posb_ap, freq, bconst, 32, T, W["cos"], W["sin"], W["btab"])
    tp.close()
    W["q"] = (ph.sb([128, T], BF16, "q"), S.buf("a_q"))
    W["kp"] = (ph.sb([128, T + 2 * PADA], BF16, "kp"), S.buf("a_kp"))
    W["vp"] = (ph.sb([128, T + 2 * PADA], BF16, "vp"), S.buf("a_vp"))
    for nm in ("kp", "vp"):
        t, b = W[nm]
        S.op("pool", lambda g: g.memset(t[:, 0:PADA], 0.0), writes=[b])
        S.op("pool", lambda g: g.memset(t[:, PADA + T:], 0.0), writes=[b])
    W["num"] = (ph.sb([128, T], F32, "num"), S.buf("a_num"))
    W["den"] = (ph.sb([128, T], F32, "den"), S.buf("a_den"))
    W["ao"] = (ph.sb([128, T], BF16, "ao"), S.buf("a_ao"))
    W["rp"] = ph.pool("rp", 1, [128, 512], F32, psum=True)
    W["rt"] = ph.pool("rt", 4, [128, 512], F32)
    W["po"] = ph.pool("po", 2, [128, 128], F32, psum=True)
    W["pd"] = ph.pool("pd", 2, [128, 128], F32, psum=True)
    W["pv"] = ph.pool("pv", 1, [128, 128], BF16, psum=True)
    W["pss"] = ph.pool("pss", 2, [128, 128], F32, psum=True)
    W["vb"] = ph.pool("vb", 4, [128, 128], BF16)
    W["pt"] = ph.pool("pt", 3, [128, 128], BF16)
    return W


def _retention_consts():
    c = {}
    m = np.arange(128, dtype=np.float32)[:, None]
    cc = np.arange(128, dtype=np.float32)[None, :]
    diff = cc - m
    c["ret_dpos"] = np.maximum(diff, 0.0)
    c["ret_dneg"] = np.maximum(-diff, 0.0)
    c["ret_mge"] = (diff >= 0).astype(np.float32)
    c["ret_mlt"] = (diff < 0).astype(np.float32)
    c["ret_cp1"] = np.broadcast_to(cc + 1.0, (128, 128)).copy()
    c["ret_128mc"] = np.broadcast_to(128.0 - cc, (128, 128)).copy()
    col = np.zeros((128, 4), np.float32)
    col[:, 0] = 127.0 - m[:, 0]
    col[:, 1] = m[:, 0]
    col[:, 2] = 128.0
    c["ret_cols"] = col
    c["ones_f"] = np.ones((128, 128), np.float32)
    return c


def mixer_b_setup(C, ph, posb_ap, T):
    S = C.S
    W = {}
    bconst = S.buf("b_const")
    W["bconst"] = bconst
    W["pmat"] = load_const(C, ph, "pmat_b", [128, 128], BF16, bconst)
    for nm in ("ret_dpos", "ret_dneg", "ret_mge", "ret_mlt", "ret_cp1", "ret_128mc", "ones_f"):
        W[nm] = load_const(C, ph, nm, [128, 128], F32, bconst)
    W["ret_cols"] = load_const(C, ph, "ret_cols", [128, 4], F32, bconst)
    freq = load_const(C, ph, "freq_b", [128, 1], F32, bconst)
    W["cos"] = ph.sb([128, T], F32, "cos")
    W["sin"] = ph.sb([128, T], F32, "sin")
    W["btab"] = S.buf("b_tab")
    tp = Phase(C, ph.name + "_trig")
    make_trig_tables(C, tp, posb_ap, freq, bconst, 128, T, W["cos"], W["sin"], W["btab"])
    tp.close()
    return W


def mixer_b_head(C, ph0, ld, decay_t, bdec, hidx, gnw_t, bgn, out_ap, T, W):
    S = C.S
    ph = Phase(C, ph0.name + "_h")
    pp = Phase(C, ph0.name + "_pa")
    nch = T // 128
    sc = 128.0 ** -0.5
    bc = W["bconst"]
    sm = ph.sb([128, 16], F32, "sm")
    dm = ph.sb([128, 128], F32, "dm")
    tmp = ph.sb([128, 128], F32, "tmp")
    qdf = ph.sb([128, 128], F32, "qdf")
    qdb = ph.sb([128, 128], F32, "qdb")
    rT, brT = ph.sb([128, 2, T], F32, "rT"), S.buf("b_rT")
    qt, bq = pp.sb([128, T], BF16, "q"), S.buf("b_q")
    kt, bk = pp.sb([128, T], BF16, "k"), S.buf("b_k")
    vt, bv = pp.sb([128, 2, T], BF16, "v"), S.buf("b_v")
    ld("q", qt[:, :], bq)
    ld("k", kt[:, :], bk)
    ld("v0", vt[:, 0, :], bv)
    ld("v1", vt[:, 1, :], bv)
    rpools = {"rp": pp.pool("rp", 1, [128, 512], F32, psum=True), "rt": pp.pool("rt", 4, [128, 512], F32)}
    rope_inplace(C, ph, qt, bq, 128, T, W["pmat"], bc, W["cos"], W["sin"], W["btab"], rpools)
    rope_inplace(C, ph, kt, bk, 128, T, W["pmat"], bc, W["cos"], W["sin"], W["btab"], rpools)
    bsm = S.buf("b_sm")
    nh2 = decay_t.shape[1] // 2
    for dr in (0, 1):
        col = dr * nh2 + hidx
        S.op("act", lambda a: a.activation(out=sm[:, dr:dr + 1], in_=decay_t[:, col:col + 1], func=AF.Exp, scale=-1.0),
             reads=[bdec], writes=[bsm])
        S.op("dve", lambda v: v.tensor_scalar(out=sm[:, dr:dr + 1], in0=sm[:, dr:dr + 1], scalar1=1.0, scalar2=None,
                                              op0=ALU.add), reads=[bsm], writes=[bsm])
        S.op("act", lambda a: a.activation(out=sm[:, dr:dr + 1], in_=sm[:, dr:dr + 1], func=AF.Ln), reads=[bsm], writes=[bsm])
        S.op("dve", lambda v: v.tensor_scalar(out=sm[:, dr:dr + 1], in0=sm[:, dr:dr + 1], scalar1=-1.0, scalar2=None,
                                              op0=ALU.mult), reads=[bsm], writes=[bsm])
    lgf, lgb = sm[:, 0:1], sm[:, 1:2]
    cols = W["ret_cols"]
    S.op("act", lambda a: a.activation(out=sm[:, 2:3], in_=cols[:, 0:1], func=AF.Exp, scale=lgf), reads=[bsm, bc], writes=[bsm])
    S.op("act", lambda a: a.activation(out=sm[:, 3:4], in_=cols[:, 1:2], func=AF.Exp, scale=lgb), reads=[bsm, bc], writes=[bsm])
    S.op("act", lambda a: a.activation(out=sm[:, 4:5], in_=cols[:, 2:3], func=AF.Exp, scale=lgf), reads=[bsm, bc], writes=[bsm])
    S.op("act", lambda a: a.activation(out=sm[:, 5:6], in_=cols[:, 2:3], func=AF.Exp, scale=lgb), reads=[bsm, bc], writes=[bsm])
    bdm = S.buf("b_dm")
    S.op("act", lambda a: a.activation(out=dm[:], in_=W["ret_dpos"][:], func=AF.Exp, scale=lgf), reads=[bsm, bc], writes=[bdm])
    S.op("dve", lambda v: v.scalar_tensor_tensor(out=dm[:], in0=dm[:], scalar=sc, in1=W["ret_mge"][:], op0=ALU.mult, op1=ALU.mult),
         reads=[bdm, bc], writes=[bdm])
    S.op("act", lambda a: a.activation(out=tmp[:], in_=W["ret_dneg"][:], func=AF.Exp, scale=lgb), reads=[bsm, bc], writes=[bdm])
    S.op("dve", lambda v: v.scalar_tensor_tensor(out=tmp[:], in0=tmp[:], scalar=sc, in1=W["ret_mlt"][:], op0=ALU.mult, op1=ALU.mult),
         reads=[bdm, bc], writes=[bdm])
    S.op("dve", lambda v: v.tensor_tensor(out=dm[:], in0=dm[:], in1=tmp[:], op=ALU.add), reads=[bdm], writes=[bdm])
    S.op("act", lambda a: a.activation(out=qdf[:], in_=W["ret_cp1"][:], func=AF.Exp, scale=lgf), reads=[bsm, bc], writes=[bdm])
    S.op("act", lambda a: a.activation(out=qdb[:], in_=W["ret_128mc"][:], func=AF.Exp, scale=lgb), reads=[bsm, bc], writes=[bdm])
    S.op("dve", lambda v: v.tensor_scalar(out=qdf[:], in0=qdf[:], scalar1=sc, scalar2=None, op0=ALU.mult), reads=[bdm], writes=[bdm])
    S.op("dve", lambda v: v.tensor_scalar(out=qdb[:], in0=qdb[:], scalar1=sc, scalar2=None, op0=ALU.mult), reads=[bdm], writes=[bdm])
    qf, bqf = pp.sb([128, T], BF16, "qf"), S.buf("b_qf")
    qb, bqb = pp.sb([128, T], BF16, "qb"), S.buf("b_qb")
    for n in range(nch):
        sl = slice(n * 128, (n + 1) * 128)
        S.op("dve", lambda v: v.tensor_tensor(out=qf[:, sl], in0=qt[:, sl], in1=qdf[:], op=ALU.mult), reads=[bq, bdm], writes=[bqf])
        S.op("pool", lambda g: g.tensor_tensor(out=qb[:, sl], in0=qt[:, sl], in1=qdb[:], op=ALU.mult), reads=[bq, bdm], writes=[bqb])
    kf, bkf = pp.sb([128, nch, 128], BF16, "kf"), S.buf("b_kf")
    kb, bkb = pp.sb([128, nch, 128], BF16, "kb"), S.buf("b_kb")
    vtm, bvtm = pp.sb([128, nch, 256], BF16, "vtm"), S.buf("b_vtm")
    ptr = pp.pool("ptr", 1, [128, 3, 128], BF16, psum=True)
    for n in range(nch):
        sl = slice(n * 128, (n + 1) * 128)
        pt, bpt = ptr.next()
        S.op("pe", lambda p: p.transpose(out=pt[:, 0, :], in_=kt[:, sl], identity=C.ident_bf[:]), reads=[bk, C.b_const], writes=[bpt])
        S.op("pe", lambda p: p.transpose(out=pt[:, 1, :], in_=vt[:, 0, sl], identity=C.ident_bf[:]), reads=[bv, C.b_const], writes=[bpt])
        S.op("pe", lambda p: p.transpose(out=pt[:, 2, :], in_=vt[:, 1, sl], identity=C.ident_bf[:]), reads=[bv, C.b_const], writes=[bpt])
        S.op("act", lambda a: a.activation(out=kf[:, n, :], in_=pt[:, 0, :], func=AF.Copy, scale=sm[:, 2:3]), reads=[bpt, bsm], writes=[bkf])
        S.op("act", lambda a: a.activation(out=kb[:, n, :], in_=pt[:, 0, :], func=AF.Copy, scale=sm[:, 3:4]), reads=[bpt, bsm], writes=[bkb])
        S.op("dve", lambda v: v.tensor_copy(out=vtm[:, n, :], in_=pt[:, 1:3, :]), reads=[bpt], writes=[bvtm])
    sf, bsf = pp.sb([128, nch, 256], BF16, "sf"), S.buf("b_sf")
    sbk, bsb = pp.sb([128, nch, 256], BF16, "sbk"), S.buf("b_sb")
    st, bst = pp.sb([128, 256], F32, "st"), S.buf("b_st")
    pkv = pp.pool("pkv", 1, [128, 256], F32, psum=True)
    S.op("dve", lambda v: v.memset(st[:], 0.0), writes=[bst])
    S.op("pool", lambda g: g.memset(sf[:, 0, :], 0.0), writes=[bsf])
    for n in range(1, nch):
        pk, bpk = pkv.next()
        S.op("pe", lambda p: p.matmul(pk[:], lhsT=kf[:, n - 1, :], rhs=vtm[:, n - 1, :], start=True, stop=True),
             reads=[bkf, bvtm], writes=[bpk])
        S.op("dve", lambda v: v.scalar_tensor_tensor(out=st[:], in0=st[:], scalar=sm[:, 4:5], in1=pk[:], op0=ALU.mult, op1=ALU.add),
             reads=[bst, bsm, bpk], writes=[bst])
        S.op("act", lambda a: a.copy(out=sf[:, n, :], in_=st[:]), reads=[bst], writes=[bsf])
    S.op("dve", lambda v: v.memset(st[:], 0.0), reads=[bst], writes=[bst])
    S.op("pool", lambda g: g.memset(sbk[:, nch - 1, :], 0.0), writes=[bsb])
    for n in range(nch - 2, -1, -1):
        pk, bpk = pkv.next()
        S.op("pe", lambda p: p.matmul(pk[:], lhsT=kb[:, n + 1, :], rhs=vtm[:, n + 1, :], start=True, stop=True),
             reads=[bkb, bvtm], writes=[bpk])
        S.op("dve", lambda v: v.scalar_tensor_tensor(out=st[:], in0=st[:], scalar=sm[:, 5:6], in1=pk[:], op0=ALU.mult, op1=ALU.add),
             reads=[bst, bsm, bpk], writes=[bst])
        S.op("act", lambda a: a.copy(out=sbk[:, n, :], in_=st[:]), reads=[bst], writes=[bsb])
    pss = pp.pool("pss", 2, [128, 128], F32, psum=True)
    pout = pp.pool("pout", 2, [128, 128], F32, psum=True)
    pmt = pp.pool("pmt", 3, [128, 128], BF16)
    for n in range(nch):
        sl = slice(n * 128, (n + 1) * 128)
        ps_, bps = pss.next()
        S.op("pe", lambda p: p.matmul(ps_[:], lhsT=kt[:, sl], rhs=qt[:, sl], start=True, stop=True), reads=[bk, bq], writes=[bps])
        pm, bpm = pmt.next()
        S.op("dve", lambda v: v.tensor_tensor(out=pm[:], in0=ps_[:], in1=dm[:], op=ALU.mult), reads=[bps, bdm], writes=[bpm])
        for hf in (0, 1):
            po, bpo = pout.next()
            cs = slice(hf * 128, (hf + 1) * 128)
            S.op("pe", lambda p: p.matmul(po[:], lhsT=vtm[:, n, cs], rhs=pm[:], start=True, stop=False), reads=[bvtm, bpm], writes=[bpo])
            S.op("pe", lambda p: p.matmul(po[:], lhsT=sf[:, n, cs], rhs=qf[:, sl], start=False, stop=False), reads=[bsf, bqf], writes=[bpo])
            S.op("pe", lambda p: p.matmul(po[:], lhsT=sbk[:, n, cs], rhs=qb[:, sl], start=False, stop=True), reads=[bsb, bqb], writes=[bpo])
            if hf == 0:
                S.op("act", lambda a: a.copy(out=rT[:, hf, sl], in_=po[:]), reads=[bpo], writes=[brT])
            else:
                S.op("dve", lambda v: v.tensor_copy(out=rT[:, hf, sl], in_=po[:]), reads=[bpo], writes=[brT])
    pp.close()
    gt, bg = ph.sb([128, 2, T], BF16, "g"), S.buf("b_g")
    ld("g0", gt[:, 0, :], bg)
    ld("g1", gt[:, 1, :], bg)
    sq, bsq = ph.sb([128, 2, 512], F32, "sq"), S.buf("b_sq")
    pst = ph.pool("pst", 2, [128, 2, 512], F32, psum=True)
    mv, bmv = ph.sb([128, 4, 512], F32, "mv"), S.buf("b_mv")
    ot, bo = ph.sb([128, 2, T], BF16, "ot"), S.buf("b_ot")
    sg, bsg = ph.sb([128, 2, 512], F32, "sg"), S.buf("b_sg")
    onesf = W["ones_f"]
    for c0 in range(0, T, 512):
        cs = slice(c0, c0 + 512)
        S.op("act", lambda a: a.activation(out=sq[:], in_=rT[:, :, cs], func=AF.Square), reads=[brT], writes=[bsq])
        p2, bp2 = pst.next()
        for hf in (0, 1):
            S.op("pe", lambda p: p.matmul(p2[:, 0, :], lhsT=onesf[:], rhs=rT[:, hf, cs], start=(hf == 0), stop=(hf == 1)),
                 reads=[bc, brT], writes=[bp2])
        for hf in (0, 1):
            S.op("pe", lambda p: p.matmul(p2[:, 1, :], lhsT=onesf[:], rhs=sq[:, hf, :], start=(hf == 0), stop=(hf == 1)),
                 reads=[bc, bsq], writes=[bp2])
        S.op("dve", lambda v: v.tensor_scalar(out=mv[:, 0, :], in0=p2[:, 0, :], scalar1=1.0 / 256, scalar2=None, op0=ALU.mult),
             reads=[bp2], writes=[bmv])
        S.op("dve", lambda v: v.tensor_tensor(out=mv[:, 1, :], in0=mv[:, 0, :], in1=mv[:, 0, :], op=ALU.mult), reads=[bmv], writes=[bmv])
        S.op("dve", lambda v: v.scalar_tensor_tensor(out=mv[:, 1, :], in0=p2[:, 1, :], scalar=1.0 / 256, in1=mv[:, 1, :],
                                                     op0=ALU.mult, op1=ALU.subtract), reads=[bp2, bmv], writes=[bmv])
        S.op("act", lambda a: a.activation(out=mv[:, 1, :], in_=mv[:, 1, :], func=AF.Sqrt, bias=C.eps_t[:], scale=1.0),
             reads=[bmv, C.b_const], writes=[bmv])
        S.op("dve", lambda v: v.reciprocal(out=mv[:, 1, :], in_=mv[:, 1, :]), reads=[bmv], writes=[bmv])
        S.op("act", lambda a: a.activation(out=sg[:], in_=gt[:, :, cs], func=AF.Silu), reads=[bg], writes=[bsg])
        for hf in (0, 1):
            S.op("dve", lambda v: v.tensor_tensor(out=mv[:, 2, :], in0=rT[:, hf, cs], in1=mv[:, 0, :], op=ALU.subtract),
                 reads=[brT, bmv], writes=[bmv])
            S.op("dve", lambda v: v.scalar_tensor_tensor(out=mv[:, 2, :], in0=mv[:, 2, :], scalar=gnw_t[:, hf:hf + 1], in1=mv[:, 1, :],
                                                         op0=ALU.mult, op1=ALU.mult), reads=[bmv, bgn], writes=[bmv])
            S.op("dve", lambda v: v.tensor_tensor(out=ot[:, hf, cs], in0=mv[:, 2, :], in1=sg[:, hf, :], op=ALU.mult),
                 reads=[bmv, bsg], writes=[bo])
    ev = S.dma("sp", lambda q: q.dma_start(out=out_ap.rearrange("(h p) t -> p h t", p=128), in_=ot[:]), bo, reads=[bo])
    ph.close()
    return ev


def _s5_consts(T):
    return {"tidx": np.broadcast_to(np.arange(T, dtype=np.int32), (128, T)).copy()}


def sincos_col(C, ph, ang, bang, s_out, c_out, bout, tmp, btmp):
    S = C.S
    a2, qi, qf, rr = tmp["f"][:, 0:1], tmp["i"][:, 0:1], tmp["f"][:, 1:2], tmp["f"][:, 2:3]
    m = tmp["f"][:, 3:4]
    S.op("dve", lambda v: v.tensor_scalar(out=a2, in0=ang, scalar1=8 * math.pi, scalar2=None, op0=ALU.add), reads=[bang], writes=[btmp])
    S.op("dve", lambda v: v.tensor_scalar(out=qi, in0=a2, scalar1=1.0 / TWO_PI, scalar2=None, op0=ALU.mult), reads=[btmp], writes=[btmp])
    S.op("dve", lambda v: v.tensor_copy(out=qf, in_=qi), reads=[btmp], writes=[btmp])
    S.op("dve", lambda v: v.scalar_tensor_tensor(out=rr, in0=qf, scalar=-CW1, in1=a2, op0=ALU.mult, op1=ALU.add), reads=[btmp], writes=[btmp])
    S.op("dve", lambda v: v.scalar_tensor_tensor(out=rr, in0=qf, scalar=-CW2, in1=rr, op0=ALU.mult, op1=ALU.add), reads=[btmp], writes=[btmp])
    S.op("dve", lambda v: v.tensor_scalar(out=m, in0=rr, scalar1=math.pi, scalar2=None, op0=ALU.is_gt), reads=[btmp], writes=[btmp])
    S.op("dve", lambda v: v.scalar_tensor_tensor(out=a2, in0=m, scalar=-TWO_PI, in1=rr, op0=ALU.mult, op1=ALU.add), reads=[btmp], writes=[btmp])
    S.op("act", lambda a: a.activation(out=s_out, in_=a2, func=AF.Sin), reads=[btmp], writes=[bout])
    S.op("dve", lambda v: v.tensor_scalar(out=m, in0=rr, scalar1=math.pi / 2, scalar2=None, op0=ALU.is_gt), reads=[btmp], writes=[btmp])
    S.op("dve", lambda v: v.scalar_tensor_tensor(out=a2, in0=m, scalar=-TWO_PI, in1=rr, op0=ALU.mult, op1=ALU.add), reads=[btmp, bout], writes=[btmp])
    S.op("act", lambda a: a.activation(out=c_out, in_=a2, func=AF.Sin, bias=C.halfpi_t[:, :]), reads=[btmp, C.b_const], writes=[bout])


def mixer_c_pair(C, ph0, ld_u, prm, dcol_ap, tidx_ap, out_ap, T):
    S = C.S
    ph = Phase(C, ph0.name + "_c")
    ut, bu = ph.sb([32, T], BF16, "u"), S.buf("c_u")
    ld_u(ut, bu)
    Y, bY = ph.sb([32, T], F32, "Y"), S.buf("c_Y")
    cos_t = ph.sb([128, T], F32, "cos")
    sin_t = ph.sb([128, T], F32, "sin")
    btab = S.buf("c_tab")
    pr = ph.sb([128, 24], F32, "pr")
    bpr = S.buf("c_pr")
    tmpd = {"f": ph.sb([128, 4], F32, "tf"), "i": ph.sb([128, 1], I32, "ti")}
    btmp = S.buf("c_tmp")
    bmat = ph.sb([128, 4, 16], F32, "bmat")
    bd = ph.sb([128, 4, 32], F32, "bd")
    bbd = S.buf("c_bd")
    lhs_b = ph.sb([32, 2, 128], BF16, "lhsb")
    lhs_c = ph.sb([128, 2, 32], BF16, "lhsc")
    blhs = S.buf("c_lhs")
    RB = ph.sb([128, 512], F32, "RB")
    bRB = S.buf("c_RB")
    carry = ph.sb([128, 2], F32, "carry")
    bcar = S.buf("c_car")
    dcol = ph.sb([32, 1], F32, "dcol")
    bdc = S.buf("c_dcol")
    S.dma("sp", lambda q: q.dma_start(out=dcol[:], in_=dcol_ap), bdc, writes=[bdc])
    wk = ph.pool("wk", 12, [128, 512], F32)
    hb = ph.pool("hb", 4, [128, 512], BF16)
    px = ph.pool("px", 4, [128, 512], F32, psum=True)
    py = ph.pool("py", 2, [32, 512], F32, psum=True)
    ptp = ph.pool("ptp", 1, [32, 128], F32, psum=True)
    for dr in (0, 1):
        P = prm[dr]
        sg = 1.0 if dr == 0 else -1.0
        for i, nm in enumerate(("lam_re", "lam_im", "logdt")):
            S.dma("sp", lambda q, i=i, nm=nm: q.dma_start(out=pr[:, i:i + 1], in_=P[nm]), bpr, writes=[bpr])
        for i, nm in enumerate(("b_re", "b_im", "c_reT", "c_imT")):
            S.dma("sp", lambda q, i=i, nm=nm: q.dma_start(out=bmat[:, i, :], in_=P[nm]), bbd, writes=[bbd])
        c_ = lambda i: pr[:, i:i + 1]
        S.op("act", lambda a: a.activation(out=c_(2), in_=c_(2), func=AF.Exp), reads=[bpr], writes=[bpr])
        S.op("dve", lambda v: v.tensor_tensor(out=c_(3), in0=c_(0), in1=c_(2), op=ALU.mult), reads=[bpr], writes=[bpr])
        S.op("act", lambda a: a.activation(out=c_(3), in_=c_(3), func=AF.Exp), reads=[bpr], writes=[bpr])
        S.op("dve", lambda v: v.tensor_tensor(out=c_(4), in0=c_(1), in1=c_(2), op=ALU.mult), reads=[bpr], writes=[bpr])
        sincos_col(C, ph, c_(4), bpr, c_(5), c_(6), bpr, tmpd, btmp)
        S.op("dve", lambda v: v.tensor_tensor(out=c_(7), in0=c_(3), in1=c_(6), op=ALU.mult), reads=[bpr], writes=[bpr])
        S.op("dve", lambda v: v.tensor_tensor(out=c_(8), in0=c_(3), in1=c_(5), op=ALU.mult), reads=[bpr], writes=[bpr])
        S.op("dve", lambda v: v.tensor_tensor(out=c_(9), in0=c_(0), in1=c_(0), op=ALU.mult), reads=[bpr], writes=[bpr])
        S.op("dve", lambda v: v.scalar_tensor_tensor(out=c_(9), in0=c_(1), scalar=c_(1), in1=c_(9), op0=ALU.mult, op1=ALU.add), reads=[bpr], writes=[bpr])
        S.op("dve", lambda v: v.reciprocal(out=c_(9), in_=c_(9)), reads=[bpr], writes=[bpr])
        S.op("dve", lambda v: v.tensor_scalar(out=c_(10), in0=c_(7), scalar1=-1.0, scalar2=None, op0=ALU.add), reads=[bpr], writes=[bpr])
        S.op("dve", lambda v: v.tensor_tensor(out=c_(13), in0=c_(10), in1=c_(0), op=ALU.mult), reads=[bpr], writes=[bpr])
        S.op("dve", lambda v: v.scalar_tensor_tensor(out=c_(13), in0=c_(8), scalar=c_(1), in1=c_(13), op0=ALU.mult, op1=ALU.add), reads=[bpr], writes=[bpr])
        S.op("dve", lambda v: v.tensor_tensor(out=c_(11), in0=c_(13), in1=c_(9), op=ALU.mult), reads=[bpr], writes=[bpr])
        S.op("dve", lambda v: v.tensor_tensor(out=c_(13), in0=c_(8), in1=c_(0), op=ALU.mult), reads=[bpr], writes=[bpr])
        S.op("dve", lambda v: v.tensor_tensor(out=c_(14), in0=c_(10), in1=c_(1), op=ALU.mult), reads=[bpr], writes=[bpr])
        S.op("dve", lambda v: v.tensor_tensor(out=c_(13), in0=c_(13), in1=c_(14), op=ALU.subtract), reads=[bpr], writes=[bpr])
        S.op("dve", lambda v: v.tensor_tensor(out=c_(12), in0=c_(13), in1=c_(9), op=ALU.mult), reads=[bpr], writes=[bpr])
        S.op("dve", lambda v: v.tensor_scalar(out=c_(15), in0=c_(12), scalar1=-1.0, scalar2=None, op0=ALU.mult), reads=[bpr], writes=[bpr])
        S.op("pool", lambda g: g.memset(bd[:], 0.0), reads=[bbd], writes=[bbd])
        for gi in (0, 1):
            rs = slice(gi * 64, (gi + 1) * 64)
            cs = slice(gi * 16, (gi + 1) * 16)
            S.op("dve", lambda v: v.tensor_scalar(out=bd[rs, 0, cs], in0=bmat[rs, 0, :], scalar1=pr[rs, 11:12], scalar2=None, op0=ALU.mult),
                 reads=[bbd, bpr], writes=[bbd])
            S.op("dve", lambda v: v.scalar_tensor_tensor(out=bd[rs, 0, cs], in0=bmat[rs, 1, :], scalar=pr[rs, 15:16], in1=bd[rs, 0, cs],
                                                         op0=ALU.mult, op1=ALU.add), reads=[bbd, bpr], writes=[bbd])
            S.op("dve", lambda v: v.tensor_scalar(out=bd[rs, 1, cs], in0=bmat[rs, 1, :], scalar1=pr[rs, 11:12], scalar2=None, op0=ALU.mult),
                 reads=[bbd, bpr], writes=[bbd])
            S.op("dve", lambda v: v.scalar_tensor_tensor(out=bd[rs, 1, cs], in0=bmat[rs, 0, :], scalar=pr[rs, 12:13], in1=bd[rs, 1, cs],
                                                         op0=ALU.mult, op1=ALU.add), reads=[bbd, bpr], writes=[bbd])
            S.op("dve", lambda v: v.tensor_copy(out=bd[rs, 2, cs], in_=bmat[rs, 2, :]), reads=[bbd], writes=[bbd])
            S.op("dve", lambda v: v.tensor_scalar(out=bd[rs, 3, cs], in0=bmat[rs, 3, :], scalar1=-1.0, scalar2=None, op0=ALU.mult),
                 reads=[bbd], writes=[bbd])
        for i in (0, 1):
            pt, bpt = ptp.next()
            S.op("pe", lambda p: p.transpose(out=pt[:], in_=bd[:, i, :], identity=C.ident_f[:]), reads=[bbd, C.b_const], writes=[bpt])
            S.op("dve", lambda v: v.tensor_copy(out=lhs_b[:, i, :], in_=pt[:]), reads=[bpt], writes=[blhs])
        S.op("dve", lambda v: v.tensor_copy(out=lhs_c[:, :, :], in_=bd[:, 2:4, :]), reads=[bbd], writes=[blhs])
        S.op("dve", lambda v: v.memset(RB[:], 1.0), reads=[bRB], writes=[bRB])
        S.op("dve", lambda v: v.tensor_scalar(out=RB[:], in0=RB[:], scalar1=pr[:, 3:4], scalar2=None, op0=ALU.mult), reads=[bRB, bpr], writes=[bRB])
        tp = Phase(C, ph.name + "_trig%d" % dr)
        make_trig_tables(C, tp, tidx_ap, pr[:, 4:5], bpr, 128, T, cos_t, sin_t, btab, offset=8 * math.pi)
        tp.close()
        S.op("dve", lambda v: v.memset(carry[:], 0.0), reads=[bcar], writes=[bcar])
        ntile = T // 512
        order = range(ntile) if dr == 0 else range(ntile - 1, -1, -1)
        for ti in order:
            cs = slice(ti * 512, (ti + 1) * 512)
            pxr, bpxr = px.next()
            pxi, bpxi = px.next()
            S.op("pe", lambda p: p.matmul(pxr[:], lhsT=lhs_b[:, 0, :], rhs=ut[:, cs], start=True, stop=True), reads=[blhs, bu], writes=[bpxr])
            S.op("pe", lambda p: p.matmul(pxi[:], lhsT=lhs_b[:, 1, :], rhs=ut[:, cs], start=True, stop=True), reads=[blhs, bu], writes=[bpxi])
            (a1, ba1), (a2, ba2), (a3, ba3), (a4, ba4) = wk.next(), wk.next(), wk.next(), wk.next()
            S.op("dve", lambda v: v.tensor_tensor(out=a1[:], in0=pxr[:], in1=cos_t[:, cs], op=ALU.mult), reads=[bpxr, btab], writes=[ba1])
            S.op("dve", lambda v: v.tensor_tensor(out=a2[:], in0=pxi[:], in1=sin_t[:, cs], op=ALU.mult), reads=[bpxi, btab], writes=[ba2])
            S.op("dve", lambda v: v.tensor_tensor(out=a3[:], in0=pxi[:], in1=cos_t[:, cs], op=ALU.mult), reads=[bpxi, btab], writes=[ba3])
            S.op("dve", lambda v: v.tensor_tensor(out=a4[:], in0=pxr[:], in1=sin_t[:, cs], op=ALU.mult), reads=[bpxr, btab], writes=[ba4])
            opr = ALU.add if dr == 0 else ALU.subtract
            opi = ALU.subtract if dr == 0 else ALU.add
            S.op("pool", lambda g: g.tensor_tensor(out=a1[:], in0=a1[:], in1=a2[:], op=opr), reads=[ba1, ba2], writes=[ba1])
            S.op("pool", lambda g: g.tensor_tensor(out=a3[:], in0=a3[:], in1=a4[:], op=opi), reads=[ba3, ba4], writes=[ba3])
            (hr_, bhr), (hi_, bhi) = wk.next(), wk.next()
            rev = (lambda t: t[:, ::-1]) if dr == 1 else (lambda t: t[:, :])
            last = 0 if dr == 1 else 511
            S.op("dve", lambda v: v.tensor_tensor_scan(out=rev(hr_), data0=RB[:], data1=rev(a1), initial=carry[:, 0:1], op0=ALU.mult, op1=ALU.add),
                 reads=[bRB, ba1, bcar], writes=[bhr])
            S.op("dve", lambda v: v.tensor_tensor_scan(out=rev(hi_), data0=RB[:], data1=rev(a3), initial=carry[:, 1:2], op0=ALU.mult, op1=ALU.add),
                 reads=[bRB, ba3, bcar], writes=[bhi])
            S.op("dve", lambda v: v.tensor_copy(out=carry[:, 0:1], in_=hr_[:, last:last + 1]), reads=[bhr], writes=[bcar])
            S.op("dve", lambda v: v.tensor_copy(out=carry[:, 1:2], in_=hi_[:, last:last + 1]), reads=[bhi], writes=[bcar])
            (b1, bb1), (b2, bb2), (b3, bb3), (b4, bb4) = wk.next(), wk.next(), wk.next(), wk.next()
            S.op("pool", lambda g: g.tensor_tensor(out=b1[:], in0=hr_[:], in1=cos_t[:, cs], op=ALU.mult), reads=[bhr, btab], writes=[bb1])
            S.op("pool", lambda g: g.tensor_tensor(out=b2[:], in0=hi_[:], in1=sin_t[:, cs], op=ALU.mult), reads=[bhi, btab], writes=[bb2])
            S.op("pool", lambda g: g.tensor_tensor(out=b3[:], in0=hi_[:], in1=cos_t[:, cs], op=ALU.mult), reads=[bhi, btab], writes=[bb3])
            S.op("pool", lambda g: g.tensor_tensor(out=b4[:], in0=hr_[:], in1=sin_t[:, cs], op=ALU.mult), reads=[bhr, btab], writes=[bb4])
            (h1, bh1), (h2, bh2) = hb.next(), hb.next()
            S.op("dve", lambda v: v.scalar_tensor_tensor(out=h1[:], in0=b2[:], scalar=-sg, in1=b1[:], op0=ALU.mult, op1=ALU.add),
                 reads=[bb1, bb2], writes=[bh1])
            S.op("dve", lambda v: v.scalar_tensor_tensor(out=h2[:], in0=b4[:], scalar=sg, in1=b3[:], op0=ALU.mult, op1=ALU.add),
                 reads=[bb3, bb4], writes=[bh2])
            pyt, bpy = py.next()
            S.op("pe", lambda p: p.matmul(pyt[:], lhsT=lhs_c[:, 0, :], rhs=h1[:], start=True, stop=False), reads=[blhs, bh1], writes=[bpy])
            S.op("pe", lambda p: p.matmul(pyt[:], lhsT=lhs_c[:, 1, :], rhs=h2[:], start=False, stop=True), reads=[blhs, bh2], writes=[bpy])
            if dr == 0:
                S.op("act", lambda a: a.copy(out=Y[:, cs], in_=pyt[:]), reads=[bpy], writes=[bY])
            else:
                S.op("dve", lambda v: v.tensor_tensor(out=Y[:, cs], in0=pyt[:], in1=Y[:, cs], op=ALU.add), reads=[bpy, bY], writes=[bY])
    yo, byo = ph.sb([32, T], BF16, "yo"), S.buf("c_yo")
    for c0 in range(0, T, 2048):
        cs = slice(c0, c0 + 2048)
        S.op("dve", lambda v: v.scalar_tensor_tensor(out=Y[:, cs], in0=ut[:, cs], scalar=dcol[:, 0:1], in1=Y[:, cs], op0=ALU.mult, op1=ALU.add),
             reads=[bu, bdc, bY], writes=[bY])
        S.op("act", lambda a: a.activation(out=yo[:, cs], in_=Y[:, cs], func=AF.Gelu), reads=[bY], writes=[byo])
    ev = S.dma("sp", lambda q: q.dma_start(out=out_ap, in_=yo[:]), byo, reads=[byo])
    ph.close()
    return ev


def load_w(C, wt_ap, bw, src_ap):
    if src_ap.dtype == BF16:
        C.S.dma("sp", lambda q: q.dma_start(out=wt_ap, in_=src_ap), bw, writes=[bw])
    else:
        C.S.dma("pool", lambda q: q.dma_start(out=wt_ap, in_=src_ap), bw, writes=[bw])


class WMat:
    def __init__(self, ap3, col0=0):
        self.ap3 = ap3
        self.cw = ap3.shape[2]
        self.col0 = col0

    def blk(self, c0, wn):
        c0 += self.col0
        bi, off = c0 // self.cw, c0 % self.cw
        return self.ap3[bi].rearrange("(kc p) c -> p kc c", p=128)[:, :, off:off + wn]


def wblk(w_ap, c0, wn):
    if hasattr(w_ap, "blk"):
        return w_ap.blk(c0, wn)
    return w_ap.rearrange("(kc p) n -> p kc n", p=128)[:, :, c0:c0 + wn]


def gemm_tok(C, act_fn, bact, nk, ntt, w_ap, nn, cb, wpool, pspool, wn=512):
    S = C.S
    tiles = {}

    def issue(n):
        wt, bw = wpool.next()
        load_w(C, wt[:, 0:nk, 0:wn], bw, wblk(w_ap, n * wn, wn))
        tiles[n] = (wt, bw)

    issue(0)
    for n in range(nn):
        if n + 1 < nn:
            issue(n + 1)
        wt, bw = tiles.pop(n)
        for tt in range(ntt):
            ps, bps = pspool.next()
            for k in range(nk):
                S.op("pe", lambda p, k=k: p.matmul(ps[:, 0:wn], lhsT=act_fn(k, tt), rhs=wt[:, k, 0:wn],
                                                   start=(k == 0), stop=(k == nk - 1)),
                     reads=[bw, bact], writes=[bps])
            cb(tt, n, ps, bps)


def gemm_fm2(C, act_fn, bact, nk, ntok, w_ap, nmb, cb, wpool, pspool, wn=512):
    S = C.S
    tiles = {}

    def issue(n):
        wt, bw = wpool.next()
        load_w(C, wt[:, 0:nk, 0:wn], bw, wblk(w_ap, n * wn, wn))
        tiles[n] = (wt, bw)

    issue(0)
    for nb in range(nmb):
        if nb + 1 < nmb:
            issue(nb + 1)
        wt, bw = tiles.pop(nb)
        for mi in range(wn // 128):
            ps, bps = pspool.next()
            for k in range(nk):
                S.op("pe", lambda p, k=k: p.matmul(ps[:, 0:ntok], lhsT=wt[:, k, mi * 128:(mi + 1) * 128], rhs=act_fn(k),
                                                   start=(k == 0), stop=(k == nk - 1)),
                     reads=[bw, bact], writes=[bps])
            cb(nb * (wn // 128) + mi, ps, bps)


def phase_mem_kv(C, mem_ap, nwb_ap, wkv_ap, kmT_d, vm_d, bkm, bvm):
    S = C.S
    ph = Phase(C, "mkv")
    wb = ph.sb([128, D], F32, "wb")
    bwb = S.buf("mkv_wb")
    S.dma("sp", lambda q: q.dma_start(out=wb[:], in_=nwb_ap), bwb, writes=[bwb])
    pools = {"x": ph.pool("x", 2, [128, D], F32), "h": ph.pool("h", 2, [128, D], BF16),
             "pst": ph.pool("pst", 2, [128, 8, 128], BF16, psum=True),
             "small": {"ss": ph.sb([128, 1], F32), "rs": ph.sb([128, 1], F32), "junk": ph.sb([128, D], BF16), "b": S.buf("mkv_small")}}
    mT = ph.sb([128, 32, 256], BF16, "mT")
    bmT = S.buf("mkv_mT")
    norm_block_to_hT(C, mem_ap, 0, 2, wb, bwb, mT, bmT, pools)
    wpool = ph.pool("w", 2, [128, 32, 512], BF16)
    pspool = ph.pool("ps", 3, [128, 512], F32, psum=True)
    opool = ph.pool("o", 3, [128, 512], BF16)

    def cb_k(m, ps, bps):
        ot, bo = opool.next()
        S.op("act", lambda a: a.copy(out=ot[:, 0:256], in_=ps[:, 0:256]), reads=[bps], writes=[bo])
        S.dma("sp", lambda q: q.dma_start(out=kmT_d[m * 128:(m + 1) * 128, :], in_=ot[:, 0:256]), bo, reads=[bo], writes=[bkm])

    gemm_fm2(C, lambda k: mT[:, k, :], bmT, 32, 256, wkv_ap[0] if isinstance(wkv_ap, tuple) else wkv_ap[:, 0:D], 8, cb_k, wpool, pspool)

    def cb_v(tt, n, ps, bps):
        ot, bo = opool.next()
        S.op("dve", lambda v: v.tensor_copy(out=ot[:], in_=ps[:]), reads=[bps], writes=[bo])
        S.dma("sp", lambda q: q.dma_start(out=vm_d[tt * 128:(tt + 1) * 128, n * 512:(n + 1) * 512], in_=ot[:]), bo, reads=[bo], writes=[bvm])

    gemm_tok(C, lambda k, tt: mT[:, k, tt * 128:(tt + 1) * 128], bmT, 32, 2, wkv_ap[1] if isinstance(wkv_ap, tuple) else wkv_ap[:, D:2 * D], 8, cb_v, wpool, pspool)
    ph.close()


TB3 = 512


def phase_k3(C, x_ap, cat_src, glu_ap, wout_ap, nwc_ap, wq_ap, kmT_d, vm_d, bkm, bvm, wo_ap, nwf_ap, rw_ap,
             x1_d, x2_d, hffn_d, aff_d, affT_d, ntok):
    S = C.S
    ph = Phase(C, "k3")
    bx1 = S.buf("k3_x1d")
    bx2 = S.buf("k3_x2d")
    bhf = S.buf("k3_hffn")
    baf = S.buf("k3_aff")
    glu = ph.sb([128, 8, 1024], BF16, "glu")
    bglu = S.buf("k3_glu")
    for nb_ in range(2):
        load_w(C, glu[:, :, nb_ * 512:(nb_ + 1) * 512], bglu, wblk(glu_ap, nb_ * 512, 512))
    wbc = ph.sb([128, D], F32, "wbc")
    wbf = wbc
    bwb = S.buf("k3_wb")
    brw = S.buf("k3_rw")
    rw = ph.sb([128, 32, 16], BF16, "rw")
    load_w(C, rw[:], brw, rw_ap.rearrange("(kc p) n -> p kc n", p=128))
    actT = ph.sb([128, 32, TB3], BF16, "actT")
    bact = S.buf("k3_act")
    qT = ph.sb([128, 32, TB3], BF16, "qT")
    bqT = S.buf("k3_qT")
    wpool = ph.pool("w", 2, [128, 32, 256], BF16)
    pspool = ph.pool("ps", 3, [128, 512], F32, psum=True)
    xpool = ph.pool("x", 1, [128, D], F32)
    hpool = ph.pool("h", 1, [128, D], BF16)
    small = {"ss": ph.sb([128, 1], F32), "rs": ph.sb([128, 1], F32), "junk": ph.sb([128, D], BF16), "b": S.buf("k3_small")}
    pst = ph.pool("pst", 2, [128, 8, 128], BF16, psum=True)
    npools = {"x": xpool, "h": hpool, "pst": pst, "small": small}
    sgp = ph.pool("sg", 2, [128, 512], F32)
    xo = ph.pool("xo", 3, [128, 512], F32)
    kmh = ph.pool("kmh", 2, [128, 8, 256], BF16)
    vmh = ph.pool("vmh", 2, [128, 2, 1024], BF16)
    ptp = ph.pool("ptp", 2, [128, 2, 512], BF16)
    rdn = ph.pool("rdn", 2, [128, 512], F32)
    rsm = ph.sb([128, 8], F32, "rsm")
    lg = ph.sb([128, 16], F32, "lg")
    aft = ph.sb([16, 128], F32, "aft")
    brs = S.buf("k3_rsm")
    ntt = TB3 // 128
    for t0 in range(0, ntok, TB3):
        for kc in range(32):
            cat_src(kc, t0, (qT if kc >= 24 else actT)[:, kc, :], bqT if kc >= 24 else bact)
        for m in range(8):
            ps, bps = pspool.next()
            for k in range(8):
                S.op("pe", lambda p, k=k: p.matmul(ps[:], lhsT=glu[:, k, m * 128:(m + 1) * 128], rhs=qT[:, 24 + k, :],
                                                   start=(k == 0), stop=(k == 7)), reads=[bglu, bqT], writes=[bps])
            sg, bsg = sgp.next()
            S.op("act", lambda a: a.activation(out=sg[:], in_=ps[:], func=AF.Sigmoid), reads=[bps], writes=[bsg])
            S.op("dve", lambda v: v.tensor_tensor(out=actT[:, 24 + m, :], in0=sg[:], in1=qT[:, 24 + m, :], op=ALU.mult),
                 reads=[bsg, bqT], writes=[bact])

        def cb_res(src_ap, dst_ap, bdst, bsrc=None):
            def cb(tt, n, ps, bps):
                xt, bxt = xo.next()
                r0 = t0 + tt * 128
                S.dma("sp", lambda q: q.dma_start(out=xt[:, 0:256], in_=src_ap[r0:r0 + 128, n * 256:(n + 1) * 256]), bxt,
                      reads=([bsrc] if bsrc else []), writes=[bxt])
                S.op("dve", lambda v: v.tensor_tensor(out=xt[:, 0:256], in0=ps[:, 0:256], in1=xt[:, 0:256], op=ALU.add), reads=[bps, bxt], writes=[bxt])
                S.dma("sp", lambda q: q.dma_start(out=dst_ap[r0:r0 + 128, n * 256:(n + 1) * 256], in_=xt[:, 0:256]), bxt,
                      reads=[bxt], writes=[bdst])
            return cb

        gemm_tok(C, lambda k, tt: actT[:, k, tt * 128:(tt + 1) * 128], bact, 32, ntt, wout_ap, 16, cb_res(x_ap, x1_d, bx1), wpool, pspool, wn=256)
        S.dma("sp", lambda q: q.dma_start(out=wbc[:], in_=nwc_ap), bwb, writes=[bwb])
        for i in range(ntt):
            xt, bx = xpool.next()
            r0 = t0 + i * 128
            S.dma("sp", lambda q: q.dma_start(out=xt[:], in_=x1_d[r0:r0 + 128, :]), bx, reads=[bx1], writes=[bx])
            hb, bh = hpool.next()
            rmsnorm_tile(C, xt, bx, wbc, bwb, hb, bh, small)
            transpose_to(C, hb, bh, lambda k0, n, i=i: actT[:, k0:k0 + n, i * 128:(i + 1) * 128], bact, 32, pst)

        def cb_q(m, ps, bps):
            S.op("act" if m % 2 else "dve",
                 (lambda a: a.copy(out=qT[:, m, :], in_=ps[:])) if m % 2 else (lambda v: v.tensor_copy(out=qT[:, m, :], in_=ps[:])),
                 reads=[bps], writes=[bqT])

        gemm_fm2(C, lambda k: actT[:, k, :], bact, 32, TB3, wq_ap, 16, cb_q, wpool, pspool, wn=256)
        for h in range(4):
            km, bkmh = kmh.next()
            vm, bvmh = vmh.next()
            S.dma("sp", lambda q: q.dma_start(out=km[:], in_=kmT_d[h * 1024:(h + 1) * 1024, :].rearrange("(c p) m -> p c m", p=128)),
                  bkmh, reads=[bkm], writes=[bkmh])
            S.dma("sp", lambda q: q.dma_start(out=vm[:], in_=vm_d[:, h * 1024:(h + 1) * 1024].rearrange("(b p) f -> p b f", p=128)),
                  bvmh, reads=[bvm], writes=[bvmh])
            pt, bpt = ptp.next()
            for mb in range(2):
                ps, bps = pspool.next()
                for c in range(8):
                    S.op("pe", lambda p, c=c: p.matmul(ps[:], lhsT=km[:, c, mb * 128:(mb + 1) * 128], rhs=qT[:, h * 8 + c, :],
                                                       start=(c == 0), stop=(c == 7)), reads=[bkmh, bqT], writes=[bps])
                S.op("act", lambda a: a.activation(out=pt[:, mb, :], in_=ps[:], func=AF.Exp, scale=1.0 / 32.0), reads=[bps], writes=[bpt])
            ps, bps = pspool.next()
            for mb in range(2):
                S.op("pe", lambda p: p.matmul(ps[:], lhsT=C.ones_bf[:], rhs=pt[:, mb, :], start=(mb == 0), stop=(mb == 1)),
                     reads=[C.b_const, bpt], writes=[bps])
            rd, brd = rdn.next()
            S.op("dve", lambda v: v.reciprocal(out=rd[:], in_=ps[:]), reads=[bps], writes=[brd])
            for c in range(8):
                ps, bps = pspool.next()
                for mb in range(2):
                    S.op("pe", lambda p: p.matmul(ps[:], lhsT=vm[:, mb, c * 128:(c + 1) * 128], rhs=pt[:, mb, :],
                                                  start=(mb == 0), stop=(mb == 1)), reads=[bvmh, bpt], writes=[bps])
                S.op("dve", lambda v: v.tensor_tensor(out=actT[:, h * 8 + c, :], in0=ps[:], in1=rd[:], op=ALU.mult),
                     reads=[bps, brd], writes=[bact])
        gemm_tok(C, lambda k, tt: actT[:, k, tt * 128:(tt + 1) * 128], bact, 32, ntt, wo_ap, 16, cb_res(x1_d, x2_d, bx2, bx1), wpool, pspool, wn=256)
        S.dma("sp", lambda q: q.dma_start(out=wbc[:], in_=nwf_ap), bwb, writes=[bwb])
        for i in range(ntt):
            xt, bx = xpool.next()
            r0 = t0 + i * 128
            S.dma("sp", lambda q: q.dma_start(out=xt[:], in_=x2_d[r0:r0 + 128, :]), bx, reads=[bx2], writes=[bx])
            hb, bh = hpool.next()
            rmsnorm_tile(C, xt, bx, wbf, bwb, hb, bh, small)
            S.dma("sp", lambda q: q.dma_start(out=hffn_d[r0:r0 + 128, :], in_=hb[:]), bh, reads=[bh], writes=[bhf])
            transpose_to(C, hb, bh, lambda k0, n, i=i: actT[:, k0:k0 + n, i * 128:(i + 1) * 128], bact, 32, pst)
            ps, bps = pspool.next()
            for k in range(32):
                S.op("pe", lambda p, k=k: p.matmul(ps[:, 0:16], lhsT=actT[:, k, i * 128:(i + 1) * 128], rhs=rw[:, k, :],
                                                   start=(k == 0), stop=(k == 31)), reads=[bact, brw], writes=[bps])
            S.op("dve", lambda v: v.tensor_reduce(out=rsm[:, 0:1], in_=ps[:, 0:16], axis=AX.X, op=ALU.max), reads=[bps], writes=[brs])
            S.op("dve", lambda v: v.tensor_scalar(out=rsm[:, 1:2], in0=rsm[:, 0:1], scalar1=-1.0, scalar2=None, op0=ALU.mult), reads=[brs], writes=[brs])
            S.op("act", lambda a: a.activation(out=lg[:], in_=ps[:, 0:16], func=AF.Exp, bias=rsm[:, 1:2], accum_out=rsm[:, 2:3]),
                 reads=[bps, brs], writes=[brs])
            S.op("dve", lambda v: v.reciprocal(out=rsm[:, 3:4], in_=rsm[:, 2:3]), reads=[brs], writes=[brs])
            S.op("dve", lambda v: v.tensor_scalar(out=lg[:], in0=lg[:], scalar1=rsm[:, 3:4], scalar2=None, op0=ALU.mult), reads=[brs], writes=[brs])
            S.dma("sp", lambda q: q.dma_start(out=aff_d[r0:r0 + 128, :], in_=lg[:]), brs, reads=[brs], writes=[baf])
            pa, bpa = pspool.next()
            S.op("pe", lambda p: p.transpose(out=pa[0:16, 0:128], in_=lg[:], identity=C.ident_f[:]), reads=[brs, C.b_const], writes=[bpa])
            S.op("dve", lambda v: v.tensor_copy(out=aft[:], in_=pa[0:16, 0:128]), reads=[bpa, brs], writes=[brs])
            S.dma("sp", lambda q: q.dma_start(out=affT_d[:, r0:r0 + 128], in_=aft[:]), brs, reads=[brs], writes=[baf])
    ph.close()


CAP = 512
TOWN = 4096
HALF = 2048
SW = 256


def _moe_consts():
    c = {}
    c["moe_iota"] = np.broadcast_to(np.arange(CAP, dtype=np.float32), (128, CAP)).copy()
    rh = np.zeros((128, TOWN // 128, 3), np.float32)
    rh[:, :, 0] = np.arange(128)[:, None]
    rh[:, :, 1] = np.arange(TOWN // 128)[None, :]
    rh[:, :, 2] = 1.0
    c["moe_rh"] = rh.astype(ml_dtypes.bfloat16)
    dm = np.zeros((128, 4), np.float32)
    for sc in range(4):
        dm[:, sc] = TOWN + sc * 128 + np.arange(128)
    c["moe_dmy"] = dm
    c["moe_ncol"] = np.broadcast_to(np.arange(D // SW, dtype=np.float32), (128, D // SW)).copy()
    return c


def phase_k4(C, affT_pair, affT_own, aff_own, hffn_d, xacc_d, wexp, experts, bxacc_in, dep_bufs=()):
    S = C.S
    ph = Phase(C, "k4")
    bc = S.buf("k4_const")
    iota = load_const(C, ph, "moe_iota", [128, CAP], F32, bc)
    rhc = load_const(C, ph, "moe_rh", [128, TOWN // 128, 3], BF16, bc)
    dmy = load_const(C, ph, "moe_dmy", [128, 4], F32, bc)
    ncol = load_const(C, ph, "moe_ncol", [128, D // SW], F32, bc)
    ntt = TOWN // 128
    pos_tok = ph.sb([128, ntt, 16], F32, "pos_tok")
    sel_tok = ph.sb([128, ntt, 16], F32, "sel_tok")
    aff_tok = ph.sb([128, ntt, 16], F32, "aff_tok")
    ahi = ph.sb([128, ntt, 16], BF16, "ahi")
    alo = ph.sb([128, ntt, 16], BF16, "alo")
    btok = S.buf("k4_tok")
    S.dma("sp", lambda q: q.dma_start(out=aff_tok[:], in_=aff_own.rearrange("(t p) e -> p t e", p=128)), btok, reads=list(dep_bufs), writes=[btok])
    S.op("dve", lambda v: v.tensor_copy(out=ahi[:], in_=aff_tok[:]), reads=[btok], writes=[btok])
    S.op("dve", lambda v: v.tensor_tensor(out=alo[:], in0=aff_tok[:], in1=ahi[:], op=ALU.subtract), reads=[btok], writes=[btok])
    p1 = Phase(C, "k4a")
    AT = p1.sb([16, TOWN], F32, "AT")
    junk = p1.sb([16, TOWN], F32, "junk")
    bAT = S.buf("k4_AT")
    for r in (0, 1):
        S.dma("sp", lambda q: q.dma_start(out=AT[:, r * HALF:(r + 1) * HALF], in_=affT_pair[r]), bAT, reads=list(dep_bufs), writes=[bAT])
    bs = p1.sb([16, 8], F32, "bs")
    bbs = S.buf("k4_bs")
    c_ = lambda i: bs[:, i:i + 1]
    S.op("dve", lambda v: v.memset(bs[:], 0.0), writes=[bbs])
    S.op("dve", lambda v: v.memset(c_(1), 1.0), reads=[bbs], writes=[bbs])
    S.op("dve", lambda v: v.memset(c_(6), 0.5), reads=[bbs], writes=[bbs])
    for it in range(30):
        S.op("dve", lambda v: v.scalar_tensor_tensor(out=c_(2), in0=c_(0), scalar=c_(1), in1=c_(6), op0=ALU.add, op1=ALU.mult), reads=[bbs], writes=[bbs])
        S.op("dve", lambda v: v.tensor_scalar(out=junk[:], in0=AT[:], scalar1=c_(2), scalar2=0.0, op0=ALU.is_gt, op1=ALU.add, accum_out=c_(3)),
             reads=[bAT, bbs], writes=[bbs])
        S.op("dve", lambda v: v.tensor_scalar(out=c_(4), in0=c_(3), scalar1=CAP - 0.5, scalar2=None, op0=ALU.is_gt), reads=[bbs], writes=[bbs])
        S.op("dve", lambda v: v.tensor_tensor(out=c_(5), in0=c_(2), in1=c_(0), op=ALU.subtract), reads=[bbs], writes=[bbs])
        S.op("dve", lambda v: v.scalar_tensor_tensor(out=c_(0), in0=c_(5), scalar=c_(4), in1=c_(0), op0=ALU.mult, op1=ALU.add), reads=[bbs], writes=[bbs])
        S.op("dve", lambda v: v.tensor_tensor(out=c_(5), in0=c_(1), in1=c_(2), op=ALU.subtract), reads=[bbs], writes=[bbs])
        S.op("dve", lambda v: v.scalar_tensor_tensor(out=c_(1), in0=c_(5), scalar=c_(4), in1=c_(2), op0=ALU.mult, op1=ALU.add), reads=[bbs], writes=[bbs])
    selT = p1.sb([16, TOWN], F32, "selT")
    posT = p1.sb([16, TOWN], F32, "posT")
    ones = p1.sb([16, TOWN], F32, "ones")
    bsel = S.buf("k4_sel")
    S.op("dve", lambda v: v.tensor_scalar(out=selT[:], in0=AT[:], scalar1=c_(0), scalar2=None, op0=ALU.is_gt), reads=[bAT, bbs], writes=[bsel])
    S.op("dve", lambda v: v.memset(ones[:], 1.0), writes=[bsel])
    S.op("dve", lambda v: v.tensor_tensor_scan(out=posT[:], data0=ones[:], data1=selT[:], initial=0.0, op0=ALU.mult, op1=ALU.add),
         reads=[bsel], writes=[bsel])
    S.op("dve", lambda v: v.tensor_tensor(out=posT[:], in0=posT[:], in1=selT[:], op=ALU.subtract), reads=[bsel], writes=[bsel])
    ptr = p1.pool("ptr", 2, [128, 2, 16], F32, psum=True)
    for tt in range(ntt):
        pt, bpt = ptr.next()
        cs = slice(tt * 128, (tt + 1) * 128)
        S.op("pe", lambda p: p.transpose(out=pt[:, 0, :], in_=posT[:, cs], identity=C.ident_f[0:16, 0:16]), reads=[bsel, C.b_const], writes=[bpt])
        S.op("pe", lambda p: p.transpose(out=pt[:, 1, :], in_=selT[:, cs], identity=C.ident_f[0:16, 0:16]), reads=[bsel, C.b_const], writes=[bpt])
        S.op("dve", lambda v: v.tensor_copy(out=pos_tok[:, tt, :], in_=pt[:, 0, :]), reads=[bpt], writes=[btok])
        S.op("dve", lambda v: v.tensor_copy(out=sel_tok[:, tt, :], in_=pt[:, 1, :]), reads=[bpt], writes=[btok])
    p1.close()
    rh = ph.sb([128, ntt, 5], BF16, "rh")
    brh = S.buf("k4_rh")
    S.op("dve", lambda v: v.tensor_copy(out=rh[:, :, 0:3], in_=rhc[:]), reads=[bc], writes=[brh])
    ohp = ph.pool("oh", ntt + 2, [128, CAP], BF16)
    pidx = ph.pool("pidx", 1, [128, 8], F32, psum=True)
    ixf = ph.sb([128, 4, 8], F32, "ixf")
    idx_g = ph.sb([128, 4], I32, "idx_g")
    idx_s = ph.sb([128, 4, D // SW], I32, "idx_s")
    idx_sf = ph.sb([128, D // SW], F32, "idx_sf")
    gate = ph.sb([128, 4], F32, "gate")
    bix = S.buf("k4_ix")
    xgp = ph.pool("xg", 2, [128, D], BF16)
    xeT = ph.sb([128, 32, CAP], BF16, "xeT")
    bxe = S.buf("k4_xeT")
    pst = ph.pool("pst", 2, [128, 8, 128], BF16, psum=True)
    wpool = ph.pool("w", 2, [128, 32, 256], BF16)
    pspool = ph.pool("ps", 3, [128, 512], F32, psum=True)
    sgl = ph.sb([128, 8, CAP], F32, "sgl")
    bsg = S.buf("k4_sgl")
    actT = ph.sb([128, 8, CAP], BF16, "actT")
    bact = S.buf("k4_act")
    yep = ph.pool("ye", 4, [128, SW], F32)
    bacc = [S.buf("k4_acc%d" % n) for n in range(D // SW)]
    for b in bacc:
        b.w = bxacc_in.w
    xacc_v = xacc_d.rearrange("r (a w) -> (r a) w", w=SW)
    bhf = S.buf("k4_hf")
    for e in experts:
        S.op("dve", lambda v: v.tensor_copy(out=rh[:, :, 3], in_=ahi[:, :, e]), reads=[btok, brh], writes=[brh])
        S.op("dve", lambda v: v.tensor_copy(out=rh[:, :, 4], in_=alo[:, :, e]), reads=[btok, brh], writes=[brh])
        ohs = []
        for tt in range(ntt):
            oh, boh = ohp.next()
            S.op("dve", lambda v: v.tensor_scalar(out=oh[:], in0=iota[:], scalar1=pos_tok[:, tt, e:e + 1], scalar2=sel_tok[:, tt, e:e + 1],
                                                  op0=ALU.is_equal, op1=ALU.mult), reads=[bc, btok], writes=[boh])
            ohs.append((oh, boh))
        for sc in range(4):
            pi, bpi = pidx.next()
            for tt in range(ntt):
                oh, boh = ohs[tt]
                S.op("pe", lambda p: p.matmul(pi[:, 0:5], lhsT=oh[:, sc * 128:(sc + 1) * 128], rhs=rh[:, tt, :], start=(tt == 0), stop=(tt == ntt - 1)),
                     reads=[boh, brh], writes=[bpi])
            S.op("dve", lambda v: v.tensor_copy(out=ixf[:, sc, 0:5], in_=pi[:, 0:5]), reads=[bpi, bix], writes=[bix])
            f = lambda i: ixf[:, sc, i:i + 1]
            S.op("dve", lambda v: v.scalar_tensor_tensor(out=f(5), in0=f(1), scalar=128.0, in1=f(0), op0=ALU.mult, op1=ALU.add), reads=[bix], writes=[bix])
            S.op("dve", lambda v: v.tensor_copy(out=idx_g[:, sc:sc + 1], in_=f(5)), reads=[bix], writes=[bix])
            S.op("dve", lambda v: v.tensor_tensor(out=gate[:, sc:sc + 1], in0=f(3), in1=f(4), op=ALU.add), reads=[bix], writes=[bix])
            S.op("dve", lambda v: v.tensor_tensor(out=f(6), in0=f(2), in1=dmy[:, sc:sc + 1], op=ALU.mult), reads=[bix, bc], writes=[bix])
            S.op("dve", lambda v: v.tensor_tensor(out=f(7), in0=dmy[:, sc:sc + 1], in1=f(6), op=ALU.subtract), reads=[bix, bc], writes=[bix])
            S.op("dve", lambda v: v.tensor_tensor(out=f(7), in0=f(7), in1=f(5), op=ALU.add), reads=[bix], writes=[bix])
            S.op("dve", lambda v: v.tensor_copy(out=idx_sf[:], in_=ncol[:]), reads=[bc, bix], writes=[bix])
            S.op("dve", lambda v: v.tensor_scalar(out=f(6), in0=f(7), scalar1=float(D // SW), scalar2=None, op0=ALU.mult), reads=[bix], writes=[bix])
            S.op("dve", lambda v: v.tensor_scalar(out=idx_sf[:], in0=idx_sf[:], scalar1=f(6), scalar2=None, op0=ALU.add), reads=[bix], writes=[bix])
            S.op("dve", lambda v: v.tensor_copy(out=idx_s[:, sc, :], in_=idx_sf[:]), reads=[bix], writes=[bix])
            xg, bxg = xgp.next()
            S.dma("pool", lambda q: q.indirect_dma_start(out=xg[:], out_offset=None, in_=hffn_d,
                                                         in_offset=bass.IndirectOffsetOnAxis(ap=idx_g[:, sc:sc + 1], axis=0)),
                  bxg, reads=[bix, bhf] + list(dep_bufs), writes=[bxg])
            transpose_to(C, xg, bxg, lambda k0, n: xeT[:, k0:k0 + n, sc * 128:(sc + 1) * 128], bxe, 32, pst)
        wg_e, wu_e, wd_e = wexp(e)

        def cb_g(m, ps, bps):
            S.op("act", lambda a: a.activation(out=sgl[:, m, :], in_=ps[:], func=AF.Silu), reads=[bps], writes=[bsg])

        gemm_fm2(C, lambda k: xeT[:, k, :], bxe, 32, CAP, wg_e, 4, cb_g, wpool, pspool, wn=256)

        def cb_u(m, ps, bps):
            S.op("dve", lambda v: v.tensor_tensor(out=actT[:, m, :], in0=ps[:], in1=sgl[:, m, :], op=ALU.mult), reads=[bps, bsg], writes=[bact])

        gemm_fm2(C, lambda k: xeT[:, k, :], bxe, 32, CAP, wu_e, 4, cb_u, wpool, pspool, wn=256)

        def cb_d(sc, n, ps, bps):
            ye, bye = yep.next()
            S.op("act", lambda a: a.activation(out=ye[:], in_=ps[:, 0:SW], func=AF.Copy, scale=gate[:, sc:sc + 1]), reads=[bps, bix], writes=[bye])
            S.dma("pool", lambda q: q.indirect_dma_start(out=xacc_v, out_offset=bass.IndirectOffsetOnAxis(ap=idx_s[:, sc, n:n + 1], axis=0),
                                                         in_=ye[:], in_offset=None, compute_op=ALU.add),
                  bye, reads=[bye, bix], writes=[bacc[n]])

        gemm_tok(C, lambda k, tt: actT[:, k, tt * 128:(tt + 1) * 128], bact, 8, 4, wd_e, D // SW, cb_d, wpool, pspool, wn=SW)
    ph.close()
    return bacc


def _coll(self, kind, groups, in_ap, out_ap, reads, writes):
    e = "pool"
    self._waits(e, reads, writes)
    if not hasattr(self, "cc_sem"):
        self.cc_sem = self.nc.alloc_semaphore("cc_sem")
        self.cc_cnt = 0
    ins = self.eng[e].collective_compute(kind, ALU.bypass, replica_groups=groups, ins=[in_ap.opt()], outs=[out_ap.opt()])
    ins.then_inc(self.cc_sem)
    self.cc_cnt += 1
    ev = ("cc", self.cc_sem, self.cc_cnt)
    self._record(ev, reads, writes)
    self.all_dma["cc"] = ev
    return ev


Sched.coll = _coll
G4 = [[0, 1, 2, 3], [4, 5, 6, 7]]
GP4 = [[0, 4], [1, 5], [2, 6], [3, 7]]
GP1 = [[0, 1], [2, 3], [4, 5], [6, 7]]

OFF_QA, OFF_KA, OFF_VA, OFF_QB, OFF_KB, OFF_VB, OFF_GB, OFF_UC = 0, 1536, 3072, 4608, 5376, 6144, 7680, 9216


def phase_k1(C, x_ap, bx_in, nw_ap, w_ap, bw_in, projT_d, bproj):
    S = C.S
    ph = Phase(C, "k1")
    Tc = 2048
    TBK = 1024
    wb = ph.sb([128, D], F32, "wb")
    bwb = S.buf("k1_wb")
    S.dma("sp", lambda q: q.dma_start(out=wb[:], in_=nw_ap), bwb, writes=[bwb])
    pools = {"x": ph.pool("x", 2, [128, D], F32), "h": ph.pool("h", 2, [128, D], BF16),
             "pst": ph.pool("pst", 2, [128, 8, 128], BF16, psum=True),
             "small": {"ss": ph.sb([128, 1], F32), "rs": ph.sb([128, 1], F32), "junk": ph.sb([128, D], BF16), "b": S.buf("k1_small")}}
    hT = ph.sb([128, 32, TBK], BF16, "hT")
    bhT = S.buf("k1_hT")
    wpool = ph.pool("w", 2, [128, 32, 256], BF16)
    pspool = ph.pool("ps", 4, [128, 512], F32, psum=True)
    opool = ph.pool("ot", 4, [128, 512], BF16)
    cnt = 0
    for tb in range(Tc // TBK):
        norm_block_to_hT(C, x_ap, tb * TBK, TBK // 128, wb, bwb, hT, bhT, pools)
        for nb in range(40):
            wt, bw = wpool.next()
            load_w(C, wt[:, :, :], bw, wblk(w_ap, nb * 256, 256))
            for mi in range(2):
                for th in range(TBK // 512):
                    ps, bps = pspool.next()
                    for k in range(32):
                        S.op("pe", lambda p: p.matmul(ps[:], lhsT=wt[:, k, mi * 128:(mi + 1) * 128], rhs=hT[:, k, th * 512:(th + 1) * 512],
                                                      start=(k == 0), stop=(k == 31)), reads=[bw, bhT], writes=[bps])
                    ot, bo = opool.next()
                    if cnt % 2:
                        S.op("act", lambda a: a.copy(out=ot[:], in_=ps[:]), reads=[bps], writes=[bo])
                    else:
                        S.op("dve", lambda v: v.tensor_copy(out=ot[:], in_=ps[:]), reads=[bps], writes=[bo])
                    cnt += 1
                    m = nb * 2 + mi
                    c0 = tb * TBK + th * 512
                    S.dma("sp", lambda q: q.dma_start(out=projT_d[m * 128:(m + 1) * 128, c0:c0 + 512], in_=ot[:]), bo, reads=[bo], writes=[bproj])
    ph.close()


def phase_k2(C, G1, bG1, posb, idx2_ap, P, mixT_d, bmix):
    S = C.S
    T = SEQ
    G1v = G1
    top = Phase(C, "k2")
    idx2 = top.sb([128, 80], I32, "idx2")
    bidx = S.buf("k2_idx")
    S.dma("sp", lambda q: q.dma_start(out=idx2[:], in_=idx2_ap), bidx, writes=[bidx])

    def gath(dst_fn, b, col):
        for h in (0, 1):
            S.dma("pool", lambda q: q.indirect_dma_start(out=dst_fn(h), out_offset=None, in_=G1v,
                                                         in_offset=bass.IndirectOffsetOnAxis(ap=idx2[:, col * 2 + h:col * 2 + h + 1], axis=0)),
                  b, reads=[bidx, bG1], writes=[b])

    ph = Phase(C, "k2a")
    W = mixer_a_setup(C, ph, posb, T)
    for j in range(6):
        def ld(which, t, b, off, j=j):
            ti = {"q": 0, "k": 1, "v": 2}[which]
            gath(lambda h: t[:, off + h * 2048: off + (h + 1) * 2048], b, j * 3 + ti)
        mixer_a_head(C, ph, ld, mixT_d[j * 128:(j + 1) * 128, :], T, W)
    S.barrier()
    bmix.w = None
    ph.close()
    ph = Phase(C, "k2b")
    Wb = mixer_b_setup(C, ph, posb, T)
    dect = ph.sb([128, 6], F32, "dect")
    gnt = ph.sb([128, 6], F32, "gnt")
    bd = S.buf("k2_dec")
    S.dma("sp", lambda q: q.dma_start(out=dect[:], in_=P["ret_dec"]), bd, writes=[bd])
    S.dma("sp", lambda q: q.dma_start(out=gnt[:], in_=P["gnw"]), bd, writes=[bd])
    for j in range(3):
        def ldb(which, ap, b, j=j):
            wi = {"q": 0, "k": 1, "v0": 2, "v1": 3, "g0": 4, "g1": 5}[which]
            gath(lambda h: ap[:, h * 2048:(h + 1) * 2048], b, 18 + j * 6 + wi)
        mixer_b_head(C, ph, ldb, dect, bd, j, gnt[:, 2 * j:2 * j + 2], bd, mixT_d[768 + 256 * j:768 + 256 * (j + 1), :], T, Wb)
    ph.close()
    ph = Phase(C, "k2c")
    stg = ph.sb([128, T], BF16, "stg")
    bstg = S.buf("k2_stg")
    for gp in range(16):
        if gp % 4 == 0:
            gath(lambda h: stg[:, h * 2048:(h + 1) * 2048], bstg, 36 + gp // 4)

        def ld_u(ut, bu, gp=gp):
            r0 = (gp % 4) * 32
            S.dma("sp", lambda q: q.dma_start(out=ut[:], in_=stg[r0:r0 + 32, :]), bu, reads=[bstg], writes=[bu])

        prm = []
        for dr in (0, 1):
            d = {}
            for i, nm in enumerate(("lam_re", "lam_im", "logdt")):
                d[nm] = P["s5_cols"][gp, dr, i]
            for i, nm in enumerate(("b_re", "b_im", "c_reT", "c_imT")):
                d[nm] = P["s5_mats"][gp, dr, :, i, :]
            prm.append(d)
        mixer_c_pair(C, ph, ld_u, prm, P["s5_d"][gp], P["tidx"], mixT_d[1536 + 32 * gp:1536 + 32 * (gp + 1), :], T)
    ph.close()
    top.close()


NC4 = 4
GRP = [[0, 1, 2, 3]]
WSPEC = (("w_in", 4096, 10240, 512, 1), ("w_out", 4096, D, 512, 1), ("wq", 4096, D, 512, 1), ("wkv", 4096, 2 * D, 512, 1),
         ("wo", 4096, D, 512, 1), ("glu", 1024, 1024, 512, 1), ("wg", 4096, 1024, 512, 16), ("wu", 4096, 1024, 512, 16),
         ("wd", 1024, D, 2048, 16))


def prologue_weight(C, name, src_ap, K, N, cw, ne, pool):
    S = C.S
    Ks = K // 4
    NB = N // cw
    sh = C.scratch(name + "_sh", [ne * NB, Ks, cw], BF16)
    full = C.scratch(name + "_full", [ne * NB, K, cw], BF16)
    for e in range(ne):
        bsh = S.buf("%s_sh%d" % (name, e))
        for r0 in range(0, Ks, 128):
            t, bt = pool.next()
            S.dma("pool", lambda q: q.dma_start(out=t[:, 0:N], in_=src_ap[e * Ks + r0:e * Ks + r0 + 128, :]), bt, writes=[bt])
            S.dma("sp", lambda q: q.dma_start(out=sh[e * NB:(e + 1) * NB, r0:r0 + 128, :].rearrange("nb p c -> p nb c"),
                                              in_=t[:, 0:N].rearrange("p (nb c) -> p nb c", c=cw)), bt, reads=[bt], writes=[bsh])
        for nb in range(NB):
            S.coll("AllGather", GRP, sh[e * NB + nb], full[e * NB + nb], [bsh], [])
    return full


def build_full(depth=2):
    C = Ctx()
    S = C.S
    x_in = C.inp("x", [SEQ, D], F32)
    mem_in = C.inp("mem", [256, D], F32)
    posb = C.inp("posb", [128, SEQ], I32)
    tidx = C.inp("tidx", [128, SEQ], I32)
    idx2_in = C.inp("idx2", [2, 128, 80], I32)
    idx3_in = C.inp("idx3", [2, 128, 32], F32)
    final_nw = C.inp("final_nw", [128, D], F32)
    out = C.out("out", [SEQ, D], F32)
    for nm, arr in list(_mixer_consts().items()) + list(_retention_consts().items()) + list(_moe_consts().items()):
        C.inp(nm, list(arr.shape), BF16 if arr.dtype == ml_dtypes.bfloat16 else (I32 if arr.dtype == np.int32 else F32))
    G1 = C.scratch("G1", [2 * 10240, 2048], BF16)
    G2 = C.scratch("G2", [2 * 2048, SEQ], BF16)
    x1_d = C.scratch("x1_d", [2048, D], F32)
    xacc_all = C.scratch("xacc", [SEQ + CAP, D], F32)
    hffn_all = C.scratch("hffn", [SEQ, D], BF16)
    aff_all = C.scratch("aff", [SEQ, 16], F32)
    xacc = [xacc_all[r * 2048:(r + 1) * 2048, :] for r in (0, 1)]
    hffn = [hffn_all[r * 2048:(r + 1) * 2048, :] for r in (0, 1)]
    aff = [aff_all[r * 2048:(r + 1) * 2048, :] for r in (0, 1)]
    affT_pair = C.scratch("affT_pair", [32, 2048], F32)
    kmT_d = C.scratch("kmT_d", [D, 256], BF16)
    vm_d = C.scratch("vm_d", [256, D], BF16)
    LW = []
    ph = Phase(C, "pro")
    cpool = ph.pool("cast", 3, [128, 10240], BF16)
    for l in range(depth):
        Wl = {}
        for nm, K, N, cw, ne in WSPEC:
            src = C.inp("%s_%d" % (nm, l), [ne * K // 4, N], F32)
            Wl[nm] = prologue_weight(C, "%s_%d" % (nm, l), src, K, N, cw, ne, cpool)
        LW.append(Wl)
    ph.close()
    for l in range(depth):
        Wl = LW[l]
        Pr = []
        rd_in = C.inp("ret_dec_%d" % l, [2, 128, 6], F32)
        gn_in = C.inp("gnw_%d" % l, [2, 128, 6], F32)
        sc_in = C.inp("s5_cols_%d" % l, [2, 16, 2, 3, 128, 1], F32)
        sm_in = C.inp("s5_mats_%d" % l, [2, 16, 2, 128, 4, 16], F32)
        sd_in = C.inp("s5_d_%d" % l, [2, 16, 32, 1], F32)
        for r in (0, 1):
            Pr.append({"ret_dec": rd_in[r], "gnw": gn_in[r], "s5_cols": sc_in[r], "s5_mats": sm_in[r], "s5_d": sd_in[r], "tidx": tidx})
        nw_mix = C.inp("nw_mix_%d" % l, [128, D], F32)
        nw_cross = C.inp("nw_cross_%d" % l, [128, D], F32)
        nw_mem = C.inp("nw_mem_%d" % l, [128, D], F32)
        nw_ffn = C.inp("nw_ffn_%d" % l, [128, D], F32)
        rw = C.inp("rw_%d" % l, [D, 16], F32)
        xs = [x_in[0:2048, :], x_in[2048:4096, :]] if l == 0 else [xacc[0], xacc[1]]
        dummy = S.buf("dummy")
        for r in (0, 1):
            phase_k1(C, xs[r], None, nw_mix, WMat(Wl["w_in"]), dummy, G1[r * 10240:(r + 1) * 10240, :], S.buf("proj"))
        for r in (0, 1):
            phase_k2(C, G1, S.buf("G1"), posb, idx2_in[r], Pr[r], G2[r * 2048:(r + 1) * 2048, :], S.buf("mix"))
        bkm, bvm = S.buf("kmd"), S.buf("vmd")
        phase_mem_kv(C, mem_in, nw_mem, (WMat(Wl["wkv"]), WMat(Wl["wkv"], col0=D)), kmT_d, vm_d, bkm, bvm)
        G2v = G2.rearrange("r (a w) -> (r a) w", w=512)
        for r in (0, 1):
            php = Phase(C, "k3idx")
            idx3f = php.sb([128, 32], F32, "idx3f")
            idx3b = php.sb([128, 4, 32], I32, "idx3b")
            bi3 = S.buf("k3_idx")
            S.dma("sp", lambda q: q.dma_start(out=idx3f[:], in_=idx3_in[r]), bi3, writes=[bi3])
            for blk in range(4):
                S.op("dve", lambda v: v.tensor_scalar(out=idx3b[:, blk, :], in0=idx3f[:], scalar1=float(blk), scalar2=None, op0=ALU.add),
                     reads=[bi3], writes=[bi3])

            def cat_load(kc, t0, dst, bd):
                blk = t0 // 512
                S.dma("pool", lambda q: q.indirect_dma_start(out=dst, out_offset=None, in_=G2v,
                                                             in_offset=bass.IndirectOffsetOnAxis(ap=idx3b[:, blk, kc:kc + 1], axis=0)),
                      bd, reads=[bi3], writes=[bd])

            phase_k3(C, xs[r], cat_load, WMat(Wl["glu"]), WMat(Wl["w_out"]), nw_cross, WMat(Wl["wq"]), kmT_d, vm_d, bkm, bvm,
                     WMat(Wl["wo"]), nw_ffn, rw, x1_d, xacc[r], hffn[r], aff[r], affT_pair[r * 16:(r + 1) * 16, :], 2048)
            php.close()
        affp_v = affT_pair.rearrange("(r e) t -> r e t", r=2)

        def wexp(e):
            return (WMat(Wl["wg"][2 * e:2 * e + 2]), WMat(Wl["wu"][2 * e:2 * e + 2]), WMat(Wl["wd"][2 * e:2 * e + 2]))

        phase_k4(C, affp_v, None, aff_all, hffn_all, xacc_all, wexp, list(range(16)), S.buf("xacc"))
    ph = Phase(C, "fin")
    wb = ph.sb([128, D], F32, "wb")
    bwb = S.buf("fin_wb")
    S.dma("sp", lambda q: q.dma_start(out=wb[:], in_=final_nw), bwb, writes=[bwb])
    xp = ph.pool("x", 2, [128, D], F32)
    hp = ph.pool("h", 2, [128, D], F32)
    small = {"ss": ph.sb([128, 1], F32), "rs": ph.sb([128, 1], F32), "junk": ph.sb([128, D], BF16), "b": S.buf("fin_small")}
    for r in (0, 1):
        for i in range(16):
            xt, bxt = xp.next()
            S.dma("sp", lambda q: q.dma_start(out=xt[:], in_=xacc[r][i * 128:(i + 1) * 128, :]), bxt, writes=[bxt])
            hb, bh = hp.next()
            rmsnorm_tile(C, xt, bxt, wb, bwb, hb, bh, small)
            r0 = r * 2048 + i * 128
            C.out_evs.append(S.dma("sp", lambda q: q.dma_start(out=out[r0:r0 + 128, :], in_=hb[:]), bh, reads=[bh]))
    ph.close()
    return C


def _idx_tables(r):
    idx2 = np.zeros((128, 80), np.int32)
    p = np.arange(128)
    cols = []
    for j in range(6):
        H = 6 * r + j
        cols += [OFF_QA + 128 * H, OFF_KA + 128 * H, OFF_VA + 128 * H]
    for j in range(3):
        H = 3 * r + j
        cols += [OFF_QB + 128 * H, OFF_KB + 128 * H, OFF_VB + 256 * H, OFF_VB + 256 * H + 128, OFF_GB + 256 * H, OFF_GB + 256 * H + 128]
    for cb in range(4):
        cols += [OFF_UC + 512 * r + 128 * cb]
    for ci, row0 in enumerate(cols):
        for h in (0, 1):
            idx2[:, ci * 2 + h] = h * 10240 + row0 + p
    idx3 = np.zeros((128, 32), np.float32)
    for kc in range(32):
        if kc < 12:
            rr, row0 = kc // 6, 128 * (kc % 6)
        elif kc < 24:
            rr, row0 = (kc - 12) // 6, 768 + 128 * ((kc - 12) % 6)
        else:
            rr, row0 = (kc - 24) // 4, 1536 + 128 * ((kc - 24) % 4)
        idx3[:, kc] = (rr * 2048 + row0 + p) * 8 + r * 4
    return idx2, idx3


_PROG = {}


def make_maps(inp, depth=2):
    f32 = lambda a: np.ascontiguousarray(np.asarray(a), dtype=np.float32)
    bc = lambda v: np.ascontiguousarray(np.broadcast_to(np.asarray(v, dtype=np.float32), (128, D)))
    consts = {}
    consts.update(_mixer_consts())
    consts.update(_retention_consts())
    consts.update(_moe_consts())
    consts.update(_s5_consts(SEQ))
    consts.update(_host_consts())
    t2 = [_idx_tables(r) for r in (0, 1)]
    consts["idx2"] = np.stack([t2[0][0], t2[1][0]])
    consts["idx3"] = np.stack([t2[0][1], t2[1][1]])
    consts["final_nw"] = bc(inp["final_norm_w"])
    per_layer = []
    for l in range(depth):
        d = {}
        d["rw_%d" % l] = f32(inp["router_w"][l])
        d["nw_mix_%d" % l] = bc(inp["norm_mix_w"][l])
        d["nw_cross_%d" % l] = bc(inp["norm_cross_w"][l])
        d["nw_mem_%d" % l] = bc(inp["norm_mem_w"][l])
        d["nw_ffn_%d" % l] = bc(inp["norm_ffn_w"][l])
        rd = np.asarray(inp["ret_decay"][l], dtype=np.float32)
        gw = np.asarray(inp["ret_gn_w"][l], dtype=np.float32)
        dec = np.zeros((2, 128, 6), np.float32)
        gn = np.zeros((2, 128, 6), np.float32)
        cols = np.zeros((2, 16, 2, 3, 128, 1), np.float32)
        mats = np.zeros((2, 16, 2, 128, 4, 16), np.float32)
        sd = np.zeros((2, 16, 32, 1), np.float32)
        lre, lim, ldt = (np.asarray(inp[k][l]) for k in ("s5_lam_re", "s5_lam_im", "s5_log_dt"))
        bre, bim, cre, cim = (np.asarray(inp[k][l]) for k in ("s5_b_re", "s5_b_im", "s5_c_re", "s5_c_im"))
        s5d = np.asarray(inp["s5_d"][l])
        for r in (0, 1):
            for j in range(3):
                H = 3 * r + j
                for dr in (0, 1):
                    dec[r, :, dr * 3 + j] = rd[dr, H]
                for hf in (0, 1):
                    gn[r, :, 2 * j + hf] = gw[H * 256 + hf * 128:H * 256 + (hf + 1) * 128]
            for gp in range(16):
                g0 = 32 * r + 2 * gp
                sd[r, gp, :, 0] = s5d[16 * g0:16 * g0 + 32]
                for dr in (0, 1):
                    for gi in (0, 1):
                        g = g0 + gi
                        ps = slice(gi * 64, (gi + 1) * 64)
                        cols[r, gp, dr, 0, ps, 0] = lre[dr][g]
                        cols[r, gp, dr, 1, ps, 0] = lim[dr][g]
                        cols[r, gp, dr, 2, ps, 0] = ldt[dr][g]
                        mats[r, gp, dr, ps, 0] = bre[dr][g]
                        mats[r, gp, dr, ps, 1] = bim[dr][g]
                        mats[r, gp, dr, ps, 2] = cre[dr][g].T
                        mats[r, gp, dr, ps, 3] = cim[dr][g].T
        d["ret_dec_%d" % l], d["gnw_%d" % l] = dec, gn
        d["s5_cols_%d" % l], d["s5_mats_%d" % l], d["s5_d_%d" % l] = cols, mats, sd
        per_layer.append(d)
    maps = []
    for c in range(NC4):
        m = dict(consts)
        m["x"] = f32(inp["x"][c])
        m["mem"] = f32(inp["mem"][c])
        m["posb"] = np.ascontiguousarray(np.broadcast_to(np.asarray(inp["positions"][c], dtype=np.int32), (128, SEQ)))
        for l in range(depth):
            m.update(per_layer[l])
            for nm, key, K in (("w_in", "w_in", 4096), ("w_out", "w_out", 4096), ("wq", "cross_wq", 4096), ("wkv", "cross_wkv", 4096),
                               ("wo", "cross_wo", 4096), ("glu", "s5_glu_w", 1024)):
                Ks = K // 4
                m["%s_%d" % (nm, l)] = f32(inp[key][l][c * Ks:(c + 1) * Ks])
            for nm, key, K in (("wg", "expert_w_gate", 4096), ("wu", "expert_w_up", 4096), ("wd", "expert_w_down", 1024)):
                Ks = K // 4
                a = np.asarray(inp[key][l])[:, c * Ks:(c + 1) * Ks, :]
                m["%s_%d" % (nm, l)] = f32(a.reshape(16 * Ks, a.shape[2]))
        maps.append(m)
    return maps


def kernel(**inp):
    depth = 2
    if "full" not in _PROG:
        C = build_full(depth)
        C.S.finish(C.out_evs)
        _PROG["full"] = C
    C = _PROG["full"]
    maps = make_maps(inp, depth)
    res = run_bass_kernel_spmd(C.nc, maps, core_ids=list(range(NC4)))
    full = np.zeros((4, SEQ, D), np.float32)
    for c in range(NC4):
        full[c] = np.asarray(res.results[c]["out"])
    return full
```

```python
import numpy as np
import ml_dtypes
import concourse.bass as bass
import concourse.mybir as mybir
from concourse.bass_utils import run_bass_kernel_spmd

F32 = mybir.dt.float32
BF16 = mybir.dt.bfloat16
I32 = mybir.dt.int32
U32 = mybir.dt.uint32
AF = mybir.ActivationFunctionType
ALU = mybir.AluOpType
AX = mybir.AxisListType

D = 4096
NCORES = 8
EPS = 1e-6


class Buf:
    __slots__ = ("name", "w", "r", "dsem", "dcnt", "ws")

    def __init__(self, name):
        self.name = name
        self.ws = {}
        self.w = None
        self.r = {}
        self.dsem = None
        self.dcnt = 0


class Sched:
    def __init__(self, nc):
        self.nc = nc
        self.eng = {"pe": nc.tensor, "dve": nc.vector, "act": nc.scalar,
                    "pool": nc.gpsimd, "sp": nc.sync}
        self.sem = {k: nc.alloc_semaphore("s_" + k) for k in self.eng}
        self.cnt = {k: 0 for k in self.eng}
        self.seen = {k: {} for k in self.eng}
        self.nbuf = 0
        self.ndsem = 0
        self.final = []

    def buf(self, name=None):
        self.nbuf += 1
        return Buf(name or ("b%d" % self.nbuf))

    def bufs(self, n, name="b"):
        return [self.buf("%s%d" % (name, i)) for i in range(n)]

    def _waits(self, e, reads, writes):
        need = {}

        def add(ev):
            if ev is None:
                return
            k = ev[0]
            if k not in need or need[k][2] < ev[2]:
                need[k] = ev

        for b in reads:
            add(b.w)
            for ev in b.ws.values():
                add(ev)
        for b in writes:
            add(b.w)
            for ev in b.ws.values():
                add(ev)
            for ev in b.r.values():
                add(ev)
        eng = self.eng[e]
        seen = self.seen[e]
        for k, ev in need.items():
            if e == "pe" and k == "pe":
                continue
            if seen.get(k, 0) >= ev[2]:
                continue
            eng.wait_ge(ev[1], ev[2])
            seen[k] = ev[2]

    def _record(self, ev, reads, writes):
        for b in reads:
            b.r[ev[0]] = ev
        for b in writes:
            b.w = ev
            b.ws[ev[0]] = ev
            b.r = {}

    def op(self, e, fn, reads=(), writes=()):
        self._waits(e, reads, writes)
        ins = fn(self.eng[e])
        self.cnt[e] += 1
        ev = (e, self.sem[e], self.cnt[e])
        ins.then_inc(self.sem[e], 1)
        if e != "pe":
            pass
        self._record(ev, reads, writes)
        return ev

    def dma(self, e, fn, sb, reads=(), writes=()):
        self._waits(e, reads, writes)
        if sb.dsem is None:
            sb.dsem = self.nc.alloc_semaphore("d_%s" % sb.name)
            self.ndsem += 1
        ins = fn(self.eng[e])
        sb.dcnt += 16
        ev = ("d_" + sb.name, sb.dsem, sb.dcnt)
        ins.then_inc(sb.dsem, 16)
        self._record(ev, reads, writes)
        return ev

    def finish(self, evs, e="sp"):
        eng = self.eng[e]
        best = {}
        for ev in evs:
            if ev[0] not in best or best[ev[0]][2] < ev[2]:
                best[ev[0]] = ev
        for ev in best.values():
            eng.wait_ge(ev[1], ev[2])


def _host_consts():
    c = {}
    c["ident_bf"] = np.eye(128, dtype=np.float32).astype(ml_dtypes.bfloat16)
    c["ident_f"] = np.eye(128, dtype=np.float32)
    c["ones_bf"] = np.ones((128, 128), np.float32).astype(ml_dtypes.bfloat16)
    return c


class Ctx:
    def __init__(self):
        self.nc = bass.Bass("TRN2", target_bir_lowering=False)
        self.S = Sched(self.nc)
        self.ins = {}
        self.outs = []
        self.out_evs = []
        nc, S = self.nc, self.S
        self.ident_bf = nc.alloc_sbuf_tensor("sb_ident_bf", [128, 128], BF16)
        self.ident_f = nc.alloc_sbuf_tensor("sb_ident_f", [128, 128], F32)
        self.ones_bf = nc.alloc_sbuf_tensor("sb_ones_bf", [128, 128], BF16)
        self.eps_t = nc.alloc_sbuf_tensor("eps_t", [128, 1], F32)
        self.b_const = S.buf("consts")
        for nm, t, dt in (("ident_bf", self.ident_bf, BF16), ("ident_f", self.ident_f, F32),
                          ("ones_bf", self.ones_bf, BF16)):
            d = self.inp(nm, [128, 128], dt)
            S.dma("sp", lambda q, t=t, d=d: q.dma_start(out=t[:], in_=d), self.b_const,
                  writes=[self.b_const])
        S.op("dve", lambda v: v.memset(self.eps_t[:], EPS), writes=[self.b_const])
        self.halfpi_t = nc.alloc_sbuf_tensor("halfpi_t", [128, 1], F32)
        S.op("dve", lambda v: v.memset(self.halfpi_t[:], 1.5707963267948966), writes=[self.b_const])

    def inp(self, name, shape, dt):
        t = self.nc.dram_tensor(name, list(shape), dt, kind="ExternalInput")
        self.ins[name] = t
        return t.ap()

    def out(self, name, shape, dt):
        t = self.nc.dram_tensor(name, list(shape), dt, kind="ExternalOutput")
        self.outs.append(name)
        return t.ap()

    def scratch(self, name, shape, dt):
        return self.nc.dram_tensor(name, list(shape), dt, kind="Internal").ap()

    def run(self, in_maps):
        self.S.finish(self.out_evs)
        cst = _host_consts()
        maps = []
        for m in in_maps:
            mm = dict(cst)
            mm.update(m)
            maps.append(mm)
        res = run_bass_kernel_spmd(self.nc, maps, core_ids=list(range(len(maps))))
        return res.results


class Pool:
    def __init__(self, C, name, n, shape, dt, psum=False):
        self.tiles = []
        for i in range(n):
            nm = "%s%d" % (name, i)
            if psum:
                t = C.nc.alloc_psum_tensor(nm, list(shape), dt)
            else:
                t = C.nc.alloc_sbuf_tensor(nm, list(shape), dt)
            self.tiles.append((t, C.S.buf(nm)))
        self.i = 0

    def next(self):
        t = self.tiles[self.i % len(self.tiles)]
        self.i += 1
        return t


def rmsnorm_tile(C, xt, bx, wb, bw, hb, bh, P, out_f32=None):
    S = C.S
    ss, rs, junk = P["ss"], P["rs"], P["junk"]
    bs = P["b"]
    S.op("act", lambda a: a.activation(out=junk[:], in_=xt[:], func=AF.Square, accum_out=ss[:]),
         reads=[bx], writes=[bs])
    S.op("act", lambda a: a.activation(out=rs[:], in_=ss[:], func=AF.Sqrt, bias=C.eps_t[:], scale=1.0 / D),
         reads=[bs, C.b_const], writes=[bs])
    S.op("dve", lambda v: v.reciprocal(out=rs[:], in_=rs[:]), reads=[bs], writes=[bs])
    S.op("dve", lambda v: v.scalar_tensor_tensor(out=hb[:], in0=xt[:], scalar=rs[:, 0:1], in1=wb[:],
                                                 op0=ALU.mult, op1=ALU.mult),
         reads=[bx, bs, bw], writes=[bh])


def transpose_to(C, src, bsrc, dst_fn, bdst, nk, pst_pool, dt=BF16, evac=("act", "dve")):
    S = C.S
    ident = C.ident_bf if dt == BF16 else C.ident_f
    per = 8 if dt == BF16 else 4
    gi = 0
    for k0 in range(0, nk, per):
        n = min(per, nk - k0)
        pt, bpt = pst_pool.next()
        for j in range(n):
            S.op("pe", lambda p, j=j, pt=pt: p.transpose(out=pt[:, j, :], in_=src[:, (k0 + j) * 128:(k0 + j + 1) * 128],
                                                          identity=ident[:]),
                 reads=[bsrc, C.b_const], writes=[bpt])
        e = evac[gi % len(evac)]
        gi += 1
        dst = dst_fn(k0, n)
        if e == "act":
            S.op("act", lambda a, pt=pt, dst=dst, n=n: a.copy(out=dst, in_=pt[:, 0:n, :]), reads=[bpt], writes=[bdst])
        else:
            S.op("dve", lambda v, pt=pt, dst=dst, n=n: v.tensor_copy(out=dst, in_=pt[:, 0:n, :]), reads=[bpt], writes=[bdst])


def load_w_cast(C, wt, bw, src_ap):
    C.S.dma("pool", lambda q: q.dma_start(out=wt, in_=src_ap), bw, writes=[bw])


TB = 1024


def norm_block_to_hT(C, x_ap, t0, ntile, wb, bwb, hT, bhT, pools, also_tok=None):
    S = C.S
    for i in range(ntile):
        xt, bx = pools["x"].next()
        r0 = t0 + i * 128
        S.dma("sp", lambda q, xt=xt, r0=r0: q.dma_start(out=xt[:], in_=x_ap[r0:r0 + 128, :]), bx, writes=[bx])
        hb, bh = pools["h"].next()
        rmsnorm_tile(C, xt, bx, wb, bwb, hb, bh, pools["small"])
        if also_tok is not None:
            also_tok(i, hb, bh)
        transpose_to(C, hb, bh, lambda k0, n, i=i: hT[:, k0:k0 + n, i * 128:(i + 1) * 128], bhT, 32, pools["pst"])


def make_norm_pools(C, tag=""):
    nc = C.nc
    pools = {
        "x": Pool(C, "xt" + tag, 2, [128, D], F32),
        "h": Pool(C, "hb" + tag, 2, [128, D], BF16),
        "pst": Pool(C, "pst" + tag, 2, [128, 8, 128], BF16, psum=True),
    }
    pools["small"] = {
        "ss": nc.alloc_sbuf_tensor("ss" + tag, [128, 1], F32),
        "rs": nc.alloc_sbuf_tensor("rs" + tag, [128, 1], F32),
        "junk": nc.alloc_sbuf_tensor("junk" + tag, [128, D], BF16),
        "b": C.S.buf("small" + tag),
    }
    return pools


def gemm_fm(C, hT, bhT, nk, ntok, w_ap, m0, nm, out_cb, wpool, pspool):
    S = C.S
    wv = w_ap.rearrange("(kc p) m -> p kc m", p=128)
    for mi in range(nm):
        m = m0 + mi
        wt, bw = wpool.next()
        load_w_cast(C, wt[:, 0:nk, :], bw, wv[:, :, m * 128:(m + 1) * 128])
        for ts in range(ntok // 512):
            ps, bps = pspool.next()
            for k in range(nk):
                S.op("pe", lambda p, k=k, ps=ps, wt=wt, ts=ts: p.matmul(
                    ps[:], lhsT=wt[:, k, :], rhs=hT[:, k, ts * 512:(ts + 1) * 512],
                    start=(k == 0), stop=(k == nk - 1)),
                    reads=[bw, bhT], writes=[bps])
            out_cb(mi, ts, ps, bps)


def build_k1(l):
    C = Ctx()
    nc, S = C.nc, C.S
    Tc = 2048
    x = C.inp("x", [Tc, D], F32)
    nw = C.inp("nw", [128, D], F32)
    w_in = C.inp("w_in", [D, 10240], F32)
    projT = C.out("projT", [10240, Tc], BF16)
    wb = nc.alloc_sbuf_tensor("wb", [128, D], F32)
    bwb = S.buf("wb")
    S.dma("sp", lambda q: q.dma_start(out=wb[:], in_=nw), bwb, writes=[bwb])
    pools = make_norm_pools(C)
    hT = nc.alloc_sbuf_tensor("hT", [128, 32, TB], BF16)
    bhT = S.buf("hT")
    wpool = Pool(C, "w", 3, [128, 32, 128], BF16)
    pspool = Pool(C, "ps", 4, [128, 512], F32, psum=True)
    opool = Pool(C, "ot", 4, [128, 512], BF16)
    bproj = S.buf("projT")
    evs = []
    cnt = [0]
    for tb in range(Tc // TB):
        norm_block_to_hT(C, x, tb * TB, TB // 128, wb, bwb, hT, bhT, pools)

        def out_cb(mi, ts, ps, bps, tb=tb):
            ot, bo = opool.next()
            e = ("act", "dve")[cnt[0] % 2]
            cnt[0] += 1
            if e == "act":
                S.op("act", lambda a: a.copy(out=ot[:], in_=ps[:]), reads=[bps], writes=[bo])
            else:
                S.op("dve", lambda v: v.tensor_copy(out=ot[:], in_=ps[:]), reads=[bps], writes=[bo])
            c0 = tb * TB + ts * 512
            evs.append(S.dma("sp", lambda q: q.dma_start(out=projT[mi * 128:(mi + 1) * 128, c0:c0 + 512], in_=ot[:]),
                             bo, reads=[bo]))

        gemm_fm(C, hT, bhT, 32, TB, w_in, 0, 80, out_cb, wpool, pspool)
    C.out_evs += evs[-8:] + evs
    return C


_UID = [0]


class Phase:
    def __init__(self, C, name):
        self.C = C
        self.name = name
        self.cms = []
        self.n = 0

    def sb(self, shape, dt, nm=None):
        self.n += 1
        _UID[0] += 1
        cm = self.C.nc.sbuf_tensor("%s_%s%d_%d" % (self.name, nm or "t", self.n, _UID[0]), list(shape), dt)
        t = cm.__enter__()
        self.cms.append(cm)
        return t

    def ps(self, shape, dt, nm=None):
        self.n += 1
        _UID[0] += 1
        cm = self.C.nc.psum_tensor("%s_%s%d_%d" % (self.name, nm or "p", self.n, _UID[0]), list(shape), dt)
        t = cm.__enter__()
        self.cms.append(cm)
        return t

    def pool(self, nm, n, shape, dt, psum=False):
        p = Pool.__new__(Pool)
        p.tiles = []
        p.i = 0
        for i in range(n):
            t = self.ps(shape, dt, nm) if psum else self.sb(shape, dt, nm)
            p.tiles.append((t, self.C.S.buf("%s_%s%d" % (self.name, nm, i))))
        return p

    def close(self):
        self.C.S.barrier()
        for cm in reversed(self.cms):
            cm.__exit__(None, None, None)
        self.cms = []


def _sched_barrier(self):
    evs = [(k, self.sem[k], self.cnt[k]) for k in self.eng if self.cnt[k] > 0]
    evs += list(self.all_dma.values())
    for e, eng in self.eng.items():
        seen = self.seen[e]
        for ev in evs:
            if ev[0] == e:
                continue
            if seen.get(ev[0], 0) >= ev[2]:
                continue
            eng.wait_ge(ev[1], ev[2])
            seen[ev[0]] = ev[2]


Sched.barrier = _sched_barrier
_old_dma = Sched.dma


def _dma_track(self, e, fn, sb, reads=(), writes=()):
    ev = _old_dma(self, e, fn, sb, reads, writes)
    if not hasattr(self, "all_dma"):
        self.all_dma = {}
    self.all_dma[ev[0]] = ev
    return ev


Sched.dma = _dma_track
_old_init = Sched.__init__


def _init2(self, nc):
    _old_init(self, nc)
    self.all_dma = {}


Sched.__init__ = _init2


class _DSem:
    __slots__ = ("sem", "cnt", "key")


def _dma_v2(self, e, fn, sb, reads=(), writes=()):
    self._waits(e, reads, writes)
    if sb.dsem is None:
        if self.free_dsems:
            sb.dsem = self.free_dsems.pop()
        else:
            d = _DSem()
            d.key = "d%d" % self.ndsem
            d.sem = self.nc.alloc_semaphore("dsem%d" % self.ndsem)
            d.cnt = 0
            self.ndsem += 1
            sb.dsem = d
        self.bound.append(sb)
    d = sb.dsem
    ins = fn(self.eng[e])
    d.cnt += 16
    ev = (d.key, d.sem, d.cnt)
    ins.then_inc(d.sem, 16)
    self._record(ev, reads, writes)
    self.all_dma[d.key] = ev
    return ev


def _barrier_v2(self):
    _sched_barrier(self)
    for b in self.bound:
        self.free_dsems.append(b.dsem)
        b.dsem = None
    self.bound = []


def _init3(self, nc):
    _old_init(self, nc)
    self.all_dma = {}
    self.free_dsems = []
    self.bound = []


Sched.dma = _dma_v2
Sched.barrier = _barrier_v2
Sched.__init__ = _init3


import math

TWO_PI = 2.0 * math.pi
CW1 = 6.28125
CW2 = TWO_PI - CW1


def _mixer_consts():
    c = {}
    fa = 500000.0 ** (-(np.arange(16, dtype=np.float32) * 2.0 / 32.0))
    fcol = np.zeros((128, 1), np.float32)
    fcol[0:16, 0] = fa
    fcol[16:32, 0] = fa
    c["freq_a"] = fcol
    pa = np.zeros((128, 128), np.float32)
    for d in range(16):
        pa[d + 16, d] = -1.0
        pa[d, d + 16] = 1.0
    c["pmat_a"] = pa.astype(ml_dtypes.bfloat16)
    fb = 10000.0 ** (-np.linspace(0.0, 1.0, 64, dtype=np.float32))
    fcolb = np.concatenate([fb, fb]).reshape(128, 1).astype(np.float32)
    c["freq_b"] = fcolb
    pb = np.zeros((128, 128), np.float32)
    for d in range(64):
        pb[d + 64, d] = -1.0
        pb[d, d + 64] = 1.0
    c["pmat_b"] = pb.astype(ml_dtypes.bfloat16)
    a = np.arange(128)[:, None]
    b = np.arange(128)[None, :]
    m = np.stack([(a >= b), (a <= b), (a >= b) & (a >= 64), (a <= b) & (a < 64)]).astype(np.float32)
    c["amask"] = np.ascontiguousarray(m.transpose(1, 0, 2)).astype(ml_dtypes.bfloat16)
    return c


def load_const(C, ph, name, shape, dt, b):
    d = C.inp(name, shape, dt) if name not in C.ins else C.ins[name].ap()
    t = ph.sb(shape, dt, name)
    C.S.dma("sp", lambda q: q.dma_start(out=t[:], in_=d), b, writes=[b])
    return t


def make_trig_tables(C, ph, posb_ap, freq_t, bfreq, nrows, T, cos_t, sin_t, btab, offset=0.0):
    S = C.S
    CH = 2048
    pi_t = ph.sb([nrows, CH], I32, "posi")
    ang = ph.sb([nrows, CH], F32, "ang")
    qi = ph.sb([nrows, CH], I32, "qi")
    qf = ph.sb([nrows, CH], F32, "qf")
    rr = ph.sb([nrows, CH], F32, "rr")
    b = S.buf("trig_tmp")
    bp = S.buf("trig_pos")
    for c0 in range(0, T, CH):
        S.dma("sp", lambda q: q.dma_start(out=pi_t[:], in_=posb_ap[0:nrows, c0:c0 + CH]), bp, writes=[bp])
        S.op("dve", lambda v: v.tensor_copy(out=ang[:], in_=pi_t[:]), reads=[bp], writes=[b])
        S.op("dve", lambda v: v.tensor_scalar(out=ang[:], in0=ang[:], scalar1=freq_t[0:nrows, 0:1], scalar2=offset,
                                              op0=ALU.mult, op1=ALU.add), reads=[b, bfreq], writes=[b])
        S.op("dve", lambda v: v.tensor_scalar(out=qi[:], in0=ang[:], scalar1=1.0 / TWO_PI, scalar2=None,
                                              op0=ALU.mult), reads=[b], writes=[b])
        S.op("dve", lambda v: v.tensor_copy(out=qf[:], in_=qi[:]), reads=[b], writes=[b])
        S.op("dve", lambda v: v.scalar_tensor_tensor(out=rr[:], in0=qf[:], scalar=-CW1, in1=ang[:],
                                                     op0=ALU.mult, op1=ALU.add), reads=[b], writes=[b])
        S.op("dve", lambda v: v.scalar_tensor_tensor(out=rr[:], in0=qf[:], scalar=-CW2, in1=rr[:],
                                                     op0=ALU.mult, op1=ALU.add), reads=[b], writes=[b])
        S.op("dve", lambda v: v.tensor_scalar(out=qf[:], in0=rr[:], scalar1=math.pi, scalar2=None, op0=ALU.is_gt),
             reads=[b], writes=[b])
        S.op("dve", lambda v: v.scalar_tensor_tensor(out=ang[:], in0=qf[:], scalar=-TWO_PI, in1=rr[:],
                                                     op0=ALU.mult, op1=ALU.add), reads=[b], writes=[b])
        S.op("act", lambda a: a.activation(out=sin_t[:, c0:c0 + CH], in_=ang[:], func=AF.Sin), reads=[b], writes=[btab])
        S.op("dve", lambda v: v.tensor_scalar(out=qf[:], in0=rr[:], scalar1=math.pi / 2, scalar2=None, op0=ALU.is_gt),
             reads=[b], writes=[b])
        S.op("dve", lambda v: v.scalar_tensor_tensor(out=ang[:], in0=qf[:], scalar=-TWO_PI, in1=rr[:],
                                                     op0=ALU.mult, op1=ALU.add), reads=[b, btab], writes=[b])
        S.op("act", lambda a: a.activation(out=cos_t[:, c0:c0 + CH], in_=ang[:], func=AF.Sin, bias=C.halfpi_t[0:nrows, :]),
             reads=[b, C.b_const], writes=[btab])


def rope_inplace(C, ph, xt, bx, nrows, T, pmat, bconst, cos_t, sin_t, btab, pools):
    S = C.S
    for c0 in range(0, T, 512):
        pp, bpp = pools["rp"].next()
        S.op("pe", lambda p: p.matmul(pp[0:nrows, :], lhsT=pmat[0:nrows, 0:nrows], rhs=xt[0:nrows, c0:c0 + 512],
                                      start=True, stop=True), reads=[bx, bconst], writes=[bpp])
        t1, bt1 = pools["rt"].next()
        t2, bt2 = pools["rt"].next()
        S.op("dve", lambda v: v.tensor_tensor(out=t1[0:nrows, :], in0=pp[0:nrows, :], in1=sin_t[0:nrows, c0:c0 + 512],
                                              op=ALU.mult), reads=[bpp, btab], writes=[bt1])
        S.op("pool", lambda g: g.tensor_tensor(out=t2[0:nrows, :], in0=xt[0:nrows, c0:c0 + 512],
                                               in1=cos_t[0:nrows, c0:c0 + 512], op=ALU.mult),
             reads=[bx, btab], writes=[bt2])
        S.op("dve", lambda v: v.tensor_tensor(out=xt[0:nrows, c0:c0 + 512], in0=t1[0:nrows, :], in1=t2[0:nrows, :],
                                              op=ALU.add), reads=[bt1, bt2], writes=[bx])


SEQ = 4096
PADA = 1024


def mixer_a_head(C, ph, ld, out_ap, T, W):
    S = C.S
    qt, bq = W["q"]
    kp, bk = W["kp"]
    vp, bv = W["vp"]
    ld("q", qt, bq, 0)
    ld("k", kp, bk, PADA)
    ld("v", vp, bv, PADA)
    rope_inplace(C, ph, qt, bq, 32, T, W["pmat"], W["bconst"], W["cos"], W["sin"], W["btab"], W)
    rope_inplace(C, ph, kp[:, PADA:PADA + T], bk, 32, T, W["pmat"], W["bconst"], W["cos"], W["sin"], W["btab"], W)
    num, bnum = W["num"]
    den, bden = W["den"]
    amask = W["amask"]
    scale = 128.0 ** -0.5
    first = True
    for d in (1, 4, 16):
        sub = T // d
        nb = sub // 128
        for r in range(d):
            vprev = None
            for j in range(nb):
                po, bpo = W["po"].next()
                pd, bpd = W["pd"].next()
                for side in (0, 1):
                    K0 = 128 * j - 64 + 128 * side
                    c_lo = PADA + r + d * K0
                    ksl = kp[:, c_lo:c_lo + d * 127 + 1:d]
                    vsl = vp[:, c_lo:c_lo + d * 127 + 1:d]
                    q_lo = r + d * 128 * j
                    qsl = qt[:, q_lo:q_lo + d * 127 + 1:d]
                    if side == 0 and vprev is not None:
                        vb, bvb = vprev
                    else:
                        pv, bpv = W["pv"].next()
                        S.op("pe", lambda p: p.transpose(out=pv[:], in_=vsl, identity=C.ident_bf[:]),
                             reads=[bv, C.b_const], writes=[bpv])
                        vb, bvb = W["vb"].next()
                        S.op("dve", lambda v: v.tensor_copy(out=vb[:], in_=pv[:]), reads=[bpv], writes=[bvb])
                    if side == 1:
                        vprev = (vb, bvb)
                    pss, bps = W["pss"].next()
                    S.op("pe", lambda p: p.matmul(pss[:], lhsT=ksl, rhs=qsl, start=True, stop=True),
                         reads=[bk, bq], writes=[bps])
                    pt, bpt = W["pt"].next()
                    S.op("act", lambda a: a.activation(out=pt[:], in_=pss[:], func=AF.Exp, scale=scale),
                         reads=[bps], writes=[bpt])
                    mi = side
                    if j == 0 and side == 0:
                        mi = 2
                    if j == nb - 1 and side == 1:
                        mi = 3
                    S.op("pool", lambda g: g.tensor_tensor(out=pt[:], in0=pt[:], in1=amask[:, mi, :], op=ALU.mult),
                         reads=[bpt, W["bconst"]], writes=[bpt])
                    S.op("pe", lambda p: p.matmul(po[:], lhsT=vb[:], rhs=pt[:], start=(side == 0), stop=(side == 1)),
                         reads=[bvb, bpt], writes=[bpo])
                    S.op("pe", lambda p: p.matmul(pd[:], lhsT=C.ones_bf[:], rhs=pt[:], start=(side == 0), stop=(side == 1)),
                         reads=[C.b_const, bpt], writes=[bpd])
                q_lo = r + d * 128 * j
                nsl = num[:, q_lo:q_lo + d * 127 + 1:d]
                dsl = den[:, q_lo:q_lo + d * 127 + 1:d]
                if first:
                    S.op("act", lambda a: a.copy(out=nsl, in_=po[:]), reads=[bpo], writes=[bnum])
                    S.op("dve", lambda v: v.tensor_copy(out=dsl, in_=pd[:]), reads=[bpd], writes=[bden])
                else:
                    S.op("dve", lambda v: v.tensor_tensor(out=nsl, in0=po[:], in1=nsl, op=ALU.add),
                         reads=[bpo, bnum], writes=[bnum])
                    S.op("dve", lambda v: v.tensor_tensor(out=dsl, in0=pd[:], in1=dsl, op=ALU.add),
                         reads=[bpd, bden], writes=[bden])
        first = False
    ot, bo = W["ao"]
    for c0 in range(0, T, 1024):
        S.op("dve", lambda v: v.reciprocal(out=den[:, c0:c0 + 1024], in_=den[:, c0:c0 + 1024]), reads=[bden], writes=[bden])
        S.op("dve", lambda v: v.tensor_tensor(out=ot[:, c0:c0 + 1024], in0=num[:, c0:c0 + 1024],
                                              in1=den[:, c0:c0 + 1024], op=ALU.mult),
             reads=[bnum, bden], writes=[bo])
    return S.dma("sp", lambda q: q.dma_start(out=out_ap, in_=ot[:]), bo, reads=[bo])


def mixer_a_setup(C, ph, posb_ap, T):
    S = C.S
    W = {}
    bconst = S.buf("a_const")
    W["bconst"] = bconst
    W["pmat"] = load_const(C, ph, "pmat_a", [128, 128], BF16, bconst)
    W["amask"] = load_const(C, ph, "amask", [128, 4, 128], BF16, bconst)
    freq = load_const(C, ph, "freq_a", [128, 1], F32, bconst)
    W["cos"] = ph.sb([32, T], F32, "cos")
    W["sin"] = ph.sb([32, T], F32, "sin")
    W["btab"] = S.buf("a_tab")
    tp = Phase(C, ph.name + "_trig")
    make_trig_tables(C, tp, posb_ap, freq, bconst, 32, T, W["cos"], W["sin"], W["btab"])
    tp.close()
    W["q"] = (ph.sb([128, T], BF16, "q"), S.buf("a_q"))
    W["kp"] = (ph.sb([128, T + 2 * PADA], BF16, "kp"), S.buf("a_kp"))
    W["vp"] = (ph.sb([128, T + 2 * PADA], BF16, "vp"), S.buf("a_vp"))
    for nm in ("kp", "vp"):
        t, b = W[nm]
        S.op("pool", lambda g: g.memset(t[:, 0:PADA], 0.0), writes=[b])
        S.op("pool", lambda g: g.memset(t[:, PADA + T:], 0.0), writes=[b])
    W["num"] = (ph.sb([128, T], F32, "num"), S.buf("a_num"))
    W["den"] = (ph.sb([128, T], F32, "den"), S.buf("a_den"))
    W["ao"] = (ph.sb([128, T], BF16, "ao"), S.buf("a_ao"))
    W["rp"] = ph.pool("rp", 1, [128, 512], F32, psum=True)
    W["rt"] = ph.pool("rt", 4, [128, 512], F32)
    W["po"] = ph.pool("po", 2, [128, 128], F32, psum=True)
    W["pd"] = ph.pool("pd", 2, [128, 128], F32, psum=True)
    W["pv"] = ph.pool("pv", 1, [128, 128], BF16, psum=True)
    W["pss"] = ph.pool("pss", 2, [128, 128], F32, psum=True)
    W["vb"] = ph.pool("vb", 4, [128, 128], BF16)
    W["pt"] = ph.pool("pt", 3, [128, 128], BF16)
    return W


def _retention_consts():
    c = {}
    m = np.arange(128, dtype=np.float32)[:, None]
    cc = np.arange(128, dtype=np.float32)[None, :]
    diff = cc - m
    c["ret_dpos"] = np.maximum(diff, 0.0)
    c["ret_dneg"] = np.maximum(-diff, 0.0)
    c["ret_mge"] = (diff >= 0).astype(np.float32)
    c["ret_mlt"] = (diff < 0).astype(np.float32)
    c["ret_cp1"] = np.broadcast_to(cc + 1.0, (128, 128)).copy()
    c["ret_128mc"] = np.broadcast_to(128.0 - cc, (128, 128)).copy()
    col = np.zeros((128, 4), np.float32)
    col[:, 0] = 127.0 - m[:, 0]
    col[:, 1] = m[:, 0]
    col[:, 2] = 128.0
    c["ret_cols"] = col
    c["ones_f"] = np.ones((128, 128), np.float32)
    return c


def mixer_b_setup(C, ph, posb_ap, T):
    S = C.S
    W = {}
    bconst = S.buf("b_const")
    W["bconst"] = bconst
    W["pmat"] = load_const(C, ph, "pmat_b", [128, 128], BF16, bconst)
    for nm in ("ret_dpos", "ret_dneg", "ret_mge", "ret_mlt", "ret_cp1", "ret_128mc", "ones_f"):
        W[nm] = load_const(C, ph, nm, [128, 128], F32, bconst)
    W["ret_cols"] = load_const(C, ph, "ret_cols", [128, 4], F32, bconst)
    freq = load_const(C, ph, "freq_b", [128, 1], F32, bconst)
    W["cos"] = ph.sb([128, T], F32, "cos")
    W["sin"] = ph.sb([128, T], F32, "sin")
    W["btab"] = S.buf("b_tab")
    tp = Phase(C, ph.name + "_trig")
    make_trig_tables(C, tp, posb_ap, freq, bconst, 128, T, W["cos"], W["sin"], W["btab"])
    tp.close()
    return W


def mixer_b_head(C, ph0, ld, decay_t, bdec, hidx, gnw_t, bgn, out_ap, T, W):
    S = C.S
    ph = Phase(C, ph0.name + "_h")
    pp = Phase(C, ph0.name + "_pa")
    nch = T // 128
    sc = 128.0 ** -0.5
    bc = W["bconst"]
    sm = ph.sb([128, 16], F32, "sm")
    dm = ph.sb([128, 128], F32, "dm")
    tmp = ph.sb([128, 128], F32, "tmp")
    qdf = ph.sb([128, 128], F32, "qdf")
    qdb = ph.sb([128, 128], F32, "qdb")
    rT, brT = ph.sb([128, 2, T], F32, "rT"), S.buf("b_rT")
    qt, bq = pp.sb([128, T], BF16, "q"), S.buf("b_q")
    kt, bk = pp.sb([128, T], BF16, "k"), S.buf("b_k")
    vt, bv = pp.sb([128, 2, T], BF16, "v"), S.buf("b_v")
    ld("q", qt[:, :], bq)
    ld("k", kt[:, :], bk)
    ld("v0", vt[:, 0, :], bv)
    ld("v1", vt[:, 1, :], bv)
    rpools = {"rp": pp.pool("rp", 1, [128, 512], F32, psum=True), "rt": pp.pool("rt", 4, [128, 512], F32)}
    rope_inplace(C, ph, qt, bq, 128, T, W["pmat"], bc, W["cos"], W["sin"], W["btab"], rpools)
    rope_inplace(C, ph, kt, bk, 128, T, W["pmat"], bc, W["cos"], W["sin"], W["btab"], rpools)
    bsm = S.buf("b_sm")
    nh2 = decay_t.shape[1] // 2
    for dr in (0, 1):
        col = dr * nh2 + hidx
        S.op("act", lambda a: a.activation(out=sm[:, dr:dr + 1], in_=decay_t[:, col:col + 1], func=AF.Exp, scale=-1.0),
             reads=[bdec], writes=[bsm])
        S.op("dve", lambda v: v.tensor_scalar(out=sm[:, dr:dr + 1], in0=sm[:, dr:dr + 1], scalar1=1.0, scalar2=None,
                                              op0=ALU.add), reads=[bsm], writes=[bsm])
        S.op("act", lambda a: a.activation(out=sm[:, dr:dr + 1], in_=sm[:, dr:dr + 1], func=AF.Ln), reads=[bsm], writes=[bsm])
        S.op("dve", lambda v: v.tensor_scalar(out=sm[:, dr:dr + 1], in0=sm[:, dr:dr + 1], scalar1=-1.0, scalar2=None,
                                              op0=ALU.mult), reads=[bsm], writes=[bsm])
    lgf, lgb = sm[:, 0:1], sm[:, 1:2]
    cols = W["ret_cols"]
    S.op("act", lambda a: a.activation(out=sm[:, 2:3], in_=cols[:, 0:1], func=AF.Exp, scale=lgf), reads=[bsm, bc], writes=[bsm])
    S.op("act", lambda a: a.activation(out=sm[:, 3:4], in_=cols[:, 1:2], func=AF.Exp, scale=lgb), reads=[bsm, bc], writes=[bsm])
    S.op("act", lambda a: a.activation(out=sm[:, 4:5], in_=cols[:, 2:3], func=AF.Exp, scale=lgf), reads=[bsm, bc], writes=[bsm])
    S.op("act", lambda a: a.activation(out=sm[:, 5:6], in_=cols[:, 2:3], func=AF.Exp, scale=lgb), reads=[bsm, bc], writes=[bsm])
    bdm = S.buf("b_dm")
    S.op("act", lambda a: a.activation(out=dm[:], in_=W["ret_dpos"][:], func=AF.Exp, scale=lgf), reads=[bsm, bc], writes=[bdm])
    S.op("dve", lambda v: v.scalar_tensor_tensor(out=dm[:], in0=dm[:], scalar=sc, in1=W["ret_mge"][:], op0=ALU.mult, op1=ALU.mult),
         reads=[bdm, bc], writes=[bdm])
    S.op("act", lambda a: a.activation(out=tmp[:], in_=W["ret_dneg"][:], func=AF.Exp, scale=lgb), reads=[bsm, bc], writes=[bdm])
    S.op("dve", lambda v: v.scalar_tensor_tensor(out=tmp[:], in0=tmp[:], scalar=sc, in1=W["ret_mlt"][:], op0=ALU.mult, op1=ALU.mult),
         reads=[bdm, bc], writes=[bdm])
    S.op("dve", lambda v: v.tensor_tensor(out=dm[:], in0=dm[:], in1=tmp[:], op=ALU.add), reads=[bdm], writes=[bdm])
    S.op("act", lambda a: a.activation(out=qdf[:], in_=W["ret_cp1"][:], func=AF.Exp, scale=lgf), reads=[bsm, bc], writes=[bdm])
    S.op("act", lambda a: a.activation(out=qdb[:], in_=W["ret_128mc"][:], func=AF.Exp, scale=lgb), reads=[bsm, bc], writes=[bdm])
    S.op("dve", lambda v: v.tensor_scalar(out=qdf[:], in0=qdf[:], scalar1=sc, scalar2=None, op0=ALU.mult), reads=[bdm], writes=[bdm])
    S.op("dve", lambda v: v.tensor_scalar(out=qdb[:], in0=qdb[:], scalar1=sc, scalar2=None, op0=ALU.mult), reads=[bdm], writes=[bdm])
    qf, bqf = pp.sb([128, T], BF16, "qf"), S.buf("b_qf")
    qb, bqb = pp.sb([128, T], BF16, "qb"), S.buf("b_qb")
    for n in range(nch):
        sl = slice(n * 128, (n + 1) * 128)
        S.op("dve", lambda v: v.tensor_tensor(out=qf[:, sl], in0=qt[:, sl], in1=qdf[:], op=ALU.mult), reads=[bq, bdm], writes=[bqf])
        S.op("pool", lambda g: g.tensor_tensor(out=qb[:, sl], in0=qt[:, sl], in1=qdb[:], op=ALU.mult), reads=[bq, bdm], writes=[bqb])
    kf, bkf = pp.sb([128, nch, 128], BF16, "kf"), S.buf("b_kf")
    kb, bkb = pp.sb([128, nch, 128], BF16, "kb"), S.buf("b_kb")
    vtm, bvtm = pp.sb([128, nch, 256], BF16, "vtm"), S.buf("b_vtm")
    ptr = pp.pool("ptr", 1, [128, 3, 128], BF16, psum=True)
    for n in range(nch):
        sl = slice(n * 128, (n + 1) * 128)
        pt, bpt = ptr.next()
        S.op("pe", lambda p: p.transpose(out=pt[:, 0, :], in_=kt[:, sl], identity=C.ident_bf[:]), reads=[bk, C.b_const], writes=[bpt])
        S.op("pe", lambda p: p.transpose(out=pt[:, 1, :], in_=vt[:, 0, sl], identity=C.ident_bf[:]), reads=[bv, C.b_const], writes=[bpt])
        S.op("pe", lambda p: p.transpose(out=pt[:, 2, :], in_=vt[:, 1, sl], identity=C.ident_bf[:]), reads=[bv, C.b_const], writes=[bpt])
        S.op("act", lambda a: a.activation(out=kf[:, n, :], in_=pt[:, 0, :], func=AF.Copy, scale=sm[:, 2:3]), reads=[bpt, bsm], writes=[bkf])
        S.op("act", lambda a: a.activation(out=kb[:, n, :], in_=pt[:, 0, :], func=AF.Copy, scale=sm[:, 3:4]), reads=[bpt, bsm], writes=[bkb])
        S.op("dve", lambda v: v.tensor_copy(out=vtm[:, n, :], in_=pt[:, 1:3, :]), reads=[bpt], writes=[bvtm])
    sf, bsf = pp.sb([128, nch, 256], BF16, "sf"), S.buf("b_sf")
    sbk, bsb = pp.sb([128, nch, 256], BF16, "sbk"), S.buf("b_sb")
    st, bst = pp.sb([128, 256], F32, "st"), S.buf("b_st")
    pkv = pp.pool("pkv", 1, [128, 256], F32, psum=True)
    S.op("dve", lambda v: v.memset(st[:], 0.0), writes=[bst])
    S.op("pool", lambda g: g.memset(sf[:, 0, :], 0.0), writes=[bsf])
    for n in range(1, nch):
        pk, bpk = pkv.next()
        S.op("pe", lambda p: p.matmul(pk[:], lhsT=kf[:, n - 1, :], rhs=vtm[:, n - 1, :], start=True, stop=True),
             reads=[bkf, bvtm], writes=[bpk])
        S.op("dve", lambda v: v.scalar_tensor_tensor(out=st[:], in0=st[:], scalar=sm[:, 4:5], in1=pk[:], op0=ALU.mult, op1=ALU.add),
             reads=[bst, bsm, bpk], writes=[bst])
        S.op("act", lambda a: a.copy(out=sf[:, n, :], in_=st[:]), reads=[bst], writes=[bsf])
    S.op("dve", lambda v: v.memset(st[:], 0.0), reads=[bst], writes=[bst])
    S.op("pool", lambda g: g.memset(sbk[:, nch - 1, :], 0.0), writes=[bsb])
    for n in range(nch - 2, -1, -1):
        pk, bpk = pkv.next()
        S.op("pe", lambda p: p.matmul(pk[:], lhsT=kb[:, n + 1, :], rhs=vtm[:, n + 1, :], start=True, stop=True),
             reads=[bkb, bvtm], writes=[bpk])
        S.op("dve", lambda v: v.scalar_tensor_tensor(out=st[:], in0=st[:], scalar=sm[:, 5:6], in1=pk[:], op0=ALU.mult, op1=ALU.add),
             reads=[bst, bsm, bpk], writes=[bst])
        S.op("act", lambda a: a.copy(out=sbk[:, n, :], in_=st[:]), reads=[bst], writes=[bsb])
    pss = pp.pool("pss", 2, [128, 128], F32, psum=True)
    pout = pp.pool("pout", 2, [128, 128], F32, psum=True)
    pmt = pp.pool("pmt", 3, [128, 128], BF16)
    for n in range(nch):
        sl = slice(n * 128, (n + 1) * 128)
        ps_, bps = pss.next()
        S.op("pe", lambda p: p.matmul(ps_[:], lhsT=kt[:, sl], rhs=qt[:, sl], start=True, stop=True), reads=[bk, bq], writes=[bps])
        pm, bpm = pmt.next()
        S.op("dve", lambda v: v.tensor_tensor(out=pm[:], in0=ps_[:], in1=dm[:], op=ALU.mult), reads=[bps, bdm], writes=[bpm])
        for hf in (0, 1):
            po, bpo = pout.next()
            cs = slice(hf * 128, (hf + 1) * 128)
            S.op("pe", lambda p: p.matmul(po[:], lhsT=vtm[:, n, cs], rhs=pm[:], start=True, stop=False), reads=[bvtm, bpm], writes=[bpo])
            S.op("pe", lambda p: p.matmul(po[:], lhsT=sf[:, n, cs], rhs=qf[:, sl], start=False, stop=False), reads=[bsf, bqf], writes=[bpo])
            S.op("pe", lambda p: p.matmul(po[:], lhsT=sbk[:, n, cs], rhs=qb[:, sl], start=False, stop=True), reads=[bsb, bqb], writes=[bpo])
            if hf == 0:
                S.op("act", lambda a: a.copy(out=rT[:, hf, sl], in_=po[:]), reads=[bpo], writes=[brT])
            else:
                S.op("dve", lambda v: v.tensor_copy(out=rT[:, hf, sl], in_=po[:]), reads=[bpo], writes=[brT])
    pp.close()
    gt, bg = ph.sb([128, 2, T], BF16, "g"), S.buf("b_g")
    ld("g0", gt[:, 0, :], bg)
    ld("g1", gt[:, 1, :], bg)
    sq, bsq = ph.sb([128, 2, 512], F32, "sq"), S.buf("b_sq")
    pst = ph.pool("pst", 2, [128, 2, 512], F32, psum=True)
    mv, bmv = ph.sb([128, 4, 512], F32, "mv"), S.buf("b_mv")
    ot, bo = ph.sb([128, 2, T], BF16, "ot"), S.buf("b_ot")
    sg, bsg = ph.sb([128, 2, 512], F32, "sg"), S.buf("b_sg")
    onesf = W["ones_f"]
    for c0 in range(0, T, 512):
        cs = slice(c0, c0 + 512)
        S.op("act", lambda a: a.activation(out=sq[:], in_=rT[:, :, cs], func=AF.Square), reads=[brT], writes=[bsq])
        p2, bp2 = pst.next()
        for hf in (0, 1):
            S.op("pe", lambda p: p.matmul(p2[:, 0, :], lhsT=onesf[:], rhs=rT[:, hf, cs], start=(hf == 0), stop=(hf == 1)),
                 reads=[bc, brT], writes=[bp2])
        for hf in (0, 1):
            S.op("pe", lambda p: p.matmul(p2[:, 1, :], lhsT=onesf[:], rhs=sq[:, hf, :], start=(hf == 0), stop=(hf == 1)),
                 reads=[bc, bsq], writes=[bp2])
        S.op("dve", lambda v: v.tensor_scalar(out=mv[:, 0, :], in0=p2[:, 0, :], scalar1=1.0 / 256, scalar2=None, op0=ALU.mult),
             reads=[bp2], writes=[bmv])
        S.op("dve", lambda v: v.tensor_tensor(out=mv[:, 1, :], in0=mv[:, 0, :], in1=mv[:, 0, :], op=ALU.mult), reads=[bmv], writes=[bmv])
        S.op("dve", lambda v: v.scalar_tensor_tensor(out=mv[:, 1, :], in0=p2[:, 1, :], scalar=1.0 / 256, in1=mv[:, 1, :],
                                                     op0=ALU.mult, op1=ALU.subtract), reads=[bp2, bmv], writes=[bmv])
        S.op("act", lambda a: a.activation(out=mv[:, 1, :], in_=mv[:, 1, :], func=AF.Sqrt, bias=C.eps_t[:], scale=1.0),
             reads=[bmv, C.b_const], writes=[bmv])
        S.op("dve", lambda v: v.reciprocal(out=mv[:, 1, :], in_=mv[:, 1, :]), reads=[bmv], writes=[bmv])
        S.op("act", lambda a: a.activation(out=sg[:], in_=gt[:, :, cs], func=AF.Silu), reads=[bg], writes=[bsg])
        for hf in (0, 1):
            S.op("dve", lambda v: v.tensor_tensor(out=mv[:, 2, :], in0=rT[:, hf, cs], in1=mv[:, 0, :], op=ALU.subtract),
                 reads=[brT, bmv], writes=[bmv])
            S.op("dve", lambda v: v.scalar_tensor_tensor(out=mv[:, 2, :], in0=mv[:, 2, :], scalar=gnw_t[:, hf:hf + 1], in1=mv[:, 1, :],
                                                         op0=ALU.mult, op1=ALU.mult), reads=[bmv, bgn], writes=[bmv])
            S.op("dve", lambda v: v.tensor_tensor(out=ot[:, hf, cs], in0=mv[:, 2, :], in1=sg[:, hf, :], op=ALU.mult),
                 reads=[bmv, bsg], writes=[bo])
    ev = S.dma("sp", lambda q: q.dma_start(out=out_ap.rearrange("(h p) t -> p h t", p=128), in_=ot[:]), bo, reads=[bo])
    ph.close()
    return ev


def _s5_consts(T):
    return {"tidx": np.broadcast_to(np.arange(T, dtype=np.int32), (128, T)).copy()}


def sincos_col(C, ph, ang, bang, s_out, c_out, bout, tmp, btmp):
    S = C.S
    a2, qi, qf, rr = tmp["f"][:, 0:1], tmp["i"][:, 0:1], tmp["f"][:, 1:2], tmp["f"][:, 2:3]
    m = tmp["f"][:, 3:4]
    S.op("dve", lambda v: v.tensor_scalar(out=a2, in0=ang, scalar1=8 * math.pi, scalar2=None, op0=ALU.add), reads=[bang], writes=[btmp])
    S.op("dve", lambda v: v.tensor_scalar(out=qi, in0=a2, scalar1=1.0 / TWO_PI, scalar2=None, op0=ALU.mult), reads=[btmp], writes=[btmp])
    S.op("dve", lambda v: v.tensor_copy(out=qf, in_=qi), reads=[btmp], writes=[btmp])
    S.op("dve", lambda v: v.scalar_tensor_tensor(out=rr, in0=qf, scalar=-CW1, in1=a2, op0=ALU.mult, op1=ALU.add), reads=[btmp], writes=[btmp])
    S.op("dve", lambda v: v.scalar_tensor_tensor(out=rr, in0=qf, scalar=-CW2, in1=rr, op0=ALU.mult, op1=ALU.add), reads=[btmp], writes=[btmp])
    S.op("dve", lambda v: v.tensor_scalar(out=m, in0=rr, scalar1=math.pi, scalar2=None, op0=ALU.is_gt), reads=[btmp], writes=[btmp])
    S.op("dve", lambda v: v.scalar_tensor_tensor(out=a2, in0=m, scalar=-TWO_PI, in1=rr, op0=ALU.mult, op1=ALU.add), reads=[btmp], writes=[btmp])
    S.op("act", lambda a: a.activation(out=s_out, in_=a2, func=AF.Sin), reads=[btmp], writes=[bout])
    S.op("dve", lambda v: v.tensor_scalar(out=m, in0=rr, scalar1=math.pi / 2, scalar2=None, op0=ALU.is_gt), reads=[btmp], writes=[btmp])
    S.op("dve", lambda v: v.scalar_tensor_tensor(out=a2, in0=m, scalar=-TWO_PI, in1=rr, op0=ALU.mult, op1=ALU.add), reads=[btmp, bout], writes=[btmp])
    S.op("act", lambda a: a.activation(out=c_out, in_=a2, func=AF.Sin, bias=C.halfpi_t[:, :]), reads=[btmp, C.b_const], writes=[bout])


def mixer_c_pair(C, ph0, ld_u, prm, dcol_ap, tidx_ap, out_ap, T):
    S = C.S
    ph = Phase(C, ph0.name + "_c")
    ut, bu = ph.sb([32, T], BF16, "u"), S.buf("c_u")
    ld_u(ut, bu)
    Y, bY = ph.sb([32, T], F32, "Y"), S.buf("c_Y")
    cos_t = ph.sb([128, T], F32, "cos")
    sin_t = ph.sb([128, T], F32, "sin")
    btab = S.buf("c_tab")
    pr = ph.sb([128, 24], F32, "pr")
    bpr = S.buf("c_pr")
    tmpd = {"f": ph.sb([128, 4], F32, "tf"), "i": ph.sb([128, 1], I32, "ti")}
    btmp = S.buf("c_tmp")
    bmat = ph.sb([128, 4, 16], F32, "bmat")
    bd = ph.sb([128, 4, 32], F32, "bd")
    bbd = S.buf("c_bd")
    lhs_b = ph.sb([32, 2, 128], BF16, "lhsb")
    lhs_c = ph.sb([128, 2, 32], BF16, "lhsc")
    blhs = S.buf("c_lhs")
    RB = ph.sb([128, 512], F32, "RB")
    bRB = S.buf("c_RB")
    carry = ph.sb([128, 2], F32, "carry")
    bcar = S.buf("c_car")
    dcol = ph.sb([32, 1], F32, "dcol")
    bdc = S.buf("c_dcol")
    S.dma("sp", lambda q: q.dma_start(out=dcol[:], in_=dcol_ap), bdc, writes=[bdc])
    wk = ph.pool("wk", 24, [128, 512], F32)
    hb = ph.pool("hb", 6, [128, 512], BF16)
    px = ph.pool("px", 4, [128, 512], F32, psum=True)
    py = ph.pool("py", 2, [32, 512], F32, psum=True)
    ptp = ph.pool("ptp", 1, [32, 128], F32, psum=True)
    for dr in (0, 1):
        P = prm[dr]
        sg = 1.0 if dr == 0 else -1.0
        for i, nm in enumerate(("lam_re", "lam_im", "logdt")):
            S.dma("sp", lambda q, i=i, nm=nm: q.dma_start(out=pr[:, i:i + 1], in_=P[nm]), bpr, writes=[bpr])
        for i, nm in enumerate(("b_re", "b_im", "c_reT", "c_imT")):
            S.dma("sp", lambda q, i=i, nm=nm: q.dma_start(out=bmat[:, i, :], in_=P[nm]), bbd, writes=[bbd])
        c_ = lambda i: pr[:, i:i + 1]
        S.op("act", lambda a: a.activation(out=c_(2), in_=c_(2), func=AF.Exp), reads=[bpr], writes=[bpr])
        S.op("dve", lambda v: v.tensor_tensor(out=c_(3), in0=c_(0), in1=c_(2), op=ALU.mult), reads=[bpr], writes=[bpr])
        S.op("act", lambda a: a.activation(out=c_(3), in_=c_(3), func=AF.Exp), reads=[bpr], writes=[bpr])
        S.op("dve", lambda v: v.tensor_tensor(out=c_(4), in0=c_(1), in1=c_(2), op=ALU.mult), reads=[bpr], writes=[bpr])
        sincos_col(C, ph, c_(4), bpr, c_(5), c_(6), bpr, tmpd, btmp)
        S.op("dve", lambda v: v.tensor_tensor(out=c_(7), in0=c_(3), in1=c_(6), op=ALU.mult), reads=[bpr], writes=[bpr])
        S.op("dve", lambda v: v.tensor_tensor(out=c_(8), in0=c_(3), in1=c_(5), op=ALU.mult), reads=[bpr], writes=[bpr])
        S.op("dve", lambda v: v.tensor_tensor(out=c_(9), in0=c_(0), in1=c_(0), op=ALU.mult), reads=[bpr], writes=[bpr])
        S.op("dve", lambda v: v.scalar_tensor_tensor(out=c_(9), in0=c_(1), scalar=c_(1), in1=c_(9), op0=ALU.mult, op1=ALU.add), reads=[bpr], writes=[bpr])
        S.op("dve", lambda v: v.reciprocal(out=c_(9), in_=c_(9)), reads=[bpr], writes=[bpr])
        S.op("dve", lambda v: v.tensor_scalar(out=c_(10), in0=c_(7), scalar1=-1.0, scalar2=None, op0=ALU.add), reads=[bpr], writes=[bpr])
        S.op("dve", lambda v: v.tensor_tensor(out=c_(13), in0=c_(10), in1=c_(0), op=ALU.mult), reads=[bpr], writes=[bpr])
        S.op("dve", lambda v: v.scalar_tensor_tensor(out=c_(13), in0=c_(8), scalar=c_(1), in1=c_(13), op0=ALU.mult, op1=ALU.add), reads=[bpr], writes=[bpr])
        S.op("dve", lambda v: v.tensor_tensor(out=c_(11), in0=c_(13), in1=c_(9), op=ALU.mult), reads=[bpr], writes=[bpr])
        S.op("dve", lambda v: v.tensor_tensor(out=c_(13), in0=c_(8), in1=c_(0), op=ALU.mult), reads=[bpr], writes=[bpr])
        S.op("dve", lambda v: v.tensor_tensor(out=c_(14), in0=c_(10), in1=c_(1), op=ALU.mult), reads=[bpr], writes=[bpr])
        S.op("dve", lambda v: v.tensor_tensor(out=c_(13), in0=c_(13), in1=c_(14), op=ALU.subtract), reads=[bpr], writes=[bpr])
        S.op("dve", lambda v: v.tensor_tensor(out=c_(12), in0=c_(13), in1=c_(9), op=ALU.mult), reads=[bpr], writes=[bpr])
        S.op("dve", lambda v: v.tensor_scalar(out=c_(15), in0=c_(12), scalar1=-1.0, scalar2=None, op0=ALU.mult), reads=[bpr], writes=[bpr])
        S.op("pool", lambda g: g.memset(bd[:], 0.0), reads=[bbd], writes=[bbd])
        for gi in (0, 1):
            rs = slice(gi * 64, (gi + 1) * 64)
            cs = slice(gi * 16, (gi + 1) * 16)
            S.op("dve", lambda v: v.tensor_scalar(out=bd[rs, 0, cs], in0=bmat[rs, 0, :], scalar1=pr[rs, 11:12], scalar2=None, op0=ALU.mult),
                 reads=[bbd, bpr], writes=[bbd])
            S.op("dve", lambda v: v.scalar_tensor_tensor(out=bd[rs, 0, cs], in0=bmat[rs, 1, :], scalar=pr[rs, 15:16], in1=bd[rs, 0, cs],
                                                         op0=ALU.mult, op1=ALU.add), reads=[bbd, bpr], writes=[bbd])
            S.op("dve", lambda v: v.tensor_scalar(out=bd[rs, 1, cs], in0=bmat[rs, 1, :], scalar1=pr[rs, 11:12], scalar2=None, op0=ALU.mult),
                 reads=[bbd, bpr], writes=[bbd])
            S.op("dve", lambda v: v.scalar_tensor_tensor(out=bd[rs, 1, cs], in0=bmat[rs, 0, :], scalar=pr[rs, 12:13], in1=bd[rs, 1, cs],
                                                         op0=ALU.mult, op1=ALU.add), reads=[bbd, bpr], writes=[bbd])
            S.op("dve", lambda v: v.tensor_copy(out=bd[rs, 2, cs], in_=bmat[rs, 2, :]), reads=[bbd], writes=[bbd])
            S.op("dve", lambda v: v.tensor_scalar(out=bd[rs, 3, cs], in0=bmat[rs, 3, :], scalar1=-1.0, scalar2=None, op0=ALU.mult),
                 reads=[bbd], writes=[bbd])
        for i in (0, 1):
            pt, bpt = ptp.next()
            S.op("pe", lambda p: p.transpose(out=pt[:], in_=bd[:, i, :], identity=C.ident_f[:]), reads=[bbd, C.b_const], writes=[bpt])
            S.op("dve", lambda v: v.tensor_copy(out=lhs_b[:, i, :], in_=pt[:]), reads=[bpt], writes=[blhs])
        S.op("dve", lambda v: v.tensor_copy(out=lhs_c[:, :, :], in_=bd[:, 2:4, :]), reads=[bbd], writes=[blhs])
        S.op("dve", lambda v: v.memset(RB[:], 1.0), reads=[bRB], writes=[bRB])
        S.op("dve", lambda v: v.tensor_scalar(out=RB[:], in0=RB[:], scalar1=pr[:, 3:4], scalar2=None, op0=ALU.mult), reads=[bRB, bpr], writes=[bRB])
        tp = Phase(C, ph.name + "_trig%d" % dr)
        make_trig_tables(C, tp, tidx_ap, pr[:, 4:5], bpr, 128, T, cos_t, sin_t, btab, offset=8 * math.pi)
        tp.close()
        S.op("dve", lambda v: v.memset(carry[:], 0.0), reads=[bcar], writes=[bcar])
        ntile = T // 512
        order = range(ntile) if dr == 0 else range(ntile - 1, -1, -1)
        for ti in order:
            cs = slice(ti * 512, (ti + 1) * 512)
            pxr, bpxr = px.next()
            pxi, bpxi = px.next()
            S.op("pe", lambda p: p.matmul(pxr[:], lhsT=lhs_b[:, 0, :], rhs=ut[:, cs], start=True, stop=True), reads=[blhs, bu], writes=[bpxr])
            S.op("pe", lambda p: p.matmul(pxi[:], lhsT=lhs_b[:, 1, :], rhs=ut[:, cs], start=True, stop=True), reads=[blhs, bu], writes=[bpxi])
            (a1, ba1), (a2, ba2), (a3, ba3), (a4, ba4) = wk.next(), wk.next(), wk.next(), wk.next()
            S.op("dve", lambda v: v.tensor_tensor(out=a1[:], in0=pxr[:], in1=cos_t[:, cs], op=ALU.mult), reads=[bpxr, btab], writes=[ba1])
            S.op("dve", lambda v: v.tensor_tensor(out=a2[:], in0=pxi[:], in1=sin_t[:, cs], op=ALU.mult), reads=[bpxi, btab], writes=[ba2])
            S.op("dve", lambda v: v.tensor_tensor(out=a3[:], in0=pxi[:], in1=cos_t[:, cs], op=ALU.mult), reads=[bpxi, btab], writes=[ba3])
            S.op("dve", lambda v: v.tensor_tensor(out=a4[:], in0=pxr[:], in1=sin_t[:, cs], op=ALU.mult), reads=[bpxr, btab], writes=[ba4])
            opr = ALU.add if dr == 0 else ALU.subtract
            opi = ALU.subtract if dr == 0 else ALU.add
            S.op("pool", lambda g: g.tensor_tensor(out=a1[:], in0=a1[:], in1=a2[:], op=opr), reads=[ba1, ba2], writes=[ba1])
            S.op("pool", lambda g: g.tensor_tensor(out=a3[:], in0=a3[:], in1=a4[:], op=opi), reads=[ba3, ba4], writes=[ba3])
            (hr_, bhr), (hi_, bhi) = wk.next(), wk.next()
            rev = (lambda t: t[:, ::-1]) if dr == 1 else (lambda t: t[:, :])
            last = 0 if dr == 1 else 511
            S.op("dve", lambda v: v.tensor_tensor_scan(out=rev(hr_), data0=RB[:], data1=rev(a1), initial=carry[:, 0:1], op0=ALU.mult, op1=ALU.add),
                 reads=[bRB, ba1, bcar], writes=[bhr])
            S.op("dve", lambda v: v.tensor_tensor_scan(out=rev(hi_), data0=RB[:], data1=rev(a3), initial=carry[:, 1:2], op0=ALU.mult, op1=ALU.add),
                 reads=[bRB, ba3, bcar], writes=[bhi])
            S.op("dve", lambda v: v.tensor_copy(out=carry[:, 0:1], in_=hr_[:, last:last + 1]), reads=[bhr], writes=[bcar])
            S.op("dve", lambda v: v.tensor_copy(out=carry[:, 1:2], in_=hi_[:, last:last + 1]), reads=[bhi], writes=[bcar])
            (b1, bb1), (b2, bb2), (b3, bb3), (b4, bb4) = wk.next(), wk.next(), wk.next(), wk.next()
            S.op("pool", lambda g: g.tensor_tensor(out=b1[:], in0=hr_[:], in1=cos_t[:, cs], op=ALU.mult), reads=[bhr, btab], writes=[bb1])
            S.op("pool", lambda g: g.tensor_tensor(out=b2[:], in0=hi_[:], in1=sin_t[:, cs], op=ALU.mult), reads=[bhi, btab], writes=[bb2])
            S.op("pool", lambda g: g.tensor_tensor(out=b3[:], in0=hi_[:], in1=cos_t[:, cs], op=ALU.mult), reads=[bhi, btab], writes=[bb3])
            S.op("pool", lambda g: g.tensor_tensor(out=b4[:], in0=hr_[:], in1=sin_t[:, cs], op=ALU.mult), reads=[bhr, btab], writes=[bb4])
            (h1, bh1), (h2, bh2) = hb.next(), hb.next()
            S.op("dve", lambda v: v.scalar_tensor_tensor(out=h1[:], in0=b2[:], scalar=-sg, in1=b1[:], op0=ALU.mult, op1=ALU.add),
                 reads=[bb1, bb2], writes=[bh1])
            S.op("dve", lambda v: v.scalar_tensor_tensor(out=h2[:], in0=b4[:], scalar=sg, in1=b3[:], op0=ALU.mult, op1=ALU.add),
                 reads=[bb3, bb4], writes=[bh2])
            pyt, bpy = py.next()
            S.op("pe", lambda p: p.matmul(pyt[:], lhsT=lhs_c[:, 0, :], rhs=h1[:], start=True, stop=False), reads=[blhs, bh1], writes=[bpy])
            S.op("pe", lambda p: p.matmul(pyt[:], lhsT=lhs_c[:, 1, :], rhs=h2[:], start=False, stop=True), reads=[blhs, bh2], writes=[bpy])
            if dr == 0:
                S.op("act", lambda a: a.copy(out=Y[:, cs], in_=pyt[:]), reads=[bpy], writes=[bY])
            else:
                S.op("dve", lambda v: v.tensor_tensor(out=Y[:, cs], in0=pyt[:], in1=Y[:, cs], op=ALU.add), reads=[bpy, bY], writes=[bY])
    yo, byo = ph.sb([32, T], BF16, "yo"), S.buf("c_yo")
    for c0 in range(0, T, 2048):
        cs = slice(c0, c0 + 2048)
        S.op("dve", lambda v: v.scalar_tensor_tensor(out=Y[:, cs], in0=ut[:, cs], scalar=dcol[:, 0:1], in1=Y[:, cs], op0=ALU.mult, op1=ALU.add),
             reads=[bu, bdc, bY], writes=[bY])
        S.op("act", lambda a: a.activation(out=yo[:, cs], in_=Y[:, cs], func=AF.Gelu), reads=[bY], writes=[byo])
    ev = S.dma("sp", lambda q: q.dma_start(out=out_ap, in_=yo[:]), byo, reads=[byo])
    ph.close()
    return ev


def load_w(C, wt_ap, bw, src_ap):
    if src_ap.dtype == BF16:
        C.S.dma("sp", lambda q: q.dma_start(out=wt_ap, in_=src_ap), bw, writes=[bw])
    else:
        C.S.dma("pool", lambda q: q.dma_start(out=wt_ap, in_=src_ap), bw, writes=[bw])


class WMat:
    def __init__(self, ap3, col0=0):
        self.ap3 = ap3
        self.cw = ap3.shape[2]
        self.col0 = col0

    def blk(self, c0, wn):
        c0 += self.col0
        bi, off = c0 // self.cw, c0 % self.cw
        return self.ap3[bi].rearrange("(kc p) c -> p kc c", p=128)[:, :, off:off + wn]


def wblk(w_ap, c0, wn):
    if hasattr(w_ap, "blk"):
        return w_ap.blk(c0, wn)
    return w_ap.rearrange("(kc p) n -> p kc n", p=128)[:, :, c0:c0 + wn]


def gemm_tok(C, act_fn, bact, nk, ntt, w_ap, nn, cb, wpool, pspool, wn=512):
    S = C.S
    tiles = {}

    def issue(n):
        wt, bw = wpool.next()
        load_w(C, wt[:, 0:nk, 0:wn], bw, wblk(w_ap, n * wn, wn))
        tiles[n] = (wt, bw)

    issue(0)
    for n in range(nn):
        if n + 1 < nn:
            issue(n + 1)
        wt, bw = tiles.pop(n)
        for tt in range(ntt):
            ps, bps = pspool.next()
            for k in range(nk):
                S.op("pe", lambda p, k=k: p.matmul(ps[:, 0:wn], lhsT=act_fn(k, tt), rhs=wt[:, k, 0:wn],
                                                   start=(k == 0), stop=(k == nk - 1)),
                     reads=[bw, bact], writes=[bps])
            cb(tt, n, ps, bps)


def gemm_fm2(C, act_fn, bact, nk, ntok, w_ap, nmb, cb, wpool, pspool, wn=512):
    S = C.S
    tiles = {}

    def issue(n):
        wt, bw = wpool.next()
        load_w(C, wt[:, 0:nk, 0:wn], bw, wblk(w_ap, n * wn, wn))
        tiles[n] = (wt, bw)

    issue(0)
    for nb in range(nmb):
        if nb + 1 < nmb:
            issue(nb + 1)
        wt, bw = tiles.pop(nb)
        for mi in range(wn // 128):
            ps, bps = pspool.next()
            for k in range(nk):
                S.op("pe", lambda p, k=k: p.matmul(ps[:, 0:ntok], lhsT=wt[:, k, mi * 128:(mi + 1) * 128], rhs=act_fn(k),
                                                   start=(k == 0), stop=(k == nk - 1)),
                     reads=[bw, bact], writes=[bps])
            cb(nb * (wn // 128) + mi, ps, bps)


def phase_mem_kv(C, mem_ap, nwb_ap, wkv_ap, kmT_d, vm_d, bkm, bvm):
    S = C.S
    ph = Phase(C, "mkv")
    wb = ph.sb([128, D], F32, "wb")
    bwb = S.buf("mkv_wb")
    S.dma("sp", lambda q: q.dma_start(out=wb[:], in_=nwb_ap), bwb, writes=[bwb])
    pools = {"x": ph.pool("x", 2, [128, D], F32), "h": ph.pool("h", 2, [128, D], BF16),
             "pst": ph.pool("pst", 2, [128, 8, 128], BF16, psum=True),
             "small": {"ss": ph.sb([128, 1], F32), "rs": ph.sb([128, 1], F32), "junk": ph.sb([128, D], BF16), "b": S.buf("mkv_small")}}
    mT = ph.sb([128, 32, 256], BF16, "mT")
    bmT = S.buf("mkv_mT")
    norm_block_to_hT(C, mem_ap, 0, 2, wb, bwb, mT, bmT, pools)
    wpool = ph.pool("w", 2, [128, 32, 512], BF16)
    pspool = ph.pool("ps", 3, [128, 512], F32, psum=True)
    opool = ph.pool("o", 3, [128, 512], BF16)

    def cb_k(m, ps, bps):
        ot, bo = opool.next()
        S.op("act", lambda a: a.copy(out=ot[:, 0:256], in_=ps[:, 0:256]), reads=[bps], writes=[bo])
        S.dma("sp", lambda q: q.dma_start(out=kmT_d[m * 128:(m + 1) * 128, :], in_=ot[:, 0:256]), bo, reads=[bo], writes=[bkm])

    gemm_fm2(C, lambda k: mT[:, k, :], bmT, 32, 256, wkv_ap[0] if isinstance(wkv_ap, tuple) else wkv_ap[:, 0:D], 8, cb_k, wpool, pspool)

    def cb_v(tt, n, ps, bps):
        ot, bo = opool.next()
        S.op("dve", lambda v: v.tensor_copy(out=ot[:], in_=ps[:]), reads=[bps], writes=[bo])
        S.dma("sp", lambda q: q.dma_start(out=vm_d[tt * 128:(tt + 1) * 128, n * 512:(n + 1) * 512], in_=ot[:]), bo, reads=[bo], writes=[bvm])

    gemm_tok(C, lambda k, tt: mT[:, k, tt * 128:(tt + 1) * 128], bmT, 32, 2, wkv_ap[1] if isinstance(wkv_ap, tuple) else wkv_ap[:, D:2 * D], 8, cb_v, wpool, pspool)
    ph.close()


TB3 = 512


def phase_k3(C, x_ap, cat_src, glu_ap, wout_ap, nwc_ap, wq_ap, kmT_d, vm_d, bkm, bvm, wo_ap, nwf_ap, rw_ap,
             x1_d, x2_d, hffn_d, aff_d, affT_d, ntok):
    S = C.S
    ph = Phase(C, "k3")
    bx1 = S.buf("k3_x1d")
    bx2 = S.buf("k3_x2d")
    bhf = S.buf("k3_hffn")
    baf = S.buf("k3_aff")
    glu = ph.sb([128, 8, 1024], BF16, "glu")
    bglu = S.buf("k3_glu")
    for nb_ in range(2):
        load_w(C, glu[:, :, nb_ * 512:(nb_ + 1) * 512], bglu, wblk(glu_ap, nb_ * 512, 512))
    wbc = ph.sb([128, D], F32, "wbc")
    wbf = wbc
    bwb = S.buf("k3_wb")
    brw = S.buf("k3_rw")
    rw = ph.sb([128, 32, 16], BF16, "rw")
    load_w(C, rw[:], brw, rw_ap.rearrange("(kc p) n -> p kc n", p=128))
    actT = ph.sb([128, 32, TB3], BF16, "actT")
    bact = S.buf("k3_act")
    qT = ph.sb([128, 32, TB3], BF16, "qT")
    bqT = S.buf("k3_qT")
    wpool = ph.pool("w", 2, [128, 32, 256], BF16)
    pspool = ph.pool("ps", 3, [128, 512], F32, psum=True)
    xpool = ph.pool("x", 1, [128, D], F32)
    hpool = ph.pool("h", 1, [128, D], BF16)
    small = {"ss": ph.sb([128, 1], F32), "rs": ph.sb([128, 1], F32), "junk": ph.sb([128, D], BF16), "b": S.buf("k3_small")}
    pst = ph.pool("pst", 2, [128, 8, 128], BF16, psum=True)
    npools = {"x": xpool, "h": hpool, "pst": pst, "small": small}
    sgp = ph.pool("sg", 2, [128, 512], F32)
    xo = ph.pool("xo", 3, [128, 512], F32)
    kmh = ph.pool("kmh", 2, [128, 8, 256], BF16)
    vmh = ph.pool("vmh", 2, [128, 2, 1024], BF16)
    ptp = ph.pool("ptp", 2, [128, 2, 512], BF16)
    rdn = ph.pool("rdn", 2, [128, 512], F32)
    rsm = ph.sb([128, 8], F32, "rsm")
    lg = ph.sb([128, 16], F32, "lg")
    aft = ph.sb([16, 128], F32, "aft")
    brs = S.buf("k3_rsm")
    ntt = TB3 // 128
    for t0 in range(0, ntok, TB3):
        for kc in range(32):
            cat_src(kc, t0, (qT if kc >= 24 else actT)[:, kc, :], bqT if kc >= 24 else bact)
        for m in range(8):
            ps, bps = pspool.next()
            for k in range(8):
                S.op("pe", lambda p, k=k: p.matmul(ps[:], lhsT=glu[:, k, m * 128:(m + 1) * 128], rhs=qT[:, 24 + k, :],
                                                   start=(k == 0), stop=(k == 7)), reads=[bglu, bqT], writes=[bps])
            sg, bsg = sgp.next()
            S.op("act", lambda a: a.activation(out=sg[:], in_=ps[:], func=AF.Sigmoid), reads=[bps], writes=[bsg])
            S.op("dve", lambda v: v.tensor_tensor(out=actT[:, 24 + m, :], in0=sg[:], in1=qT[:, 24 + m, :], op=ALU.mult),
                 reads=[bsg, bqT], writes=[bact])

        def cb_res(src_ap, dst_ap, bdst, bsrc=None):
            def cb(tt, n, ps, bps):
                xt, bxt = xo.next()
                r0 = t0 + tt * 128
                S.dma("sp", lambda q: q.dma_start(out=xt[:, 0:256], in_=src_ap[r0:r0 + 128, n * 256:(n + 1) * 256]), bxt,
                      reads=([bsrc] if bsrc else []), writes=[bxt])
                S.op("dve", lambda v: v.tensor_tensor(out=xt[:, 0:256], in0=ps[:, 0:256], in1=xt[:, 0:256], op=ALU.add), reads=[bps, bxt], writes=[bxt])
                S.dma("sp", lambda q: q.dma_start(out=dst_ap[r0:r0 + 128, n * 256:(n + 1) * 256], in_=xt[:, 0:256]), bxt,
                      reads=[bxt], writes=[bdst])
            return cb

        gemm_tok(C, lambda k, tt: actT[:, k, tt * 128:(tt + 1) * 128], bact, 32, ntt, wout_ap, 16, cb_res(x_ap, x1_d, bx1), wpool, pspool, wn=256)
        S.dma("sp", lambda q: q.dma_start(out=wbc[:], in_=nwc_ap), bwb, writes=[bwb])
        for i in range(ntt):
            xt, bx = xpool.next()
            r0 = t0 + i * 128
            S.dma("sp", lambda q: q.dma_start(out=xt[:], in_=x1_d[r0:r0 + 128, :]), bx, reads=[bx1], writes=[bx])
            hb, bh = hpool.next()
            rmsnorm_tile(C, xt, bx, wbc, bwb, hb, bh, small)
            transpose_to(C, hb, bh, lambda k0, n, i=i: actT[:, k0:k0 + n, i * 128:(i + 1) * 128], bact, 32, pst)

        def cb_q(m, ps, bps):
            S.op("act" if m % 2 else "dve",
                 (lambda a: a.copy(out=qT[:, m, :], in_=ps[:])) if m % 2 else (lambda v: v.tensor_copy(out=qT[:, m, :], in_=ps[:])),
                 reads=[bps], writes=[bqT])

        gemm_fm2(C, lambda k: actT[:, k, :], bact, 32, TB3, wq_ap, 16, cb_q, wpool, pspool, wn=256)
        for h in range(4):
            km, bkmh = kmh.next()
            vm, bvmh = vmh.next()
            S.dma("sp", lambda q: q.dma_start(out=km[:], in_=kmT_d[h * 1024:(h + 1) * 1024, :].rearrange("(c p) m -> p c m", p=128)),
                  bkmh, reads=[bkm], writes=[bkmh])
            S.dma("sp", lambda q: q.dma_start(out=vm[:], in_=vm_d[:, h * 1024:(h + 1) * 1024].rearrange("(b p) f -> p b f", p=128)),
                  bvmh, reads=[bvm], writes=[bvmh])
            pt, bpt = ptp.next()
            for mb in range(2):
                ps, bps = pspool.next()
                for c in range(8):
                    S.op("pe", lambda p, c=c: p.matmul(ps[:], lhsT=km[:, c, mb * 128:(mb + 1) * 128], rhs=qT[:, h * 8 + c, :],
                                                       start=(c == 0), stop=(c == 7)), reads=[bkmh, bqT], writes=[bps])
                S.op("act", lambda a: a.activation(out=pt[:, mb, :], in_=ps[:], func=AF.Exp, scale=1.0 / 32.0), reads=[bps], writes=[bpt])
            ps, bps = pspool.next()
            for mb in range(2):
                S.op("pe", lambda p: p.matmul(ps[:], lhsT=C.ones_bf[:], rhs=pt[:, mb, :], start=(mb == 0), stop=(mb == 1)),
                     reads=[C.b_const, bpt], writes=[bps])
            rd, brd = rdn.next()
            S.op("dve", lambda v: v.reciprocal(out=rd[:], in_=ps[:]), reads=[bps], writes=[brd])
            for c in range(8):
                ps, bps = pspool.next()
                for mb in range(2):
                    S.op("pe", lambda p: p.matmul(ps[:], lhsT=vm[:, mb, c * 128:(c + 1) * 128], rhs=pt[:, mb, :],
                                                  start=(mb == 0), stop=(mb == 1)), reads=[bvmh, bpt], writes=[bps])
                S.op("dve", lambda v: v.tensor_tensor(out=actT[:, h * 8 + c, :], in0=ps[:], in1=rd[:], op=ALU.mult),
                     reads=[bps, brd], writes=[bact])
        gemm_tok(C, lambda k, tt: actT[:, k, tt * 128:(tt + 1) * 128], bact, 32, ntt, wo_ap, 16, cb_res(x1_d, x2_d, bx2, bx1), wpool, pspool, wn=256)
        S.dma("sp", lambda q: q.dma_start(out=wbc[:], in_=nwf_ap), bwb, writes=[bwb])
        for i in range(ntt):
            xt, bx = xpool.next()
            r0 = t0 + i * 128
            S.dma("sp", lambda q: q.dma_start(out=xt[:], in_=x2_d[r0:r0 + 128, :]), bx, reads=[bx2], writes=[bx])
            hb, bh = hpool.next()
            rmsnorm_tile(C, xt, bx, wbf, bwb, hb, bh, small)
            S.dma("sp", lambda q: q.dma_start(out=hffn_d[r0:r0 + 128, :], in_=hb[:]), bh, reads=[bh], writes=[bhf])
            transpose_to(C, hb, bh, lambda k0, n, i=i: actT[:, k0:k0 + n, i * 128:(i + 1) * 128], bact, 32, pst)
            ps, bps = pspool.next()
            for k in range(32):
                S.op("pe", lambda p, k=k: p.matmul(ps[:, 0:16], lhsT=actT[:, k, i * 128:(i + 1) * 128], rhs=rw[:, k, :],
                                                   start=(k == 0), stop=(k == 31)), reads=[bact, brw], writes=[bps])
            S.op("dve", lambda v: v.tensor_reduce(out=rsm[:, 0:1], in_=ps[:, 0:16], axis=AX.X, op=ALU.max), reads=[bps], writes=[brs])
            S.op("dve", lambda v: v.tensor_scalar(out=rsm[:, 1:2], in0=rsm[:, 0:1], scalar1=-1.0, scalar2=None, op0=ALU.mult), reads=[brs], writes=[brs])
            S.op("act", lambda a: a.activation(out=lg[:], in_=ps[:, 0:16], func=AF.Exp, bias=rsm[:, 1:2], accum_out=rsm[:, 2:3]),
                 reads=[bps, brs], writes=[brs])
            S.op("dve", lambda v: v.reciprocal(out=rsm[:, 3:4], in_=rsm[:, 2:3]), reads=[brs], writes=[brs])
            S.op("dve", lambda v: v.tensor_scalar(out=lg[:], in0=lg[:], scalar1=rsm[:, 3:4], scalar2=None, op0=ALU.mult), reads=[brs], writes=[brs])
            S.dma("sp", lambda q: q.dma_start(out=aff_d[r0:r0 + 128, :], in_=lg[:]), brs, reads=[brs], writes=[baf])
            pa, bpa = pspool.next()
            S.op("pe", lambda p: p.transpose(out=pa[0:16, 0:128], in_=lg[:], identity=C.ident_f[:]), reads=[brs, C.b_const], writes=[bpa])
            S.op("dve", lambda v: v.tensor_copy(out=aft[:], in_=pa[0:16, 0:128]), reads=[bpa, brs], writes=[brs])
            S.dma("sp", lambda q: q.dma_start(out=affT_d[:, r0:r0 + 128], in_=aft[:]), brs, reads=[brs], writes=[baf])
    ph.close()


CAP = 512
TOWN = 4096
HALF = 2048
SW = 256


def _moe_consts():
    c = {}
    c["moe_iota"] = np.broadcast_to(np.arange(CAP, dtype=np.float32), (128, CAP)).copy()
    rh = np.zeros((128, TOWN // 128, 3), np.float32)
    rh[:, :, 0] = np.arange(128)[:, None]
    rh[:, :, 1] = np.arange(TOWN // 128)[None, :]
    rh[:, :, 2] = 1.0
    c["moe_rh"] = rh.astype(ml_dtypes.bfloat16)
    dm = np.zeros((128, 4), np.float32)
    for sc in range(4):
        dm[:, sc] = TOWN + sc * 128 + np.arange(128)
    c["moe_dmy"] = dm
    c["moe_ncol"] = np.broadcast_to(np.arange(D // SW, dtype=np.float32), (128, D // SW)).copy()
    return c


def phase_k4(C, affT_pair, affT_own, aff_own, hffn_d, xacc_d, wexp, experts, bxacc_in, dep_bufs=()):
    S = C.S
    ph = Phase(C, "k4")
    bc = S.buf("k4_const")
    iota = load_const(C, ph, "moe_iota", [128, CAP], F32, bc)
    rhc = load_const(C, ph, "moe_rh", [128, TOWN // 128, 3], BF16, bc)
    dmy = load_const(C, ph, "moe_dmy", [128, 4], F32, bc)
    ncol = load_const(C, ph, "moe_ncol", [128, D // SW], F32, bc)
    ntt = TOWN // 128
    pos_tok = ph.sb([128, ntt, 16], F32, "pos_tok")
    sel_tok = ph.sb([128, ntt, 16], F32, "sel_tok")
    aff_tok = ph.sb([128, ntt, 16], F32, "aff_tok")
    ahi = ph.sb([128, ntt, 16], BF16, "ahi")
    alo = ph.sb([128, ntt, 16], BF16, "alo")
    btok = S.buf("k4_tok")
    S.dma("sp", lambda q: q.dma_start(out=aff_tok[:], in_=aff_own.rearrange("(t p) e -> p t e", p=128)), btok, reads=list(dep_bufs), writes=[btok])
    S.op("dve", lambda v: v.tensor_copy(out=ahi[:], in_=aff_tok[:]), reads=[btok], writes=[btok])
    S.op("dve", lambda v: v.tensor_tensor(out=alo[:], in0=aff_tok[:], in1=ahi[:], op=ALU.subtract), reads=[btok], writes=[btok])
    p1 = Phase(C, "k4a")
    AT = p1.sb([16, TOWN], F32, "AT")
    junk = p1.sb([16, TOWN], F32, "junk")
    bAT = S.buf("k4_AT")
    for r in (0, 1):
        S.dma("sp", lambda q: q.dma_start(out=AT[:, r * HALF:(r + 1) * HALF], in_=affT_pair[r]), bAT, reads=list(dep_bufs), writes=[bAT])
    bs = p1.sb([16, 8], F32, "bs")
    bbs = S.buf("k4_bs")
    c_ = lambda i: bs[:, i:i + 1]
    S.op("dve", lambda v: v.memset(bs[:], 0.0), writes=[bbs])
    S.op("dve", lambda v: v.memset(c_(1), 1.0), reads=[bbs], writes=[bbs])
    S.op("dve", lambda v: v.memset(c_(6), 0.5), reads=[bbs], writes=[bbs])
    for it in range(30):
        S.op("dve", lambda v: v.scalar_tensor_tensor(out=c_(2), in0=c_(0), scalar=c_(1), in1=c_(6), op0=ALU.add, op1=ALU.mult), reads=[bbs], writes=[bbs])
        S.op("dve", lambda v: v.tensor_scalar(out=junk[:], in0=AT[:], scalar1=c_(2), scalar2=0.0, op0=ALU.is_gt, op1=ALU.add, accum_out=c_(3)),
             reads=[bAT, bbs], writes=[bbs])
        S.op("dve", lambda v: v.tensor_scalar(out=c_(4), in0=c_(3), scalar1=CAP - 0.5, scalar2=None, op0=ALU.is_gt), reads=[bbs], writes=[bbs])
        S.op("dve", lambda v: v.tensor_tensor(out=c_(5), in0=c_(2), in1=c_(0), op=ALU.subtract), reads=[bbs], writes=[bbs])
        S.op("dve", lambda v: v.scalar_tensor_tensor(out=c_(0), in0=c_(5), scalar=c_(4), in1=c_(0), op0=ALU.mult, op1=ALU.add), reads=[bbs], writes=[bbs])
        S.op("dve", lambda v: v.tensor_tensor(out=c_(5), in0=c_(1), in1=c_(2), op=ALU.subtract), reads=[bbs], writes=[bbs])
        S.op("dve", lambda v: v.scalar_tensor_tensor(out=c_(1), in0=c_(5), scalar=c_(4), in1=c_(2), op0=ALU.mult, op1=ALU.add), reads=[bbs], writes=[bbs])
    selT = p1.sb([16, TOWN], F32, "selT")
    posT = p1.sb([16, TOWN], F32, "posT")
    ones = p1.sb([16, TOWN], F32, "ones")
    bsel = S.buf("k4_sel")
    S.op("dve", lambda v: v.tensor_scalar(out=selT[:], in0=AT[:], scalar1=c_(0), scalar2=None, op0=ALU.is_gt), reads=[bAT, bbs], writes=[bsel])
    S.op("dve", lambda v: v.memset(ones[:], 1.0), writes=[bsel])
    S.op("dve", lambda v: v.tensor_tensor_scan(out=posT[:], data0=ones[:], data1=selT[:], initial=0.0, op0=ALU.mult, op1=ALU.add),
         reads=[bsel], writes=[bsel])
    S.op("dve", lambda v: v.tensor_tensor(out=posT[:], in0=posT[:], in1=selT[:], op=ALU.subtract), reads=[bsel], writes=[bsel])
    ptr = p1.pool("ptr", 2, [128, 2, 16], F32, psum=True)
    for tt in range(ntt):
        pt, bpt = ptr.next()
        cs = slice(tt * 128, (tt + 1) * 128)
        S.op("pe", lambda p: p.transpose(out=pt[:, 0, :], in_=posT[:, cs], identity=C.ident_f[0:16, 0:16]), reads=[bsel, C.b_const], writes=[bpt])
        S.op("pe", lambda p: p.transpose(out=pt[:, 1, :], in_=selT[:, cs], identity=C.ident_f[0:16, 0:16]), reads=[bsel, C.b_const], writes=[bpt])
        S.op("dve", lambda v: v.tensor_copy(out=pos_tok[:, tt, :], in_=pt[:, 0, :]), reads=[bpt], writes=[btok])
        S.op("dve", lambda v: v.tensor_copy(out=sel_tok[:, tt, :], in_=pt[:, 1, :]), reads=[bpt], writes=[btok])
    p1.close()
    rh = ph.sb([128, ntt, 5], BF16, "rh")
    brh = S.buf("k4_rh")
    S.op("dve", lambda v: v.tensor_copy(out=rh[:, :, 0:3], in_=rhc[:]), reads=[bc], writes=[brh])
    ohp = ph.pool("oh", ntt + 2, [128, CAP], BF16)
    pidx = ph.pool("pidx", 1, [128, 8], F32, psum=True)
    ixf = ph.sb([128, 4, 8], F32, "ixf")
    idx_g = ph.sb([128, 4], I32, "idx_g")
    idx_s = ph.sb([128, 4, D // SW], I32, "idx_s")
    idx_sf = ph.sb([128, D // SW], F32, "idx_sf")
    gate = ph.sb([128, 4], F32, "gate")
    bix = S.buf("k4_ix")
    xgp = ph.pool("xg", 2, [128, D], BF16)
    xeT = ph.sb([128, 32, CAP], BF16, "xeT")
    bxe = S.buf("k4_xeT")
    pst = ph.pool("pst", 2, [128, 8, 128], BF16, psum=True)
    wpool = ph.pool("w", 2, [128, 32, 256], BF16)
    pspool = ph.pool("ps", 3, [128, 512], F32, psum=True)
    sgl = ph.sb([128, 8, CAP], F32, "sgl")
    bsg = S.buf("k4_sgl")
    actT = ph.sb([128, 8, CAP], BF16, "actT")
    bact = S.buf("k4_act")
    yep = ph.pool("ye", 4, [128, SW], F32)
    bacc = [S.buf("k4_acc%d" % n) for n in range(D // SW)]
    for b in bacc:
        b.w = bxacc_in.w
    xacc_v = xacc_d.rearrange("r (a w) -> (r a) w", w=SW)
    bhf = S.buf("k4_hf")
    for e in experts:
        S.op("dve", lambda v: v.tensor_copy(out=rh[:, :, 3], in_=ahi[:, :, e]), reads=[btok, brh], writes=[brh])
        S.op("dve", lambda v: v.tensor_copy(out=rh[:, :, 4], in_=alo[:, :, e]), reads=[btok, brh], writes=[brh])
        ohs = []
        for tt in range(ntt):
            oh, boh = ohp.next()
            S.op("dve", lambda v: v.tensor_scalar(out=oh[:], in0=iota[:], scalar1=pos_tok[:, tt, e:e + 1], scalar2=sel_tok[:, tt, e:e + 1],
                                                  op0=ALU.is_equal, op1=ALU.mult), reads=[bc, btok], writes=[boh])
            ohs.append((oh, boh))
        for sc in range(4):
            pi, bpi = pidx.next()
            for tt in range(ntt):
                oh, boh = ohs[tt]
                S.op("pe", lambda p: p.matmul(pi[:, 0:5], lhsT=oh[:, sc * 128:(sc + 1) * 128], rhs=rh[:, tt, :], start=(tt == 0), stop=(tt == ntt - 1)),
                     reads=[boh, brh], writes=[bpi])
            S.op("dve", lambda v: v.tensor_copy(out=ixf[:, sc, 0:5], in_=pi[:, 0:5]), reads=[bpi, bix], writes=[bix])
            f = lambda i: ixf[:, sc, i:i + 1]
            S.op("dve", lambda v: v.scalar_tensor_tensor(out=f(5), in0=f(1), scalar=128.0, in1=f(0), op0=ALU.mult, op1=ALU.add), reads=[bix], writes=[bix])
            S.op("dve", lambda v: v.tensor_copy(out=idx_g[:, sc:sc + 1], in_=f(5)), reads=[bix], writes=[bix])
            S.op("dve", lambda v: v.tensor_tensor(out=gate[:, sc:sc + 1], in0=f(3), in1=f(4), op=ALU.add), reads=[bix], writes=[bix])
            S.op("dve", lambda v: v.tensor_tensor(out=f(6), in0=f(2), in1=dmy[:, sc:sc + 1], op=ALU.mult), reads=[bix, bc], writes=[bix])
            S.op("dve", lambda v: v.tensor_tensor(out=f(7), in0=dmy[:, sc:sc + 1], in1=f(6), op=ALU.subtract), reads=[bix, bc], writes=[bix])
            S.op("dve", lambda v: v.tensor_tensor(out=f(7), in0=f(7), in1=f(5), op=ALU.add), reads=[bix], writes=[bix])
            S.op("dve", lambda v: v.tensor_copy(out=idx_sf[:], in_=ncol[:]), reads=[bc, bix], writes=[bix])
            S.op("dve", lambda v: v.tensor_scalar(out=f(6), in0=f(7), scalar1=float(D // SW), scalar2=None, op0=ALU.mult), reads=[bix], writes=[bix])
            S.op("dve", lambda v: v.tensor_scalar(out=idx_sf[:], in0=idx_sf[:], scalar1=f(6), scalar2=None, op0=ALU.add), reads=[bix], writes=[bix])
            S.op("dve", lambda v: v.tensor_copy(out=idx_s[:, sc, :], in_=idx_sf[:]), reads=[bix], writes=[bix])
            xg, bxg = xgp.next()
            S.dma("pool", lambda q: q.indirect_dma_start(out=xg[:], out_offset=None, in_=hffn_d,
                                                         in_offset=bass.IndirectOffsetOnAxis(ap=idx_g[:, sc:sc + 1], axis=0)),
                  bxg, reads=[bix, bhf] + list(dep_bufs), writes=[bxg])
            transpose_to(C, xg, bxg, lambda k0, n: xeT[:, k0:k0 + n, sc * 128:(sc + 1) * 128], bxe, 32, pst)
        wg_e, wu_e, wd_e = wexp(e)

        def cb_g(m, ps, bps):
            S.op("act", lambda a: a.activation(out=sgl[:, m, :], in_=ps[:], func=AF.Silu), reads=[bps], writes=[bsg])

        gemm_fm2(C, lambda k: xeT[:, k, :], bxe, 32, CAP, wg_e, 4, cb_g, wpool, pspool, wn=256)

        def cb_u(m, ps, bps):
            S.op("dve", lambda v: v.tensor_tensor(out=actT[:, m, :], in0=ps[:], in1=sgl[:, m, :], op=ALU.mult), reads=[bps, bsg], writes=[bact])

        gemm_fm2(C, lambda k: xeT[:, k, :], bxe, 32, CAP, wu_e, 4, cb_u, wpool, pspool, wn=256)

        def cb_d(sc, n, ps, bps):
            ye, bye = yep.next()
            S.op("act", lambda a: a.activation(out=ye[:], in_=ps[:, 0:SW], func=AF.Copy, scale=gate[:, sc:sc + 1]), reads=[bps, bix], writes=[bye])
            S.dma("pool", lambda q: q.indirect_dma_start(out=xacc_v, out_offset=bass.IndirectOffsetOnAxis(ap=idx_s[:, sc, n:n + 1], axis=0),
                                                         in_=ye[:], in_offset=None, compute_op=ALU.add),
                  bye, reads=[bye, bix], writes=[bacc[n]])

        gemm_tok(C, lambda k, tt: actT[:, k, tt * 128:(tt + 1) * 128], bact, 8, 4, wd_e, D // SW, cb_d, wpool, pspool, wn=SW)
    ph.close()
    return bacc


def _coll(self, kind, groups, in_ap, out_ap, reads, writes):
    e = "pool"
    self._waits(e, reads, writes)
    if not hasattr(self, "cc_sem"):
        self.cc_sem = self.nc.alloc_semaphore("cc_sem")
        self.cc_cnt = 0
    ins = self.eng[e].collective_compute(kind, ALU.bypass, replica_groups=groups, ins=[in_ap.opt()], outs=[out_ap.opt()])
    ins.then_inc(self.cc_sem)
    self.cc_cnt += 1
    ev = ("cc", self.cc_sem, self.cc_cnt)
    self._record(ev, reads, writes)
    self.all_dma["cc"] = ev
    return ev


Sched.coll = _coll
G4 = [[0, 1, 2, 3], [4, 5, 6, 7]]
GP4 = [[0, 4], [1, 5], [2, 6], [3, 7]]
GP1 = [[0, 1], [2, 3], [4, 5], [6, 7]]

OFF_QA, OFF_KA, OFF_VA, OFF_QB, OFF_KB, OFF_VB, OFF_GB, OFF_UC = 0, 1536, 3072, 4608, 5376, 6144, 7680, 9216


def phase_k1(C, x_ap, bx_in, nw_ap, w_ap, bw_in, projT_d, bproj):
    S = C.S
    ph = Phase(C, "k1")
    Tc = 2048
    TBK = 1024
    wb = ph.sb([128, D], F32, "wb")
    bwb = S.buf("k1_wb")
    S.dma("sp", lambda q: q.dma_start(out=wb[:], in_=nw_ap), bwb, writes=[bwb])
    pools = {"x": ph.pool("x", 2, [128, D], F32), "h": ph.pool("h", 2, [128, D], BF16),
             "pst": ph.pool("pst", 2, [128, 8, 128], BF16, psum=True),
             "small": {"ss": ph.sb([128, 1], F32), "rs": ph.sb([128, 1], F32), "junk": ph.sb([128, D], BF16), "b": S.buf("k1_small")}}
    hT = ph.sb([128, 32, TBK], BF16, "hT")
    bhT = S.buf("k1_hT")
    wpool = ph.pool("w", 2, [128, 32, 256], BF16)
    pspool = ph.pool("ps", 4, [128, 512], F32, psum=True)
    opool = ph.pool("ot", 4, [128, 512], BF16)
    cnt = 0
    for tb in range(Tc // TBK):
        norm_block_to_hT(C, x_ap, tb * TBK, TBK // 128, wb, bwb, hT, bhT, pools)
        tiles = {}

        def issue(n):
            wt_, bw_ = wpool.next()
            load_w(C, wt_[:, :, :], bw_, wblk(w_ap, n * 256, 256))
            tiles[n] = (wt_, bw_)

        issue(0)
        for nb in range(40):
            if nb + 1 < 40:
                issue(nb + 1)
            wt, bw = tiles.pop(nb)
            for mi in range(2):
                for th in range(TBK // 512):
                    ps, bps = pspool.next()
                    for k in range(32):
                        S.op("pe", lambda p: p.matmul(ps[:], lhsT=wt[:, k, mi * 128:(mi + 1) * 128], rhs=hT[:, k, th * 512:(th + 1) * 512],
                                                      start=(k == 0), stop=(k == 31)), reads=[bw, bhT], writes=[bps])
                    ot, bo = opool.next()
                    if cnt % 2:
                        S.op("act", lambda a: a.copy(out=ot[:], in_=ps[:]), reads=[bps], writes=[bo])
                    else:
                        S.op("dve", lambda v: v.tensor_copy(out=ot[:], in_=ps[:]), reads=[bps], writes=[bo])
                    cnt += 1
                    m = nb * 2 + mi
                    c0 = tb * TBK + th * 512
                    S.dma("sp", lambda q: q.dma_start(out=projT_d[m * 128:(m + 1) * 128, c0:c0 + 512], in_=ot[:]), bo, reads=[bo], writes=[bproj])
    ph.close()


def phase_k2(C, G1, bG1, posb, idx2_ap, P, mixT_d, bmix):
    S = C.S
    T = SEQ
    G1v = G1
    top = Phase(C, "k2")
    idx2 = top.sb([128, 80], I32, "idx2")
    bidx = S.buf("k2_idx")
    S.dma("sp", lambda q: q.dma_start(out=idx2[:], in_=idx2_ap), bidx, writes=[bidx])

    def gath(dst_fn, b, col):
        for h in (0, 1):
            S.dma("pool", lambda q: q.indirect_dma_start(out=dst_fn(h), out_offset=None, in_=G1v,
                                                         in_offset=bass.IndirectOffsetOnAxis(ap=idx2[:, col * 2 + h:col * 2 + h + 1], axis=0)),
                  b, reads=[bidx, bG1], writes=[b])

    ph = Phase(C, "k2a")
    W = mixer_a_setup(C, ph, posb, T)
    for j in range(6):
        def ld(which, t, b, off, j=j):
            ti = {"q": 0, "k": 1, "v": 2}[which]
            gath(lambda h: t[:, off + h * 2048: off + (h + 1) * 2048], b, j * 3 + ti)
        mixer_a_head(C, ph, ld, mixT_d[j * 128:(j + 1) * 128, :], T, W)
    S.barrier()
    bmix.w = None
    ph.close()
    ph = Phase(C, "k2b")
    Wb = mixer_b_setup(C, ph, posb, T)
    dect = ph.sb([128, 6], F32, "dect")
    gnt = ph.sb([128, 6], F32, "gnt")
    bd = S.buf("k2_dec")
    S.dma("sp", lambda q: q.dma_start(out=dect[:], in_=P["ret_dec"]), bd, writes=[bd])
    S.dma("sp", lambda q: q.dma_start(out=gnt[:], in_=P["gnw"]), bd, writes=[bd])
    for j in range(3):
        def ldb(which, ap, b, j=j):
            wi = {"q": 0, "k": 1, "v0": 2, "v1": 3, "g0": 4, "g1": 5}[which]
            gath(lambda h: ap[:, h * 2048:(h + 1) * 2048], b, 18 + j * 6 + wi)
        mixer_b_head(C, ph, ldb, dect, bd, j, gnt[:, 2 * j:2 * j + 2], bd, mixT_d[768 + 256 * j:768 + 256 * (j + 1), :], T, Wb)
    ph.close()
    ph = Phase(C, "k2c")
    stg = ph.sb([128, T], BF16, "stg")
    bstg = S.buf("k2_stg")
    for gp in range(16):
        if gp % 4 == 0:
            gath(lambda h: stg[:, h * 2048:(h + 1) * 2048], bstg, 36 + gp // 4)

        def ld_u(ut, bu, gp=gp):
            r0 = (gp % 4) * 32
            S.dma("sp", lambda q: q.dma_start(out=ut[:], in_=stg[r0:r0 + 32, :]), bu, reads=[bstg], writes=[bu])

        prm = []
        for dr in (0, 1):
            d = {}
            for i, nm in enumerate(("lam_re", "lam_im", "logdt")):
                d[nm] = P["s5_cols"][gp, dr, i]
            for i, nm in enumerate(("b_re", "b_im", "c_reT", "c_imT")):
                d[nm] = P["s5_mats"][gp, dr, :, i, :]
            prm.append(d)
        mixer_c_pair(C, ph, ld_u, prm, P["s5_d"][gp], P["tidx"], mixT_d[1536 + 32 * gp:1536 + 32 * (gp + 1), :], T)
    ph.close()
    top.close()


NC4 = 4
GRP = [[0, 1, 2, 3]]
WSPEC = (("w_in", 4096, 10240, 512, 1), ("w_out", 4096, D, 512, 1), ("wq", 4096, D, 512, 1), ("wkv", 4096, 2 * D, 512, 1),
         ("wo", 4096, D, 512, 1), ("glu", 1024, 1024, 512, 1), ("wg", 4096, 1024, 512, 16), ("wu", 4096, 1024, 512, 16),
         ("wd", 1024, D, 2048, 16))


def prologue_weight(C, name, src_ap, K, N, cw, ne, pool):
    S = C.S
    Ks = K // 4
    NB = N // cw
    sh = C.scratch(name + "_sh", [ne * NB, Ks, cw], BF16)
    full = C.scratch(name + "_full", [ne * NB, K, cw], BF16)
    for e in range(ne):
        bsh = S.buf("%s_sh%d" % (name, e))
        for r0 in range(0, Ks, 128):
            t, bt = pool.next()
            S.dma("pool", lambda q: q.dma_start(out=t[:, 0:N], in_=src_ap[e * Ks + r0:e * Ks + r0 + 128, :]), bt, writes=[bt])
            S.dma("sp", lambda q: q.dma_start(out=sh[e * NB:(e + 1) * NB, r0:r0 + 128, :].rearrange("nb p c -> p nb c"),
                                              in_=t[:, 0:N].rearrange("p (nb c) -> p nb c", c=cw)), bt, reads=[bt], writes=[bsh])
        for nb in range(NB):
            S.coll("AllGather", GRP, sh[e * NB + nb], full[e * NB + nb], [bsh], [])
    return full


def build_full(depth=2):
    C = Ctx()
    S = C.S
    x_in = C.inp("x", [SEQ, D], F32)
    mem_in = C.inp("mem", [256, D], F32)
    posb = C.inp("posb", [128, SEQ], I32)
    tidx = C.inp("tidx", [128, SEQ], I32)
    idx2_in = C.inp("idx2", [2, 128, 80], I32)
    idx3_in = C.inp("idx3", [2, 128, 32], F32)
    final_nw = C.inp("final_nw", [128, D], F32)
    out = C.out("out", [SEQ, D], F32)
    for nm, arr in list(_mixer_consts().items()) + list(_retention_consts().items()) + list(_moe_consts().items()):
        C.inp(nm, list(arr.shape), BF16 if arr.dtype == ml_dtypes.bfloat16 else (I32 if arr.dtype == np.int32 else F32))
    G1 = C.scratch("G1", [2 * 10240, 2048], BF16)
    G2 = C.scratch("G2", [2 * 2048, SEQ], BF16)
    x1_d = C.scratch("x1_d", [2048, D], F32)
    xacc_all = C.scratch("xacc", [SEQ + CAP, D], F32)
    hffn_all = C.scratch("hffn", [SEQ, D], BF16)
    aff_all = C.scratch("aff", [SEQ, 16], F32)
    xacc = [xacc_all[r * 2048:(r + 1) * 2048, :] for r in (0, 1)]
    hffn = [hffn_all[r * 2048:(r + 1) * 2048, :] for r in (0, 1)]
    aff = [aff_all[r * 2048:(r + 1) * 2048, :] for r in (0, 1)]
    affT_pair = C.scratch("affT_pair", [32, 2048], F32)
    kmT_d = C.scratch("kmT_d", [D, 256], BF16)
    vm_d = C.scratch("vm_d", [256, D], BF16)
    LW = []
    ph = Phase(C, "pro")
    cpool = ph.pool("cast", 3, [128, 10240], BF16)
    for l in range(depth):
        Wl = {}
        for nm, K, N, cw, ne in WSPEC:
            src = C.inp("%s_%d" % (nm, l), [ne * K // 4, N], F32)
            Wl[nm] = prologue_weight(C, "%s_%d" % (nm, l), src, K, N, cw, ne, cpool)
        LW.append(Wl)
    ph.close()
    for l in range(depth):
        Wl = LW[l]
        Pr = []
        rd_in = C.inp("ret_dec_%d" % l, [2, 128, 6], F32)
        gn_in = C.inp("gnw_%d" % l, [2, 128, 6], F32)
        sc_in = C.inp("s5_cols_%d" % l, [2, 16, 2, 3, 128, 1], F32)
        sm_in = C.inp("s5_mats_%d" % l, [2, 16, 2, 128, 4, 16], F32)
        sd_in = C.inp("s5_d_%d" % l, [2, 16, 32, 1], F32)
        for r in (0, 1):
            Pr.append({"ret_dec": rd_in[r], "gnw": gn_in[r], "s5_cols": sc_in[r], "s5_mats": sm_in[r], "s5_d": sd_in[r], "tidx": tidx})
        nw_mix = C.inp("nw_mix_%d" % l, [128, D], F32)
        nw_cross = C.inp("nw_cross_%d" % l, [128, D], F32)
        nw_mem = C.inp("nw_mem_%d" % l, [128, D], F32)
        nw_ffn = C.inp("nw_ffn_%d" % l, [128, D], F32)
        rw = C.inp("rw_%d" % l, [D, 16], F32)
        xs = [x_in[0:2048, :], x_in[2048:4096, :]] if l == 0 else [xacc[0], xacc[1]]
        dummy = S.buf("dummy")
        for r in (0, 1):
            phase_k1(C, xs[r], None, nw_mix, WMat(Wl["w_in"]), dummy, G1[r * 10240:(r + 1) * 10240, :], S.buf("proj"))
        for r in (0, 1):
            phase_k2(C, G1, S.buf("G1"), posb, idx2_in[r], Pr[r], G2[r * 2048:(r + 1) * 2048, :], S.buf("mix"))
        bkm, bvm = S.buf("kmd"), S.buf("vmd")
        phase_mem_kv(C, mem_in, nw_mem, (WMat(Wl["wkv"]), WMat(Wl["wkv"], col0=D)), kmT_d, vm_d, bkm, bvm)
        G2v = G2.rearrange("r (a w) -> (r a) w", w=512)
        for r in (0, 1):
            php = Phase(C, "k3idx")
            idx3f = php.sb([128, 32], F32, "idx3f")
            idx3b = php.sb([128, 4, 32], I32, "idx3b")
            bi3 = S.buf("k3_idx")
            S.dma("sp", lambda q: q.dma_start(out=idx3f[:], in_=idx3_in[r]), bi3, writes=[bi3])
            for blk in range(4):
                S.op("dve", lambda v: v.tensor_scalar(out=idx3b[:, blk, :], in0=idx3f[:], scalar1=float(blk), scalar2=None, op0=ALU.add),
                     reads=[bi3], writes=[bi3])

            def cat_load(kc, t0, dst, bd):
                blk = t0 // 512
                S.dma("pool", lambda q: q.indirect_dma_start(out=dst, out_offset=None, in_=G2v,
                                                             in_offset=bass.IndirectOffsetOnAxis(ap=idx3b[:, blk, kc:kc + 1], axis=0)),
                      bd, reads=[bi3], writes=[bd])

            phase_k3(C, xs[r], cat_load, WMat(Wl["glu"]), WMat(Wl["w_out"]), nw_cross, WMat(Wl["wq"]), kmT_d, vm_d, bkm, bvm,
                     WMat(Wl["wo"]), nw_ffn, rw, x1_d, xacc[r], hffn[r], aff[r], affT_pair[r * 16:(r + 1) * 16, :], 2048)
            php.close()
        affp_v = affT_pair.rearrange("(r e) t -> r e t", r=2)

        def wexp(e):
            return (WMat(Wl["wg"][2 * e:2 * e + 2]), WMat(Wl["wu"][2 * e:2 * e + 2]), WMat(Wl["wd"][2 * e:2 * e + 2]))

        phase_k4(C, affp_v, None, aff_all, hffn_all, xacc_all, wexp, list(range(16)), S.buf("xacc"))
    ph = Phase(C, "fin")
    wb = ph.sb([128, D], F32, "wb")
    bwb = S.buf("fin_wb")
    S.dma("sp", lambda q: q.dma_start(out=wb[:], in_=final_nw), bwb, writes=[bwb])
    xp = ph.pool("x", 2, [128, D], F32)
    hp = ph.pool("h", 2, [128, D], F32)
    small = {"ss": ph.sb([128, 1], F32), "rs": ph.sb([128, 1], F32), "junk": ph.sb([128, D], BF16), "b": S.buf("fin_small")}
    for r in (0, 1):
        for i in range(16):
            xt, bxt = xp.next()
            S.dma("sp", lambda q: q.dma_start(out=xt[:], in_=xacc[r][i * 128:(i + 1) * 128, :]), bxt, writes=[bxt])
            hb, bh = hp.next()
            rmsnorm_tile(C, xt, bxt, wb, bwb, hb, bh, small)
            r0 = r * 2048 + i * 128
            C.out_evs.append(S.dma("sp", lambda q: q.dma_start(out=out[r0:r0 + 128, :], in_=hb[:]), bh, reads=[bh]))
    ph.close()
    return C


def _idx_tables(r):
    idx2 = np.zeros((128, 80), np.int32)
    p = np.arange(128)
    cols = []
    for j in range(6):
        H = 6 * r + j
        cols += [OFF_QA + 128 * H, OFF_KA + 128 * H, OFF_VA + 128 * H]
    for j in range(3):
        H = 3 * r + j
        cols += [OFF_QB + 128 * H, OFF_KB + 128 * H, OFF_VB + 256 * H, OFF_VB + 256 * H + 128, OFF_GB + 256 * H, OFF_GB + 256 * H + 128]
    for cb in range(4):
        cols += [OFF_UC + 512 * r + 128 * cb]
    for ci, row0 in enumerate(cols):
        for h in (0, 1):
            idx2[:, ci * 2 + h] = h * 10240 + row0 + p
    idx3 = np.zeros((128, 32), np.float32)
    for kc in range(32):
        if kc < 12:
            rr, row0 = kc // 6, 128 * (kc % 6)
        elif kc < 24:
            rr, row0 = (kc - 12) // 6, 768 + 128 * ((kc - 12) % 6)
        else:
            rr, row0 = (kc - 24) // 4, 1536 + 128 * ((kc - 24) % 4)
        idx3[:, kc] = (rr * 2048 + row0 + p) * 8 + r * 4
    return idx2, idx3


_PROG = {}


def make_maps(inp, depth=2):
    f32 = lambda a: np.ascontiguousarray(np.asarray(a), dtype=np.float32)
    bc = lambda v: np.ascontiguousarray(np.broadcast_to(np.asarray(v, dtype=np.float32), (128, D)))
    consts = {}
    consts.update(_mixer_consts())
    consts.update(_retention_consts())
    consts.update(_moe_consts())
    consts.update(_s5_consts(SEQ))
    consts.update(_host_consts())
    t2 = [_idx_tables(r) for r in (0, 1)]
    consts["idx2"] = np.stack([t2[0][0], t2[1][0]])
    consts["idx3"] = np.stack([t2[0][1], t2[1][1]])
    consts["final_nw"] = bc(inp["final_norm_w"])
    per_layer = []
    for l in range(depth):
        d = {}
        d["rw_%d" % l] = f32(inp["router_w"][l])
        d["nw_mix_%d" % l] = bc(inp["norm_mix_w"][l])
        d["nw_cross_%d" % l] = bc(inp["norm_cross_w"][l])
        d["nw_mem_%d" % l] = bc(inp["norm_mem_w"][l])
        d["nw_ffn_%d" % l] = bc(inp["norm_ffn_w"][l])
        rd = np.asarray(inp["ret_decay"][l], dtype=np.float32)
        gw = np.asarray(inp["ret_gn_w"][l], dtype=np.float32)
        dec = np.zeros((2, 128, 6), np.float32)
        gn = np.zeros((2, 128, 6), np.float32)
        cols = np.zeros((2, 16, 2, 3, 128, 1), np.float32)
        mats = np.zeros((2, 16, 2, 128, 4, 16), np.float32)
        sd = np.zeros((2, 16, 32, 1), np.float32)
        lre, lim, ldt = (np.asarray(inp[k][l]) for k in ("s5_lam_re", "s5_lam_im", "s5_log_dt"))
        bre, bim, cre, cim = (np.asarray(inp[k][l]) for k in ("s5_b_re", "s5_b_im", "s5_c_re", "s5_c_im"))
        s5d = np.asarray(inp["s5_d"][l])
        for r in (0, 1):
            for j in range(3):
                H = 3 * r + j
                for dr in (0, 1):
                    dec[r, :, dr * 3 + j] = rd[dr, H]
                for hf in (0, 1):
                    gn[r, :, 2 * j + hf] = gw[H * 256 + hf * 128:H * 256 + (hf + 1) * 128]
            for gp in range(16):
                g0 = 32 * r + 2 * gp
                sd[r, gp, :, 0] = s5d[16 * g0:16 * g0 + 32]
                for dr in (0, 1):
                    for gi in (0, 1):
                        g = g0 + gi
                        ps = slice(gi * 64, (gi + 1) * 64)
                        cols[r, gp, dr, 0, ps, 0] = lre[dr][g]
                        cols[r, gp, dr, 1, ps, 0] = lim[dr][g]
                        cols[r, gp, dr, 2, ps, 0] = ldt[dr][g]
                        mats[r, gp, dr, ps, 0] = bre[dr][g]
                        mats[r, gp, dr, ps, 1] = bim[dr][g]
                        mats[r, gp, dr, ps, 2] = cre[dr][g].T
                        mats[r, gp, dr, ps, 3] = cim[dr][g].T
        d["ret_dec_%d" % l], d["gnw_%d" % l] = dec, gn
        d["s5_cols_%d" % l], d["s5_mats_%d" % l], d["s5_d_%d" % l] = cols, mats, sd
        per_layer.append(d)
    maps = []
    for c in range(NC4):
        m = dict(consts)
        m["x"] = f32(inp["x"][c])
        m["mem"] = f32(inp["mem"][c])
        m["posb"] = np.ascontiguousarray(np.broadcast_to(np.asarray(inp["positions"][c], dtype=np.int32), (128, SEQ)))
        for l in range(depth):
            m.update(per_layer[l])
            for nm, key, K in (("w_in", "w_in", 4096), ("w_out", "w_out", 4096), ("wq", "cross_wq", 4096), ("wkv", "cross_wkv", 4096),
                               ("wo", "cross_wo", 4096), ("glu", "s5_glu_w", 1024)):
                Ks = K // 4
                m["%s_%d" % (nm, l)] = f32(inp[key][l][c * Ks:(c + 1) * Ks])
            for nm, key, K in (("wg", "expert_w_gate", 4096), ("wu", "expert_w_up", 4096), ("wd", "expert_w_down", 1024)):
                Ks = K // 4
                a = np.asarray(inp[key][l])[:, c * Ks:(c + 1) * Ks, :]
                m["%s_%d" % (nm, l)] = f32(a.reshape(16 * Ks, a.shape[2]))
        maps.append(m)
    return maps


def kernel(**inp):
    depth = 2
    if "full" not in _PROG:
        C = build_full(depth)
        C.S.finish(C.out_evs)
        _PROG["full"] = C
    C = _PROG["full"]
    maps = make_maps(inp, depth)
    res = run_bass_kernel_spmd(C.nc, maps, core_ids=list(range(NC4)))
    full = np.zeros((4, SEQ, D), np.float32)
    for c in range(NC4):
        full[c] = np.asarray(res.results[c]["out"])
    return full
```

```python
import numpy as np
import ml_dtypes
import concourse.bass as bass
import concourse.mybir as mybir
from concourse.bass_utils import run_bass_kernel_spmd

F32 = mybir.dt.float32
BF16 = mybir.dt.bfloat16
I32 = mybir.dt.int32
U32 = mybir.dt.uint32
AF = mybir.ActivationFunctionType
ALU = mybir.AluOpType
AX = mybir.AxisListType

D = 4096
NCORES = 8
EPS = 1e-6


class Buf:
    __slots__ = ("name", "w", "r", "dsem", "dcnt", "ws")

    def __init__(self, name):
        self.name = name
        self.ws = {}
        self.w = None
        self.r = {}
        self.dsem = None
        self.dcnt = 0


class Sched:
    def __init__(self, nc):
        self.nc = nc
        self.eng = {"pe": nc.tensor, "dve": nc.vector, "act": nc.scalar,
                    "pool": nc.gpsimd, "sp": nc.sync}
        self.sem = {k: nc.alloc_semaphore("s_" + k) for k in self.eng}
        self.cnt = {k: 0 for k in self.eng}
        self.seen = {k: {} for k in self.eng}
        self.nbuf = 0
        self.ndsem = 0
        self.final = []

    def buf(self, name=None):
        self.nbuf += 1
        return Buf(name or ("b%d" % self.nbuf))

    def bufs(self, n, name="b"):
        return [self.buf("%s%d" % (name, i)) for i in range(n)]

    def _waits(self, e, reads, writes):
        need = {}

        def add(ev):
            if ev is None:
                return
            k = ev[0]
            if k not in need or need[k][2] < ev[2]:
                need[k] = ev

        for b in reads:
            add(b.w)
            for ev in b.ws.values():
                add(ev)
        for b in writes:
            add(b.w)
            for ev in b.ws.values():
                add(ev)
            for ev in b.r.values():
                add(ev)
        eng = self.eng[e]
        seen = self.seen[e]
        for k, ev in need.items():
            if e == "pe" and k == "pe":
                continue
            if seen.get(k, 0) >= ev[2]:
                continue
            eng.wait_ge(ev[1], ev[2])
            seen[k] = ev[2]

    def _record(self, ev, reads, writes):
        for b in reads:
            b.r[ev[0]] = ev
        for b in writes:
            b.w = ev
            b.ws[ev[0]] = ev
            b.r = {}

    def op(self, e, fn, reads=(), writes=()):
        self._waits(e, reads, writes)
        ins = fn(self.eng[e])
        self.cnt[e] += 1
        ev = (e, self.sem[e], self.cnt[e])
        ins.then_inc(self.sem[e], 1)
        if e != "pe":
            pass
        self._record(ev, reads, writes)
        return ev

    def dma(self, e, fn, sb, reads=(), writes=()):
        self._waits(e, reads, writes)
        if sb.dsem is None:
            sb.dsem = self.nc.alloc_semaphore("d_%s" % sb.name)
            self.ndsem += 1
        ins = fn(self.eng[e])
        sb.dcnt += 16
        ev = ("d_" + sb.name, sb.dsem, sb.dcnt)
        ins.then_inc(sb.dsem, 16)
        self._record(ev, reads, writes)
        return ev

    def finish(self, evs, e="sp"):
        eng = self.eng[e]
        best = {}
        for ev in evs:
            if ev[0] not in best or best[ev[0]][2] < ev[2]:
                best[ev[0]] = ev
        for ev in best.values():
            eng.wait_ge(ev[1], ev[2])


def _host_consts():
    c = {}
    c["ident_bf"] = np.eye(128, dtype=np.float32).astype(ml_dtypes.bfloat16)
    c["ident_f"] = np.eye(128, dtype=np.float32)
    c["ones_bf"] = np.ones((128, 128), np.float32).astype(ml_dtypes.bfloat16)
    return c


class Ctx:
    def __init__(self):
        self.nc = bass.Bass("TRN2", target_bir_lowering=False)
        self.S = Sched(self.nc)
        self.ins = {}
        self.outs = []
        self.out_evs = []
        nc, S = self.nc, self.S
        self.ident_bf = nc.alloc_sbuf_tensor("sb_ident_bf", [128, 128], BF16)
        self.ident_f = nc.alloc_sbuf_tensor("sb_ident_f", [128, 128], F32)
        self.ones_bf = nc.alloc_sbuf_tensor("sb_ones_bf", [128, 128], BF16)
        self.eps_t = nc.alloc_sbuf_tensor("eps_t", [128, 1], F32)
        self.b_const = S.buf("consts")
        for nm, t, dt in (("ident_bf", self.ident_bf, BF16), ("ident_f", self.ident_f, F32),
                          ("ones_bf", self.ones_bf, BF16)):
            d = self.inp(nm, [128, 128], dt)
            S.dma("sp", lambda q, t=t, d=d: q.dma_start(out=t[:], in_=d), self.b_const,
                  writes=[self.b_const])
        S.op("dve", lambda v: v.memset(self.eps_t[:], EPS), writes=[self.b_const])
        self.halfpi_t = nc.alloc_sbuf_tensor("halfpi_t", [128, 1], F32)
        S.op("dve", lambda v: v.memset(self.halfpi_t[:], 1.5707963267948966), writes=[self.b_const])

    def inp(self, name, shape, dt):
        t = self.nc.dram_tensor(name, list(shape), dt, kind="ExternalInput")
        self.ins[name] = t
        return t.ap()

    def out(self, name, shape, dt):
        t = self.nc.dram_tensor(name, list(shape), dt, kind="ExternalOutput")
        self.outs.append(name)
        return t.ap()

    def scratch(self, name, shape, dt):
        return self.nc.dram_tensor(name, list(shape), dt, kind="Internal").ap()

    def run(self, in_maps):
        self.S.finish(self.out_evs)
        cst = _host_consts()
        maps = []
        for m in in_maps:
            mm = dict(cst)
            mm.update(m)
            maps.append(mm)
        res = run_bass_kernel_spmd(self.nc, maps, core_ids=list(range(len(maps))))
        return res.results


class Pool:
    def __init__(self, C, name, n, shape, dt, psum=False):
        self.tiles = []
        for i in range(n):
            nm = "%s%d" % (name, i)
            if psum:
                t = C.nc.alloc_psum_tensor(nm, list(shape), dt)
            else:
                t = C.nc.alloc_sbuf_tensor(nm, list(shape), dt)
            self.tiles.append((t, C.S.buf(nm)))
        self.i = 0

    def next(self):
        t = self.tiles[self.i % len(self.tiles)]
        self.i += 1
        return t


def rmsnorm_tile(C, xt, bx, wb, bw, hb, bh, P, out_f32=None):
    S = C.S
    ss, rs, junk = P["ss"], P["rs"], P["junk"]
    bs = P["b"]
    S.op("act", lambda a: a.activation(out=junk[:], in_=xt[:], func=AF.Square, accum_out=ss[:]),
         reads=[bx], writes=[bs])
    S.op("act", lambda a: a.activation(out=rs[:], in_=ss[:], func=AF.Sqrt, bias=C.eps_t[:], scale=1.0 / D),
         reads=[bs, C.b_const], writes=[bs])
    S.op("dve", lambda v: v.reciprocal(out=rs[:], in_=rs[:]), reads=[bs], writes=[bs])
    S.op("dve", lambda v: v.scalar_tensor_tensor(out=hb[:], in0=xt[:], scalar=rs[:, 0:1], in1=wb[:],
                                                 op0=ALU.mult, op1=ALU.mult),
         reads=[bx, bs, bw], writes=[bh])


def transpose_to(C, src, bsrc, dst_fn, bdst, nk, pst_pool, dt=BF16, evac=("act", "dve")):
    S = C.S
    ident = C.ident_bf if dt == BF16 else C.ident_f
    per = 8 if dt == BF16 else 4
    gi = 0
    for k0 in range(0, nk, per):
        n = min(per, nk - k0)
        pt, bpt = pst_pool.next()
        for j in range(n):
            S.op("pe", lambda p, j=j, pt=pt: p.transpose(out=pt[:, j, :], in_=src[:, (k0 + j) * 128:(k0 + j + 1) * 128],
                                                          identity=ident[:]),
                 reads=[bsrc, C.b_const], writes=[bpt])
        e = evac[gi % len(evac)]
        gi += 1
        dst = dst_fn(k0, n)
        if e == "act":
            S.op("act", lambda a, pt=pt, dst=dst, n=n: a.copy(out=dst, in_=pt[:, 0:n, :]), reads=[bpt], writes=[bdst])
        else:
            S.op("dve", lambda v, pt=pt, dst=dst, n=n: v.tensor_copy(out=dst, in_=pt[:, 0:n, :]), reads=[bpt], writes=[bdst])


def load_w_cast(C, wt, bw, src_ap):
    C.S.dma("pool", lambda q: q.dma_start(out=wt, in_=src_ap), bw, writes=[bw])


TB = 1024


def norm_block_to_hT(C, x_ap, t0, ntile, wb, bwb, hT, bhT, pools, also_tok=None):
    S = C.S
    for i in range(ntile):
        xt, bx = pools["x"].next()
        r0 = t0 + i * 128
        S.dma("sp", lambda q, xt=xt, r0=r0: q.dma_start(out=xt[:], in_=x_ap[r0:r0 + 128, :]), bx, writes=[bx])
        hb, bh = pools["h"].next()
        rmsnorm_tile(C, xt, bx, wb, bwb, hb, bh, pools["small"])
        if also_tok is not None:
            also_tok(i, hb, bh)
        transpose_to(C, hb, bh, lambda k0, n, i=i: hT[:, k0:k0 + n, i * 128:(i + 1) * 128], bhT, 32, pools["pst"])


def make_norm_pools(C, tag=""):
    nc = C.nc
    pools = {
        "x": Pool(C, "xt" + tag, 2, [128, D], F32),
        "h": Pool(C, "hb" + tag, 2, [128, D], BF16),
        "pst": Pool(C, "pst" + tag, 2, [128, 8, 128], BF16, psum=True),
    }
    pools["small"] = {
        "ss": nc.alloc_sbuf_tensor("ss" + tag, [128, 1], F32),
        "rs": nc.alloc_sbuf_tensor("rs" + tag, [128, 1], F32),
        "junk": nc.alloc_sbuf_tensor("junk" + tag, [128, D], BF16),
        "b": C.S.buf("small" + tag),
    }
    return pools


def gemm_fm(C, hT, bhT, nk, ntok, w_ap, m0, nm, out_cb, wpool, pspool):
    S = C.S
    wv = w_ap.rearrange("(kc p) m -> p kc m", p=128)
    for mi in range(nm):
        m = m0 + mi
        wt, bw = wpool.next()
        load_w_cast(C, wt[:, 0:nk, :], bw, wv[:, :, m * 128:(m + 1) * 128])
        for ts in range(ntok // 512):
            ps, bps = pspool.next()
            for k in range(nk):
                S.op("pe", lambda p, k=k, ps=ps, wt=wt, ts=ts: p.matmul(
                    ps[:], lhsT=wt[:, k, :], rhs=hT[:, k, ts * 512:(ts + 1) * 512],
                    start=(k == 0), stop=(k == nk - 1)),
                    reads=[bw, bhT], writes=[bps])
            out_cb(mi, ts, ps, bps)


def build_k1(l):
    C = Ctx()
    nc, S = C.nc, C.S
    Tc = 2048
    x = C.inp("x", [Tc, D], F32)
    nw = C.inp("nw", [128, D], F32)
    w_in = C.inp("w_in", [D, 10240], F32)
    projT = C.out("projT", [10240, Tc], BF16)
    wb = nc.alloc_sbuf_tensor("wb", [128, D], F32)
    bwb = S.buf("wb")
    S.dma("sp", lambda q: q.dma_start(out=wb[:], in_=nw), bwb, writes=[bwb])
    pools = make_norm_pools(C)
    hT = nc.alloc_sbuf_tensor("hT", [128, 32, TB], BF16)
    bhT = S.buf("hT")
    wpool = Pool(C, "w", 3, [128, 32, 128], BF16)
    pspool = Pool(C, "ps", 4, [128, 512], F32, psum=True)
    opool = Pool(C, "ot", 4, [128, 512], BF16)
    bproj = S.buf("projT")
    evs = []
    cnt = [0]
    for tb in range(Tc // TB):
        norm_block_to_hT(C, x, tb * TB, TB // 128, wb, bwb, hT, bhT, pools)

        def out_cb(mi, ts, ps, bps, tb=tb):
            ot, bo = opool.next()
            e = ("act", "dve")[cnt[0] % 2]
            cnt[0] += 1
            if e == "act":
                S.op("act", lambda a: a.copy(out=ot[:], in_=ps[:]), reads=[bps], writes=[bo])
            else:
                S.op("dve", lambda v: v.tensor_copy(out=ot[:], in_=ps[:]), reads=[bps], writes=[bo])
            c0 = tb * TB + ts * 512
            evs.append(S.dma("sp", lambda q: q.dma_start(out=projT[mi * 128:(mi + 1) * 128, c0:c0 + 512], in_=ot[:]),
                             bo, reads=[bo]))

        gemm_fm(C, hT, bhT, 32, TB, w_in, 0, 80, out_cb, wpool, pspool)
    C.out_evs += evs[-8:] + evs
    return C


_UID = [0]


class Phase:
    def __init__(self, C, name):
        self.C = C
        self.name = name
        self.cms = []
        self.n = 0

    def sb(self, shape, dt, nm=None):
        self.n += 1
        _UID[0] += 1
        cm = self.C.nc.sbuf_tensor("%s_%s%d_%d" % (self.name, nm or "t", self.n, _UID[0]), list(shape), dt)
        t = cm.__enter__()
        self.cms.append(cm)
        return t

    def ps(self, shape, dt, nm=None):
        self.n += 1
        _UID[0] += 1
        cm = self.C.nc.psum_tensor("%s_%s%d_%d" % (self.name, nm or "p", self.n, _UID[0]), list(shape), dt)
        t = cm.__enter__()
        self.cms.append(cm)
        return t

    def pool(self, nm, n, shape, dt, psum=False):
        p = Pool.__new__(Pool)
        p.tiles = []
        p.i = 0
        for i in range(n):
            t = self.ps(shape, dt, nm) if psum else self.sb(shape, dt, nm)
            p.tiles.append((t, self.C.S.buf("%s_%s%d" % (self.name, nm, i))))
        return p

    def close(self):
        self.C.S.barrier()
        for cm in reversed(self.cms):
            cm.__exit__(None, None, None)
        self.cms = []


def _sched_barrier(self):
    evs = [(k, self.sem[k], self.cnt[k]) for k in self.eng if self.cnt[k] > 0]
    evs += list(self.all_dma.values())
    for e, eng in self.eng.items():
        seen = self.seen[e]
        for ev in evs:
            if ev[0] == e:
                continue
            if seen.get(ev[0], 0) >= ev[2]:
                continue
            eng.wait_ge(ev[1], ev[2])
            seen[ev[0]] = ev[2]


Sched.barrier = _sched_barrier
_old_dma = Sched.dma


def _dma_track(self, e, fn, sb, reads=(), writes=()):
    ev = _old_dma(self, e, fn, sb, reads, writes)
    if not hasattr(self, "all_dma"):
        self.all_dma = {}
    self.all_dma[ev[0]] = ev
    return ev


Sched.dma = _dma_track
_old_init = Sched.__init__


def _init2(self, nc):
    _old_init(self, nc)
    self.all_dma = {}


Sched.__init__ = _init2


class _DSem:
    __slots__ = ("sem", "cnt", "key")


def _dma_v2(self, e, fn, sb, reads=(), writes=()):
    self._waits(e, reads, writes)
    if sb.dsem is None:
        if self.free_dsems:
            sb.dsem = self.free_dsems.pop()
        else:
            d = _DSem()
            d.key = "d%d" % self.ndsem
            d.sem = self.nc.alloc_semaphore("dsem%d" % self.ndsem)
            d.cnt = 0
            self.ndsem += 1
            sb.dsem = d
        self.bound.append(sb)
    d = sb.dsem
    ins = fn(self.eng[e])
    d.cnt += 16
    ev = (d.key, d.sem, d.cnt)
    ins.then_inc(d.sem, 16)
    self._record(ev, reads, writes)
    self.all_dma[d.key] = ev
    return ev


def _barrier_v2(self):
    _sched_barrier(self)
    for b in self.bound:
        self.free_dsems.append(b.dsem)
        b.dsem = None
    self.bound = []


def _init3(self, nc):
    _old_init(self, nc)
    self.all_dma = {}
    self.free_dsems = []
    self.bound = []


Sched.dma = _dma_v2
Sched.barrier = _barrier_v2
Sched.__init__ = _init3


import math

TWO_PI = 2.0 * math.pi
CW1 = 6.28125
CW2 = TWO_PI - CW1


def _mixer_consts():
    c = {}
    fa = 500000.0 ** (-(np.arange(16, dtype=np.float32) * 2.0 / 32.0))
    fcol = np.zeros((128, 1), np.float32)
    fcol[0:16, 0] = fa
    fcol[16:32, 0] = fa
    c["freq_a"] = fcol
    pa = np.zeros((128, 128), np.float32)
    for d in range(16):
        pa[d + 16, d] = -1.0
        pa[d, d + 16] = 1.0
    c["pmat_a"] = pa.astype(ml_dtypes.bfloat16)
    fb = 10000.0 ** (-np.linspace(0.0, 1.0, 64, dtype=np.float32))
    fcolb = np.concatenate([fb, fb]).reshape(128, 1).astype(np.float32)
    c["freq_b"] = fcolb
    pb = np.zeros((128, 128), np.float32)
    for d in range(64):
        pb[d + 64, d] = -1.0
        pb[d, d + 64] = 1.0
    c["pmat_b"] = pb.astype(ml_dtypes.bfloat16)
    a = np.arange(128)[:, None]
    b = np.arange(128)[None, :]
    m = np.stack([(a >= b), (a <= b), (a >= b) & (a >= 64), (a <= b) & (a < 64)]).astype(np.float32)
    c["amask"] = np.ascontiguousarray(m.transpose(1, 0, 2)).astype(ml_dtypes.bfloat16)
    return c


def load_const(C, ph, name, shape, dt, b):
    d = C.inp(name, shape, dt) if name not in C.ins else C.ins[name].ap()
    t = ph.sb(shape, dt, name)
    C.S.dma("sp", lambda q: q.dma_start(out=t[:], in_=d), b, writes=[b])
    return t


def make_trig_tables(C, ph, posb_ap, freq_t, bfreq, nrows, T, cos_t, sin_t, btab, offset=0.0):
    S = C.S
    CH = 2048
    pi_t = ph.sb([nrows, CH], I32, "posi")
    ang = ph.sb([nrows, CH], F32, "ang")
    qi = ph.sb([nrows, CH], I32, "qi")
    qf = ph.sb([nrows, CH], F32, "qf")
    rr = ph.sb([nrows, CH], F32, "rr")
    b = S.buf("trig_tmp")
    bp = S.buf("trig_pos")
    for c0 in range(0, T, CH):
        S.dma("sp", lambda q: q.dma_start(out=pi_t[:], in_=posb_ap[0:nrows, c0:c0 + CH]), bp, writes=[bp])
        S.op("dve", lambda v: v.tensor_copy(out=ang[:], in_=pi_t[:]), reads=[bp], writes=[b])
        S.op("dve", lambda v: v.tensor_scalar(out=ang[:], in0=ang[:], scalar1=freq_t[0:nrows, 0:1], scalar2=offset,
                                              op0=ALU.mult, op1=ALU.add), reads=[b, bfreq], writes=[b])
        S.op("dve", lambda v: v.tensor_scalar(out=qi[:], in0=ang[:], scalar1=1.0 / TWO_PI, scalar2=None,
                                              op0=ALU.mult), reads=[b], writes=[b])
        S.op("dve", lambda v: v.tensor_copy(out=qf[:], in_=qi[:]), reads=[b], writes=[b])
        S.op("dve", lambda v: v.scalar_tensor_tensor(out=rr[:], in0=qf[:], scalar=-CW1, in1=ang[:],
                                                     op0=ALU.mult, op1=ALU.add), reads=[b], writes=[b])
        S.op("dve", lambda v: v.scalar_tensor_tensor(out=rr[:], in0=qf[:], scalar=-CW2, in1=rr[:],
                                                     op0=ALU.mult, op1=ALU.add), reads=[b], writes=[b])
        S.op("dve", lambda v: v.tensor_scalar(out=qf[:], in0=rr[:], scalar1=math.pi, scalar2=None, op0=ALU.is_gt),
             reads=[b], writes=[b])
        S.op("dve", lambda v: v.scalar_tensor_tensor(out=ang[:], in0=qf[:], scalar=-TWO_PI, in1=rr[:],
                                                     op0=ALU.mult, op1=ALU.add), reads=[b], writes=[b])
        S.op("act", lambda a: a.activation(out=sin_t[:, c0:c0 + CH], in_=ang[:], func=AF.Sin), reads=[b], writes=[btab])
        S.op("dve", lambda v: v.tensor_scalar(out=qf[:], in0=rr[:], scalar1=math.pi / 2, scalar2=None, op0=ALU.is_gt),
             reads=[b], writes=[b])
        S.op("dve", lambda v: v.scalar_tensor_tensor(out=ang[:], in0=qf[:], scalar=-TWO_PI, in1=rr[:],
                                                     op0=ALU.mult, op1=ALU.add), reads=[b, btab], writes=[b])
        S.op("act", lambda a: a.activation(out=cos_t[:, c0:c0 + CH], in_=ang[:], func=AF.Sin, bias=C.halfpi_t[0:nrows, :]),
             reads=[b, C.b_const], writes=[btab])


def rope_inplace(C, ph, xt, bx, nrows, T, pmat, bconst, cos_t, sin_t, btab, pools):
    S = C.S
    for c0 in range(0, T, 512):
        pp, bpp = pools["rp"].next()
        S.op("pe", lambda p: p.matmul(pp[0:nrows, :], lhsT=pmat[0:nrows, 0:nrows], rhs=xt[0:nrows, c0:c0 + 512],
                                      start=True, stop=True), reads=[bx, bconst], writes=[bpp])
        t1, bt1 = pools["rt"].next()
        t2, bt2 = pools["rt"].next()
        S.op("dve", lambda v: v.tensor_tensor(out=t1[0:nrows, :], in0=pp[0:nrows, :], in1=sin_t[0:nrows, c0:c0 + 512],
                                              op=ALU.mult), reads=[bpp, btab], writes=[bt1])
        S.op("pool", lambda g: g.tensor_tensor(out=t2[0:nrows, :], in0=xt[0:nrows, c0:c0 + 512],
                                               in1=cos_t[0:nrows, c0:c0 + 512], op=ALU.mult),
             reads=[bx, btab], writes=[bt2])
        S.op("dve", lambda v: v.tensor_tensor(out=xt[0:nrows, c0:c0 + 512], in0=t1[0:nrows, :], in1=t2[0:nrows, :],
                                              op=ALU.add), reads=[bt1, bt2], writes=[bx])


SEQ = 4096
PADA = 1024


def mixer_a_head(C, ph, ld, out_ap, T, W):
    S = C.S
    qt, bq = W["q"]
    kp, bk = W["kp"]
    vp, bv = W["vp"]
    ld("q", qt, bq, 0)
    ld("k", kp, bk, PADA)
    ld("v", vp, bv, PADA)
    rope_inplace(C, ph, qt, bq, 32, T, W["pmat"], W["bconst"], W["cos"], W["sin"], W["btab"], W)
    rope_inplace(C, ph, kp[:, PADA:PADA + T], bk, 32, T, W["pmat"], W["bconst"], W["cos"], W["sin"], W["btab"], W)
    num, bnum = W["num"]
    den, bden = W["den"]
    amask = W["amask"]
    scale = 128.0 ** -0.5
    first = True
    for d in (1, 4, 16):
        sub = T // d
        nb = sub // 128
        for r in range(d):
            vprev = None
            for j in range(nb):
                po, bpo = W["po"].next()
                pd, bpd = W["pd"].next()
                for side in (0, 1):
                    K0 = 128 * j - 64 + 128 * side
                    c_lo = PADA + r + d * K0
                    ksl = kp[:, c_lo:c_lo + d * 127 + 1:d]
                    vsl = vp[:, c_lo:c_lo + d * 127 + 1:d]
                    q_lo = r + d * 128 * j
                    qsl = qt[:, q_lo:q_lo + d * 127 + 1:d]
                    if side == 0 and vprev is not None:
                        vb, bvb = vprev
                    else:
                        pv, bpv = W["pv"].next()
                        S.op("pe", lambda p: p.transpose(out=pv[:], in_=vsl, identity=C.ident_bf[:]),
                             reads=[bv, C.b_const], writes=[bpv])
                        vb, bvb = W["vb"].next()
                        S.op("dve", lambda v: v.tensor_copy(out=vb[:], in_=pv[:]), reads=[bpv], writes=[bvb])
                    if side == 1:
                        vprev = (vb, bvb)
                    pss, bps = W["pss"].next()
                    S.op("pe", lambda p: p.matmul(pss[:], lhsT=ksl, rhs=qsl, start=True, stop=True),
                         reads=[bk, bq], writes=[bps])
                    pt, bpt = W["pt"].next()
                    S.op("act", lambda a: a.activation(out=pt[:], in_=pss[:], func=AF.Exp, scale=scale),
                         reads=[bps], writes=[bpt])
                    mi = side
                    if j == 0 and side == 0:
                        mi = 2
                    if j == nb - 1 and side == 1:
                        mi = 3
                    S.op("pool", lambda g: g.tensor_tensor(out=pt[:], in0=pt[:], in1=amask[:, mi, :], op=ALU.mult),
                         reads=[bpt, W["bconst"]], writes=[bpt])
                    S.op("pe", lambda p: p.matmul(po[:], lhsT=vb[:], rhs=pt[:], start=(side == 0), stop=(side == 1)),
                         reads=[bvb, bpt], writes=[bpo])
                    S.op("pe", lambda p: p.matmul(pd[:], lhsT=C.ones_bf[:], rhs=pt[:], start=(side == 0), stop=(side == 1)),
                         reads=[C.b_const, bpt], writes=[bpd])
                q_lo = r + d * 128 * j
                nsl = num[:, q_lo:q_lo + d * 127 + 1:d]
                dsl = den[:, q_lo:q_lo + d * 127 + 1:d]
                if first:
                    S.op("act", lambda a: a.copy(out=nsl, in_=po[:]), reads=[bpo], writes=[bnum])
                    S.op("dve", lambda v: v.tensor_copy(out=dsl, in_=pd[:]), reads=[bpd], writes=[bden])
                else:
                    S.op("dve", lambda v: v.tensor_tensor(out=nsl, in0=po[:], in1=nsl, op=ALU.add),
                         reads=[bpo, bnum], writes=[bnum])
                    S.op("dve", lambda v: v.tensor_tensor(out=dsl, in0=pd[:], in1=dsl, op=ALU.add),
                         reads=[bpd, bden], writes=[bden])
        first = False
    ot, bo = W["ao"]
    for c0 in range(0, T, 1024):
        S.op("dve", lambda v: v.reciprocal(out=den[:, c0:c0 + 1024], in_=den[:, c0:c0 + 1024]), reads=[bden], writes=[bden])
        S.op("dve", lambda v: v.tensor_tensor(out=ot[:, c0:c0 + 1024], in0=num[:, c0:c0 + 1024],
                                              in1=den[:, c0:c0 + 1024], op=ALU.mult),
             reads=[bnum, bden], writes=[bo])
    return S.dma("sp", lambda q: q.dma_start(out=out_ap, in_=ot[:]), bo, reads=[bo])


def mixer_a_setup(C, ph, posb_ap, T):
    S = C.S
    W = {}
    bconst = S.buf("a_const")
    W["bconst"] = bconst
    W["pmat"] = load_const(C, ph, "pmat_a", [128, 128], BF16, bconst)
    W["amask"] = load_const(C, ph, "amask", [128, 4, 128], BF16, bconst)
    freq = load_const(C, ph, "freq_a", [128, 1], F32, bconst)
    W["cos"] = ph.sb([32, T], F32, "cos")
    W["sin"] = ph.sb([32, T], F32, "sin")
    W["btab"] = S.buf("a_tab")
    tp = Phase(C, ph.name + "_trig")
    make_trig_tables(C, tp, posb_ap, freq, bconst, 32, T, W["cos"], W["sin"], W["btab"])
    tp.close()
    W["q"] = (ph.sb([128, T], BF16, "q"), S.buf("a_q"))
    W["kp"] = (ph.sb([128, T + 2 * PADA], BF16, "kp"), S.buf("a_kp"))
    W["vp"] = (ph.sb([128, T + 2 * PADA], BF16, "vp"), S.buf("a_vp"))
    for nm in ("kp", "vp"):
        t, b = W[nm]
        S.op("pool", lambda g: g.memset(t[:, 0:PADA], 0.0), writes=[b])
        S.op("pool", lambda g: g.memset(t[:, PADA + T:], 0.0), writes=[b])
    W["num"] = (ph.sb([128, T], F32, "num"), S.buf("a_num"))
    W["den"] = (ph.sb([128, T], F32, "den"), S.buf("a_den"))
    W["ao"] = (ph.sb([128, T], BF16, "ao"), S.buf("a_ao"))
    W["rp"] = ph.pool("rp", 1, [128, 512], F32, psum=True)
    W["rt"] = ph.pool("rt", 4, [128, 512], F32)
    W["po"] = ph.pool("po", 2, [128, 128], F32, psum=True)
    W["pd"] = ph.pool("pd", 2, [128, 128], F32, psum=True)
    W["pv"] = ph.pool("pv", 1, [128, 128], BF16, psum=True)
    W["pss"] = ph.pool("pss", 2, [128, 128], F32, psum=True)
    W["vb"] = ph.pool("vb", 4, [128, 128], BF16)
    W["pt"] = ph.pool("pt", 3, [128, 128], BF16)
    return W


def _retention_consts():
    c = {}
    m = np.arange(128, dtype=np.float32)[:, None]
    cc = np.arange(128, dtype=np.float32)[None, :]
    diff = cc - m
    c["ret_dpos"] = np.maximum(diff, 0.0)
    c["ret_dneg"] = np.maximum(-diff, 0.0)
    c["ret_mge"] = (diff >= 0).astype(np.float32)
    c["ret_mlt"] = (diff < 0).astype(np.float32)
    c["ret_cp1"] = np.broadcast_to(cc + 1.0, (128, 128)).copy()
    c["ret_128mc"] = np.broadcast_to(128.0 - cc, (128, 128)).copy()
    col = np.zeros((128, 4), np.float32)
    col[:, 0] = 127.0 - m[:, 0]
    col[:, 1] = m[:, 0]
    col[:, 2] = 128.0
    c["ret_cols"] = col
    c["ones_f"] = np.ones((128, 128), np.float32)
    return c


def mixer_b_setup(C, ph, posb_ap, T):
    S = C.S
    W = {}
    bconst = S.buf("b_const")
    W["bconst"] = bconst
    W["pmat"] = load_const(C, ph, "pmat_b", [128, 128], BF16, bconst)
    for nm in ("ret_dpos", "ret_dneg", "ret_mge", "ret_mlt", "ret_cp1", "ret_128mc", "ones_f"):
        W[nm] = load_const(C, ph, nm, [128, 128], F32, bconst)
    W["ret_cols"] = load_const(C, ph, "ret_cols", [128, 4], F32, bconst)
    freq = load_const(C, ph, "freq_b", [128, 1], F32, bconst)
    W["cos"] = ph.sb([128, T], F32, "cos")
    W["sin"] = ph.sb([128, T], F32, "sin")
    W["btab"] = S.buf("b_tab")
    tp = Phase(C, ph.name + "_trig")
    make_trig_tables(C, tp, posb_ap, freq, bconst, 128, T, W["cos"], W["sin"], W["btab"])
    tp.close()
    return W


def mixer_b_head(C, ph0, ld, decay_t, bdec, hidx, gnw_t, bgn, out_ap, T, W):
    S = C.S
    ph = Phase(C, ph0.name + "_h")
    pp = Phase(C, ph0.name + "_pa")
    nch = T // 128
    sc = 128.0 ** -0.5
    bc = W["bconst"]
    sm = ph.sb([128, 16], F32, "sm")
    dm = ph.sb([128, 128], F32, "dm")
    tmp = ph.sb([128, 128], F32, "tmp")
    qdf = ph.sb([128, 128], F32, "qdf")
    qdb = ph.sb([128, 128], F32, "qdb")
    rT, brT = ph.sb([128, 2, T], F32, "rT"), S.buf("b_rT")
    qt, bq = pp.sb([128, T], BF16, "q"), S.buf("b_q")
    kt, bk = pp.sb([128, T], BF16, "k"), S.buf("b_k")
    vt, bv = pp.sb([128, 2, T], BF16, "v"), S.buf("b_v")
    ld("q", qt[:, :], bq)
    ld("k", kt[:, :], bk)
    ld("v0", vt[:, 0, :], bv)
    ld("v1", vt[:, 1, :], bv)
    rpools = {"rp": pp.pool("rp", 1, [128, 512], F32, psum=True), "rt": pp.pool("rt", 4, [128, 512], F32)}
    rope_inplace(C, ph, qt, bq, 128, T, W["pmat"], bc, W["cos"], W["sin"], W["btab"], rpools)
    rope_inplace(C, ph, kt, bk, 128, T, W["pmat"], bc, W["cos"], W["sin"], W["btab"], rpools)
    bsm = S.buf("b_sm")
    nh2 = decay_t.shape[1] // 2
    for dr in (0, 1):
        col = dr * nh2 + hidx
        S.op("act", lambda a: a.activation(out=sm[:, dr:dr + 1], in_=decay_t[:, col:col + 1], func=AF.Exp, scale=-1.0),
             reads=[bdec], writes=[bsm])
        S.op("dve", lambda v: v.tensor_scalar(out=sm[:, dr:dr + 1], in0=sm[:, dr:dr + 1], scalar1=1.0, scalar2=None,
                                              op0=ALU.add), reads=[bsm], writes=[bsm])
        S.op("act", lambda a: a.activation(out=sm[:, dr:dr + 1], in_=sm[:, dr:dr + 1], func=AF.Ln), reads=[bsm], writes=[bsm])
        S.op("dve", lambda v: v.tensor_scalar(out=sm[:, dr:dr + 1], in0=sm[:, dr:dr + 1], scalar1=-1.0, scalar2=None,
                                              op0=ALU.mult), reads=[bsm], writes=[bsm])
    lgf, lgb = sm[:, 0:1], sm[:, 1:2]
    cols = W["ret_cols"]
    S.op("act", lambda a: a.activation(out=sm[:, 2:3], in_=cols[:, 0:1], func=AF.Exp, scale=lgf), reads=[bsm, bc], writes=[bsm])
    S.op("act", lambda a: a.activation(out=sm[:, 3:4], in_=cols[:, 1:2], func=AF.Exp, scale=lgb), reads=[bsm, bc], writes=[bsm])
    S.op("act", lambda a: a.activation(out=sm[:, 4:5], in_=cols[:, 2:3], func=AF.Exp, scale=lgf), reads=[bsm, bc], writes=[bsm])
    S.op("act", lambda a: a.activation(out=sm[:, 5:6], in_=cols[:, 2:3], func=AF.Exp, scale=lgb), reads=[bsm, bc], writes=[bsm])
    bdm = S.buf("b_dm")
    S.op("act", lambda a: a.activation(out=dm[:], in_=W["ret_dpos"][:], func=AF.Exp, scale=lgf), reads=[bsm, bc], writes=[bdm])
    S.op("dve", lambda v: v.scalar_tensor_tensor(out=dm[:], in0=dm[:], scalar=sc, in1=W["ret_mge"][:], op0=ALU.mult, op1=ALU.mult),
         reads=[bdm, bc], writes=[bdm])
    S.op("act", lambda a: a.activation(out=tmp[:], in_=W["ret_dneg"][:], func=AF.Exp, scale=lgb), reads=[bsm, bc], writes=[bdm])
    S.op("dve", lambda v: v.scalar_tensor_tensor(out=tmp[:], in0=tmp[:], scalar=sc, in1=W["ret_mlt"][:], op0=ALU.mult, op1=ALU.mult),
         reads=[bdm, bc], writes=[bdm])
    S.op("dve", lambda v: v.tensor_tensor(out=dm[:], in0=dm[:], in1=tmp[:], op=ALU.add), reads=[bdm], writes=[bdm])
    S.op("act", lambda a: a.activation(out=qdf[:], in_=W["ret_cp1"][:], func=AF.Exp, scale=lgf), reads=[bsm, bc], writes=[bdm])
    S.op("act", lambda a: a.activation(out=qdb[:], in_=W["ret_128mc"][:], func=AF.Exp, scale=lgb), reads=[bsm, bc], writes=[bdm])
    S.op("dve", lambda v: v.tensor_scalar(out=qdf[:], in0=qdf[:], scalar1=sc, scalar2=None, op0=ALU.mult), reads=[bdm], writes=[bdm])
    S.op("dve", lambda v: v.tensor_scalar(out=qdb[:], in0=qdb[:], scalar1=sc, scalar2=None, op0=ALU.mult), reads=[bdm], writes=[bdm])
    qf, bqf = pp.sb([128, T], BF16, "qf"), S.buf("b_qf")
    qb, bqb = pp.sb([128, T], BF16, "qb"), S.buf("b_qb")
    for n in range(nch):
        sl = slice(n * 128, (n + 1) * 128)
        S.op("dve", lambda v: v.tensor_tensor(out=qf[:, sl], in0=qt[:, sl], in1=qdf[:], op=ALU.mult), reads=[bq, bdm], writes=[bqf])
        S.op("pool", lambda g: g.tensor_tensor(out=qb[:, sl], in0=qt[:, sl], in1=qdb[:], op=ALU.mult), reads=[bq, bdm], writes=[bqb])
    kf, bkf = pp.sb([128, nch, 128], BF16, "kf"), S.buf("b_kf")
    kb, bkb = pp.sb([128, nch, 128], BF16, "kb"), S.buf("b_kb")
    vtm, bvtm = pp.sb([128, nch, 256], BF16, "vtm"), S.buf("b_vtm")
    ptr = pp.pool("ptr", 1, [128, 3, 128], BF16, psum=True)
    for n in range(nch):
        sl = slice(n * 128, (n + 1) * 128)
        pt, bpt = ptr.next()
        S.op("pe", lambda p: p.transpose(out=pt[:, 0, :], in_=kt[:, sl], identity=C.ident_bf[:]), reads=[bk, C.b_const], writes=[bpt])
        S.op("pe", lambda p: p.transpose(out=pt[:, 1, :], in_=vt[:, 0, sl], identity=C.ident_bf[:]), reads=[bv, C.b_const], writes=[bpt])
        S.op("pe", lambda p: p.transpose(out=pt[:, 2, :], in_=vt[:, 1, sl], identity=C.ident_bf[:]), reads=[bv, C.b_const], writes=[bpt])
        S.op("act", lambda a: a.activation(out=kf[:, n, :], in_=pt[:, 0, :], func=AF.Copy, scale=sm[:, 2:3]), reads=[bpt, bsm], writes=[bkf])
        S.op("act", lambda a: a.activation(out=kb[:, n, :], in_=pt[:, 0, :], func=AF.Copy, scale=sm[:, 3:4]), reads=[bpt, bsm], writes=[bkb])
        S.op("dve", lambda v: v.tensor_copy(out=vtm[:, n, :], in_=pt[:, 1:3, :]), reads=[bpt], writes=[bvtm])
    sf, bsf = pp.sb([128, nch, 256], BF16, "sf"), S.buf("b_sf")
    sbk, bsb = pp.sb([128, nch, 256], BF16, "sbk"), S.buf("b_sb")
    st, bst = pp.sb([128, 256], F32, "st"), S.buf("b_st")
    pkv = pp.pool("pkv", 1, [128, 256], F32, psum=True)
    S.op("dve", lambda v: v.memset(st[:], 0.0), writes=[bst])
    S.op("pool", lambda g: g.memset(sf[:, 0, :], 0.0), writes=[bsf])
    for n in range(1, nch):
        pk, bpk = pkv.next()
        S.op("pe", lambda p: p.matmul(pk[:], lhsT=kf[:, n - 1, :], rhs=vtm[:, n - 1, :], start=True, stop=True),
             reads=[bkf, bvtm], writes=[bpk])
        S.op("dve", lambda v: v.scalar_tensor_tensor(out=st[:], in0=st[:], scalar=sm[:, 4:5], in1=pk[:], op0=ALU.mult, op1=ALU.add),
             reads=[bst, bsm, bpk], writes=[bst])
        S.op("act", lambda a: a.copy(out=sf[:, n, :], in_=st[:]), reads=[bst], writes=[bsf])
    S.op("dve", lambda v: v.memset(st[:], 0.0), reads=[bst], writes=[bst])
    S.op("pool", lambda g: g.memset(sbk[:, nch - 1, :], 0.0), writes=[bsb])
    for n in range(nch - 2, -1, -1):
        pk, bpk = pkv.next()
        S.op("pe", lambda p: p.matmul(pk[:], lhsT=kb[:, n + 1, :], rhs=vtm[:, n + 1, :], start=True, stop=True),
             reads=[bkb, bvtm], writes=[bpk])
        S.op("dve", lambda v: v.scalar_tensor_tensor(out=st[:], in0=st[:], scalar=sm[:, 5:6], in1=pk[:], op0=ALU.mult, op1=ALU.add),
             reads=[bst, bsm, bpk], writes=[bst])
        S.op("act", lambda a: a.copy(out=sbk[:, n, :], in_=st[:]), reads=[bst], writes=[bsb])
    pss = pp.pool("pss", 2, [128, 128], F32, psum=True)
    pout = pp.pool("pout", 2, [128, 128], F32, psum=True)
    pmt = pp.pool("pmt", 3, [128, 128], BF16)
    for n in range(nch):
        sl = slice(n * 128, (n + 1) * 128)
        ps_, bps = pss.next()
        S.op("pe", lambda p: p.matmul(ps_[:], lhsT=kt[:, sl], rhs=qt[:, sl], start=True, stop=True), reads=[bk, bq], writes=[bps])
        pm, bpm = pmt.next()
        S.op("dve", lambda v: v.tensor_tensor(out=pm[:], in0=ps_[:], in1=dm[:], op=ALU.mult), reads=[bps, bdm], writes=[bpm])
        for hf in (0, 1):
            po, bpo = pout.next()
            cs = slice(hf * 128, (hf + 1) * 128)
            S.op("pe", lambda p: p.matmul(po[:], lhsT=vtm[:, n, cs], rhs=pm[:], start=True, stop=False), reads=[bvtm, bpm], writes=[bpo])
            S.op("pe", lambda p: p.matmul(po[:], lhsT=sf[:, n, cs], rhs=qf[:, sl], start=False, stop=False), reads=[bsf, bqf], writes=[bpo])
            S.op("pe", lambda p: p.matmul(po[:], lhsT=sbk[:, n, cs], rhs=qb[:, sl], start=False, stop=True), reads=[bsb, bqb], writes=[bpo])
            if hf == 0:
                S.op("act", lambda a: a.copy(out=rT[:, hf, sl], in_=po[:]), reads=[bpo], writes=[brT])
            else:
                S.op("dve", lambda v: v.tensor_copy(out=rT[:, hf, sl], in_=po[:]), reads=[bpo], writes=[brT])
    pp.close()
    gt, bg = ph.sb([128, 2, T], BF16, "g"), S.buf("b_g")
    ld("g0", gt[:, 0, :], bg)
    ld("g1", gt[:, 1, :], bg)
    sq, bsq = ph.sb([128, 2, 512], F32, "sq"), S.buf("b_sq")
    pst = ph.pool("pst", 2, [128, 2, 512], F32, psum=True)
    mv, bmv = ph.sb([128, 4, 512], F32, "mv"), S.buf("b_mv")
    ot, bo = ph.sb([128, 2, T], BF16, "ot"), S.buf("b_ot")
    sg, bsg = ph.sb([128, 2, 512], F32, "sg"), S.buf("b_sg")
    onesf = W["ones_f"]
    for c0 in range(0, T, 512):
        cs = slice(c0, c0 + 512)
        S.op("act", lambda a: a.activation(out=sq[:], in_=rT[:, :, cs], func=AF.Square), reads=[brT], writes=[bsq])
        p2, bp2 = pst.next()
        for hf in (0, 1):
            S.op("pe", lambda p: p.matmul(p2[:, 0, :], lhsT=onesf[:], rhs=rT[:, hf, cs], start=(hf == 0), stop=(hf == 1)),
                 reads=[bc, brT], writes=[bp2])
        for hf in (0, 1):
            S.op("pe", lambda p: p.matmul(p2[:, 1, :], lhsT=onesf[:], rhs=sq[:, hf, :], start=(hf == 0), stop=(hf == 1)),
                 reads=[bc, bsq], writes=[bp2])
        S.op("dve", lambda v: v.tensor_scalar(out=mv[:, 0, :], in0=p2[:, 0, :], scalar1=1.0 / 256, scalar2=None, op0=ALU.mult),
             reads=[bp2], writes=[bmv])
        S.op("dve", lambda v: v.tensor_tensor(out=mv[:, 1, :], in0=mv[:, 0, :], in1=mv[:, 0, :], op=ALU.mult), reads=[bmv], writes=[bmv])
        S.op("dve", lambda v: v.scalar_tensor_tensor(out=mv[:, 1, :], in0=p2[:, 1, :], scalar=1.0 / 256, in1=mv[:, 1, :],
                                                     op0=ALU.mult, op1=ALU.subtract), reads=[bp2, bmv], writes=[bmv])
        S.op("act", lambda a: a.activation(out=mv[:, 1, :], in_=mv[:, 1, :], func=AF.Sqrt, bias=C.eps_t[:], scale=1.0),
             reads=[bmv, C.b_const], writes=[bmv])
        S.op("dve", lambda v: v.reciprocal(out=mv[:, 1, :], in_=mv[:, 1, :]), reads=[bmv], writes=[bmv])
        S.op("act", lambda a: a.activation(out=sg[:], in_=gt[:, :, cs], func=AF.Silu), reads=[bg], writes=[bsg])
        for hf in (0, 1):
            S.op("dve", lambda v: v.tensor_tensor(out=mv[:, 2, :], in0=rT[:, hf, cs], in1=mv[:, 0, :], op=ALU.subtract),
                 reads=[brT, bmv], writes=[bmv])
            S.op("dve", lambda v: v.scalar_tensor_tensor(out=mv[:, 2, :], in0=mv[:, 2, :], scalar=gnw_t[:, hf:hf + 1], in1=mv[:, 1, :],
                                                         op0=ALU.mult, op1=ALU.mult), reads=[bmv, bgn], writes=[bmv])
            S.op("dve", lambda v: v.tensor_tensor(out=ot[:, hf, cs], in0=mv[:, 2, :], in1=sg[:, hf, :], op=ALU.mult),
                 reads=[bmv, bsg], writes=[bo])
    ev = S.dma("sp", lambda q: q.dma_start(out=out_ap.rearrange("(h p) t -> p h t", p=128), in_=ot[:]), bo, reads=[bo])
    ph.close()
    return ev


def _s5_consts(T):
    return {"tidx": np.broadcast_to(np.arange(T, dtype=np.int32), (128, T)).copy()}


def sincos_col(C, ph, ang, bang, s_out, c_out, bout, tmp, btmp):
    S = C.S
    a2, qi, qf, rr = tmp["f"][:, 0:1], tmp["i"][:, 0:1], tmp["f"][:, 1:2], tmp["f"][:, 2:3]
    m = tmp["f"][:, 3:4]
    S.op("dve", lambda v: v.tensor_scalar(out=a2, in0=ang, scalar1=8 * math.pi, scalar2=None, op0=ALU.add), reads=[bang], writes=[btmp])
    S.op("dve", lambda v: v.tensor_scalar(out=qi, in0=a2, scalar1=1.0 / TWO_PI, scalar2=None, op0=ALU.mult), reads=[btmp], writes=[btmp])
    S.op("dve", lambda v: v.tensor_copy(out=qf, in_=qi), reads=[btmp], writes=[btmp])
    S.op("dve", lambda v: v.scalar_tensor_tensor(out=rr, in0=qf, scalar=-CW1, in1=a2, op0=ALU.mult, op1=ALU.add), reads=[btmp], writes=[btmp])
    S.op("dve", lambda v: v.scalar_tensor_tensor(out=rr, in0=qf, scalar=-CW2, in1=rr, op0=ALU.mult, op1=ALU.add), reads=[btmp], writes=[btmp])
    S.op("dve", lambda v: v.tensor_scalar(out=m, in0=rr, scalar1=math.pi, scalar2=None, op0=ALU.is_gt), reads=[btmp], writes=[btmp])
    S.op("dve", lambda v: v.scalar_tensor_tensor(out=a2, in0=m, scalar=-TWO_PI, in1=rr, op0=ALU.mult, op1=ALU.add), reads=[btmp], writes=[btmp])
    S.op("act", lambda a: a.activation(out=s_out, in_=a2, func=AF.Sin), reads=[btmp], writes=[bout])
    S.op("dve", lambda v: v.tensor_scalar(out=m, in0=rr, scalar1=math.pi / 2, scalar2=None, op0=ALU.is_gt), reads=[btmp], writes=[btmp])
    S.op("dve", lambda v: v.scalar_tensor_tensor(out=a2, in0=m, scalar=-TWO_PI, in1=rr, op0=ALU.mult, op1=ALU.add), reads=[btmp, bout], writes=[btmp])
    S.op("act", lambda a: a.activation(out=c_out, in_=a2, func=AF.Sin, bias=C.halfpi_t[:, :]), reads=[btmp, C.b_const], writes=[bout])


def mixer_c_pair(C, ph0, ld_u, prm, dcol_ap, tidx_ap, out_ap, T):
    S = C.S
    ph = Phase(C, ph0.name + "_c")
    ut, bu = ph.sb([32, T], BF16, "u"), S.buf("c_u")
    ld_u(ut, bu)
    Y, bY = ph.sb([32, T], F32, "Y"), S.buf("c_Y")
    cos_t = ph.sb([128, T], F32, "cos")
    sin_t = ph.sb([128, T], F32, "sin")
    btab = S.buf("c_tab")
    pr = ph.sb([128, 24], F32, "pr")
    bpr = S.buf("c_pr")
    tmpd = {"f": ph.sb([128, 4], F32, "tf"), "i": ph.sb([128, 1], I32, "ti")}
    btmp = S.buf("c_tmp")
    bmat = ph.sb([128, 4, 16], F32, "bmat")
    bd = ph.sb([128, 4, 32], F32, "bd")
    bbd = S.buf("c_bd")
    lhs_b = ph.sb([32, 2, 128], BF16, "lhsb")
    lhs_c = ph.sb([128, 2, 32], BF16, "lhsc")
    blhs = S.buf("c_lhs")
    RB = ph.sb([128, 512], F32, "RB")
    bRB = S.buf("c_RB")
    carry = ph.sb([128, 2], F32, "carry")
    bcar = S.buf("c_car")
    dcol = ph.sb([32, 1], F32, "dcol")
    bdc = S.buf("c_dcol")
    S.dma("sp", lambda q: q.dma_start(out=dcol[:], in_=dcol_ap), bdc, writes=[bdc])
    wk = ph.pool("wk", 24, [128, 512], F32)
    hb = ph.pool("hb", 6, [128, 512], BF16)
    px = ph.pool("px", 4, [128, 512], F32, psum=True)
    py = ph.pool("py", 2, [32, 512], F32, psum=True)
    ptp = ph.pool("ptp", 1, [32, 128], F32, psum=True)
    for dr in (0, 1):
        P = prm[dr]
        sg = 1.0 if dr == 0 else -1.0
        for i, nm in enumerate(("lam_re", "lam_im", "logdt")):
            S.dma("sp", lambda q, i=i, nm=nm: q.dma_start(out=pr[:, i:i + 1], in_=P[nm]), bpr, writes=[bpr])
        for i, nm in enumerate(("b_re", "b_im", "c_reT", "c_imT")):
            S.dma("sp", lambda q, i=i, nm=nm: q.dma_start(out=bmat[:, i, :], in_=P[nm]), bbd, writes=[bbd])
        c_ = lambda i: pr[:, i:i + 1]
        S.op("act", lambda a: a.activation(out=c_(2), in_=c_(2), func=AF.Exp), reads=[bpr], writes=[bpr])
        S.op("dve", lambda v: v.tensor_tensor(out=c_(3), in0=c_(0), in1=c_(2), op=ALU.mult), reads=[bpr], writes=[bpr])
        S.op("act", lambda a: a.activation(out=c_(3), in_=c_(3), func=AF.Exp), reads=[bpr], writes=[bpr])
        S.op("dve", lambda v: v.tensor_tensor(out=c_(4), in0=c_(1), in1=c_(2), op=ALU.mult), reads=[bpr], writes=[bpr])
        sincos_col(C, ph, c_(4), bpr, c_(5), c_(6), bpr, tmpd, btmp)
        S.op("dve", lambda v: v.tensor_tensor(out=c_(7), in0=c_(3), in1=c_(6), op=ALU.mult), reads=[bpr], writes=[bpr])
        S.op("dve", lambda v: v.tensor_tensor(out=c_(8), in0=c_(3), in1=c_(5), op=ALU.mult), reads=[bpr], writes=[bpr])
        S.op("dve", lambda v: v.tensor_tensor(out=c_(9), in0=c_(0), in1=c_(0), op=ALU.mult), reads=[bpr], writes=[bpr])
        S.op("dve", lambda v: v.scalar_tensor_tensor(out=c_(9), in0=c_(1), scalar=c_(1), in1=c_(9), op0=ALU.mult, op1=ALU.add), reads=[bpr], writes=[bpr])
        S.op("dve", lambda v: v.reciprocal(out=c_(9), in_=c_(9)), reads=[bpr], writes=[bpr])
        S.op("dve", lambda v: v.tensor_scalar(out=c_(10), in0=c_(7), scalar1=-1.0, scalar2=None, op0=ALU.add), reads=[bpr], writes=[bpr])
        S.op("dve", lambda v: v.tensor_tensor(out=c_(13), in0=c_(10), in1=c_(0), op=ALU.mult), reads=[bpr], writes=[bpr])
        S.op("dve", lambda v: v.scalar_tensor_tensor(out=c_(13), in0=c_(8), scalar=c_(1), in1=c_(13), op0=ALU.mult, op1=ALU.add), reads=[bpr], writes=[bpr])
        S.op("dve", lambda v: v.tensor_tensor(out=c_(11), in0=c_(13), in1=c_(9), op=ALU.mult), reads=[bpr], writes=[bpr])
        S.op("dve", lambda v: v.tensor_tensor(out=c_(13), in0=c_(8), in1=c_(0), op=ALU.mult), reads=[bpr], writes=[bpr])
        S.op("dve", lambda v: v.tensor_tensor(out=c_(14), in0=c_(10), in1=c_(1), op=ALU.mult), reads=[bpr], writes=[bpr])
        S.op("dve", lambda v: v.tensor_tensor(out=c_(13), in0=c_(13), in1=c_(14), op=ALU.subtract), reads=[bpr], writes=[bpr])
        S.op("dve", lambda v: v.tensor_tensor(out=c_(12), in0=c_(13), in1=c_(9), op=ALU.mult), reads=[bpr], writes=[bpr])
        S.op("dve", lambda v: v.tensor_scalar(out=c_(15), in0=c_(12), scalar1=-1.0, scalar2=None, op0=ALU.mult), reads=[bpr], writes=[bpr])
        S.op("pool", lambda g: g.memset(bd[:], 0.0), reads=[bbd], writes=[bbd])
        for gi in (0, 1):
            rs = slice(gi * 64, (gi + 1) * 64)
            cs = slice(gi * 16, (gi + 1) * 16)
            S.op("dve", lambda v: v.tensor_scalar(out=bd[rs, 0, cs], in0=bmat[rs, 0, :], scalar1=pr[rs, 11:12], scalar2=None, op0=ALU.mult),
                 reads=[bbd, bpr], writes=[bbd])
            S.op("dve", lambda v: v.scalar_tensor_tensor(out=bd[rs, 0, cs], in0=bmat[rs, 1, :], scalar=pr[rs, 15:16], in1=bd[rs, 0, cs],
                                                         op0=ALU.mult, op1=ALU.add), reads=[bbd, bpr], writes=[bbd])
            S.op("dve", lambda v: v.tensor_scalar(out=bd[rs, 1, cs], in0=bmat[rs, 1, :], scalar1=pr[rs, 11:12], scalar2=None, op0=ALU.mult),
                 reads=[bbd, bpr], writes=[bbd])
            S.op("dve", lambda v: v.scalar_tensor_tensor(out=bd[rs, 1, cs], in0=bmat[rs, 0, :], scalar=pr[rs, 12:13], in1=bd[rs, 1, cs],
                                                         op0=ALU.mult, op1=ALU.add), reads=[bbd, bpr], writes=[bbd])
            S.op("dve", lambda v: v.tensor_copy(out=bd[rs, 2, cs], in_=bmat[rs, 2, :]), reads=[bbd], writes=[bbd])
            S.op("dve", lambda v: v.tensor_scalar(out=bd[rs, 3, cs], in0=bmat[rs, 3, :], scalar1=-1.0, scalar2=None, op0=ALU.mult),
                 reads=[bbd], writes=[bbd])
        for i in (0, 1):
            pt, bpt = ptp.next()
            S.op("pe", lambda p: p.transpose(out=pt[:], in_=bd[:, i, :], identity=C.ident_f[:]), reads=[bbd, C.b_const], writes=[bpt])
            S.op("dve", lambda v: v.tensor_copy(out=lhs_b[:, i, :], in_=pt[:]), reads=[bpt], writes=[blhs])
        S.op("dve", lambda v: v.tensor_copy(out=lhs_c[:, :, :], in_=bd[:, 2:4, :]), reads=[bbd], writes=[blhs])
        S.op("dve", lambda v: v.memset(RB[:], 1.0), reads=[bRB], writes=[bRB])
        S.op("dve", lambda v: v.tensor_scalar(out=RB[:], in0=RB[:], scalar1=pr[:, 3:4], scalar2=None, op0=ALU.mult), reads=[bRB, bpr], writes=[bRB])
        tp = Phase(C, ph.name + "_trig%d" % dr)
        make_trig_tables(C, tp, tidx_ap, pr[:, 4:5], bpr, 128, T, cos_t, sin_t, btab, offset=8 * math.pi)
        tp.close()
        S.op("dve", lambda v: v.memset(carry[:], 0.0), reads=[bcar], writes=[bcar])
        ntile = T // 512
        order = range(ntile) if dr == 0 else range(ntile - 1, -1, -1)
        for ti in order:
            cs = slice(ti * 512, (ti + 1) * 512)
            pxr, bpxr = px.next()
            pxi, bpxi = px.next()
            S.op("pe", lambda p: p.matmul(pxr[:], lhsT=lhs_b[:, 0, :], rhs=ut[:, cs], start=True, stop=True), reads=[blhs, bu], writes=[bpxr])
            S.op("pe", lambda p: p.matmul(pxi[:], lhsT=lhs_b[:, 1, :], rhs=ut[:, cs], start=True, stop=True), reads=[blhs, bu], writes=[bpxi])
            (a1, ba1), (a2, ba2), (a3, ba3), (a4, ba4) = wk.next(), wk.next(), wk.next(), wk.next()
            S.op("dve", lambda v: v.tensor_tensor(out=a1[:], in0=pxr[:], in1=cos_t[:, cs], op=ALU.mult), reads=[bpxr, btab], writes=[ba1])
            S.op("dve", lambda v: v.tensor_tensor(out=a2[:], in0=pxi[:], in1=sin_t[:, cs], op=ALU.mult), reads=[bpxi, btab], writes=[ba2])
            S.op("dve", lambda v: v.tensor_tensor(out=a3[:], in0=pxi[:], in1=cos_t[:, cs], op=ALU.mult), reads=[bpxi, btab], writes=[ba3])
            S.op("dve", lambda v: v.tensor_tensor(out=a4[:], in0=pxr[:], in1=sin_t[:, cs], op=ALU.mult), reads=[bpxr, btab], writes=[ba4])
            opr = ALU.add if dr == 0 else ALU.subtract
            opi = ALU.subtract if dr == 0 else ALU.add
            S.op("pool", lambda g: g.tensor_tensor(out=a1[:], in0=a1[:], in1=a2[:], op=opr), reads=[ba1, ba2], writes=[ba1])
            S.op("pool", lambda g: g.tensor_tensor(out=a3[:], in0=a3[:], in1=a4[:], op=opi), reads=[ba3, ba4], writes=[ba3])
            (hr_, bhr), (hi_, bhi) = wk.next(), wk.next()
            rev = (lambda t: t[:, ::-1]) if dr == 1 else (lambda t: t[:, :])
            last = 0 if dr == 1 else 511
            S.op("dve", lambda v: v.tensor_tensor_scan(out=rev(hr_), data0=RB[:], data1=rev(a1), initial=carry[:, 0:1], op0=ALU.mult, op1=ALU.add),
                 reads=[bRB, ba1, bcar], writes=[bhr])
            S.op("dve", lambda v: v.tensor_tensor_scan(out=rev(hi_), data0=RB[:], data1=rev(a3), initial=carry[:, 1:2], op0=ALU.mult, op1=ALU.add),
                 reads=[bRB, ba3, bcar], writes=[bhi])
            S.op("dve", lambda v: v.tensor_copy(out=carry[:, 0:1], in_=hr_[:, last:last + 1]), reads=[bhr], writes=[bcar])
            S.op("dve", lambda v: v.tensor_copy(out=carry[:, 1:2], in_=hi_[:, last:last + 1]), reads=[bhi], writes=[bcar])
            (b1, bb1), (b2, bb2), (b3, bb3), (b4, bb4) = wk.next(), wk.next(), wk.next(), wk.next()
            S.op("pool", lambda g: g.tensor_tensor(out=b1[:], in0=hr_[:], in1=cos_t[:, cs], op=ALU.mult), reads=[bhr, btab], writes=[bb1])
            S.op("pool", lambda g: g.tensor_tensor(out=b2[:], in0=hi_[:], in1=sin_t[:, cs], op=ALU.mult), reads=[bhi, btab], writes=[bb2])
            S.op("pool", lambda g: g.tensor_tensor(out=b3[:], in0=hi_[:], in1=cos_t[:, cs], op=ALU.mult), reads=[bhi, btab], writes=[bb3])
            S.op("pool", lambda g: g.tensor_tensor(out=b4[:], in0=hr_[:], in1=sin_t[:, cs], op=ALU.mult), reads=[bhr, btab], writes=[bb4])
            (h1, bh1), (h2, bh2) = hb.next(), hb.next()
            S.op("dve", lambda v: v.scalar_tensor_tensor(out=h1[:], in0=b2[:], scalar=-sg, in1=b1[:], op0=ALU.mult, op1=ALU.add),
                 reads=[bb1, bb2], writes=[bh1])
            S.op("dve", lambda v: v.scalar_tensor_tensor(out=h2[:], in0=b4[:], scalar=sg, in1=b3[:], op0=ALU.mult, op1=ALU.add),
                 reads=[bb3, bb4], writes=[bh2])
            pyt, bpy = py.next()
            S.op("pe", lambda p: p.matmul(pyt[:], lhsT=lhs_c[:, 0, :], rhs=h1[:], start=True, stop=False), reads=[blhs, bh1], writes=[bpy])
            S.op("pe", lambda p: p.matmul(pyt[:], lhsT=lhs_c[:, 1, :], rhs=h2[:], start=False, stop=True), reads=[blhs, bh2], writes=[bpy])
            if dr == 0:
                S.op("act", lambda a: a.copy(out=Y[:, cs], in_=pyt[:]), reads=[bpy], writes=[bY])
            else:
                S.op("dve", lambda v: v.tensor_tensor(out=Y[:, cs], in0=pyt[:], in1=Y[:, cs], op=ALU.add), reads=[bpy, bY], writes=[bY])
    yo, byo = ph.sb([32, T], BF16, "yo"), S.buf("c_yo")
    for c0 in range(0, T, 2048):
        cs = slice(c0, c0 + 2048)
        S.op("dve", lambda v: v.scalar_tensor_tensor(out=Y[:, cs], in0=ut[:, cs], scalar=dcol[:, 0:1], in1=Y[:, cs], op0=ALU.mult, op1=ALU.add),
             reads=[bu, bdc, bY], writes=[bY])
        S.op("act", lambda a: a.activation(out=yo[:, cs], in_=Y[:, cs], func=AF.Gelu), reads=[bY], writes=[byo])
    ev = S.dma("sp", lambda q: q.dma_start(out=out_ap, in_=yo[:]), byo, reads=[byo])
    ph.close()
    return ev


def load_w(C, wt_ap, bw, src_ap):
    if src_ap.dtype == BF16:
        C.S.dma("sp", lambda q: q.dma_start(out=wt_ap, in_=src_ap), bw, writes=[bw])
    else:
        C.S.dma("pool", lambda q: q.dma_start(out=wt_ap, in_=src_ap), bw, writes=[bw])


class WMat:
    def __init__(self, ap3, col0=0):
        self.ap3 = ap3
        self.cw = ap3.shape[2]
        self.col0 = col0

    def blk(self, c0, wn):
        c0 += self.col0
        bi, off = c0 // self.cw, c0 % self.cw
        return self.ap3[bi].rearrange("(kc p) c -> p kc c", p=128)[:, :, off:off + wn]


def wblk(w_ap, c0, wn):
    if hasattr(w_ap, "blk"):
        return w_ap.blk(c0, wn)
    return w_ap.rearrange("(kc p) n -> p kc n", p=128)[:, :, c0:c0 + wn]


def gemm_tok(C, act_fn, bact, nk, ntt, w_ap, nn, cb, wpool, pspool, wn=512):
    S = C.S
    tiles = {}

    def issue(n):
        wt, bw = wpool.next()
        load_w(C, wt[:, 0:nk, 0:wn], bw, wblk(w_ap, n * wn, wn))
        tiles[n] = (wt, bw)

    issue(0)
    for n in range(nn):
        if n + 1 < nn:
            issue(n + 1)
        wt, bw = tiles.pop(n)
        for tt in range(ntt):
            ps, bps = pspool.next()
            for k in range(nk):
                S.op("pe", lambda p, k=k: p.matmul(ps[:, 0:wn], lhsT=act_fn(k, tt), rhs=wt[:, k, 0:wn],
                                                   start=(k == 0), stop=(k == nk - 1)),
                     reads=[bw, bact], writes=[bps])
            cb(tt, n, ps, bps)


def gemm_fm2(C, act_fn, bact, nk, ntok, w_ap, nmb, cb, wpool, pspool, wn=512):
    S = C.S
    tiles = {}

    def issue(n):
        wt, bw = wpool.next()
        load_w(C, wt[:, 0:nk, 0:wn], bw, wblk(w_ap, n * wn, wn))
        tiles[n] = (wt, bw)

    issue(0)
    for nb in range(nmb):
        if nb + 1 < nmb:
            issue(nb + 1)
        wt, bw = tiles.pop(nb)
        for mi in range(wn // 128):
            ps, bps = pspool.next()
            for k in range(nk):
                S.op("pe", lambda p, k=k: p.matmul(ps[:, 0:ntok], lhsT=wt[:, k, mi * 128:(mi + 1) * 128], rhs=act_fn(k),
                                                   start=(k == 0), stop=(k == nk - 1)),
                     reads=[bw, bact], writes=[bps])
            cb(nb * (wn // 128) + mi, ps, bps)


def phase_mem_kv(C, mem_ap, nwb_ap, wkv_ap, kmT_d, vm_d, bkm, bvm):
    S = C.S
    ph = Phase(C, "mkv")
    wb = ph.sb([128, D], F32, "wb")
    bwb = S.buf("mkv_wb")
    S.dma("sp", lambda q: q.dma_start(out=wb[:], in_=nwb_ap), bwb, writes=[bwb])
    pools = {"x": ph.pool("x", 2, [128, D], F32), "h": ph.pool("h", 2, [128, D], BF16),
             "pst": ph.pool("pst", 2, [128, 8, 128], BF16, psum=True),
             "small": {"ss": ph.sb([128, 1], F32), "rs": ph.sb([128, 1], F32), "junk": ph.sb([128, D], BF16), "b": S.buf("mkv_small")}}
    mT = ph.sb([128, 32, 256], BF16, "mT")
    bmT = S.buf("mkv_mT")
    norm_block_to_hT(C, mem_ap, 0, 2, wb, bwb, mT, bmT, pools)
    wpool = ph.pool("w", 2, [128, 32, 512], BF16)
    pspool = ph.pool("ps", 3, [128, 512], F32, psum=True)
    opool = ph.pool("o", 3, [128, 512], BF16)

    def cb_k(m, ps, bps):
        ot, bo = opool.next()
        S.op("act", lambda a: a.copy(out=ot[:, 0:256], in_=ps[:, 0:256]), reads=[bps], writes=[bo])
        S.dma("sp", lambda q: q.dma_start(out=kmT_d[m * 128:(m + 1) * 128, :], in_=ot[:, 0:256]), bo, reads=[bo], writes=[bkm])

    gemm_fm2(C, lambda k: mT[:, k, :], bmT, 32, 256, wkv_ap[0] if isinstance(wkv_ap, tuple) else wkv_ap[:, 0:D], 8, cb_k, wpool, pspool)

    def cb_v(tt, n, ps, bps):
        ot, bo = opool.next()
        S.op("dve", lambda v: v.tensor_copy(out=ot[:], in_=ps[:]), reads=[bps], writes=[bo])
        S.dma("sp", lambda q: q.dma_start(out=vm_d[tt * 128:(tt + 1) * 128, n * 512:(n + 1) * 512], in_=ot[:]), bo, reads=[bo], writes=[bvm])

    gemm_tok(C, lambda k, tt: mT[:, k, tt * 128:(tt + 1) * 128], bmT, 32, 2, wkv_ap[1] if isinstance(wkv_ap, tuple) else wkv_ap[:, D:2 * D], 8, cb_v, wpool, pspool)
    ph.close()


TB3 = 512


def phase_k3(C, x_ap, cat_src, glu_ap, wout_ap, nwc_ap, wq_ap, kmT_d, vm_d, bkm, bvm, wo_ap, nwf_ap, rw_ap,
             x1_d, x2_d, hffn_d, aff_d, affT_d, ntok):
    S = C.S
    ph = Phase(C, "k3")
    bx1 = S.buf("k3_x1d")
    bx2 = S.buf("k3_x2d")
    bhf = S.buf("k3_hffn")
    baf = S.buf("k3_aff")
    glu = ph.sb([128, 8, 1024], BF16, "glu")
    bglu = S.buf("k3_glu")
    for nb_ in range(2):
        load_w(C, glu[:, :, nb_ * 512:(nb_ + 1) * 512], bglu, wblk(glu_ap, nb_ * 512, 512))
    wbc = ph.sb([128, D], F32, "wbc")
    wbf = wbc
    bwb = S.buf("k3_wb")
    brw = S.buf("k3_rw")
    rw = ph.sb([128, 32, 16], BF16, "rw")
    load_w(C, rw[:], brw, rw_ap.rearrange("(kc p) n -> p kc n", p=128))
    actT = ph.sb([128, 32, TB3], BF16, "actT")
    bact = S.buf("k3_act")
    qT = ph.sb([128, 32, TB3], BF16, "qT")
    bqT = S.buf("k3_qT")
    wpool = ph.pool("w", 2, [128, 32, 256], BF16)
    pspool = ph.pool("ps", 5, [128, 512], F32, psum=True)
    xpool = ph.pool("x", 1, [128, D], F32)
    hpool = ph.pool("h", 1, [128, D], BF16)
    small = {"ss": ph.sb([128, 1], F32), "rs": ph.sb([128, 1], F32), "junk": ph.sb([128, D], BF16), "b": S.buf("k3_small")}
    pst = ph.pool("pst", 2, [128, 8, 128], BF16, psum=True)
    npools = {"x": xpool, "h": hpool, "pst": pst, "small": small}
    sgp = ph.pool("sg", 2, [128, 512], F32)
    xo = ph.pool("xo", 4, [128, 512], F32)
    kmh = ph.pool("kmh", 2, [128, 8, 256], BF16)
    vmh = ph.pool("vmh", 2, [128, 2, 1024], BF16)
    ptp = ph.pool("ptp", 2, [128, 2, 512], BF16)
    rdn = ph.pool("rdn", 2, [128, 512], F32)
    rsm = ph.sb([128, 8], F32, "rsm")
    lg = ph.sb([128, 16], F32, "lg")
    aft = ph.sb([16, 128], F32, "aft")
    brs = S.buf("k3_rsm")
    ntt = TB3 // 128
    for t0 in range(0, ntok, TB3):
        for kc in range(32):
            cat_src(kc, t0, (qT if kc >= 24 else actT)[:, kc, :], bqT if kc >= 24 else bact)
        for m in range(8):
            ps, bps = pspool.next()
            for k in range(8):
                S.op("pe", lambda p, k=k: p.matmul(ps[:], lhsT=glu[:, k, m * 128:(m + 1) * 128], rhs=qT[:, 24 + k, :],
                                                   start=(k == 0), stop=(k == 7)), reads=[bglu, bqT], writes=[bps])
            sg, bsg = sgp.next()
            S.op("act", lambda a: a.activation(out=sg[:], in_=ps[:], func=AF.Sigmoid), reads=[bps], writes=[bsg])
            S.op("dve", lambda v: v.tensor_tensor(out=actT[:, 24 + m, :], in0=sg[:], in1=qT[:, 24 + m, :], op=ALU.mult),
                 reads=[bsg, bqT], writes=[bact])

        def cb_res(src_ap, dst_ap, bdst, bsrc=None):
            def cb(tt, n, ps, bps):
                xt, bxt = xo.next()
                r0 = t0 + tt * 128
                S.dma("sp", lambda q: q.dma_start(out=xt[:, 0:256], in_=src_ap[r0:r0 + 128, n * 256:(n + 1) * 256]), bxt,
                      reads=([bsrc] if bsrc else []), writes=[bxt])
                S.op("dve", lambda v: v.tensor_tensor(out=xt[:, 0:256], in0=ps[:, 0:256], in1=xt[:, 0:256], op=ALU.add), reads=[bps, bxt], writes=[bxt])
                S.dma("sp", lambda q: q.dma_start(out=dst_ap[r0:r0 + 128, n * 256:(n + 1) * 256], in_=xt[:, 0:256]), bxt,
                      reads=[bxt], writes=[bdst])
            return cb

        gemm_tok(C, lambda k, tt: actT[:, k, tt * 128:(tt + 1) * 128], bact, 32, ntt, wout_ap, 16, cb_res(x_ap, x1_d, bx1), wpool, pspool, wn=256)
        S.dma("sp", lambda q: q.dma_start(out=wbc[:], in_=nwc_ap), bwb, writes=[bwb])
        for i in range(ntt):
            xt, bx = xpool.next()
            r0 = t0 + i * 128
            S.dma("sp", lambda q: q.dma_start(out=xt[:], in_=x1_d[r0:r0 + 128, :]), bx, reads=[bx1], writes=[bx])
            hb, bh = hpool.next()
            rmsnorm_tile(C, xt, bx, wbc, bwb, hb, bh, small)
            transpose_to(C, hb, bh, lambda k0, n, i=i: actT[:, k0:k0 + n, i * 128:(i + 1) * 128], bact, 32, pst)

        def cb_q(m, ps, bps):
            S.op("act" if m % 2 else "dve",
                 (lambda a: a.copy(out=qT[:, m, :], in_=ps[:])) if m % 2 else (lambda v: v.tensor_copy(out=qT[:, m, :], in_=ps[:])),
                 reads=[bps], writes=[bqT])

        gemm_fm2(C, lambda k: actT[:, k, :], bact, 32, TB3, wq_ap, 16, cb_q, wpool, pspool, wn=256)
        for h in range(4):
            km, bkmh = kmh.next()
            vm, bvmh = vmh.next()
            S.dma("sp", lambda q: q.dma_start(out=km[:], in_=kmT_d[h * 1024:(h + 1) * 1024, :].rearrange("(c p) m -> p c m", p=128)),
                  bkmh, reads=[bkm], writes=[bkmh])
            S.dma("sp", lambda q: q.dma_start(out=vm[:], in_=vm_d[:, h * 1024:(h + 1) * 1024].rearrange("(b p) f -> p b f", p=128)),
                  bvmh, reads=[bvm], writes=[bvmh])
            pt, bpt = ptp.next()
            for mb in range(2):
                ps, bps = pspool.next()
                for c in range(8):
                    S.op("pe", lambda p, c=c: p.matmul(ps[:], lhsT=km[:, c, mb * 128:(mb + 1) * 128], rhs=qT[:, h * 8 + c, :],
                                                       start=(c == 0), stop=(c == 7)), reads=[bkmh, bqT], writes=[bps])
                S.op("act", lambda a: a.activation(out=pt[:, mb, :], in_=ps[:], func=AF.Exp, scale=1.0 / 32.0), reads=[bps], writes=[bpt])
            ps, bps = pspool.next()
            for mb in range(2):
                S.op("pe", lambda p: p.matmul(ps[:], lhsT=C.ones_bf[:], rhs=pt[:, mb, :], start=(mb == 0), stop=(mb == 1)),
                     reads=[C.b_const, bpt], writes=[bps])
            rd, brd = rdn.next()
            S.op("dve", lambda v: v.reciprocal(out=rd[:], in_=ps[:]), reads=[bps], writes=[brd])
            for c in range(8):
                ps, bps = pspool.next()
                for mb in range(2):
                    S.op("pe", lambda p: p.matmul(ps[:], lhsT=vm[:, mb, c * 128:(c + 1) * 128], rhs=pt[:, mb, :],
                                                  start=(mb == 0), stop=(mb == 1)), reads=[bvmh, bpt], writes=[bps])
                S.op("dve", lambda v: v.tensor_tensor(out=actT[:, h * 8 + c, :], in0=ps[:], in1=rd[:], op=ALU.mult),
                     reads=[bps, brd], writes=[bact])
        gemm_tok(C, lambda k, tt: actT[:, k, tt * 128:(tt + 1) * 128], bact, 32, ntt, wo_ap, 16, cb_res(x1_d, x2_d, bx2, bx1), wpool, pspool, wn=256)
        S.dma("sp", lambda q: q.dma_start(out=wbc[:], in_=nwf_ap), bwb, writes=[bwb])
        for i in range(ntt):
            xt, bx = xpool.next()
            r0 = t0 + i * 128
            S.dma("sp", lambda q: q.dma_start(out=xt[:], in_=x2_d[r0:r0 + 128, :]), bx, reads=[bx2], writes=[bx])
            hb, bh = hpool.next()
            rmsnorm_tile(C, xt, bx, wbf, bwb, hb, bh, small)
            S.dma("sp", lambda q: q.dma_start(out=hffn_d[r0:r0 + 128, :], in_=hb[:]), bh, reads=[bh], writes=[bhf])
            transpose_to(C, hb, bh, lambda k0, n, i=i: actT[:, k0:k0 + n, i * 128:(i + 1) * 128], bact, 32, pst)
            ps, bps = pspool.next()
            for k in range(32):
                S.op("pe", lambda p, k=k: p.matmul(ps[:, 0:16], lhsT=actT[:, k, i * 128:(i + 1) * 128], rhs=rw[:, k, :],
                                                   start=(k == 0), stop=(k == 31)), reads=[bact, brw], writes=[bps])
            S.op("dve", lambda v: v.tensor_reduce(out=rsm[:, 0:1], in_=ps[:, 0:16], axis=AX.X, op=ALU.max), reads=[bps], writes=[brs])
            S.op("dve", lambda v: v.tensor_scalar(out=rsm[:, 1:2], in0=rsm[:, 0:1], scalar1=-1.0, scalar2=None, op0=ALU.mult), reads=[brs], writes=[brs])
            S.op("act", lambda a: a.activation(out=lg[:], in_=ps[:, 0:16], func=AF.Exp, bias=rsm[:, 1:2], accum_out=rsm[:, 2:3]),
                 reads=[bps, brs], writes=[brs])
            S.op("dve", lambda v: v.reciprocal(out=rsm[:, 3:4], in_=rsm[:, 2:3]), reads=[brs], writes=[brs])
            S.op("dve", lambda v: v.tensor_scalar(out=lg[:], in0=lg[:], scalar1=rsm[:, 3:4], scalar2=None, op0=ALU.mult), reads=[brs], writes=[brs])
            S.dma("sp", lambda q: q.dma_start(out=aff_d[r0:r0 + 128, :], in_=lg[:]), brs, reads=[brs], writes=[baf])
            pa, bpa = pspool.next()
            S.op("pe", lambda p: p.transpose(out=pa[0:16, 0:128], in_=lg[:], identity=C.ident_f[:]), reads=[brs, C.b_const], writes=[bpa])
            S.op("dve", lambda v: v.tensor_copy(out=aft[:], in_=pa[0:16, 0:128]), reads=[bpa, brs], writes=[brs])
            S.dma("sp", lambda q: q.dma_start(out=affT_d[:, r0:r0 + 128], in_=aft[:]), brs, reads=[brs], writes=[baf])
    ph.close()


CAP = 512
TOWN = 4096
HALF = 2048
SW = 256


def _moe_consts():
    c = {}
    c["moe_iota"] = np.broadcast_to(np.arange(CAP, dtype=np.float32), (128, CAP)).copy()
    rh = np.zeros((128, TOWN // 128, 3), np.float32)
    rh[:, :, 0] = np.arange(128)[:, None]
    rh[:, :, 1] = np.arange(TOWN // 128)[None, :]
    rh[:, :, 2] = 1.0
    c["moe_rh"] = rh.astype(ml_dtypes.bfloat16)
    dm = np.zeros((128, 4), np.float32)
    for sc in range(4):
        dm[:, sc] = TOWN + sc * 128 + np.arange(128)
    c["moe_dmy"] = dm
    c["moe_ncol"] = np.broadcast_to(np.arange(D // SW, dtype=np.float32), (128, D // SW)).copy()
    return c


def phase_k4(C, affT_pair, affT_own, aff_own, hffn_d, xacc_d, wexp, experts, bxacc_in, dep_bufs=()):
    S = C.S
    ph = Phase(C, "k4")
    bc = S.buf("k4_const")
    iota = load_const(C, ph, "moe_iota", [128, CAP], F32, bc)
    rhc = load_const(C, ph, "moe_rh", [128, TOWN // 128, 3], BF16, bc)
    dmy = load_const(C, ph, "moe_dmy", [128, 4], F32, bc)
    ncol = load_const(C, ph, "moe_ncol", [128, D // SW], F32, bc)
    ntt = TOWN // 128
    pos_tok = ph.sb([128, ntt, 16], F32, "pos_tok")
    sel_tok = ph.sb([128, ntt, 16], F32, "sel_tok")
    aff_tok = ph.sb([128, ntt, 16], F32, "aff_tok")
    ahi = ph.sb([128, ntt, 16], BF16, "ahi")
    alo = ph.sb([128, ntt, 16], BF16, "alo")
    btok = S.buf("k4_tok")
    S.dma("sp", lambda q: q.dma_start(out=aff_tok[:], in_=aff_own.rearrange("(t p) e -> p t e", p=128)), btok, reads=list(dep_bufs), writes=[btok])
    S.op("dve", lambda v: v.tensor_copy(out=ahi[:], in_=aff_tok[:]), reads=[btok], writes=[btok])
    S.op("dve", lambda v: v.tensor_tensor(out=alo[:], in0=aff_tok[:], in1=ahi[:], op=ALU.subtract), reads=[btok], writes=[btok])
    p1 = Phase(C, "k4a")
    AT = p1.sb([16, TOWN], F32, "AT")
    junk = p1.sb([16, TOWN], F32, "junk")
    bAT = S.buf("k4_AT")
    for r in (0, 1):
        S.dma("sp", lambda q: q.dma_start(out=AT[:, r * HALF:(r + 1) * HALF], in_=affT_pair[r]), bAT, reads=list(dep_bufs), writes=[bAT])
    bs = p1.sb([16, 8], F32, "bs")
    bbs = S.buf("k4_bs")
    c_ = lambda i: bs[:, i:i + 1]
    S.op("dve", lambda v: v.memset(bs[:], 0.0), writes=[bbs])
    S.op("dve", lambda v: v.memset(c_(1), 1.0), reads=[bbs], writes=[bbs])
    S.op("dve", lambda v: v.memset(c_(6), 0.5), reads=[bbs], writes=[bbs])
    for it in range(30):
        S.op("dve", lambda v: v.scalar_tensor_tensor(out=c_(2), in0=c_(0), scalar=c_(1), in1=c_(6), op0=ALU.add, op1=ALU.mult), reads=[bbs], writes=[bbs])
        S.op("dve", lambda v: v.tensor_scalar(out=junk[:], in0=AT[:], scalar1=c_(2), scalar2=0.0, op0=ALU.is_gt, op1=ALU.add, accum_out=c_(3)),
             reads=[bAT, bbs], writes=[bbs])
        S.op("dve", lambda v: v.tensor_scalar(out=c_(4), in0=c_(3), scalar1=CAP - 0.5, scalar2=None, op0=ALU.is_gt), reads=[bbs], writes=[bbs])
        S.op("dve", lambda v: v.tensor_tensor(out=c_(5), in0=c_(2), in1=c_(0), op=ALU.subtract), reads=[bbs], writes=[bbs])
        S.op("dve", lambda v: v.scalar_tensor_tensor(out=c_(0), in0=c_(5), scalar=c_(4), in1=c_(0), op0=ALU.mult, op1=ALU.add), reads=[bbs], writes=[bbs])
        S.op("dve", lambda v: v.tensor_tensor(out=c_(5), in0=c_(1), in1=c_(2), op=ALU.subtract), reads=[bbs], writes=[bbs])
        S.op("dve", lambda v: v.scalar_tensor_tensor(out=c_(1), in0=c_(5), scalar=c_(4), in1=c_(2), op0=ALU.mult, op1=ALU.add), reads=[bbs], writes=[bbs])
    selT = p1.sb([16, TOWN], F32, "selT")
    posT = p1.sb([16, TOWN], F32, "posT")
    ones = p1.sb([16, TOWN], F32, "ones")
    bsel = S.buf("k4_sel")
    S.op("dve", lambda v: v.tensor_scalar(out=selT[:], in0=AT[:], scalar1=c_(0), scalar2=None, op0=ALU.is_gt), reads=[bAT, bbs], writes=[bsel])
    S.op("dve", lambda v: v.memset(ones[:], 1.0), writes=[bsel])
    S.op("dve", lambda v: v.tensor_tensor_scan(out=posT[:], data0=ones[:], data1=selT[:], initial=0.0, op0=ALU.mult, op1=ALU.add),
         reads=[bsel], writes=[bsel])
    S.op("dve", lambda v: v.tensor_tensor(out=posT[:], in0=posT[:], in1=selT[:], op=ALU.subtract), reads=[bsel], writes=[bsel])
    ptr = p1.pool("ptr", 2, [128, 2, 16], F32, psum=True)
    for tt in range(ntt):
        pt, bpt = ptr.next()
        cs = slice(tt * 128, (tt + 1) * 128)
        S.op("pe", lambda p: p.transpose(out=pt[:, 0, :], in_=posT[:, cs], identity=C.ident_f[0:16, 0:16]), reads=[bsel, C.b_const], writes=[bpt])
        S.op("pe", lambda p: p.transpose(out=pt[:, 1, :], in_=selT[:, cs], identity=C.ident_f[0:16, 0:16]), reads=[bsel, C.b_const], writes=[bpt])
        S.op("dve", lambda v: v.tensor_copy(out=pos_tok[:, tt, :], in_=pt[:, 0, :]), reads=[bpt], writes=[btok])
        S.op("dve", lambda v: v.tensor_copy(out=sel_tok[:, tt, :], in_=pt[:, 1, :]), reads=[bpt], writes=[btok])
    p1.close()
    rh = ph.sb([128, ntt, 5], BF16, "rh")
    brh = S.buf("k4_rh")
    S.op("dve", lambda v: v.tensor_copy(out=rh[:, :, 0:3], in_=rhc[:]), reads=[bc], writes=[brh])
    ohp = ph.pool("oh", ntt + 2, [128, CAP], BF16)
    pidx = ph.pool("pidx", 1, [128, 8], F32, psum=True)
    ixf = ph.sb([128, 4, 8], F32, "ixf")
    idx_g = ph.sb([128, 4], I32, "idx_g")
    idx_s = ph.sb([128, 4, D // SW], I32, "idx_s")
    idx_sf = ph.sb([128, D // SW], F32, "idx_sf")
    gate = ph.sb([128, 4], F32, "gate")
    bix = S.buf("k4_ix")
    xgp = ph.pool("xg", 2, [128, D], BF16)
    xeT = ph.sb([128, 32, CAP], BF16, "xeT")
    bxe = S.buf("k4_xeT")
    pst = ph.pool("pst", 2, [128, 8, 128], BF16, psum=True)
    wpool = ph.pool("w", 2, [128, 32, 256], BF16)
    pspool = ph.pool("ps", 4, [128, 512], F32, psum=True)
    sgl = ph.sb([128, 8, CAP], F32, "sgl")
    bsg = S.buf("k4_sgl")
    actT = ph.sb([128, 8, CAP], BF16, "actT")
    bact = S.buf("k4_act")
    yep = ph.pool("ye", 4, [128, SW], F32)
    bacc = [S.buf("k4_acc%d" % n) for n in range(D // SW)]
    for b in bacc:
        b.w = bxacc_in.w
    xacc_v = xacc_d.rearrange("r (a w) -> (r a) w", w=SW)
    bhf = S.buf("k4_hf")
    for e in experts:
        S.op("dve", lambda v: v.tensor_copy(out=rh[:, :, 3], in_=ahi[:, :, e]), reads=[btok, brh], writes=[brh])
        S.op("dve", lambda v: v.tensor_copy(out=rh[:, :, 4], in_=alo[:, :, e]), reads=[btok, brh], writes=[brh])
        ohs = []
        for tt in range(ntt):
            oh, boh = ohp.next()
            S.op("dve", lambda v: v.tensor_scalar(out=oh[:], in0=iota[:], scalar1=pos_tok[:, tt, e:e + 1], scalar2=sel_tok[:, tt, e:e + 1],
                                                  op0=ALU.is_equal, op1=ALU.mult), reads=[bc, btok], writes=[boh])
            ohs.append((oh, boh))
        for sc in range(4):
            pi, bpi = pidx.next()
            for tt in range(ntt):
                oh, boh = ohs[tt]
                S.op("pe", lambda p: p.matmul(pi[:, 0:5], lhsT=oh[:, sc * 128:(sc + 1) * 128], rhs=rh[:, tt, :], start=(tt == 0), stop=(tt == ntt - 1)),
                     reads=[boh, brh], writes=[bpi])
            S.op("dve", lambda v: v.tensor_copy(out=ixf[:, sc, 0:5], in_=pi[:, 0:5]), reads=[bpi, bix], writes=[bix])
            f = lambda i: ixf[:, sc, i:i + 1]
            S.op("dve", lambda v: v.scalar_tensor_tensor(out=f(5), in0=f(1), scalar=128.0, in1=f(0), op0=ALU.mult, op1=ALU.add), reads=[bix], writes=[bix])
            S.op("dve", lambda v: v.tensor_copy(out=idx_g[:, sc:sc + 1], in_=f(5)), reads=[bix], writes=[bix])
            S.op("dve", lambda v: v.tensor_tensor(out=gate[:, sc:sc + 1], in0=f(3), in1=f(4), op=ALU.add), reads=[bix], writes=[bix])
            S.op("dve", lambda v: v.tensor_tensor(out=f(6), in0=f(2), in1=dmy[:, sc:sc + 1], op=ALU.mult), reads=[bix, bc], writes=[bix])
            S.op("dve", lambda v: v.tensor_tensor(out=f(7), in0=dmy[:, sc:sc + 1], in1=f(6), op=ALU.subtract), reads=[bix, bc], writes=[bix])
            S.op("dve", lambda v: v.tensor_tensor(out=f(7), in0=f(7), in1=f(5), op=ALU.add), reads=[bix], writes=[bix])
            S.op("dve", lambda v: v.tensor_copy(out=idx_sf[:], in_=ncol[:]), reads=[bc, bix], writes=[bix])
            S.op("dve", lambda v: v.tensor_scalar(out=f(6), in0=f(7), scalar1=float(D // SW), scalar2=None, op0=ALU.mult), reads=[bix], writes=[bix])
            S.op("dve", lambda v: v.tensor_scalar(out=idx_sf[:], in0=idx_sf[:], scalar1=f(6), scalar2=None, op0=ALU.add), reads=[bix], writes=[bix])
            S.op("dve", lambda v: v.tensor_copy(out=idx_s[:, sc, :], in_=idx_sf[:]), reads=[bix], writes=[bix])
            xg, bxg = xgp.next()
            S.dma("pool", lambda q: q.indirect_dma_start(out=xg[:], out_offset=None, in_=hffn_d,
                                                         in_offset=bass.IndirectOffsetOnAxis(ap=idx_g[:, sc:sc + 1], axis=0)),
                  bxg, reads=[bix, bhf] + list(dep_bufs), writes=[bxg])
            transpose_to(C, xg, bxg, lambda k0, n: xeT[:, k0:k0 + n, sc * 128:(sc + 1) * 128], bxe, 32, pst)
        wg_e, wu_e, wd_e = wexp(e)

        def cb_g(m, ps, bps):
            S.op("act", lambda a: a.activation(out=sgl[:, m, :], in_=ps[:], func=AF.Silu), reads=[bps], writes=[bsg])

        gemm_fm2(C, lambda k: xeT[:, k, :], bxe, 32, CAP, wg_e, 4, cb_g, wpool, pspool, wn=256)

        def cb_u(m, ps, bps):
            S.op("dve", lambda v: v.tensor_tensor(out=actT[:, m, :], in0=ps[:], in1=sgl[:, m, :], op=ALU.mult), reads=[bps, bsg], writes=[bact])

        gemm_fm2(C, lambda k: xeT[:, k, :], bxe, 32, CAP, wu_e, 4, cb_u, wpool, pspool, wn=256)

        def cb_d(sc, n, ps, bps):
            ye, bye = yep.next()
            S.op("act", lambda a: a.activation(out=ye[:], in_=ps[:, 0:SW], func=AF.Copy, scale=gate[:, sc:sc + 1]), reads=[bps, bix], writes=[bye])
            S.dma("pool", lambda q: q.indirect_dma_start(out=xacc_v, out_offset=bass.IndirectOffsetOnAxis(ap=idx_s[:, sc, n:n + 1], axis=0),
                                                         in_=ye[:], in_offset=None, compute_op=ALU.add),
                  bye, reads=[bye, bix], writes=[bacc[n]])

        gemm_tok(C, lambda k, tt: actT[:, k, tt * 128:(tt + 1) * 128], bact, 8, 4, wd_e, D // SW, cb_d, wpool, pspool, wn=SW)
    ph.close()
    return bacc


def _coll(self, kind, groups, in_ap, out_ap, reads, writes):
    e = "pool"
    self._waits(e, reads, writes)
    if not hasattr(self, "cc_sem"):
        self.cc_sem = self.nc.alloc_semaphore("cc_sem")
        self.cc_cnt = 0
    ins = self.eng[e].collective_compute(kind, ALU.bypass, replica_groups=groups, ins=[in_ap.opt()], outs=[out_ap.opt()])
    ins.then_inc(self.cc_sem)
    self.cc_cnt += 1
    ev = ("cc", self.cc_sem, self.cc_cnt)
    self._record(ev, reads, writes)
    self.all_dma["cc"] = ev
    return ev


Sched.coll = _coll
G4 = [[0, 1, 2, 3], [4, 5, 6, 7]]
GP4 = [[0, 4], [1, 5], [2, 6], [3, 7]]
GP1 = [[0, 1], [2, 3], [4, 5], [6, 7]]

OFF_QA, OFF_KA, OFF_VA, OFF_QB, OFF_KB, OFF_VB, OFF_GB, OFF_UC = 0, 1536, 3072, 4608, 5376, 6144, 7680, 9216


def phase_k1(C, x_ap, bx_in, nw_ap, w_ap, bw_in, projT_d, bproj):
    S = C.S
    ph = Phase(C, "k1")
    Tc = 2048
    TBK = 1024
    wb = ph.sb([128, D], F32, "wb")
    bwb = S.buf("k1_wb")
    S.dma("sp", lambda q: q.dma_start(out=wb[:], in_=nw_ap), bwb, writes=[bwb])
    pools = {"x": ph.pool("x", 2, [128, D], F32), "h": ph.pool("h", 2, [128, D], BF16),
             "pst": ph.pool("pst", 2, [128, 8, 128], BF16, psum=True),
             "small": {"ss": ph.sb([128, 1], F32), "rs": ph.sb([128, 1], F32), "junk": ph.sb([128, D], BF16), "b": S.buf("k1_small")}}
    hT = ph.sb([128, 32, TBK], BF16, "hT")
    bhT = S.buf("k1_hT")
    wpool = ph.pool("w", 2, [128, 32, 256], BF16)
    pspool = ph.pool("ps", 6, [128, 512], F32, psum=True)
    opool = ph.pool("ot", 4, [128, 512], BF16)
    cnt = 0
    for tb in range(Tc // TBK):
        norm_block_to_hT(C, x_ap, tb * TBK, TBK // 128, wb, bwb, hT, bhT, pools)
        tiles = {}

        def issue(n):
            wt_, bw_ = wpool.next()
            load_w(C, wt_[:, :, :], bw_, wblk(w_ap, n * 256, 256))
            tiles[n] = (wt_, bw_)

        issue(0)
        for nb in range(40):
            if nb + 1 < 40:
                issue(nb + 1)
            wt, bw = tiles.pop(nb)
            for mi in range(2):
                for th in range(TBK // 512):
                    ps, bps = pspool.next()
                    for k in range(32):
                        S.op("pe", lambda p: p.matmul(ps[:], lhsT=wt[:, k, mi * 128:(mi + 1) * 128], rhs=hT[:, k, th * 512:(th + 1) * 512],
                                                      start=(k == 0), stop=(k == 31)), reads=[bw, bhT], writes=[bps])
                    ot, bo = opool.next()
                    if cnt % 2:
                        S.op("act", lambda a: a.copy(out=ot[:], in_=ps[:]), reads=[bps], writes=[bo])
                    else:
                        S.op("dve", lambda v: v.tensor_copy(out=ot[:], in_=ps[:]), reads=[bps], writes=[bo])
                    cnt += 1
                    m = nb * 2 + mi
                    c0 = tb * TBK + th * 512
                    S.dma("sp", lambda q: q.dma_start(out=projT_d[m * 128:(m + 1) * 128, c0:c0 + 512], in_=ot[:]), bo, reads=[bo], writes=[bproj])
    ph.close()


def phase_k2(C, G1, bG1, posb, idx2_ap, P, mixT_d, bmix):
    S = C.S
    T = SEQ
    G1v = G1
    top = Phase(C, "k2")
    idx2 = top.sb([128, 80], I32, "idx2")
    bidx = S.buf("k2_idx")
    S.dma("sp", lambda q: q.dma_start(out=idx2[:], in_=idx2_ap), bidx, writes=[bidx])

    def gath(dst_fn, b, col):
        for h in (0, 1):
            S.dma("pool", lambda q: q.indirect_dma_start(out=dst_fn(h), out_offset=None, in_=G1v,
                                                         in_offset=bass.IndirectOffsetOnAxis(ap=idx2[:, col * 2 + h:col * 2 + h + 1], axis=0)),
                  b, reads=[bidx, bG1], writes=[b])

    ph = Phase(C, "k2a")
    W = mixer_a_setup(C, ph, posb, T)
    for j in range(6):
        def ld(which, t, b, off, j=j):
            ti = {"q": 0, "k": 1, "v": 2}[which]
            gath(lambda h: t[:, off + h * 2048: off + (h + 1) * 2048], b, j * 3 + ti)
        mixer_a_head(C, ph, ld, mixT_d[j * 128:(j + 1) * 128, :], T, W)
    S.barrier()
    bmix.w = None
    ph.close()
    ph = Phase(C, "k2b")
    Wb = mixer_b_setup(C, ph, posb, T)
    dect = ph.sb([128, 6], F32, "dect")
    gnt = ph.sb([128, 6], F32, "gnt")
    bd = S.buf("k2_dec")
    S.dma("sp", lambda q: q.dma_start(out=dect[:], in_=P["ret_dec"]), bd, writes=[bd])
    S.dma("sp", lambda q: q.dma_start(out=gnt[:], in_=P["gnw"]), bd, writes=[bd])
    for j in range(3):
        def ldb(which, ap, b, j=j):
            wi = {"q": 0, "k": 1, "v0": 2, "v1": 3, "g0": 4, "g1": 5}[which]
            gath(lambda h: ap[:, h * 2048:(h + 1) * 2048], b, 18 + j * 6 + wi)
        mixer_b_head(C, ph, ldb, dect, bd, j, gnt[:, 2 * j:2 * j + 2], bd, mixT_d[768 + 256 * j:768 + 256 * (j + 1), :], T, Wb)
    ph.close()
    ph = Phase(C, "k2c")
    stg = ph.sb([128, T], BF16, "stg")
    bstg = S.buf("k2_stg")
    for gp in range(16):
        if gp % 4 == 0:
            gath(lambda h: stg[:, h * 2048:(h + 1) * 2048], bstg, 36 + gp // 4)

        def ld_u(ut, bu, gp=gp):
            r0 = (gp % 4) * 32
            S.dma("sp", lambda q: q.dma_start(out=ut[:], in_=stg[r0:r0 + 32, :]), bu, reads=[bstg], writes=[bu])

        prm = []
        for dr in (0, 1):
            d = {}
            for i, nm in enumerate(("lam_re", "lam_im", "logdt")):
                d[nm] = P["s5_cols"][gp, dr, i]
            for i, nm in enumerate(("b_re", "b_im", "c_reT", "c_imT")):
                d[nm] = P["s5_mats"][gp, dr, :, i, :]
            prm.append(d)
        mixer_c_pair(C, ph, ld_u, prm, P["s5_d"][gp], P["tidx"], mixT_d[1536 + 32 * gp:1536 + 32 * (gp + 1), :], T)
    ph.close()
    top.close()


NC4 = 4
GRP = [[0, 1, 2, 3]]
WSPEC = (("w_in", 4096, 10240, 512, 1), ("w_out", 4096, D, 512, 1), ("wq", 4096, D, 512, 1), ("wkv", 4096, 2 * D, 512, 1),
         ("wo", 4096, D, 512, 1), ("glu", 1024, 1024, 512, 1), ("wg", 4096, 1024, 512, 16), ("wu", 4096, 1024, 512, 16),
         ("wd", 1024, D, 2048, 16))


def prologue_weight(C, name, src_ap, K, N, cw, ne, pool):
    S = C.S
    Ks = K // 4
    NB = N // cw
    sh = C.scratch(name + "_sh", [ne * NB, Ks, cw], BF16)
    full = C.scratch(name + "_full", [ne * NB, K, cw], BF16)
    for e in range(ne):
        bsh = S.buf("%s_sh%d" % (name, e))
        for r0 in range(0, Ks, 128):
            t, bt = pool.next()
            S.dma("pool", lambda q: q.dma_start(out=t[:, 0:N], in_=src_ap[e * Ks + r0:e * Ks + r0 + 128, :]), bt, writes=[bt])
            S.dma("sp", lambda q: q.dma_start(out=sh[e * NB:(e + 1) * NB, r0:r0 + 128, :].rearrange("nb p c -> p nb c"),
                                              in_=t[:, 0:N].rearrange("p (nb c) -> p nb c", c=cw)), bt, reads=[bt], writes=[bsh])
        for nb in range(NB):
            S.coll("AllGather", GRP, sh[e * NB + nb], full[e * NB + nb], [bsh], [])
    return full


def build_full(depth=2):
    C = Ctx()
    S = C.S
    x_in = C.inp("x", [SEQ, D], F32)
    mem_in = C.inp("mem", [256, D], F32)
    posb = C.inp("posb", [128, SEQ], I32)
    tidx = C.inp("tidx", [128, SEQ], I32)
    idx2_in = C.inp("idx2", [2, 128, 80], I32)
    idx3_in = C.inp("idx3", [2, 128, 32], F32)
    final_nw = C.inp("final_nw", [128, D], F32)
    out = C.out("out", [SEQ, D], F32)
    for nm, arr in list(_mixer_consts().items()) + list(_retention_consts().items()) + list(_moe_consts().items()):
        C.inp(nm, list(arr.shape), BF16 if arr.dtype == ml_dtypes.bfloat16 else (I32 if arr.dtype == np.int32 else F32))
    G1 = C.scratch("G1", [2 * 10240, 2048], BF16)
    G2 = C.scratch("G2", [2 * 2048, SEQ], BF16)
    x1_d = C.scratch("x1_d", [2048, D], F32)
    xacc_all = C.scratch("xacc", [SEQ + CAP, D], F32)
    hffn_all = C.scratch("hffn", [SEQ, D], BF16)
    aff_all = C.scratch("aff", [SEQ, 16], F32)
    xacc = [xacc_all[r * 2048:(r + 1) * 2048, :] for r in (0, 1)]
    hffn = [hffn_all[r * 2048:(r + 1) * 2048, :] for r in (0, 1)]
    aff = [aff_all[r * 2048:(r + 1) * 2048, :] for r in (0, 1)]
    affT_pair = C.scratch("affT_pair", [32, 2048], F32)
    kmT_d = C.scratch("kmT_d", [D, 256], BF16)
    vm_d = C.scratch("vm_d", [256, D], BF16)
    LW = []
    ph = Phase(C, "pro")
    cpool = ph.pool("cast", 3, [128, 10240], BF16)
    for l in range(depth):
        Wl = {}
        for nm, K, N, cw, ne in WSPEC:
            src = C.inp("%s_%d" % (nm, l), [ne * K // 4, N], F32)
            Wl[nm] = prologue_weight(C, "%s_%d" % (nm, l), src, K, N, cw, ne, cpool)
        LW.append(Wl)
    ph.close()
    for l in range(depth):
        Wl = LW[l]
        Pr = []
        rd_in = C.inp("ret_dec_%d" % l, [2, 128, 6], F32)
        gn_in = C.inp("gnw_%d" % l, [2, 128, 6], F32)
        sc_in = C.inp("s5_cols_%d" % l, [2, 16, 2, 3, 128, 1], F32)
        sm_in = C.inp("s5_mats_%d" % l, [2, 16, 2, 128, 4, 16], F32)
        sd_in = C.inp("s5_d_%d" % l, [2, 16, 32, 1], F32)
        for r in (0, 1):
            Pr.append({"ret_dec": rd_in[r], "gnw": gn_in[r], "s5_cols": sc_in[r], "s5_mats": sm_in[r], "s5_d": sd_in[r], "tidx": tidx})
        nw_mix = C.inp("nw_mix_%d" % l, [128, D], F32)
        nw_cross = C.inp("nw_cross_%d" % l, [128, D], F32)
        nw_mem = C.inp("nw_mem_%d" % l, [128, D], F32)
        nw_ffn = C.inp("nw_ffn_%d" % l, [128, D], F32)
        rw = C.inp("rw_%d" % l, [D, 16], F32)
        xs = [x_in[0:2048, :], x_in[2048:4096, :]] if l == 0 else [xacc[0], xacc[1]]
        dummy = S.buf("dummy")
        for r in (0, 1):
            phase_k1(C, xs[r], None, nw_mix, WMat(Wl["w_in"]), dummy, G1[r * 10240:(r + 1) * 10240, :], S.buf("proj"))
        for r in (0, 1):
            phase_k2(C, G1, S.buf("G1"), posb, idx2_in[r], Pr[r], G2[r * 2048:(r + 1) * 2048, :], S.buf("mix"))
        bkm, bvm = S.buf("kmd"), S.buf("vmd")
        phase_mem_kv(C, mem_in, nw_mem, (WMat(Wl["wkv"]), WMat(Wl["wkv"], col0=D)), kmT_d, vm_d, bkm, bvm)
        G2v = G2.rearrange("r (a w) -> (r a) w", w=512)
        for r in (0, 1):
            php = Phase(C, "k3idx")
            idx3f = php.sb([128, 32], F32, "idx3f")
            idx3b = php.sb([128, 4, 32], I32, "idx3b")
            bi3 = S.buf("k3_idx")
            S.dma("sp", lambda q: q.dma_start(out=idx3f[:], in_=idx3_in[r]), bi3, writes=[bi3])
            for blk in range(4):
                S.op("dve", lambda v: v.tensor_scalar(out=idx3b[:, blk, :], in0=idx3f[:], scalar1=float(blk), scalar2=None, op0=ALU.add),
                     reads=[bi3], writes=[bi3])

            def cat_load(kc, t0, dst, bd):
                blk = t0 // 512
                S.dma("pool", lambda q: q.indirect_dma_start(out=dst, out_offset=None, in_=G2v,
                                                             in_offset=bass.IndirectOffsetOnAxis(ap=idx3b[:, blk, kc:kc + 1], axis=0)),
                      bd, reads=[bi3], writes=[bd])

            phase_k3(C, xs[r], cat_load, WMat(Wl["glu"]), WMat(Wl["w_out"]), nw_cross, WMat(Wl["wq"]), kmT_d, vm_d, bkm, bvm,
                     WMat(Wl["wo"]), nw_ffn, rw, x1_d, xacc[r], hffn[r], aff[r], affT_pair[r * 16:(r + 1) * 16, :], 2048)
            php.close()
        affp_v = affT_pair.rearrange("(r e) t -> r e t", r=2)

        def wexp(e):
            return (WMat(Wl["wg"][2 * e:2 * e + 2]), WMat(Wl["wu"][2 * e:2 * e + 2]), WMat(Wl["wd"][2 * e:2 * e + 2]))

        phase_k4(C, affp_v, None, aff_all, hffn_all, xacc_all, wexp, list(range(16)), S.buf("xacc"))
    ph = Phase(C, "fin")
    wb = ph.sb([128, D], F32, "wb")
    bwb = S.buf("fin_wb")
    S.dma("sp", lambda q: q.dma_start(out=wb[:], in_=final_nw), bwb, writes=[bwb])
    xp = ph.pool("x", 2, [128, D], F32)
    hp = ph.pool("h", 2, [128, D], F32)
    small = {"ss": ph.sb([128, 1], F32), "rs": ph.sb([128, 1], F32), "junk": ph.sb([128, D], BF16), "b": S.buf("fin_small")}
    for r in (0, 1):
        for i in range(16):
            xt, bxt = xp.next()
            S.dma("sp", lambda q: q.dma_start(out=xt[:], in_=xacc[r][i * 128:(i + 1) * 128, :]), bxt, writes=[bxt])
            hb, bh = hp.next()
            rmsnorm_tile(C, xt, bxt, wb, bwb, hb, bh, small)
            r0 = r * 2048 + i * 128
            C.out_evs.append(S.dma("sp", lambda q: q.dma_start(out=out[r0:r0 + 128, :], in_=hb[:]), bh, reads=[bh]))
    ph.close()
    return C


def _idx_tables(r):
    idx2 = np.zeros((128, 80), np.int32)
    p = np.arange(128)
    cols = []
    for j in range(6):
        H = 6 * r + j
        cols += [OFF_QA + 128 * H, OFF_KA + 128 * H, OFF_VA + 128 * H]
    for j in range(3):
        H = 3 * r + j
        cols += [OFF_QB + 128 * H, OFF_KB + 128 * H, OFF_VB + 256 * H, OFF_VB + 256 * H + 128, OFF_GB + 256 * H, OFF_GB + 256 * H + 128]
    for cb in range(4):
        cols += [OFF_UC + 512 * r + 128 * cb]
    for ci, row0 in enumerate(cols):
        for h in (0, 1):
            idx2[:, ci * 2 + h] = h * 10240 + row0 + p
    idx3 = np.zeros((128, 32), np.float32)
    for kc in range(32):
        if kc < 12:
            rr, row0 = kc // 6, 128 * (kc % 6)
        elif kc < 24:
            rr, row0 = (kc - 12) // 6, 768 + 128 * ((kc - 12) % 6)
        else:
            rr, row0 = (kc - 24) // 4, 1536 + 128 * ((kc - 24) % 4)
        idx3[:, kc] = (rr * 2048 + row0 + p) * 8 + r * 4
    return idx2, idx3


_PROG = {}


def make_maps(inp, depth=2):
    f32 = lambda a: np.ascontiguousarray(np.asarray(a), dtype=np.float32)
    bc = lambda v: np.ascontiguousarray(np.broadcast_to(np.asarray(v, dtype=np.float32), (128, D)))
    consts = {}
    consts.update(_mixer_consts())
    consts.update(_retention_consts())
    consts.update(_moe_consts())
    consts.update(_s5_consts(SEQ))
    consts.update(_host_consts())
    t2 = [_idx_tables(r) for r in (0, 1)]
    consts["idx2"] = np.stack([t2[0][0], t2[1][0]])
    consts["idx3"] = np.stack([t2[0][1], t2[1][1]])
    consts["final_nw"] = bc(inp["final_norm_w"])
    per_layer = []
    for l in range(depth):
        d = {}
        d["rw_%d" % l] = f32(inp["router_w"][l])
        d["nw_mix_%d" % l] = bc(inp["norm_mix_w"][l])
        d["nw_cross_%d" % l] = bc(inp["norm_cross_w"][l])
        d["nw_mem_%d" % l] = bc(inp["norm_mem_w"][l])
        d["nw_ffn_%d" % l] = bc(inp["norm_ffn_w"][l])
        rd = np.asarray(inp["ret_decay"][l], dtype=np.float32)
        gw = np.asarray(inp["ret_gn_w"][l], dtype=np.float32)
        dec = np.zeros((2, 128, 6), np.float32)
        gn = np.zeros((2, 128, 6), np.float32)
        cols = np.zeros((2, 16, 2, 3, 128, 1), np.float32)
        mats = np.zeros((2, 16, 2, 128, 4, 16), np.float32)
        sd = np.zeros((2, 16, 32, 1), np.float32)
        lre, lim, ldt = (np.asarray(inp[k][l]) for k in ("s5_lam_re", "s5_lam_im", "s5_log_dt"))
        bre, bim, cre, cim = (np.asarray(inp[k][l]) for k in ("s5_b_re", "s5_b_im", "s5_c_re", "s5_c_im"))
        s5d = np.asarray(inp["s5_d"][l])
        for r in (0, 1):
            for j in range(3):
                H = 3 * r + j
                for dr in (0, 1):
                    dec[r, :, dr * 3 + j] = rd[dr, H]
                for hf in (0, 1):
                    gn[r, :, 2 * j + hf] = gw[H * 256 + hf * 128:H * 256 + (hf + 1) * 128]
            for gp in range(16):
                g0 = 32 * r + 2 * gp
                sd[r, gp, :, 0] = s5d[16 * g0:16 * g0 + 32]
                for dr in (0, 1):
                    for gi in (0, 1):
                        g = g0 + gi
                        ps = slice(gi * 64, (gi + 1) * 64)
                        cols[r, gp, dr, 0, ps, 0] = lre[dr][g]
                        cols[r, gp, dr, 1, ps, 0] = lim[dr][g]
                        cols[r, gp, dr, 2, ps, 0] = ldt[dr][g]
                        mats[r, gp, dr, ps, 0] = bre[dr][g]
                        mats[r, gp, dr, ps, 1] = bim[dr][g]
                        mats[r, gp, dr, ps, 2] = cre[dr][g].T
                        mats[r, gp, dr, ps, 3] = cim[dr][g].T
        d["ret_dec_%d" % l], d["gnw_%d" % l] = dec, gn
        d["s5_cols_%d" % l], d["s5_mats_%d" % l], d["s5_d_%d" % l] = cols, mats, sd
        per_layer.append(d)
    maps = []
    for c in range(NC4):
        m = dict(consts)
        m["x"] = f32(inp["x"][c])
        m["mem"] = f32(inp["mem"][c])
        m["posb"] = np.ascontiguousarray(np.broadcast_to(np.asarray(inp["positions"][c], dtype=np.int32), (128, SEQ)))
        for l in range(depth):
            m.update(per_layer[l])
            for nm, key, K in (("w_in", "w_in", 4096), ("w_out", "w_out", 4096), ("wq", "cross_wq", 4096), ("wkv", "cross_wkv", 4096),
                               ("wo", "cross_wo", 4096), ("glu", "s5_glu_w", 1024)):
                Ks = K // 4
                m["%s_%d" % (nm, l)] = f32(inp[key][l][c * Ks:(c + 1) * Ks])
            for nm, key, K in (("wg", "expert_w_gate", 4096), ("wu", "expert_w_up", 4096), ("wd", "expert_w_down", 1024)):
                Ks = K // 4
                a = np.asarray(inp[key][l])[:, c * Ks:(c + 1) * Ks, :]
                m["%s_%d" % (nm, l)] = f32(a.reshape(16 * Ks, a.shape[2]))
        maps.append(m)
    return maps


def kernel(**inp):
    depth = 2
    if "full" not in _PROG:
        C = build_full(depth)
        C.S.finish(C.out_evs)
        _PROG["full"] = C
    C = _PROG["full"]
    maps = make_maps(inp, depth)
    res = run_bass_kernel_spmd(C.nc, maps, core_ids=list(range(NC4)))
    full = np.zeros((4, SEQ, D), np.float32)
    for c in range(NC4):
        full[c] = np.asarray(res.results[c]["out"])
    return full
```
